# Optimizing a Trainium2 kernel written in Bass

```python
import math
import jax
import jax.numpy as jnp
from jax import lax
import numpy as np

D_MODEL = 1024
BATCH = 2
SEQ = 8192
DEPTH = 1

GRID_W = 64
CTX_LEN = 256

DA_HEADS = 4
DA_HEAD_DIM = 64
DA_V_DIM = 2 * DA_HEAD_DIM
DA_WIDTH = DA_HEADS * DA_V_DIM
HG_HEADS = 4
HG_HEAD_DIM = 128
HG_WIDTH = HG_HEADS * HG_HEAD_DIM
MIX_WIDTH = DA_WIDTH + HG_WIDTH
IN_WIDTH = 3 * DA_WIDTH + 5 * HG_WIDTH
SPLITS = (DA_WIDTH, 2 * DA_WIDTH, 3 * DA_WIDTH, 3 * DA_WIDTH + HG_WIDTH,
          3 * DA_WIDTH + 2 * HG_WIDTH, 3 * DA_WIDTH + 3 * HG_WIDTH, 3 * DA_WIDTH + 4 * HG_WIDTH)
CTX_KV_END = 3 * DA_WIDTH + 3 * HG_WIDTH
CTX_SPLITS = (DA_WIDTH, 2 * DA_WIDTH, 2 * DA_WIDTH + HG_WIDTH, 2 * DA_WIDTH + 2 * HG_WIDTH)
CHUNK = 64
Q_BLOCK = 128
N_EXPERTS = 16
CAPACITY_FACTOR = 2
EXPERT_FF = 1024
ROPE_THETA = 10000.0
NORM_EPS = 1e-6

kernel_name = "hybrid_diffattn_hgrn2_ecmoe_dit_layer"


def rmsnorm(x, g):
    xf = x.astype(jnp.float32)
    y = xf * lax.rsqrt(jnp.mean(xf * xf, axis=-1, keepdims=True) + NORM_EPS)
    return (y * g.astype(jnp.float32)).astype(x.dtype)


def modulate(h, shift, scale):
    return h * (1.0 + scale) + shift


def axial_rope_tables(rows):
    half = DA_HEAD_DIM // 2
    inv_freq = 1.0 / (ROPE_THETA ** (jnp.arange(0, half, 2, dtype=jnp.float32) / half))
    r, col = jnp.meshgrid(jnp.arange(rows, dtype=jnp.float32),
                          jnp.arange(GRID_W, dtype=jnp.float32), indexing="ij")
    ang_r = r.reshape(-1)[:, None] * inv_freq
    ang_c = col.reshape(-1)[:, None] * inv_freq
    ang = jnp.concatenate([ang_r, ang_r, ang_c, ang_c], axis=-1)
    return jnp.cos(ang), jnp.sin(ang)


def apply_axial_rope(x, cos, sin):
    xf = x.astype(jnp.float32)
    x1, x2, x3, x4 = jnp.split(xf, 4, axis=-1)
    rot = jnp.concatenate([-x2, x1, -x4, x3], axis=-1)
    return (xf * cos + rot * sin).astype(x.dtype)


def qk_heads(a):
    b, t = a.shape[:2]
    return a.reshape(b, t, DA_HEADS, 2, DA_HEAD_DIM).transpose(0, 2, 3, 1, 4)


def to_heads(a, n_heads, dim):
    b, t = a.shape[:2]
    return a.reshape(b, t, n_heads, dim).transpose(0, 2, 1, 3)


def merge_heads(o):
    b, h, t, d = o.shape
    return o.transpose(0, 2, 1, 3).reshape(b, t, h * d)


def diff_attend(q, k, v, lam):
    s = jnp.einsum("bhiqd,bhikd->bhiqk", q, k).astype(jnp.float32) * (DA_HEAD_DIM ** -0.5)
    p = jax.nn.softmax(s, axis=-1)
    a = p[:, :, 0] - lam * p[:, :, 1]
    return jnp.einsum("bhqk,bhkd->bhqd", a.astype(v.dtype), v)


def hgrn2_chunk_scan(k, v, log_f, s0, q=None):
    b_, h_, t_, _ = k.shape
    dv = v.shape[-1]
    n = t_ // CHUNK

    def chunks(a):
        return jnp.moveaxis(a.astype(jnp.float32).reshape(b_, h_, n, CHUNK, a.shape[-1]), 2, 0)

    lower = jnp.tril(jnp.ones((CHUNK, CHUNK), dtype=bool))[:, :, None]

    def step(S, inp):
        kc, vc, lc = inp[0], inp[1], inp[2]
        bcum = jnp.cumsum(lc, axis=2)
        b_end = bcum[:, :, -1:, :]
        S_next = (jnp.exp(b_end[:, :, 0, :, None]) * S
                  + jnp.einsum("bhsk,bhsv->bhkv", kc * jnp.exp(b_end - bcum), vc))
        if q is None:
            return S_next, None
        qc = inp[3]
        rel = bcum[:, :, :, None, :] - bcum[:, :, None, :, :]
        decay = jnp.exp(jnp.where(lower, rel, -jnp.inf))
        scores = jnp.einsum("bhrk,bhsk,bhrsk->bhrs", qc, kc, decay)
        o = (jnp.einsum("bhrk,bhkv->bhrv", qc * jnp.exp(bcum), S)
             + jnp.einsum("bhrs,bhsv->bhrv", scores, vc))
        return S_next, o

    xs = (chunks(k), chunks(v), chunks(log_f))
    if q is not None:
        xs = xs + (chunks(q),)
    S_fin, o = lax.scan(step, s0, xs)
    if q is not None:
        o = jnp.moveaxis(o, 0, 2).reshape(b_, h_, t_, dv).astype(v.dtype)
    return o, S_fin


def forget_gate(z, lb):
    f = lb + (1.0 - lb) * jax.nn.sigmoid(z.astype(jnp.float32))
    return jnp.log(f), 1.0 - f


def token_mixer(h_lat, h_ctx, w_in, w_out, lam, lambda_init, da_subln, lb, hg_norm, cos, sin, ctx_out):
    b_ = h_lat.shape[0]
    da_q, da_k, da_v, hg_ff, hg_fb, hg_i, hg_q, hg_g = jnp.split(h_lat @ w_in, SPLITS, axis=-1)
    c_da_k, c_da_v, c_ff, c_fb, c_i = jnp.split(h_ctx @ w_in[:, DA_WIDTH:CTX_KV_END], CTX_SPLITS, axis=-1)
    if ctx_out:
        c_da_q = h_ctx @ w_in[:, :DA_WIDTH]
        c_hg_q, c_hg_g = jnp.split(h_ctx @ w_in[:, CTX_KV_END:], 2, axis=-1)

    q = apply_axial_rope(qk_heads(da_q), cos, sin)
    k_ctx = qk_heads(c_da_k)
    k_all = jnp.concatenate([k_ctx, apply_axial_rope(qk_heads(da_k), cos, sin)], axis=3)
    v_ctx = to_heads(c_da_v, DA_HEADS, DA_V_DIM)
    v_all = jnp.concatenate([v_ctx, to_heads(da_v, DA_HEADS, DA_V_DIM)], axis=2)
    t_ = q.shape[3]
    n_blk = t_ // Q_BLOCK
    q_blocks = jnp.moveaxis(q.reshape(b_, DA_HEADS, 2, n_blk, Q_BLOCK, DA_HEAD_DIM), 3, 0)
    o_blocks = lax.map(lambda qb: diff_attend(qb, k_all, v_all, lam), q_blocks)
    o_da = jnp.moveaxis(o_blocks, 0, 2).reshape(b_, DA_HEADS, t_, DA_V_DIM)
    out_da = merge_heads(rmsnorm(o_da, da_subln) * (1.0 - lambda_init))

    v_lat = to_heads(hg_i, HG_HEADS, HG_HEAD_DIM)
    v_c = to_heads(c_i, HG_HEADS, HG_HEAD_DIM)
    q_lat = to_heads(hg_q, HG_HEADS, HG_HEAD_DIM)
    q_c = to_heads(c_hg_q, HG_HEADS, HG_HEAD_DIM) if ctx_out else None
    s0 = jnp.zeros((b_, HG_HEADS, HG_HEAD_DIM, HG_HEAD_DIM), jnp.float32)
    o_hg_lat = None
    o_hg_ctx = None
    for d, (z_lat, z_ctx) in enumerate(((hg_ff, c_ff), (hg_fb, c_fb))):
        flip = (lambda a: a) if d == 0 else (lambda a: jnp.flip(a, axis=2))
        lf_l, k_l = forget_gate(z_lat, lb[d])
        lf_c, k_c = forget_gate(z_ctx, lb[d])
        k_l, lf_l = to_heads(k_l, HG_HEADS, HG_HEAD_DIM), to_heads(lf_l, HG_HEADS, HG_HEAD_DIM)
        k_c, lf_c = to_heads(k_c, HG_HEADS, HG_HEAD_DIM), to_heads(lf_c, HG_HEADS, HG_HEAD_DIM)
        o_c, S_c = hgrn2_chunk_scan(flip(k_c), flip(v_c), flip(lf_c), s0,
                                    q=None if q_c is None else flip(q_c))
        o_l, _ = hgrn2_chunk_scan(flip(k_l), flip(v_lat), flip(lf_l), S_c, q=flip(q_lat))
        o_hg_lat = flip(o_l) if o_hg_lat is None else o_hg_lat + flip(o_l)
        if ctx_out:
            o_hg_ctx = flip(o_c) if o_hg_ctx is None else o_hg_ctx + flip(o_c)
    out_hg = merge_heads(rmsnorm(o_hg_lat, hg_norm)) * jax.nn.silu(hg_g)

    y_lat = jnp.concatenate([out_da, out_hg], axis=-1) @ w_out
    if not ctx_out:
        return y_lat, None
    o_c_da = diff_attend(qk_heads(c_da_q), k_ctx, v_ctx, lam)
    out_c_da = merge_heads(rmsnorm(o_c_da, da_subln) * (1.0 - lambda_init))
    out_c_hg = merge_heads(rmsnorm(o_hg_ctx, hg_norm)) * jax.nn.silu(c_hg_g)
    y_ctx = jnp.concatenate([out_c_da, out_c_hg], axis=-1) @ w_out
    return y_lat, y_ctx


def expert_choice_ffn(h, w_router, w_gate, w_up, w_down):
    b_, t_, d_ = h.shape
    cap = CAPACITY_FACTOR * t_ // N_EXPERTS
    aff = jax.nn.softmax(jnp.einsum("btd,de->bte", h, w_router).astype(jnp.float32), axis=-1)
    top_aff, top_idx = lax.top_k(jnp.swapaxes(aff, 1, 2), cap)
    xin = jax.vmap(lambda hb, ib: hb[ib])(h, top_idx)
    g = jnp.einsum("becd,edf->becf", xin, w_gate)
    u = jnp.einsum("becd,edf->becf", xin, w_up)
    y = jnp.einsum("becf,efd->becd", jax.nn.silu(g) * u, w_down) * top_aff[..., None].astype(h.dtype)
    return jax.vmap(lambda yb, ib: jnp.zeros((t_, d_), h.dtype).at[ib.reshape(-1)].add(yb.reshape(-1, d_)))(y, top_idx)


def setup_inputs(seed: int = 0) -> dict:
    key = jax.random.key(seed)
    ks = jax.random.split(key, 24)
    D = D_MODEL

    def nrm(k, shape, scale):
        return jax.random.normal(k, shape, jnp.float32) * scale

    return {
        "x": nrm(ks[0], (BATCH, SEQ, D), 1.0),
        "c": nrm(ks[1], (BATCH, D), 1.0),
        "ctx": nrm(ks[2], (BATCH, CTX_LEN, D), 1.0),
        "c_ctx": nrm(ks[3], (D,), 1.0),
        "w_ada": nrm(ks[4], (DEPTH, D, 6 * D), 0.5 * D ** -0.5),
        "b_ada": nrm(ks[5], (DEPTH, 6 * D), 0.02),
        "norm_pre_mix": 1.0 + nrm(ks[6], (DEPTH, D), 0.05),
        "norm_post_mix": 1.0 + nrm(ks[7], (DEPTH, D), 0.05),
        "norm_pre_ffn": 1.0 + nrm(ks[8], (DEPTH, D), 0.05),
        "norm_post_ffn": 1.0 + nrm(ks[9], (DEPTH, D), 0.05),
        "w_in": nrm(ks[10], (DEPTH, D, IN_WIDTH), D ** -0.5),
        "da_lambda_q1": nrm(ks[11], (DEPTH, DA_HEAD_DIM), 0.1),
        "da_lambda_k1": nrm(ks[12], (DEPTH, DA_HEAD_DIM), 0.1),
        "da_lambda_q2": nrm(ks[13], (DEPTH, DA_HEAD_DIM), 0.1),
        "da_lambda_k2": nrm(ks[14], (DEPTH, DA_HEAD_DIM), 0.1),
        "da_subln": 1.0 + nrm(ks[15], (DEPTH, DA_V_DIM), 0.05),
        "hg_lower_bound": nrm(ks[16], (DEPTH + 1, 2, HG_WIDTH), 0.5),
        "hg_norm": 1.0 + nrm(ks[17], (DEPTH, HG_HEAD_DIM), 0.05),
        "w_out": nrm(ks[18], (DEPTH, MIX_WIDTH, D), MIX_WIDTH ** -0.5),
        "w_router": nrm(ks[19], (DEPTH, D, N_EXPERTS), D ** -0.5),
        "w_gate": nrm(ks[20], (DEPTH, N_EXPERTS, D, EXPERT_FF), D ** -0.5),
        "w_up": nrm(ks[21], (DEPTH, N_EXPERTS, D, EXPERT_FF), D ** -0.5),
        "w_down": nrm(ks[22], (DEPTH, N_EXPERTS, EXPERT_FF, D), EXPERT_FF ** -0.5),
    }


def reference(x, c, ctx, c_ctx, w_ada, b_ada, norm_pre_mix, norm_post_mix, norm_pre_ffn, norm_post_ffn,
              w_in, da_lambda_q1, da_lambda_k1, da_lambda_q2, da_lambda_k2, da_subln, hg_lower_bound,
              hg_norm, w_out, w_router, w_gate, w_up, w_down):
    T = x.shape[1]
    ROWS = T // GRID_W
    cos, sin = axial_rope_tables(ROWS)
    lb_all = jnp.cumsum(jax.nn.softmax(hg_lower_bound.astype(jnp.float32), axis=0), axis=0)
    x_lat, x_ctx = x, ctx
    for l in range(DEPTH):
        last = l == DEPTH - 1
        lambda_init = 0.8 - 0.6 * math.exp(-0.3 * l)
        lam = (jnp.exp(jnp.sum(da_lambda_q1[l].astype(jnp.float32) * da_lambda_k1[l].astype(jnp.float32)))
               - jnp.exp(jnp.sum(da_lambda_q2[l].astype(jnp.float32) * da_lambda_k2[l].astype(jnp.float32)))
               + lambda_init)
        mod_lat = jax.nn.silu(c) @ w_ada[l] + b_ada[l]
        mod_ctx = jax.nn.silu(c_ctx) @ w_ada[l] + b_ada[l]
        sh1, sc1, gt1, sh2, sc2, gt2 = jnp.split(mod_lat[:, None, :], 6, axis=-1)
        csh1, csc1, cgt1, csh2, csc2, cgt2 = jnp.split(mod_ctx, 6, axis=-1)

        h_lat = modulate(rmsnorm(x_lat, norm_pre_mix[l]), sh1, sc1)
        h_ctx = modulate(rmsnorm(x_ctx, norm_pre_mix[l]), csh1, csc1)
        y_lat, y_ctx = token_mixer(h_lat, h_ctx, w_in[l], w_out[l], lam, lambda_init, da_subln[l],
                                   lb_all[l], hg_norm[l], cos, sin, not last)
        x_lat = x_lat + gt1 * rmsnorm(y_lat, norm_post_mix[l])
        h_lat = modulate(rmsnorm(x_lat, norm_pre_ffn[l]), sh2, sc2)
        f_lat = expert_choice_ffn(h_lat, w_router[l], w_gate[l], w_up[l], w_down[l])
        x_lat = x_lat + gt2 * rmsnorm(f_lat, norm_post_ffn[l])
        if not last:
            x_ctx = x_ctx + cgt1 * rmsnorm(y_ctx, norm_post_mix[l])
            h_ctx = modulate(rmsnorm(x_ctx, norm_pre_ffn[l]), csh2, csc2)
            f_ctx = expert_choice_ffn(h_ctx, w_router[l], w_gate[l], w_up[l], w_down[l])
            x_ctx = x_ctx + cgt2 * rmsnorm(f_ctx, norm_post_ffn[l])
    return x_lat
```

```python
import numpy as np
from contextlib import ExitStack
import concourse.bass as bass
import concourse.mybir as mybir
from concourse.bass import IndirectOffsetOnAxis
from concourse.bass_utils import run_bass_kernel_spmd

F32 = mybir.dt.float32
BF16 = mybir.dt.bfloat16
I32 = mybir.dt.int32
U32 = mybir.dt.uint32
AF = mybir.ActivationFunctionType
ALU = mybir.AluOpType
AX = mybir.AxisListType

D = 1024
T = 8192
TC = 256
S = T + TC
NH = 4
NE = 16
CAP = 1024
EPS = 1e-6
NTILE = T // 128
FM_BLOCKS = 7
TM_COLS = 384
HEAD_COLS = FM_BLOCKS * 128 + TM_COLS
FM_TOTAL = NH * FM_BLOCKS * 128
WCOLS = NH * HEAD_COLS


class Buf:
    __slots__ = ("w", "r")

    def __init__(self):
        self.w = None
        self.r = {}


class Prog:
    def __init__(self, nc, es, ndma=12):
        self.nc = nc
        self.eng = dict(pe=nc.tensor, act=nc.scalar, dve=nc.vector, pool=nc.gpsimd, sp=nc.sync)
        self.sem = {k: es.enter_context(nc.semaphore("s_" + k)) for k in self.eng}
        self.cnt = {k: 0 for k in self.eng}
        self.waited = {k: {} for k in self.eng}
        self.dsem = {q: [[es.enter_context(nc.semaphore("d_%s%d" % (q, i))), 0] for i in range(ndma)]
                     for q in ("sp", "pool")}
        self.dnext = {"sp": 0, "pool": 0}
        self.nwait = 0
        self.nops = 0
        import os
        self.limit = int(os.environ.get('OPLIMIT', '1000000000'))

    def _wait(self, e, ev):
        s, v = ev
        w = self.waited[e]
        if w.get(s.num, 0) < v:
            self.eng[e].wait_ge(s, v)
            w[s.num] = v
            self.nwait += 1

    def _deps(self, e, reads, writes):
        own = self.sem[e].num
        for b in reads:
            if b.w is not None:
                if not (e == "pe" and b.w[0].num == own):
                    self._wait(e, b.w)
        for b in writes:
            if b.w is not None:
                if not (e == "pe" and b.w[0].num == own):
                    self._wait(e, b.w)
            for ev in b.r.values():
                if ev[0].num == own:
                    continue
                self._wait(e, ev)

    def _mark(self, ev, reads, writes):
        k = ev[0].num
        for b in reads:
            old = b.r.get(k)
            if old is None or old[1] < ev[1]:
                b.r[k] = ev
        for b in writes:
            b.w = ev
            b.r = {}

    def op(self, e, fn, r=(), w=()):
        self.nops += 1
        if self.nops > self.limit:
            return None
        if self.nops == self.limit:
            print('LAST OP', e, fn.__code__.co_firstlineno)
        self._deps(e, r, w)
        ins = fn(self.eng[e])
        self.cnt[e] += 1
        ins.then_inc(self.sem[e], 1)
        ev = (self.sem[e], self.cnt[e])
        self._mark(ev, r, w)
        return ev

    def mm(self, out_ap, pairs, r=(), w=()):
        self.nops += 1
        if self.nops > self.limit:
            return None
        self._deps("pe", r, w)
        n = len(pairs)
        ins = None
        for i, (l, rh) in enumerate(pairs):
            ins = self.nc.tensor.matmul(out_ap, lhsT=l, rhs=rh, start=(i == 0), stop=(i == n - 1))
        self.cnt["pe"] += 1
        ins.then_inc(self.sem["pe"], 1)
        ev = (self.sem["pe"], self.cnt["pe"])
        self._mark(ev, r, w)
        return ev

    def dma(self, q, fn, r=(), w=()):
        self.nops += 1
        if self.nops > self.limit:
            return None
        slots = self.dsem[q]
        i = self.dnext[q]
        self.dnext[q] = (i + 1) % len(slots)
        s, v = slots[i]
        if v > 0:
            self._wait(q, (s, v))
        self._deps(q, r, w)
        ins = fn(self.eng[q])
        slots[i][1] = v + 16
        ins.then_inc(s, 16)
        ev = (s, v + 16)
        self._mark(ev, r, w)
        return ev

    def all_events(self):
        evs = [(self.sem[k], self.cnt[k]) for k in self.eng if self.cnt[k] > 0]
        for q in self.dsem:
            for s, v in self.dsem[q]:
                if v > 0:
                    evs.append((s, v))
        return evs

    def barrier(self, engines=None):
        evs = self.all_events()
        for e in (engines or self.eng):
            for ev in evs:
                if ev[0].num != self.sem[e].num:
                    self._wait(e, ev)


def build(stop_after=None, dbg=()):
    nc = bass.Bass("TRN2", target_bir_lowering=False)
    dbg = set(dbg)

    def din(name, shape, dt=F32):
        return nc.dram_tensor(name, list(shape), dt, kind="ExternalInput").ap()

    def dscr(name, shape, dt):
        kind = "ExternalOutput" if name in dbg else "Internal"
        return nc.dram_tensor(name, list(shape), dt, kind=kind).ap()

    x_d = din("x", [T, D])
    ctx_d = din("ctx", [TC, D])
    cc_d = din("cc", [128, 8, 2])
    wada_d = din("w_ada", [128, 8, 6 * D])
    bada_d = din("b_ada", [1, 6 * D])
    norms_d = din("norms", [1, 4 * D])
    win_d = din("w_in", [128, 8, WCOLS])
    lamv_d = din("lamv", [1, 256])
    subln_d = din("subln", [128, 1])
    hgn_d = din("hgn", [1, 128])
    hlb_d = din("hlb", [128, 16])
    wout_d = din("w_out", [128, 8, D])
    wr_d = din("w_r", [128, 8, NE])
    wg_d = din("w_gate", [NE, 128, 8, D])
    wu_d = din("w_up", [NE, 128, 8, D])
    wd_d = din("w_down", [NE, 128, 8, D])
    cos_d = din("cosT", [128, S])
    sin_d = din("sinT", [128, S])
    cm_d = din("cmasks", [128, 4, 128])
    rm_d = din("rmask", [128, 2, 1024])
    iota_d = din("iota", [128, 1024])
    tokhl_d = din("tokhl", [128, NTILE, 2])
    out_d = nc.dram_tensor("out", [T, D], F32, kind="ExternalOutput").ap()

    modrows_d = dscr("modrows", [8, D], F32)
    qkt_d = dscr("qkt", [NH, 2, 128, S], BF16)
    zt_d = dscr("zt", [NH, 3, 128, S], F32)
    vi_d = dscr("vi", [S, NH, 2, 128], BF16)
    g_d = dscr("gsil", [S, NH, 128], F32)
    mixt_d = dscr("mixt", [D, T], BF16)
    x1_d = dscr("x1", [T, D], F32)
    h2_d = dscr("h2", [T, D], BF16)
    aff_d = dscr("aff", [T, NE], F32)
    f_d = dscr("facc", [T, D], F32)

    B_modrows = Buf()
    B_qkt = [[Buf() for _ in range(17)] for _ in range(NH)]
    B_zt = [[Buf() for _ in range(17)] for _ in range(NH)]
    B_vi = [Buf() for _ in range(17)]
    B_g = [Buf() for _ in range(17)]
    B_mixt = [Buf() for _ in range(16)]
    B_x1 = [Buf() for _ in range(NTILE)]
    B_h2 = Buf()
    B_aff = Buf()
    B_f = Buf()

    es = ExitStack()
    with es:
        P = Prog(nc, es)

        def sb(stack, name, shape, dt):
            return stack.enter_context(nc.sbuf_tensor("sb_" + name, list(shape), dt))

        def ps(stack, name, shape, dt=F32):
            return stack.enter_context(nc.psum_tensor("ps_" + name, list(shape), dt))

        cm_f = sb(es, "cm_f", [128, 4, 128], F32)
        ident_bf = sb(es, "ident_bf", [128, 128], BF16)
        ones_bf = sb(es, "ones_bf", [128, 128], BF16)
        ones_f = sb(es, "ones_f", [128, 128], F32)
        neglam = sb(es, "neglam", [128, 1], F32)
        subln8 = sb(es, "subln8", [128, 1], F32)
        lbt = sb(es, "lbt", [128, 8], F32)
        omlt = sb(es, "omlt", [128, 8], F32)
        mhalf = sb(es, "mhalf", [128, 512], F32)
        B_const = Buf()
        ident_f = cm_f[:, 0, :]

        P.dma("sp", lambda q: q.dma_start(out=cm_f[:], in_=cm_d[:]), w=[B_const])
        P.op("dve", lambda v: v.tensor_copy(out=ident_bf[:], in_=cm_f[:, 0, :]), r=[B_const], w=[B_const])
        P.op("dve", lambda v: v.memset(ones_bf[:], 1.0), w=[B_const])
        P.op("dve", lambda v: v.memset(ones_f[:], 1.0), w=[B_const])
        P.op("dve", lambda v: v.memset(mhalf[:], -0.5), w=[B_const])

        def rstd_from_ss(ss_ap, n, out_ap, tmp_ap, bufs_r, bufs_w, tmpbuf):
            P.op("dve", lambda v: v.tensor_scalar(out=tmp_ap, in0=ss_ap, scalar1=1.0 / n, scalar2=EPS,
                                                  op0=ALU.mult, op1=ALU.add), r=bufs_r, w=[tmpbuf])
            shp = list(tmp_ap.shape)
            P.op("pool", lambda g: g.tensor_tensor(out=out_ap, in0=tmp_ap, in1=mhalf[0:shp[0], 0:shp[1]],
                                                   op=ALU.pow), r=[tmpbuf, B_const], w=bufs_w)

        with ExitStack() as p0:
            scf = sb(p0, "scf", [128, 8, 2], F32)
            sct = sb(p0, "sct", [128, 8, 2], F32)
            wa = [sb(p0, "wa%d" % i, [128, 8, 512], F32) for i in range(2)]
            modl = sb(p0, "modl", [1, 6 * D], F32)
            modc = sb(p0, "modc", [1, 6 * D], F32)
            bada = sb(p0, "bada", [1, 6 * D], F32)
            nrm = sb(p0, "nrm", [1, 4 * D], F32)
            rows = sb(p0, "rows", [1, 8, D], F32)
            lamv = sb(p0, "lamv", [1, 256], F32)
            lamt = sb(p0, "lamt", [1, 8], F32)
            hlb = sb(p0, "hlb", [128, 16], F32)
            sl = sb(p0, "sl", [128, 1], F32)
            pm = [ps(p0, "pm%d" % i, [1, 512]) for i in range(4)]
            pl = ps(p0, "pl", [128, 1])
            B_sc, B_wa, B_modl, B_modc, B_bada, B_nrm, B_rows, B_lam, B_hlb = (
                Buf(), [Buf(), Buf()], Buf(), Buf(), Buf(), Buf(), Buf(), Buf(), Buf())
            B_pm = [Buf() for _ in range(4)]
            B_pl = Buf()

            P.dma("sp", lambda q: q.dma_start(out=scf[:], in_=cc_d[:]), w=[B_sc])
            P.dma("sp", lambda q: q.dma_start(out=bada[:], in_=bada_d[:]), w=[B_bada])
            P.dma("sp", lambda q: q.dma_start(out=nrm[:], in_=norms_d[:]), w=[B_nrm])
            P.dma("sp", lambda q: q.dma_start(out=lamv[:], in_=lamv_d[:]), w=[B_lam])
            P.dma("sp", lambda q: q.dma_start(out=hlb[:], in_=hlb_d[:]), w=[B_hlb])
            P.dma("sp", lambda q: q.dma_start(out=sl[:], in_=subln_d[:]), w=[B_hlb])
            P.op("act", lambda a: a.activation(out=sct[:], in_=scf[:], func=AF.Exp, scale=-1.0), r=[B_sc], w=[B_rows])
            P.op("dve", lambda v: v.tensor_scalar(out=sct[:], in0=sct[:], scalar1=1.0, scalar2=None, op0=ALU.add),
                 r=[B_rows], w=[B_rows])
            P.op("dve", lambda v: v.reciprocal(out=sct[:], in_=sct[:]), r=[B_rows], w=[B_rows])
            P.op("dve", lambda v: v.tensor_tensor(out=scf[:], in0=scf[:], in1=sct[:], op=ALU.mult),
                 r=[B_rows, B_sc], w=[B_sc])
            for ch in range(12):
                wb_ = wa[ch % 2]
                P.dma("sp", lambda q: q.dma_start(out=wb_[:], in_=wada_d[:, :, ch * 512:(ch + 1) * 512]),
                      w=[B_wa[ch % 2]])
                for j, (mod, bm) in enumerate(((modl, B_modl), (modc, B_modc))):
                    pmt = pm[(2 * ch + j) % 4]
                    bp = B_pm[(2 * ch + j) % 4]
                    P.mm(pmt[:], [(scf[:, kc, j:j + 1], wb_[:, kc, :]) for kc in range(8)],
                         r=[B_sc, B_wa[ch % 2]], w=[bp])
                    P.op("dve", lambda v: v.tensor_tensor(out=mod[:, ch * 512:(ch + 1) * 512], in0=pmt[:],
                                                          in1=bada[:, ch * 512:(ch + 1) * 512], op=ALU.add),
                         r=[bp, B_bada], w=[bm])
            def stt(dst, a, b, op0):
                P.op("dve", lambda v: v.scalar_tensor_tensor(out=rows[:, dst, :], in0=a, scalar=(1.0 if op0 == ALU.add else 1.0),
                                                             in1=b, op0=op0, op1=ALU.mult),
                     r=[B_modl, B_modc, B_nrm], w=[B_rows])
            stt(0, modl[:, D:2 * D], nrm[:, 0:D], ALU.add)
            P.op("dve", lambda v: v.tensor_copy(out=rows[:, 1, :], in_=modl[:, 0:D]), r=[B_modl], w=[B_rows])
            stt(2, modc[:, D:2 * D], nrm[:, 0:D], ALU.add)
            P.op("dve", lambda v: v.tensor_copy(out=rows[:, 3, :], in_=modc[:, 0:D]), r=[B_modc], w=[B_rows])
            stt(4, modl[:, 2 * D:3 * D], nrm[:, D:2 * D], ALU.mult)
            stt(5, modl[:, 4 * D:5 * D], nrm[:, 2 * D:3 * D], ALU.add)
            P.op("dve", lambda v: v.tensor_copy(out=rows[:, 6, :], in_=modl[:, 3 * D:4 * D]), r=[B_modl], w=[B_rows])
            stt(7, modl[:, 5 * D:6 * D], nrm[:, 3 * D:4 * D], ALU.mult)
            P.dma("pool", lambda q: q.dma_start(out=modrows_d[:, :].rearrange("(o r) d -> o r d", o=1), in_=rows[:]),
                  r=[B_rows], w=[B_modrows])
            P.op("dve", lambda v: v.tensor_tensor(out=lamv[:, 0:64], in0=lamv[:, 0:64], in1=lamv[:, 64:128], op=ALU.mult),
                 r=[B_lam], w=[B_lam])
            P.op("dve", lambda v: v.tensor_tensor(out=lamv[:, 128:192], in0=lamv[:, 128:192], in1=lamv[:, 192:256], op=ALU.mult),
                 r=[B_lam], w=[B_lam])
            P.op("dve", lambda v: v.tensor_reduce(out=lamt[:, 0:1], in_=lamv[:, 0:64], axis=AX.X, op=ALU.add),
                 r=[B_lam], w=[B_lam])
            P.op("dve", lambda v: v.tensor_reduce(out=lamt[:, 1:2], in_=lamv[:, 128:192], axis=AX.X, op=ALU.add),
                 r=[B_lam], w=[B_lam])
            P.op("act", lambda a: a.activation(out=lamt[:, 2:4], in_=lamt[:, 0:2], func=AF.Exp), r=[B_lam], w=[B_lam])
            P.op("dve", lambda v: v.scalar_tensor_tensor(out=lamt[:, 4:5], in0=lamt[:, 3:4], scalar=-0.2, in1=lamt[:, 2:3],
                                                         op0=ALU.add, op1=ALU.subtract), r=[B_lam], w=[B_lam])
            P.mm(pl[:], [(ones_f[0:1, :], lamt[0:1, 4:5])], r=[B_lam, B_const], w=[B_pl])
            P.op("dve", lambda v: v.tensor_copy(out=neglam[:], in_=pl[:]), r=[B_pl], w=[B_const])
            P.op("dve", lambda v: v.tensor_scalar(out=subln8[:], in0=sl[:], scalar1=0.8, scalar2=None, op0=ALU.mult),
                 r=[B_hlb], w=[B_const])
            P.op("dve", lambda v: v.tensor_tensor(out=hlb[:, 0:8], in0=hlb[:, 8:16], in1=hlb[:, 0:8], op=ALU.subtract),
                 r=[B_hlb], w=[B_hlb])
            P.op("act", lambda a: a.activation(out=hlb[:, 0:8], in_=hlb[:, 0:8], func=AF.Exp), r=[B_hlb], w=[B_hlb])
            P.op("dve", lambda v: v.tensor_scalar(out=hlb[:, 0:8], in0=hlb[:, 0:8], scalar1=1.0, scalar2=None, op0=ALU.add),
                 r=[B_hlb], w=[B_hlb])
            P.op("dve", lambda v: v.reciprocal(out=lbt[:], in_=hlb[:, 0:8]), r=[B_hlb], w=[B_const])
            P.op("dve", lambda v: v.tensor_scalar(out=omlt[:], in0=lbt[:], scalar1=-1.0, scalar2=1.0, op0=ALU.mult, op1=ALU.add),
                 r=[B_const], w=[B_const])
            P.barrier()

        def load_bc(stack, name, row):
            t = sb(stack, name, [128, D], F32)
            b = Buf()
            P.dma("sp", lambda q: q.dma_start(out=t[:], in_=modrows_d[row:row + 1, :].partition_broadcast(128)),
                  r=[B_modrows], w=[b])
            return t, b

        def pe_group(fns, r, w):
            P._deps("pe", r, w)
            ins = None
            for fn in fns:
                ins = fn(nc.tensor)
            P.cnt["pe"] += 1
            ins.then_inc(P.sem["pe"], 1)
            ev = (P.sem["pe"], P.cnt["pe"])
            P._mark(ev, r, w)
            return ev

        SCS = [(0, 2)] + [(2 + 4 * k, 4) for k in range(16)]

        def phase_a0():
            with ExitStack() as st:
                wbf = sb(st, "wbf", [128, 8, WCOLS], BF16)
                B_w = [[Buf() for _ in range(4)] for _ in range(8)]
                for kc in range(8):
                    for pi in range(4):
                        c0 = pi * 1280
                        P.dma("pool", lambda q: q.dma_start(out=wbf[:, kc, c0:c0 + 1280], in_=win_d[:, kc, c0:c0 + 1280]),
                              w=[B_w[kc][pi]])
                B_wall = [b for l in B_w for b in l]
                gm_l, B_gml = load_bc(st, "gm_l", 0)
                sh_l, B_shl = load_bc(st, "sh_l", 1)
                gm_c, B_gmc = load_bc(st, "gm_c", 2)
                sh_c, B_shc = load_bc(st, "sh_c", 3)
                xb = [sb(st, "xb%d" % i, [128, D], F32) for i in range(2)]
                B_xb = [Buf(), Buf()]
                junk = sb(st, "junk", [128, D], BF16)
                B_junk = Buf()
                hn = [sb(st, "hn%d" % i, [128, D], F32) for i in range(2)]
                B_hn = [Buf(), Buf()]
                hb = [sb(st, "hb%d" % i, [128, D], BF16) for i in range(4)]
                B_hb = [Buf() for _ in range(4)]
                ssm = sb(st, "ssm", [128, 8], F32)
                B_ss = [Buf() for _ in range(4)]
                hT = [sb(st, "hT%d" % i, [128, 8, 512], BF16) for i in range(2)]
                B_hT = [Buf(), Buf()]
                cs = [sb(st, "cs%d" % i, [128, 2, 512], F32) for i in range(2)]
                B_cs = [Buf(), Buf()]
                qks = [sb(st, "qks%d" % i, [128, 2, 512], BF16) for i in range(2)]
                B_qks = [Buf(), Buf()]
                zs = [sb(st, "zs%d" % i, [128, 3, 512], F32) for i in range(2)]
                B_zs = [Buf(), Buf()]
                t1 = sb(st, "t1", [128, 512], F32)
                t2 = sb(st, "t2", [128, 512], F32)
                B_t1, B_t2 = Buf(), Buf()
                vis = [sb(st, "vis%d" % i, [128, NH, 2, 128], BF16) for i in range(2)]
                B_vis = [Buf(), Buf()]
                gs = [sb(st, "gs%d" % i, [128, NH, 128], F32) for i in range(2)]
                B_gs = [Buf(), Buf()]
                tp = [ps(st, "tp%d" % i, [128, 8, 128], BF16) for i in range(2)]
                B_tp = [Buf(), Buf()]
                fm = [ps(st, "fm%d" % i, [128, 512]) for i in range(3)]
                B_fm = [Buf() for _ in range(3)]
                tm = ps(st, "tm", [128, 1536])
                B_tm = Buf()
                fmi = [0]
                tile_ctr = [0]

                def stage1(k):
                    t0, nt = SCS[k]
                    for j in range(nt):
                        i = tile_ctr[0]
                        tile_ctr[0] += 1
                        x_ = xb[i % 2]
                        st_ = t0 + j
                        src = ctx_d[st_ * 128:(st_ + 1) * 128, :] if k == 0 else x_d[(st_ - 2) * 128:(st_ - 1) * 128, :]
                        gm, bgm, sh, bsh = (gm_c, B_gmc, sh_c, B_shc) if k == 0 else (gm_l, B_gml, sh_l, B_shl)
                        P.dma("sp", lambda q: q.dma_start(out=x_[:], in_=src), w=[B_xb[i % 2]])
                        ss = ssm[:, 2 * j:2 * j + 1]
                        rs = ssm[:, 2 * j + 1:2 * j + 2]
                        P.op("dve", lambda v: v.scalar_tensor_tensor(out=junk[:], in0=x_[:], scalar=1.0, in1=x_[:],
                                                                     op0=ALU.mult, op1=ALU.mult, accum_out=ss),
                             r=[B_xb[i % 2]], w=[B_junk, B_ss[j]])
                        rstd_from_ss(ss, D, rs, ss, [B_ss[j]], [B_ss[j]], B_ss[j])
                        h_ = hn[i % 2]
                        P.op("dve", lambda v: v.scalar_tensor_tensor(out=h_[:], in0=x_[:], scalar=rs, in1=gm[:],
                                                                     op0=ALU.mult, op1=ALU.mult),
                             r=[B_xb[i % 2], B_ss[j], bgm], w=[B_hn[i % 2]])
                        P.op("pool", lambda g: g.tensor_tensor(out=hb[j][:], in0=h_[:], in1=sh[:], op=ALU.add),
                             r=[B_hn[i % 2], bsh], w=[B_hb[j]])

                def stage2(k):
                    t0, nt = SCS[k]
                    for j in range(nt):
                        tpp = tp[j % 2]
                        pe_group([(lambda pe, kc=kc: pe.transpose(out=tpp[:, kc, :], in_=hb[j][:, kc * 128:(kc + 1) * 128],
                                                                   identity=ident_bf[:])) for kc in range(8)],
                                 r=[B_hb[j], B_const], w=[B_tp[j % 2]])
                        P.op("act", lambda a: a.copy(out=hT[k % 2][:, :, j * 128:(j + 1) * 128], in_=tpp[:]),
                             r=[B_tp[j % 2]], w=[B_hT[k % 2]])

                def fm_mm(k, col0, n):
                    bi = fmi[0] % 3
                    fmi[0] += 1
                    P.mm(fm[bi][:, 0:n], [(wbf[:, kc, col0:col0 + 128], hT[k % 2][:, kc, 0:n]) for kc in range(8)],
                         r=[B_hT[k % 2]] + B_wall, w=[B_fm[bi]])
                    return bi

                def stage3(k):
                    t0, nt = SCS[k]
                    n = nt * 128
                    s0 = t0 * 128
                    c_ = cs[k % 2]
                    P.dma("sp", lambda q: q.dma_start(out=c_[:, 0, 0:n], in_=cos_d[:, s0:s0 + n]), w=[B_cs[k % 2]])
                    P.dma("sp", lambda q: q.dma_start(out=c_[:, 1, 0:n], in_=sin_d[:, s0:s0 + n]), w=[B_cs[k % 2]])
                    for h in range(NH):
                        hi = k * NH + h
                        qk_ = qks[hi % 2]
                        z_ = zs[hi % 2]
                        base = h * FM_BLOCKS * 128
                        for t in range(2):
                            b0 = fm_mm(k, base + (2 * t) * 128, n)
                            b1 = fm_mm(k, base + (2 * t + 1) * 128, n)
                            P.op("dve", lambda v: v.tensor_tensor(out=t1[:, 0:n], in0=fm[b0][:, 0:n], in1=c_[:, 0, 0:n], op=ALU.mult),
                                 r=[B_fm[b0], B_cs[k % 2]], w=[B_t1])
                            P.op("dve", lambda v: v.tensor_tensor(out=t2[:, 0:n], in0=fm[b1][:, 0:n], in1=c_[:, 1, 0:n], op=ALU.mult),
                                 r=[B_fm[b1], B_cs[k % 2]], w=[B_t2])
                            P.op("pool", lambda g: g.tensor_tensor(out=qk_[:, t, 0:n], in0=t1[:, 0:n], in1=t2[:, 0:n], op=ALU.add),
                                 r=[B_t1, B_t2], w=[B_qks[hi % 2]])
                        for t in range(3):
                            b0 = fm_mm(k, base + (4 + t) * 128, n)
                            P.op("act", lambda a: a.copy(out=z_[:, t, 0:n], in_=fm[b0][:, 0:n]), r=[B_fm[b0]], w=[B_zs[hi % 2]])
                        P.dma("pool", lambda q: q.dma_start(out=qkt_d[h].rearrange("t p s -> p t s")[:, :, s0:s0 + n],
                                                            in_=qk_[:, :, 0:n]), r=[B_qks[hi % 2]], w=[B_qkt[h][k]])
                        P.dma("pool", lambda q: q.dma_start(out=zt_d[h].rearrange("t p s -> p t s")[:, :, s0:s0 + n],
                                                            in_=z_[:, :, 0:n]), r=[B_zs[hi % 2]], w=[B_zt[h][k]])
                    for j in range(nt):
                        ti = t0 + j
                        for nb in range(3):
                            P.mm(tm[:, nb * 512:(nb + 1) * 512],
                                 [(hT[k % 2][:, kc, j * 128:(j + 1) * 128], wbf[:, kc, FM_TOTAL + nb * 512:FM_TOTAL + (nb + 1) * 512])
                                  for kc in range(8)], r=[B_hT[k % 2]] + B_wall, w=[B_tm])
                        v_ = vis[ti % 2]
                        g_ = gs[ti % 2]
                        vflat = v_[:].rearrange("p h t c -> p (h t c)")
                        P.op("dve", lambda v: v.tensor_copy(out=vflat[:, 0:512], in_=tm[:, 0:512]),
                             r=[B_tm], w=[B_vis[ti % 2]])
                        P.op("dve", lambda v: v.tensor_copy(out=vflat[:, 512:1024], in_=tm[:, 512:1024]),
                             r=[B_tm], w=[B_vis[ti % 2]])
                        P.op("act", lambda a: a.activation(out=g_[:].rearrange("p h c -> p (h c)"), in_=tm[:, 1024:1536], func=AF.Silu),
                             r=[B_tm], w=[B_gs[ti % 2]])
                        P.dma("pool", lambda q: q.dma_start(out=vi_d[ti * 128:(ti + 1) * 128], in_=v_[:]),
                              r=[B_vis[ti % 2]], w=[B_vi[k]])
                        P.dma("pool", lambda q: q.dma_start(out=g_d[ti * 128:(ti + 1) * 128], in_=g_[:]),
                              r=[B_gs[ti % 2]], w=[B_g[k]])

                stage1(0)
                stage2(0)
                for k in range(17):
                    if k + 1 < 17:
                        stage1(k + 1)
                    stage3(k)
                    if k + 1 < 17:
                        stage2(k + 1)
                P.barrier()

        def mm1(out_ap, lhsT, rhs, start, stop, r, w):
            P.nops += 1
            if P.nops > P.limit:
                return None
            P._deps("pe", r, w)
            ins = nc.tensor.matmul(out_ap, lhsT=lhsT, rhs=rhs, start=start, stop=stop)
            P.cnt["pe"] += 1
            ins.then_inc(P.sem["pe"], 1)
            ev = (P.sem["pe"], P.cnt["pe"])
            P._mark(ev, r, w)
            return ev

        NKT = S // 128

        def phase_a1(heads=range(NH), qchunks=range(16)):
            with ExitStack() as st:
                kt = [sb(st, "kt%d" % i, [128, S], BF16) for i in range(2)]
                vv = [sb(st, "vv%d" % i, [128, NKT, 128], BF16) for i in range(2)]
                B_kt = [Buf(), Buf()]
                B_vv = [Buf(), Buf()]
                qt = [sb(st, "qt%d" % i, [128, 512], BF16) for i in range(2)]
                B_qt = [Buf(), Buf()]
                p1 = [sb(st, "p1_%d" % i, [128, 512], BF16) for i in range(3)]
                p2 = [sb(st, "p2_%d" % i, [128, 512], BF16) for i in range(3)]
                B_p1 = [Buf() for _ in range(3)]
                B_p2 = [Buf() for _ in range(3)]
                r1 = sb(st, "r1", [128, 512], F32)
                o1 = sb(st, "o1", [128, 512], F32)
                r2 = sb(st, "r2", [128, 512], F32)
                o2 = sb(st, "o2", [128, 512], F32)
                oo = sb(st, "oo", [128, 512], F32)
                sq = sb(st, "sq", [128, 512], F32)
                rs = sb(st, "rs", [128, 512], F32)
                ob = [sb(st, "ob%d" % i, [128, 512], BF16) for i in range(2)]
                B_r1, B_o1, B_r2, B_o2, B_oo, B_sq, B_rs = Buf(), Buf(), Buf(), Buf(), Buf(), Buf(), Buf()
                B_ob = [Buf(), Buf()]
                s1 = [ps(st, "s1_%d" % i, [128, 512]) for i in range(2)]
                s2 = [ps(st, "s2_%d" % i, [128, 512]) for i in range(2)]
                B_s1 = [Buf(), Buf()]
                B_s2 = [Buf(), Buf()]
                o1p = ps(st, "o1p", [128, 512])
                o2p = ps(st, "o2p", [128, 512])
                l1p = ps(st, "l1p", [128, 512])
                l2p = ps(st, "l2p", [128, 512])
                B_o1p, B_o2p, B_l1p, B_l2p = Buf(), Buf(), Buf(), Buf()
                cnt = 0
                for hi, h in enumerate(heads):
                    k_ = kt[hi % 2]
                    v_ = vv[hi % 2]
                    P.dma("sp", lambda q: q.dma_start(out=k_[:], in_=qkt_d[h, 1]), r=B_qkt[h], w=[B_kt[hi % 2]])
                    vsrc = vi_d[:, h, 0, :].rearrange("(t p) c -> p t c", p=128)
                    for part in range(3):
                        P.dma("sp", lambda q: q.dma_start(out=v_[:, part * 22:(part + 1) * 22, :],
                                                          in_=vsrc[:, part * 22:(part + 1) * 22, :]),
                              r=B_vi, w=[B_vv[hi % 2]])
                    for qc in qchunks:
                        q_ = qt[cnt % 2]
                        bq = B_qt[cnt % 2]
                        o_ = ob[cnt % 2]
                        bo = B_ob[cnt % 2]
                        cnt += 1
                        P.dma("sp", lambda q: q.dma_start(out=q_[:], in_=qkt_d[h, 0][:, TC + qc * 512:TC + (qc + 1) * 512]),
                              r=B_qkt[h], w=[bq])

                        def scores(i):
                            mm1(s1[i % 2][:], k_[0:64, i * 128:(i + 1) * 128], q_[0:64, :], True, True,
                                [B_kt[hi % 2], bq], [B_s1[i % 2]])
                            mm1(s2[i % 2][:], k_[64:128, i * 128:(i + 1) * 128], q_[64:128, :], True, True,
                                [B_kt[hi % 2], bq], [B_s2[i % 2]])

                        scores(0)
                        for i in range(NKT):
                            if i + 1 < NKT:
                                scores(i + 1)
                            pa, pb = p1[i % 3], p2[i % 3]
                            P.op("act", lambda a: a.activation(out=pa[:], in_=s1[i % 2][:], func=AF.Exp, scale=0.125),
                                 r=[B_s1[i % 2]], w=[B_p1[i % 3]])
                            P.op("act", lambda a: a.activation(out=pb[:], in_=s2[i % 2][:], func=AF.Exp, scale=0.125),
                                 r=[B_s2[i % 2]], w=[B_p2[i % 3]])
                            st_, sp_ = (i == 0), (i == NKT - 1)
                            mm1(o1p[:], v_[:, i, :], pa[:], st_, sp_, [B_vv[hi % 2], B_p1[i % 3]], [B_o1p])
                            mm1(l1p[:], ones_bf[:], pa[:], st_, sp_, [B_const, B_p1[i % 3]], [B_l1p])
                            mm1(o2p[:], v_[:, i, :], pb[:], st_, sp_, [B_vv[hi % 2], B_p2[i % 3]], [B_o2p])
                            mm1(l2p[:], ones_bf[:], pb[:], st_, sp_, [B_const, B_p2[i % 3]], [B_l2p])
                        P.op("dve", lambda v: v.reciprocal(out=r1[:], in_=l1p[:]), r=[B_l1p], w=[B_r1])
                        P.op("dve", lambda v: v.tensor_tensor(out=o1[:], in0=o1p[:], in1=r1[:], op=ALU.mult),
                             r=[B_o1p, B_r1], w=[B_o1])
                        P.op("dve", lambda v: v.reciprocal(out=r2[:], in_=l2p[:]), r=[B_l2p], w=[B_r2])
                        P.op("dve", lambda v: v.tensor_tensor(out=o2[:], in0=o2p[:], in1=r2[:], op=ALU.mult),
                             r=[B_o2p, B_r2], w=[B_o2])
                        P.op("dve", lambda v: v.scalar_tensor_tensor(out=oo[:], in0=o2[:], scalar=neglam[:, 0:1], in1=o1[:],
                                                                     op0=ALU.mult, op1=ALU.add),
                             r=[B_o2, B_o1, B_const], w=[B_oo])
                        P.op("pool", lambda g: g.tensor_tensor(out=sq[:], in0=oo[:], in1=oo[:], op=ALU.mult),
                             r=[B_oo], w=[B_sq])
                        P.mm(l1p[:], [(ones_f[:], sq[:])], r=[B_sq, B_const], w=[B_l1p])
                        P.op("dve", lambda v: v.tensor_scalar(out=sq[:], in0=l1p[:], scalar1=1.0 / 128, scalar2=EPS,
                                                              op0=ALU.mult, op1=ALU.add), r=[B_l1p], w=[B_sq])
                        P.op("pool", lambda g: g.tensor_tensor(out=rs[:], in0=sq[:], in1=mhalf[:], op=ALU.pow),
                             r=[B_sq, B_const], w=[B_rs])
                        P.op("dve", lambda v: v.scalar_tensor_tensor(out=o_[:], in0=oo[:], scalar=subln8[:, 0:1], in1=rs[:],
                                                                     op0=ALU.mult, op1=ALU.mult),
                             r=[B_oo, B_rs, B_const], w=[bo])
                        P.dma("pool", lambda q: q.dma_start(out=mixt_d[h * 128:(h + 1) * 128, qc * 512:(qc + 1) * 512], in_=o_[:]),
                              r=[bo], w=[B_mixt[qc]])
                P.barrier()

        def phase_a2(heads=range(NH)):
            with ExitStack() as st:
                o_acc = sb(st, "o_acc", [128, NTILE, 128], F32)
                B_oacc = [Buf() for _ in range(NTILE)]
                rm = sb(st, "rm", [128, 2, 512], F32)
                cmA = sb(st, "cmA", [128, 512], BF16)
                cmB = sb(st, "cmB", [128, 512], BF16)
                hgn = sb(st, "hgn_b", [128, 128], F32)
                B_c2 = Buf()
                P.dma("sp", lambda q: q.dma_start(out=rm[:], in_=rm_d[:, :, 0:512]), w=[B_c2])
                P.dma("sp", lambda q: q.dma_start(out=hgn[:], in_=hgn_d[0:1, :].partition_broadcast(128)), w=[B_c2])
                P.op("dve", lambda v: v.memset(cmA[:], 0.0), w=[B_c2])
                P.op("dve", lambda v: v.memset(cmB[:], 0.0), w=[B_c2])
                P.op("dve", lambda v: v.memset(cmA[:].rearrange("p (t c) -> p t c", c=128)[:, :, 0:64], 1.0), w=[B_c2])
                P.op("dve", lambda v: v.memset(cmB[:].rearrange("p (t c) -> p t c", c=128)[:, :, 64:128], 1.0), w=[B_c2])

                def dbl(name, shape, dt):
                    return [sb(st, "%s%d" % (name, i), shape, dt) for i in range(2)], [Buf(), Buf()]
                zin, B_zin = dbl("zin", [128, 2, 512], F32)
                vt, B_vt = dbl("vt", [128, 4, 128], BF16)
                gt, B_gt = dbl("gt", [128, 4, 128], F32)
                e_, B_e = dbl("e_", [128, 512], F32)
                f_, B_f_ = dbl("f_", [128, 512], F32)
                lf, B_lf = dbl("lf", [128, 512], F32)
                kk, B_kk = dbl("kk", [128, 512], F32)
                bc, B_bc = dbl("bc", [128, 512], F32)
                ep, B_ep = dbl("ep", [128, 512], F32)
                en, B_en = dbl("en", [128, 512], F32)
                kdf, B_kdf = dbl("kdf", [128, 512], F32)
                Qd, B_Qd = dbl("Qd", [128, 512], BF16)
                QdA, B_QdA = dbl("QdA", [128, 512], BF16)
                QdB, B_QdB = dbl("QdB", [128, 512], BF16)
                Kd, B_Kd = dbl("Kd", [128, 512], BF16)
                K2T, B_K2T = dbl("K2T", [128, 512], BF16)
                dec, B_dec = dbl("dec", [128, 8], F32)
                k2, B_k2 = dbl("k2", [128, 128], BF16)
                scm, B_scm = dbl("scm", [128, 128], BF16)
                Sbf, B_Sbf = dbl("Sbf", [128, 128], BF16)
                Sst = sb(st, "Sst", [128, 128], F32)
                B_S = Buf()
                ot, B_ot = dbl("ot", [128, 128], F32)
                ojunk = sb(st, "ojunk", [128, 128], F32)
                B_ojunk = Buf()
                osm = sb(st, "osm", [128, 8], F32)
                B_osm = [Buf(), Buf()]
                yb, B_yb = dbl("yb", [128, 128], BF16)
                mixs, B_mixs = dbl("mixs", [128, 512], BF16)
                tpb = [ps(st, "tpb%d" % i, [128, 1024], BF16) for i in range(2)]
                B_tpb = [Buf(), Buf()]
                scp = [ps(st, "scp%d" % i, [128, 512]) for i in range(2)]
                B_scp = [Buf(), Buf()]
                ops_ = [ps(st, "ops%d" % i, [128, 512]) for i in range(2)]
                B_ops = [Buf(), Buf()]
                ups = [ps(st, "ups%d" % i, [128, 512]) for i in range(2)]
                B_ups = [Buf(), Buf()]
                ctr = dict(sc=0, tile=0, ch=0, tp=0)

                for h in heads:
                    for d in range(2):
                        col = d * 4 + h
                        lb_ap = lbt[:, col:col + 1]
                        oml_ap = omlt[:, col:col + 1]
                        P.op("dve", lambda v: v.memset(Sst[:], 0.0), w=[B_S])
                        P.op("dve", lambda v: v.memset(Sbf[0][:], 0.0), w=[B_Sbf[0]])
                        P.op("dve", lambda v: v.memset(Sbf[1][:], 0.0), w=[B_Sbf[1]])
                        sbi = 0
                        order = list(range(17)) if d == 0 else [0] + list(range(16, 0, -1))
                        for k in order:
                            t0, nt = SCS[k]
                            n = nt * 128
                            s0 = t0 * 128
                            nch = n // 64
                            lat = k >= 1
                            i2 = ctr["sc"] % 2
                            ctr["sc"] += 1
                            z_ = zin[i2]
                            P.dma("sp", lambda q: q.dma_start(out=z_[:, 0, 0:n], in_=zt_d[h, d][:, s0:s0 + n]),
                                  r=B_zt[h], w=[B_zin[i2]])
                            P.dma("sp", lambda q: q.dma_start(out=z_[:, 1, 0:n], in_=zt_d[h, 2][:, s0:s0 + n]),
                                  r=B_zt[h], w=[B_zin[i2]])
                            P.dma("sp", lambda q: q.dma_start(
                                out=vt[i2][:, 0:nt, :], in_=vi_d[s0:s0 + n, h, 1, :].rearrange("(t p) c -> p t c", p=128)),
                                r=B_vi, w=[B_vt[i2]])
                            if lat and d == 1:
                                P.dma("sp", lambda q: q.dma_start(
                                    out=gt[i2][:, 0:nt, :], in_=g_d[s0:s0 + n, h, :].rearrange("(t p) c -> p t c", p=128)),
                                    r=B_g, w=[B_gt[i2]])
                            zz = z_[:, 0, 0:n]
                            hq = z_[:, 1, 0:n]
                            P.op("act", lambda a: a.activation(out=e_[i2][:, 0:n], in_=zz, func=AF.Exp, scale=-1.0),
                                 r=[B_zin[i2]], w=[B_e[i2]])
                            P.op("dve", lambda v: v.tensor_scalar(out=e_[i2][:, 0:n], in0=e_[i2][:, 0:n], scalar1=1.0, scalar2=None,
                                                                  op0=ALU.add), r=[B_e[i2]], w=[B_e[i2]])
                            P.op("dve", lambda v: v.reciprocal(out=e_[i2][:, 0:n], in_=e_[i2][:, 0:n]), r=[B_e[i2]], w=[B_e[i2]])
                            P.op("dve", lambda v: v.tensor_scalar(out=f_[i2][:, 0:n], in0=e_[i2][:, 0:n], scalar1=oml_ap, scalar2=lb_ap,
                                                                  op0=ALU.mult, op1=ALU.add), r=[B_e[i2], B_const], w=[B_f_[i2]])
                            P.op("act", lambda a: a.activation(out=lf[i2][:, 0:n], in_=f_[i2][:, 0:n], func=AF.Ln),
                                 r=[B_f_[i2]], w=[B_lf[i2]])
                            P.op("pool", lambda g: g.tensor_scalar(out=kk[i2][:, 0:n], in0=f_[i2][:, 0:n], scalar1=-1.0, scalar2=1.0,
                                                                   op0=ALU.mult, op1=ALU.add), r=[B_f_[i2]], w=[B_kk[i2]])
                            if d == 0:
                                P.op("dve", lambda v: v.tensor_tensor_scan(out=bc[i2][:, 0:n], data0=rm[:, 0, 0:n], data1=lf[i2][:, 0:n],
                                                                           initial=0.0, op0=ALU.mult, op1=ALU.add),
                                     r=[B_lf[i2], B_c2], w=[B_bc[i2]])
                            else:
                                P.op("dve", lambda v: v.tensor_tensor_scan(out=bc[i2][:, 0:n][:, ::-1], data0=rm[:, 1, 0:n][:, ::-1],
                                                                           data1=lf[i2][:, 0:n][:, ::-1],
                                                                           initial=0.0, op0=ALU.mult, op1=ALU.add),
                                     r=[B_lf[i2], B_c2], w=[B_bc[i2]])
                            P.op("act", lambda a: a.activation(out=ep[i2][:, 0:n], in_=bc[i2][:, 0:n], func=AF.Exp),
                                 r=[B_bc[i2]], w=[B_ep[i2]])
                            P.op("act", lambda a: a.activation(out=en[i2][:, 0:n], in_=bc[i2][:, 0:n], func=AF.Exp, scale=-1.0),
                                 r=[B_bc[i2]], w=[B_en[i2]])
                            if lat:
                                P.op("dve", lambda v: v.tensor_tensor(out=Qd[i2][:, 0:n], in0=hq, in1=ep[i2][:, 0:n], op=ALU.mult),
                                     r=[B_zin[i2], B_ep[i2]], w=[B_Qd[i2]])
                                P.op("pool", lambda g: g.tensor_tensor(out=QdA[i2][:, 0:n], in0=Qd[i2][:, 0:n], in1=cmA[:, 0:n], op=ALU.mult),
                                     r=[B_Qd[i2], B_c2], w=[B_QdA[i2]])
                                P.op("pool", lambda g: g.tensor_tensor(out=QdB[i2][:, 0:n], in0=Qd[i2][:, 0:n], in1=cmB[:, 0:n], op=ALU.mult),
                                     r=[B_Qd[i2], B_c2], w=[B_QdB[i2]])
                            P.op("pool", lambda g: g.tensor_tensor(out=kdf[i2][:, 0:n], in0=kk[i2][:, 0:n], in1=en[i2][:, 0:n], op=ALU.mult),
                                 r=[B_kk[i2], B_en[i2]], w=[B_kdf[i2]])
                            if lat:
                                P.op("pool", lambda g: g.tensor_copy(out=Kd[i2][:, 0:n], in_=kdf[i2][:, 0:n]),
                                     r=[B_kdf[i2]], w=[B_Kd[i2]])
                            endcol = 63 if d == 0 else 0
                            P.op("dve", lambda v: v.tensor_copy(out=dec[i2][:, 0:nch],
                                                                in_=ep[i2][:, 0:n].rearrange("p (c j) -> p c j", j=64)[:, :, endcol]),
                                 r=[B_ep[i2]], w=[B_dec[i2]])
                            P.op("dve", lambda v: v.tensor_tensor(
                                out=K2T[i2][:, 0:n].rearrange("p (c j) -> p c j", j=64),
                                in0=kdf[i2][:, 0:n].rearrange("p (c j) -> p c j", j=64),
                                in1=dec[i2][:, 0:nch].unsqueeze(2).to_broadcast([128, nch, 64]), op=ALU.mult),
                                r=[B_kdf[i2], B_dec[i2]], w=[B_K2T[i2]])
                            tiles = list(range(nt)) if d == 0 else list(range(nt - 1, -1, -1))
                            for j in tiles:
                                cs_ = slice(j * 128, (j + 1) * 128)
                                ti = ctr["tile"] % 2
                                ctr["tile"] += 1
                                tpi = ctr["tp"] % 2
                                ctr["tp"] += 1
                                pe_group([lambda pe: pe.transpose(out=tpb[tpi][:, 0:128], in_=K2T[i2][:, cs_], identity=ident_bf[:])],
                                         r=[B_K2T[i2], B_const], w=[B_tpb[tpi]])
                                P.op("act", lambda a: a.copy(out=k2[ti][:], in_=tpb[tpi][:, 0:128]), r=[B_tpb[tpi]], w=[B_k2[ti]])
                                if lat:
                                    gtile = (k - 1) * 4 + j
                                    mm1(scp[ti][:, 0:128], Kd[i2][:, cs_], Qd[i2][:, cs_], True, True,
                                        [B_Kd[i2], B_Qd[i2]], [B_scp[ti]])
                                    P.op("dve", lambda v: v.tensor_tensor(out=scm[ti][:], in0=scp[ti][:, 0:128], in1=cm_f[:, 1 + d, :],
                                                                          op=ALU.mult), r=[B_scp[ti], B_const], w=[B_scm[ti]])
                                    mm1(ops_[ti][:, 0:128], scm[ti][:], vt[i2][:, j, :], True, False,
                                        [B_scm[ti], B_vt[i2]], [B_ops[ti]])
                                chunks = (0, 1) if d == 0 else (1, 0)
                                for ci, c in enumerate(chunks):
                                    rows = slice(c * 64, (c + 1) * 64)
                                    gc = 2 * j + c
                                    ui = ctr["ch"] % 2
                                    ctr["ch"] += 1
                                    if lat:
                                        qsel = QdA if c == 0 else QdB
                                        bq = B_QdA if c == 0 else B_QdB
                                        mm1(ops_[ti][:, 0:128], qsel[i2][:, cs_], Sbf[sbi][:], False, ci == 1,
                                            [bq[i2], B_Sbf[sbi]], [B_ops[ti]])
                                    mm1(ups[ui][:, 0:128], k2[ti][rows, :], vt[i2][rows, j, :], True, True,
                                        [B_k2[ti], B_vt[i2]], [B_ups[ui]])
                                    P.op("dve", lambda v: v.scalar_tensor_tensor(out=Sst[:], in0=Sst[:], scalar=dec[i2][:, gc:gc + 1],
                                                                                 in1=ups[ui][:, 0:128], op0=ALU.mult, op1=ALU.add),
                                         r=[B_S, B_dec[i2], B_ups[ui]], w=[B_S])
                                    sbi = 1 - sbi
                                    P.op("act", lambda a: a.copy(out=Sbf[sbi][:], in_=Sst[:]), r=[B_S], w=[B_Sbf[sbi]])
                                if lat:
                                    if d == 0:
                                        P.op("act", lambda a: a.copy(out=o_acc[:, gtile, :], in_=ops_[ti][:, 0:128]),
                                             r=[B_ops[ti]], w=[B_oacc[gtile]])
                                    else:
                                        o_ = ot[ti]
                                        P.op("dve", lambda v: v.tensor_tensor(out=o_[:], in0=ops_[ti][:, 0:128], in1=o_acc[:, gtile, :],
                                                                              op=ALU.add), r=[B_ops[ti], B_oacc[gtile]], w=[B_ot[ti]])
                                        ss = osm[:, 2 * ti:2 * ti + 1]
                                        rsd = osm[:, 2 * ti + 1:2 * ti + 2]
                                        P.op("dve", lambda v: v.scalar_tensor_tensor(out=ojunk[:], in0=o_[:], scalar=1.0, in1=o_[:],
                                                                                     op0=ALU.mult, op1=ALU.mult, accum_out=ss),
                                             r=[B_ot[ti]], w=[B_ojunk, B_osm[ti]])
                                        rstd_from_ss(ss, 128, rsd, ss, [B_osm[ti]], [B_osm[ti]], B_osm[ti])
                                        P.op("dve", lambda v: v.scalar_tensor_tensor(out=o_[:], in0=o_[:], scalar=rsd, in1=hgn[:],
                                                                                     op0=ALU.mult, op1=ALU.mult),
                                             r=[B_ot[ti], B_osm[ti], B_c2], w=[B_ot[ti]])
                                        P.op("pool", lambda g: g.tensor_tensor(out=yb[ti][:], in0=o_[:], in1=gt[i2][:, j, :], op=ALU.mult),
                                             r=[B_ot[ti], B_gt[i2]], w=[B_yb[ti]])
                                        tpo = ctr["tp"] % 2
                                        ctr["tp"] += 1
                                        pe_group([lambda pe: pe.transpose(out=tpb[tpo][:, 0:128], in_=yb[ti][:], identity=ident_bf[:])],
                                                 r=[B_yb[ti], B_const], w=[B_tpb[tpo]])
                                        P.op("act", lambda a: a.copy(out=mixs[i2][:, cs_], in_=tpb[tpo][:, 0:128]),
                                             r=[B_tpb[tpo]], w=[B_mixs[i2]])
                            if lat and d == 1:
                                P.dma("pool", lambda q: q.dma_start(
                                    out=mixt_d[512 + h * 128:512 + (h + 1) * 128, (k - 1) * 512:k * 512], in_=mixs[i2][:]),
                                    r=[B_mixs[i2]], w=[B_mixt[k - 1]])
                P.barrier()

        AFF = sb(es, "AFF", [128, NTILE, NE], F32)
        B_AFF = [Buf() for _ in range(NTILE)]
        B_h2t = [Buf() for _ in range(NTILE)]
        B_afft = [Buf() for _ in range(NTILE)]

        def phase_b():
            with ExitStack() as st:
                wo = sb(st, "wo", [128, 8, D], BF16)
                B_wo = [Buf() for _ in range(4)]
                for pi in range(4):
                    P.dma("pool", lambda q: q.dma_start(out=wo[:, 2 * pi:2 * pi + 2, :], in_=wout_d[:, 2 * pi:2 * pi + 2, :]),
                          w=[B_wo[pi]])
                wr = sb(st, "wr", [128, 8, NE], F32)
                B_wr = Buf()
                P.dma("sp", lambda q: q.dma_start(out=wr[:], in_=wr_d[:]), w=[B_wr])
                gpm, B_gpm = load_bc(st, "gpm", 4)
                g2m, B_g2m = load_bc(st, "g2m", 5)
                sh2, B_sh2 = load_bc(st, "sh2", 6)
                mix = [sb(st, "mix%d" % i, [128, 8, 512], BF16) for i in range(2)]
                B_mix = [Buf(), Buf()]
                xb = [sb(st, "bxb%d" % i, [128, D], F32) for i in range(2)]
                B_xb = [Buf(), Buf()]
                tt = [sb(st, "btt%d" % i, [128, D], F32) for i in range(2)]
                B_tt = [Buf(), Buf()]
                x1 = [sb(st, "bx1%d" % i, [128, D], F32) for i in range(2)]
                B_x1s = [Buf(), Buf()]
                h2f = [sb(st, "h2f%d" % i, [128, D], F32) for i in range(2)]
                B_h2f = [Buf(), Buf()]
                h2b = [sb(st, "h2b%d" % i, [128, D], BF16) for i in range(2)]
                B_h2b = [Buf(), Buf()]
                junk = sb(st, "bjunk", [128, D], BF16)
                B_junk = Buf()
                h2T = [sb(st, "h2T%d" % i, [128, 8, 128], F32) for i in range(2)]
                B_h2T = [Buf(), Buf()]
                sm = sb(st, "bsm", [128, 2, 8], F32)
                B_sm = [Buf(), Buf()]
                ee = sb(st, "bee", [128, 2, NE], F32)
                yps = [ps(st, "yps%d" % i, [128, D]) for i in range(2)]
                B_yps = [Buf(), Buf()]
                trp = ps(st, "trp", [128, D])
                B_trp = Buf()
                lgp = ps(st, "lgp", [128, 512])
                B_lgp = Buf()
                for sc in range(16):
                    m_ = mix[sc % 2]
                    P.dma("sp", lambda q: q.dma_start(out=m_[:], in_=mixt_d[:, sc * 512:(sc + 1) * 512].rearrange("(kc p) t -> p kc t", p=128)),
                          r=[B_mixt[sc]], w=[B_mix[sc % 2]])
                    for j in range(4):
                        tl = sc * 4 + j
                        i2 = tl % 2
                        y_ = yps[i2]
                        for half in range(2):
                            P.mm(y_[:, half * 512:(half + 1) * 512],
                                 [(m_[:, kc, j * 128:(j + 1) * 128], wo[:, kc, half * 512:(half + 1) * 512]) for kc in range(8)],
                                 r=[B_mix[sc % 2]] + B_wo, w=[B_yps[i2]])
                        s_ = sm[:, i2, :]
                        for half in range(2):
                            P.op("act", lambda a: a.activation(out=junk[:, half * 512:(half + 1) * 512], in_=y_[:, half * 512:(half + 1) * 512],
                                                               func=AF.Square, accum_out=s_[:, half:half + 1]),
                                 r=[B_yps[i2]], w=[B_junk, B_sm[i2]])
                        P.op("dve", lambda v: v.tensor_tensor(out=s_[:, 2:3], in0=s_[:, 0:1], in1=s_[:, 1:2], op=ALU.add),
                             r=[B_sm[i2]], w=[B_sm[i2]])
                        rstd_from_ss(s_[:, 2:3], D, s_[:, 3:4], s_[:, 2:3], [B_sm[i2]], [B_sm[i2]], B_sm[i2])
                        P.dma("sp", lambda q: q.dma_start(out=xb[i2][:], in_=x_d[tl * 128:(tl + 1) * 128, :]), w=[B_xb[i2]])
                        for half in range(2):
                            hs = slice(half * 512, (half + 1) * 512)
                            P.op("dve", lambda v: v.scalar_tensor_tensor(out=tt[i2][:, hs], in0=y_[:, hs], scalar=s_[:, 3:4], in1=gpm[:, hs],
                                                                         op0=ALU.mult, op1=ALU.mult),
                                 r=[B_yps[i2], B_sm[i2], B_gpm], w=[B_tt[i2]])
                        P.op("pool", lambda g: g.tensor_tensor(out=x1[i2][:], in0=tt[i2][:], in1=xb[i2][:], op=ALU.add),
                             r=[B_tt[i2], B_xb[i2]], w=[B_x1s[i2]])
                        P.dma("pool", lambda q: q.dma_start(out=x1_d[tl * 128:(tl + 1) * 128, :], in_=x1[i2][:]),
                              r=[B_x1s[i2]], w=[B_x1[tl]])
                        P.op("dve", lambda v: v.scalar_tensor_tensor(out=junk[:], in0=x1[i2][:], scalar=1.0, in1=x1[i2][:],
                                                                     op0=ALU.mult, op1=ALU.mult, accum_out=s_[:, 4:5]),
                             r=[B_x1s[i2]], w=[B_junk, B_sm[i2]])
                        rstd_from_ss(s_[:, 4:5], D, s_[:, 5:6], s_[:, 4:5], [B_sm[i2]], [B_sm[i2]], B_sm[i2])
                        P.op("dve", lambda v: v.scalar_tensor_tensor(out=tt[i2][:], in0=x1[i2][:], scalar=s_[:, 5:6], in1=g2m[:],
                                                                     op0=ALU.mult, op1=ALU.mult),
                             r=[B_x1s[i2], B_sm[i2], B_g2m], w=[B_tt[i2]])
                        P.op("pool", lambda g: g.tensor_tensor(out=h2f[i2][:], in0=tt[i2][:], in1=sh2[:], op=ALU.add),
                             r=[B_tt[i2], B_sh2], w=[B_h2f[i2]])
                        P.op("act", lambda a: a.copy(out=h2b[i2][:], in_=h2f[i2][:]), r=[B_h2f[i2]], w=[B_h2b[i2]])
                        P.dma("pool", lambda q: q.dma_start(out=h2_d[tl * 128:(tl + 1) * 128, :], in_=h2b[i2][:]),
                              r=[B_h2b[i2]], w=[B_h2t[tl]])
                        pe_group([(lambda pe, kc=kc: pe.transpose(out=trp[:, kc * 128:(kc + 1) * 128],
                                                                   in_=h2f[i2][:, kc * 128:(kc + 1) * 128], identity=ident_f))
                                  for kc in range(8)], r=[B_h2f[i2], B_const], w=[B_trp])
                        P.op("act", lambda a: a.copy(out=h2T[i2][:, 0:4, :].rearrange("p k t -> p (k t)"), in_=trp[:, 0:512]),
                             r=[B_trp], w=[B_h2T[i2]])
                        P.op("dve", lambda v: v.tensor_copy(out=h2T[i2][:, 4:8, :].rearrange("p k t -> p (k t)"), in_=trp[:, 512:1024]),
                             r=[B_trp], w=[B_h2T[i2]])
                        P.mm(lgp[:, 0:NE], [(h2T[i2][:, kc, :], wr[:, kc, :]) for kc in range(8)],
                             r=[B_h2T[i2], B_wr], w=[B_lgp])
                        P.op("dve", lambda v: v.tensor_reduce(out=s_[:, 6:7], in_=lgp[:, 0:NE], axis=AX.X, op=ALU.max, negate=True),
                             r=[B_lgp], w=[B_sm[i2]])
                        P.op("act", lambda a: a.activation(out=ee[:, i2, :], in_=lgp[:, 0:NE], func=AF.Exp, bias=s_[:, 6:7],
                                                           accum_out=s_[:, 7:8]), r=[B_lgp, B_sm[i2]], w=[B_sm[i2]])
                        P.op("dve", lambda v: v.reciprocal(out=s_[:, 7:8], in_=s_[:, 7:8]), r=[B_sm[i2]], w=[B_sm[i2]])
                        P.op("dve", lambda v: v.tensor_scalar(out=AFF[:, tl, :], in0=ee[:, i2, :], scalar1=s_[:, 7:8], scalar2=None,
                                                              op0=ALU.mult), r=[B_sm[i2]], w=[B_AFF[tl]])
                        P.dma("pool", lambda q: q.dma_start(out=aff_d[tl * 128:(tl + 1) * 128, :], in_=AFF[:, tl, :]),
                              r=[B_AFF[tl]], w=[B_afft[tl]])
                P.barrier()

        posm = sb(es, "posm", [128, NE, NTILE], F32)
        B_posm = Buf()

        def phase_c():
            with ExitStack() as st:
                lo = sb(st, "c_lo", [128, NE], F32)
                hi = sb(st, "c_hi", [128, NE], F32)
                mid = sb(st, "c_mid", [128, NE], F32)
                ge = sb(st, "c_ge", [128, NTILE, NE], F32)
                cntp = sb(st, "c_cntp", [128, NE], F32)
                mge = sb(st, "c_mge", [128, NE], U32)
                mlt = sb(st, "c_mlt", [128, NE], U32)
                Mt = sb(st, "c_Mt", [128, NE, NTILE], F32)
                Psc = sb(st, "c_Psc", [128, NE, NTILE], F32)
                rmc = sb(st, "c_rmc", [128, 1024], F32)
                Tt = sb(st, "c_Tt", [128, NE], BF16)
                Lbf = sb(st, "c_Lbf", [128, 128], BF16)
                off = sb(st, "c_off", [128, NE], F32)
                cps = ps(st, "c_cps", [128, 512])
                B_lo, B_hi, B_mid, B_ge, B_cntp, B_m, B_cps, B_x = Buf(), Buf(), Buf(), Buf(), Buf(), Buf(), Buf(), Buf()
                P.dma("sp", lambda q: q.dma_start(out=rmc[:], in_=rm_d[:, 0, :]), w=[B_x])
                P.op("dve", lambda v: v.tensor_copy(out=Lbf[:], in_=cm_f[:, 3, :]), r=[B_const], w=[B_x])
                P.op("dve", lambda v: v.memset(lo[:], 0.0), w=[B_lo])
                P.op("dve", lambda v: v.memset(hi[:], 2.0), w=[B_hi])
                for it in range(34):
                    P.op("dve", lambda v: v.tensor_tensor(out=mid[:], in0=lo[:], in1=hi[:], op=ALU.add), r=[B_lo, B_hi], w=[B_mid])
                    P.op("dve", lambda v: v.tensor_scalar(out=mid[:], in0=mid[:], scalar1=0.5, scalar2=None, op0=ALU.mult),
                         r=[B_mid], w=[B_mid])
                    P.op("dve", lambda v: v.tensor_tensor(out=ge[:], in0=AFF[:], in1=mid[:].unsqueeze(1).to_broadcast([128, NTILE, NE]),
                                                          op=ALU.is_ge), r=B_AFF + [B_mid], w=[B_ge])
                    P.op("dve", lambda v: v.tensor_reduce(out=cntp[:], in_=ge[:].rearrange("p i e -> p e i"), axis=AX.X, op=ALU.add),
                         r=[B_ge], w=[B_cntp])
                    P.mm(cps[:, 0:NE], [(ones_f[:], cntp[:])], r=[B_cntp, B_const], w=[B_cps])
                    P.op("dve", lambda v: v.tensor_scalar(out=mge[:], in0=cps[:, 0:NE], scalar1=float(CAP), scalar2=None, op0=ALU.is_ge),
                         r=[B_cps], w=[B_m])
                    P.op("dve", lambda v: v.tensor_scalar(out=mlt[:], in0=cps[:, 0:NE], scalar1=float(CAP), scalar2=None, op0=ALU.is_lt),
                         r=[B_cps], w=[B_m])
                    P.op("dve", lambda v: v.copy_predicated(out=lo[:], mask=mge[:], data=mid[:]), r=[B_m, B_mid], w=[B_lo])
                    P.op("dve", lambda v: v.copy_predicated(out=hi[:], mask=mlt[:], data=mid[:]), r=[B_m, B_mid], w=[B_hi])
                P.op("dve", lambda v: v.tensor_tensor(out=ge[:], in0=AFF[:], in1=lo[:].unsqueeze(1).to_broadcast([128, NTILE, NE]),
                                                      op=ALU.is_ge), r=B_AFF + [B_lo], w=[B_ge])
                P.op("dve", lambda v: v.tensor_copy(out=Mt[:], in_=ge[:].rearrange("p i e -> p e i")), r=[B_ge], w=[B_x])
                P.op("dve", lambda v: v.tensor_tensor_scan(out=Psc[:].rearrange("p e i -> p (e i)"), data0=rmc[:],
                                                           data1=Mt[:].rearrange("p e i -> p (e i)"), initial=0.0,
                                                           op0=ALU.mult, op1=ALU.add), r=[B_x], w=[B_x])
                P.op("dve", lambda v: v.tensor_copy(out=Tt[:], in_=Psc[:, :, NTILE - 1]), r=[B_x], w=[B_x])
                P.mm(cps[:, 0:NE], [(Lbf[:], Tt[:])], r=[B_x], w=[B_cps])
                P.op("dve", lambda v: v.tensor_copy(out=off[:], in_=cps[:, 0:NE]), r=[B_cps], w=[B_x])
                P.op("dve", lambda v: v.tensor_tensor(out=Psc[:], in0=Psc[:], in1=off[:].unsqueeze(2).to_broadcast([128, NE, NTILE]),
                                                      op=ALU.add), r=[B_x], w=[B_x])
                P.op("dve", lambda v: v.tensor_tensor(out=Psc[:], in0=Psc[:], in1=Mt[:], op=ALU.mult), r=[B_x], w=[B_x])
                P.op("dve", lambda v: v.tensor_scalar(out=posm[:], in0=Psc[:], scalar1=-1.0, scalar2=None, op0=ALU.add),
                     r=[B_x], w=[B_posm])
                P.barrier()

        def idma(fn, r, w):
            return P.dma("pool", fn, r=r, w=w)

        def phase_d(experts=range(NE)):
            with ExitStack() as st:
                iota = sb(st, "d_iota", [128, 1024], F32)
                tokf = sb(st, "d_tokf", [128, NTILE, 2], F32)
                tokb = sb(st, "d_tokb", [128, NTILE, 2], BF16)
                zt_ = sb(st, "d_zero", [128, D], F32)
                B_dc = Buf()
                P.dma("sp", lambda q: q.dma_start(out=iota[:], in_=iota_d[:]), w=[B_dc])
                P.dma("sp", lambda q: q.dma_start(out=tokf[:], in_=tokhl_d[:]), w=[B_dc])
                P.op("dve", lambda v: v.tensor_copy(out=tokb[:], in_=tokf[:]), r=[B_dc], w=[B_dc])
                P.op("dve", lambda v: v.memset(zt_[:], 0.0), w=[B_dc])
                fview = f_d.rearrange("(t p) d -> p t d", p=128)
                for part in range(4):
                    P.dma("sp", lambda q: q.dma_start(out=fview[:, part * 16:(part + 1) * 16, :],
                                                      in_=zt_[:].unsqueeze(1).to_broadcast([128, 16, D])), r=[B_dc], w=[B_f])
                sel = [sb(st, "d_sel%d" % i, [128, 1024], BF16) for i in range(4)]
                B_sel = [Buf() for _ in range(4)]
                idxf = sb(st, "d_idxf", [2, 1024], F32)
                idx2 = sb(st, "d_idx2", [128, 8], F32)
                idxi = [sb(st, "d_idxi%d" % i, [128, 8], I32) for i in range(2)]
                B_idxf, B_idx2 = Buf(), Buf()
                B_idxi = [Buf(), Buf()]
                X = [sb(st, "d_X%d" % i, [128, D], BF16) for i in range(8)]
                B_X = [Buf() for _ in range(8)]
                gat = [sb(st, "d_gat%d" % i, [128, 8, NE], F32) for i in range(2)]
                B_gat = [Buf(), Buf()]
                XT = sb(st, "d_XT", [128, 8, 1024], BF16)
                B_XT = Buf()
                AT = sb(st, "d_AT", [128, 8, 1024], BF16)
                B_AT = Buf()
                W = [[sb(st, "d_w%d_%d" % (m, i), [128, 8, D], BF16) for m in range(3)] for i in range(2)]
                B_W = [[[Buf() for _ in range(4)] for _ in range(3)] for _ in range(2)]
                sg = [sb(st, "d_sg%d" % i, [128, 512], F32) for i in range(2)]
                B_sg = [Buf(), Buf()]
                Ysb = [sb(st, "d_Y%d" % i, [128, D], F32) for i in range(2)]
                B_Y = [Buf(), Buf()]
                ips = [ps(st, "d_ips%d" % i, [128, 512]) for i in range(2)]
                B_ips = [Buf(), Buf()]
                tpx = ps(st, "d_tpx", [128, 8, 128], BF16)
                B_tpx = Buf()
                itp = ps(st, "d_itp", [128, 512])
                B_itp = Buf()
                gps = [ps(st, "d_gps%d" % i, [128, 512]) for i in range(2)]
                B_gps = [Buf(), Buf()]
                ups = [ps(st, "d_ups%d" % i, [128, 512]) for i in range(2)]
                B_ups = [Buf(), Buf()]
                wsrc = (wg_d, wu_d, wd_d)

                def load_w(e, slot):
                    for m in range(3):
                        for pi in range(4):
                            P.dma("pool", lambda q: q.dma_start(out=W[slot][m][:, 2 * pi:2 * pi + 2, :],
                                                                in_=wsrc[m][e][:, 2 * pi:2 * pi + 2, :]), w=[B_W[slot][m][pi]])

                elist = list(experts)
                load_w(elist[0], 0)
                prev_scatter = []
                ctr = dict(sel=0, g=0, y=0)
                for ei, e in enumerate(elist):
                    slot = ei % 2
                    if ei + 1 < len(elist):
                        load_w(elist[ei + 1], 1 - slot)
                    for i in range(NTILE):
                        si = ctr["sel"] % 4
                        ctr["sel"] += 1
                        eng = "dve" if i % 2 == 0 else "pool"
                        P.op(eng, lambda v: v.tensor_scalar(out=sel[si][:], in0=iota[:], scalar1=posm[:, e, i:i + 1], scalar2=None,
                                                            op0=ALU.is_equal), r=[B_dc, B_posm], w=[B_sel[si]])
                        for half in range(2):
                            mm1(ips[half][0:2, :], tokb[:, i, :], sel[si][:, half * 512:(half + 1) * 512], i == 0, i == NTILE - 1,
                                [B_dc, B_sel[si]], [B_ips[half]])
                    for half in range(2):
                        P.op("act", lambda a: a.copy(out=idxf[:, half * 512:(half + 1) * 512], in_=ips[half][0:2, :]),
                             r=[B_ips[half]], w=[B_idxf])
                    pe_group([(lambda pe, jt=jt: pe.transpose(out=itp[:, 2 * jt:2 * jt + 2], in_=idxf[0:2, jt * 128:(jt + 1) * 128],
                                                               identity=ident_f[0:2, 0:2])) for jt in range(8)],
                             r=[B_idxf, B_const], w=[B_itp])
                    P.op("dve", lambda v: v.tensor_reduce(out=idx2[:], in_=itp[:, 0:16].rearrange("p (j t) -> p j t", t=2),
                                                          axis=AX.X, op=ALU.add), r=[B_itp], w=[B_idx2])
                    ii = idxi[slot]
                    P.op("dve", lambda v: v.tensor_copy(out=ii[:], in_=idx2[:]), r=[B_idx2], w=[B_idxi[slot]])
                    g_ = gat[slot]
                    for jt in range(8):
                        idma(lambda q: q.indirect_dma_start(out=X[jt][:], out_offset=None, in_=h2_d[:, :],
                                                            in_offset=IndirectOffsetOnAxis(ap=ii[:, jt:jt + 1], axis=0)),
                             r=[B_idxi[slot]] + B_h2t, w=[B_X[jt]])
                        idma(lambda q: q.indirect_dma_start(out=g_[:, jt, :], out_offset=None, in_=aff_d[:, :],
                                                            in_offset=IndirectOffsetOnAxis(ap=ii[:, jt:jt + 1], axis=0)),
                             r=[B_idxi[slot]] + B_afft, w=[B_gat[slot]])
                    for jt in range(8):
                        pe_group([(lambda pe, kc=kc: pe.transpose(out=tpx[:, kc, :], in_=X[jt][:, kc * 128:(kc + 1) * 128],
                                                                   identity=ident_bf[:])) for kc in range(8)],
                                 r=[B_X[jt], B_const], w=[B_tpx])
                        eng = "act" if jt % 2 == 0 else "dve"
                        if eng == "act":
                            P.op("act", lambda a: a.copy(out=XT[:, :, jt * 128:(jt + 1) * 128], in_=tpx[:]), r=[B_tpx], w=[B_XT])
                        else:
                            P.op("dve", lambda v: v.tensor_copy(out=XT[:, :, jt * 128:(jt + 1) * 128], in_=tpx[:]), r=[B_tpx], w=[B_XT])
                    wg_, wu_, wd_ = W[slot]
                    bwg, bwu, bwd = B_W[slot]
                    for fc in range(8):
                        for sh in range(2):
                            gi = ctr["g"] % 2
                            ctr["g"] += 1
                            cs_ = slice(sh * 512, (sh + 1) * 512)
                            P.mm(gps[gi][:], [(wg_[:, kc, fc * 128:(fc + 1) * 128], XT[:, kc, cs_]) for kc in range(8)],
                                 r=[B_XT] + bwg, w=[B_gps[gi]])
                            P.mm(ups[gi][:], [(wu_[:, kc, fc * 128:(fc + 1) * 128], XT[:, kc, cs_]) for kc in range(8)],
                                 r=[B_XT] + bwu, w=[B_ups[gi]])
                            P.op("act", lambda a: a.activation(out=sg[gi][:], in_=gps[gi][:], func=AF.Silu),
                                 r=[B_gps[gi]], w=[B_sg[gi]])
                            P.op("dve", lambda v: v.tensor_tensor(out=AT[:, fc, cs_], in0=ups[gi][:], in1=sg[gi][:], op=ALU.mult),
                                 r=[B_ups[gi], B_sg[gi]], w=[B_AT])
                    new_scatter = []
                    for jt in range(8):
                        yi = ctr["y"] % 2
                        ctr["y"] += 1
                        for dh in range(2):
                            gi = ctr["g"] % 2
                            ctr["g"] += 1
                            P.mm(gps[gi][:], [(AT[:, fc, jt * 128:(jt + 1) * 128], wd_[:, fc, dh * 512:(dh + 1) * 512]) for fc in range(8)],
                                 r=[B_AT] + bwd, w=[B_gps[gi]])
                            P.op("dve", lambda v: v.tensor_scalar(out=Ysb[yi][:, dh * 512:(dh + 1) * 512], in0=gps[gi][:],
                                                                  scalar1=g_[:, jt, e:e + 1], scalar2=None, op0=ALU.mult),
                                 r=[B_gps[gi], B_gat[slot]], w=[B_Y[yi]])
                        idma(lambda q: q.indirect_dma_start(out=f_d[:, :], out_offset=IndirectOffsetOnAxis(ap=ii[:, jt:jt + 1], axis=0),
                                                            in_=Ysb[yi][:], in_offset=None, compute_op=ALU.add),
                             r=[B_Y[yi], B_idxi[slot]], w=[B_f])
                P.barrier()

        def phase_e():
            with ExitStack() as st:
                gpf, B_gpf = load_bc(st, "gpf", 7)
                fb = [sb(st, "e_f%d" % i, [128, D], F32) for i in range(2)]
                xb = [sb(st, "e_x%d" % i, [128, D], F32) for i in range(2)]
                tb = [sb(st, "e_t%d" % i, [128, D], F32) for i in range(2)]
                ob_ = [sb(st, "e_o%d" % i, [128, D], F32) for i in range(2)]
                junk = sb(st, "e_junk", [128, D], BF16)
                sm = sb(st, "e_sm", [128, 2, 2], F32)
                B_fb, B_xb, B_tb, B_ob, B_sm = [Buf(), Buf()], [Buf(), Buf()], [Buf(), Buf()], [Buf(), Buf()], [Buf(), Buf()]
                B_junk = Buf()
                B_out = [Buf() for _ in range(NTILE)]
                for tl in range(NTILE):
                    i2 = tl % 2
                    rows = slice(tl * 128, (tl + 1) * 128)
                    P.dma("sp", lambda q: q.dma_start(out=fb[i2][:], in_=f_d[rows, :]), r=[B_f], w=[B_fb[i2]])
                    P.dma("sp", lambda q: q.dma_start(out=xb[i2][:], in_=x1_d[rows, :]), r=[B_x1[tl]], w=[B_xb[i2]])
                    P.op("dve", lambda v: v.scalar_tensor_tensor(out=junk[:], in0=fb[i2][:], scalar=1.0, in1=fb[i2][:],
                                                                 op0=ALU.mult, op1=ALU.mult, accum_out=sm[:, i2, 0:1]),
                         r=[B_fb[i2]], w=[B_junk, B_sm[i2]])
                    rstd_from_ss(sm[:, i2, 0:1], D, sm[:, i2, 1:2], sm[:, i2, 0:1], [B_sm[i2]], [B_sm[i2]], B_sm[i2])
                    P.op("dve", lambda v: v.scalar_tensor_tensor(out=tb[i2][:], in0=fb[i2][:], scalar=sm[:, i2, 1:2], in1=gpf[:],
                                                                 op0=ALU.mult, op1=ALU.mult),
                         r=[B_fb[i2], B_sm[i2], B_gpf], w=[B_tb[i2]])
                    P.op("pool", lambda g: g.tensor_tensor(out=ob_[i2][:], in0=tb[i2][:], in1=xb[i2][:], op=ALU.add),
                         r=[B_tb[i2], B_xb[i2]], w=[B_ob[i2]])
                    P.dma("pool", lambda q: q.dma_start(out=out_d[rows, :], in_=ob_[i2][:]), r=[B_ob[i2]], w=[B_out[tl]])
                P.barrier()

        import os
        if stop_after == "0":
            return nc
        phase_a0()
        if stop_after == "A0":
            return nc
        if stop_after == "A1":
            phase_a1(heads=[int(x) for x in os.environ.get("A1_HEADS", "0").split(",")],
                     qchunks=[int(x) for x in os.environ.get("A1_QC", "0,9").split(",")])
            return nc
        if stop_after == "A2":
            phase_a2(heads=[int(x) for x in os.environ.get("A2_HEADS", "0").split(",")])
            return nc
        if not os.environ.get("SKIP_A1"):
            phase_a1()
        phase_a2()
        phase_b()
        if stop_after == "B":
            return nc
        phase_c()
        if stop_after == "C":
            return nc
        phase_d()
        phase_e()
        return nc


def _rope_tables():
    half = 32
    inv_freq = (1.0 / (10000.0 ** (np.arange(0, half, 2, dtype=np.float32) / np.float32(half)))).astype(np.float32)
    t = np.arange(T)
    r = (t // 64).astype(np.float32)
    c = (t % 64).astype(np.float32)
    ang_r = r[:, None] * inv_freq[None, :]
    ang_c = c[:, None] * inv_freq[None, :]
    ang = np.concatenate([ang_r, ang_r, ang_c, ang_c], axis=-1).astype(np.float32)
    cos = np.cos(ang).astype(np.float32)
    sin = np.sin(ang).astype(np.float32)
    sign = np.concatenate([-np.ones(16), np.ones(16), -np.ones(16), np.ones(16)]).astype(np.float32)
    sin = sin * sign[None, :]
    cosT = np.ones((128, S), np.float32)
    sinT = np.zeros((128, S), np.float32)
    cosT[:, TC:] = np.concatenate([cos.T, cos.T], axis=0)
    sinT[:, TC:] = np.concatenate([sin.T, sin.T], axis=0)
    return cosT, sinT


def _win_cols():
    rot = np.concatenate([np.arange(16, 32), np.arange(0, 16), np.arange(48, 64), np.arange(32, 48)])
    fm, tm, tg = [], [], []
    for h in range(NH):
        for off in (0, 512):
            base = off + h * 128
            fm.append(base + np.arange(128))
            fm.append(np.concatenate([base + rot, base + 64 + rot]))
        fm.append(1536 + h * 128 + np.arange(128))
        fm.append(2048 + h * 128 + np.arange(128))
        fm.append(3072 + h * 128 + np.arange(128))
        tm.append(1024 + h * 128 + np.arange(128))
        tm.append(2560 + h * 128 + np.arange(128))
        tg.append(3584 + h * 128 + np.arange(128))
    tm = tm + tg
    return np.concatenate(fm + tm)


def _kc(a):
    n = a.shape[-1]
    return np.ascontiguousarray(a.reshape(8, 128, n).transpose(1, 0, 2))


def prep_inputs(inp, n_cores):
    f = lambda k: np.asarray(inp[k], dtype=np.float32)
    x, c, ctx, c_ctx = f("x"), f("c"), f("ctx"), f("c_ctx")
    cosT, sinT = _rope_tables()
    p = np.arange(128)
    blk = p // 64
    same = blk[:, None] == blk[None, :]
    cm = np.zeros((128, 4, 128), np.float32)
    cm[:, 0, :] = np.eye(128)
    cm[:, 1, :] = same & (p[:, None] <= p[None, :])
    cm[:, 2, :] = same & (p[:, None] >= p[None, :])
    cm[:, 3, :] = p[:, None] < p[None, :]
    j = np.arange(1024)
    rm = np.ones((128, 2, 1024), np.float32)
    rm[:, 0, j % 64 == 0] = 0.0
    rm[:, 1, j % 64 == 63] = 0.0
    iota = np.broadcast_to(j.astype(np.float32), (128, 1024)).copy()
    tt = np.arange(NTILE)[None, :] * 128 + p[:, None]
    tokhl = np.stack([64 * (tt // 64), tt % 64], axis=-1).astype(np.float32)
    hlb = f("hg_lower_bound").reshape(2, 2, 4, 128).transpose(3, 0, 1, 2).reshape(128, 16)
    shared = {
        "w_ada": _kc(f("w_ada")[0]),
        "b_ada": f("b_ada")[0][None, :],
        "norms": np.concatenate([f("norm_pre_mix")[0], f("norm_post_mix")[0], f("norm_pre_ffn")[0],
                                 f("norm_post_ffn")[0]])[None, :],
        "w_in": _kc(f("w_in")[0][:, _win_cols()]),
        "lamv": np.concatenate([f("da_lambda_q1")[0], f("da_lambda_k1")[0], f("da_lambda_q2")[0],
                                f("da_lambda_k2")[0]])[None, :],
        "subln": f("da_subln")[0][:, None],
        "hgn": f("hg_norm")[0][None, :],
        "hlb": np.ascontiguousarray(hlb),
        "w_out": _kc(f("w_out")[0]),
        "w_r": _kc(f("w_router")[0]),
        "w_gate": np.ascontiguousarray(f("w_gate")[0].reshape(NE, 8, 128, D).transpose(0, 2, 1, 3)),
        "w_up": np.ascontiguousarray(f("w_up")[0].reshape(NE, 8, 128, D).transpose(0, 2, 1, 3)),
        "w_down": np.ascontiguousarray(f("w_down")[0].reshape(NE, 8, 128, D).transpose(0, 2, 1, 3)),
        "cosT": cosT, "sinT": sinT, "cmasks": cm, "rmask": rm, "iota": iota, "tokhl": tokhl,
    }
    maps = []
    for i in range(n_cores):
        b = i % 2
        m = dict(shared)
        m["x"] = np.ascontiguousarray(x[b])
        m["ctx"] = np.ascontiguousarray(ctx[b])
        m["cc"] = _kc(np.stack([c[b], c_ctx], axis=1))
        maps.append(m)
    return maps


N_CORES = 2
_NC_CACHE = {}


def kernel(**inputs):
    if "nc" not in _NC_CACHE:
        _NC_CACHE["nc"] = build()
    nc = _NC_CACHE["nc"]
    maps = prep_inputs(inputs, N_CORES)
    res = run_bass_kernel_spmd(nc, maps, core_ids=list(range(N_CORES)))
    out = np.stack([np.asarray(res.results[b]["out"], dtype=np.float32) for b in range(2)], axis=0)
    return out
```

```python
import numpy as np
from contextlib import ExitStack
import concourse.bass as bass
import concourse.mybir as mybir
from concourse.bass import IndirectOffsetOnAxis
from concourse.bass_utils import run_bass_kernel_spmd

F32 = mybir.dt.float32
BF16 = mybir.dt.bfloat16
I32 = mybir.dt.int32
U32 = mybir.dt.uint32
AF = mybir.ActivationFunctionType
ALU = mybir.AluOpType
AX = mybir.AxisListType

D = 1024
T = 8192
TC = 256
S = T + TC
NH = 4
NE = 16
CAP = 1024
EPS = 1e-6
NTILE = T // 128
FM_BLOCKS = 7
TM_COLS = 384
HEAD_COLS = FM_BLOCKS * 128 + TM_COLS
FM_TOTAL = NH * FM_BLOCKS * 128
WCOLS = NH * HEAD_COLS


class Buf:
    __slots__ = ("w", "r")

    def __init__(self):
        self.w = None
        self.r = {}


class Prog:
    def __init__(self, nc, es, ndma=12):
        self.nc = nc
        self.eng = dict(pe=nc.tensor, act=nc.scalar, dve=nc.vector, pool=nc.gpsimd, sp=nc.sync)
        self.sem = {k: es.enter_context(nc.semaphore("s_" + k)) for k in self.eng}
        self.cnt = {k: 0 for k in self.eng}
        self.waited = {k: {} for k in self.eng}
        self.dsem = {q: [[es.enter_context(nc.semaphore("d_%s%d" % (q, i))), 0] for i in range(ndma)]
                     for q in ("sp", "pool")}
        self.dnext = {"sp": 0, "pool": 0}
        self.nwait = 0
        self.nops = 0
        import os
        self.limit = int(os.environ.get('OPLIMIT', '1000000000'))

    def _wait(self, e, ev):
        s, v = ev
        w = self.waited[e]
        if w.get(s.num, 0) < v:
            self.eng[e].wait_ge(s, v)
            w[s.num] = v
            self.nwait += 1

    def _deps(self, e, reads, writes):
        own = self.sem[e].num
        for b in reads:
            if b.w is not None:
                if not (e == "pe" and b.w[0].num == own):
                    self._wait(e, b.w)
        for b in writes:
            if b.w is not None:
                if not (e == "pe" and b.w[0].num == own):
                    self._wait(e, b.w)
            for ev in b.r.values():
                if ev[0].num == own:
                    continue
                self._wait(e, ev)

    def _mark(self, ev, reads, writes):
        k = ev[0].num
        for b in reads:
            old = b.r.get(k)
            if old is None or old[1] < ev[1]:
                b.r[k] = ev
        for b in writes:
            b.w = ev
            b.r = {}

    def op(self, e, fn, r=(), w=()):
        self.nops += 1
        if self.nops > self.limit:
            return None
        if self.nops == self.limit:
            print('LAST OP', e, fn.__code__.co_firstlineno)
        self._deps(e, r, w)
        ins = fn(self.eng[e])
        self.cnt[e] += 1
        ins.then_inc(self.sem[e], 1)
        ev = (self.sem[e], self.cnt[e])
        self._mark(ev, r, w)
        return ev

    def mm(self, out_ap, pairs, r=(), w=()):
        self.nops += 1
        if self.nops > self.limit:
            return None
        self._deps("pe", r, w)
        n = len(pairs)
        ins = None
        for i, (l, rh) in enumerate(pairs):
            ins = self.nc.tensor.matmul(out_ap, lhsT=l, rhs=rh, start=(i == 0), stop=(i == n - 1))
        self.cnt["pe"] += 1
        ins.then_inc(self.sem["pe"], 1)
        ev = (self.sem["pe"], self.cnt["pe"])
        self._mark(ev, r, w)
        return ev

    def dma(self, q, fn, r=(), w=()):
        self.nops += 1
        if self.nops > self.limit:
            return None
        slots = self.dsem[q]
        i = self.dnext[q]
        self.dnext[q] = (i + 1) % len(slots)
        s, v = slots[i]
        if v > 0:
            self._wait(q, (s, v))
        self._deps(q, r, w)
        ins = fn(self.eng[q])
        slots[i][1] = v + 16
        ins.then_inc(s, 16)
        ev = (s, v + 16)
        self._mark(ev, r, w)
        return ev

    def all_events(self):
        evs = [(self.sem[k], self.cnt[k]) for k in self.eng if self.cnt[k] > 0]
        for q in self.dsem:
            for s, v in self.dsem[q]:
                if v > 0:
                    evs.append((s, v))
        return evs

    def barrier(self, engines=None):
        evs = self.all_events()
        for e in (engines or self.eng):
            for ev in evs:
                if ev[0].num != self.sem[e].num:
                    self._wait(e, ev)


def build(stop_after=None, dbg=()):
    nc = bass.Bass("TRN2", target_bir_lowering=False)
    dbg = set(dbg)

    def din(name, shape, dt=F32):
        return nc.dram_tensor(name, list(shape), dt, kind="ExternalInput").ap()

    def dscr(name, shape, dt):
        kind = "ExternalOutput" if name in dbg else "Internal"
        return nc.dram_tensor(name, list(shape), dt, kind=kind).ap()

    x_d = din("x", [T, D])
    ctx_d = din("ctx", [TC, D])
    cc_d = din("cc", [128, 8, 2])
    wada_d = din("w_ada", [128, 8, 6 * D])
    bada_d = din("b_ada", [1, 6 * D])
    norms_d = din("norms", [1, 4 * D])
    win_d = din("w_in", [128, 8, WCOLS])
    lamv_d = din("lamv", [1, 256])
    subln_d = din("subln", [128, 1])
    hgn_d = din("hgn", [1, 128])
    hlb_d = din("hlb", [128, 16])
    wout_d = din("w_out", [128, 8, D])
    wr_d = din("w_r", [128, 8, NE])
    wg_d = din("w_gate", [NE, 128, 8, D])
    wu_d = din("w_up", [NE, 128, 8, D])
    wd_d = din("w_down", [NE, 128, 8, D])
    cos_d = din("cosT", [128, S])
    sin_d = din("sinT", [128, S])
    cm_d = din("cmasks", [128, 4, 128])
    rm_d = din("rmask", [128, 2, 1024])
    iota_d = din("iota", [128, 1024])
    tokhl_d = din("tokhl", [128, NTILE, 2])
    out_d = nc.dram_tensor("out", [T, D], F32, kind="ExternalOutput").ap()

    modrows_d = dscr("modrows", [8, D], F32)
    qkt_d = dscr("qkt", [NH, 2, 128, S], BF16)
    zt_d = dscr("zt", [NH, 3, 128, S], F32)
    vi_d = dscr("vi", [S, NH, 2, 128], BF16)
    g_d = dscr("gsil", [S, NH, 128], F32)
    mixt_d = dscr("mixt", [D, T], BF16)
    x1_d = dscr("x1", [T, D], F32)
    h2_d = dscr("h2", [T, D], BF16)
    aff_d = dscr("aff", [T, NE], F32)
    f_d = dscr("facc", [T, D], F32)

    B_modrows = Buf()
    B_qkt = [[Buf() for _ in range(17)] for _ in range(NH)]
    B_zt = [[Buf() for _ in range(17)] for _ in range(NH)]
    B_vi = [Buf() for _ in range(17)]
    B_g = [Buf() for _ in range(17)]
    B_mixt = [Buf() for _ in range(16)]
    B_x1 = [Buf() for _ in range(NTILE)]
    B_h2 = Buf()
    B_aff = Buf()
    B_f = Buf()

    es = ExitStack()
    with es:
        P = Prog(nc, es)

        def sb(stack, name, shape, dt):
            return stack.enter_context(nc.sbuf_tensor("sb_" + name, list(shape), dt))

        def ps(stack, name, shape, dt=F32):
            return stack.enter_context(nc.psum_tensor("ps_" + name, list(shape), dt))

        cm_f = sb(es, "cm_f", [128, 4, 128], F32)
        ident_bf = sb(es, "ident_bf", [128, 128], BF16)
        ones_bf = sb(es, "ones_bf", [128, 128], BF16)
        ones_f = sb(es, "ones_f", [128, 128], F32)
        neglam = sb(es, "neglam", [128, 1], F32)
        subln8 = sb(es, "subln8", [128, 1], F32)
        lbt = sb(es, "lbt", [128, 8], F32)
        omlt = sb(es, "omlt", [128, 8], F32)
        mhalf = sb(es, "mhalf", [128, 512], F32)
        B_const = Buf()
        ident_f = cm_f[:, 0, :]

        P.dma("sp", lambda q: q.dma_start(out=cm_f[:], in_=cm_d[:]), w=[B_const])
        P.op("dve", lambda v: v.tensor_copy(out=ident_bf[:], in_=cm_f[:, 0, :]), r=[B_const], w=[B_const])
        P.op("dve", lambda v: v.memset(ones_bf[:], 1.0), w=[B_const])
        P.op("dve", lambda v: v.memset(ones_f[:], 1.0), w=[B_const])
        P.op("dve", lambda v: v.memset(mhalf[:], -0.5), w=[B_const])

        def rstd_from_ss(ss_ap, n, out_ap, tmp_ap, bufs_r, bufs_w, tmpbuf):
            P.op("dve", lambda v: v.tensor_scalar(out=tmp_ap, in0=ss_ap, scalar1=1.0 / n, scalar2=EPS,
                                                  op0=ALU.mult, op1=ALU.add), r=bufs_r, w=[tmpbuf])
            shp = list(tmp_ap.shape)
            P.op("pool", lambda g: g.tensor_tensor(out=out_ap, in0=tmp_ap, in1=mhalf[0:shp[0], 0:shp[1]],
                                                   op=ALU.pow), r=[tmpbuf, B_const], w=bufs_w)

        with ExitStack() as p0:
            scf = sb(p0, "scf", [128, 8, 2], F32)
            sct = sb(p0, "sct", [128, 8, 2], F32)
            wa = [sb(p0, "wa%d" % i, [128, 8, 512], F32) for i in range(2)]
            modl = sb(p0, "modl", [1, 6 * D], F32)
            modc = sb(p0, "modc", [1, 6 * D], F32)
            bada = sb(p0, "bada", [1, 6 * D], F32)
            nrm = sb(p0, "nrm", [1, 4 * D], F32)
            rows = sb(p0, "rows", [1, 8, D], F32)
            lamv = sb(p0, "lamv", [1, 256], F32)
            lamt = sb(p0, "lamt", [1, 8], F32)
            hlb = sb(p0, "hlb", [128, 16], F32)
            sl = sb(p0, "sl", [128, 1], F32)
            pm = [ps(p0, "pm%d" % i, [1, 512]) for i in range(4)]
            pl = ps(p0, "pl", [128, 1])
            B_sc, B_wa, B_modl, B_modc, B_bada, B_nrm, B_rows, B_lam, B_hlb = (
                Buf(), [Buf(), Buf()], Buf(), Buf(), Buf(), Buf(), Buf(), Buf(), Buf())
            B_pm = [Buf() for _ in range(4)]
            B_pl = Buf()

            P.dma("sp", lambda q: q.dma_start(out=scf[:], in_=cc_d[:]), w=[B_sc])
            P.dma("sp", lambda q: q.dma_start(out=bada[:], in_=bada_d[:]), w=[B_bada])
            P.dma("sp", lambda q: q.dma_start(out=nrm[:], in_=norms_d[:]), w=[B_nrm])
            P.dma("sp", lambda q: q.dma_start(out=lamv[:], in_=lamv_d[:]), w=[B_lam])
            P.dma("sp", lambda q: q.dma_start(out=hlb[:], in_=hlb_d[:]), w=[B_hlb])
            P.dma("sp", lambda q: q.dma_start(out=sl[:], in_=subln_d[:]), w=[B_hlb])
            P.op("act", lambda a: a.activation(out=sct[:], in_=scf[:], func=AF.Exp, scale=-1.0), r=[B_sc], w=[B_rows])
            P.op("dve", lambda v: v.tensor_scalar(out=sct[:], in0=sct[:], scalar1=1.0, scalar2=None, op0=ALU.add),
                 r=[B_rows], w=[B_rows])
            P.op("dve", lambda v: v.reciprocal(out=sct[:], in_=sct[:]), r=[B_rows], w=[B_rows])
            P.op("dve", lambda v: v.tensor_tensor(out=scf[:], in0=scf[:], in1=sct[:], op=ALU.mult),
                 r=[B_rows, B_sc], w=[B_sc])
            for ch in range(12):
                wb_ = wa[ch % 2]
                P.dma("sp", lambda q: q.dma_start(out=wb_[:], in_=wada_d[:, :, ch * 512:(ch + 1) * 512]),
                      w=[B_wa[ch % 2]])
                for j, (mod, bm) in enumerate(((modl, B_modl), (modc, B_modc))):
                    pmt = pm[(2 * ch + j) % 4]
                    bp = B_pm[(2 * ch + j) % 4]
                    P.mm(pmt[:], [(scf[:, kc, j:j + 1], wb_[:, kc, :]) for kc in range(8)],
                         r=[B_sc, B_wa[ch % 2]], w=[bp])
                    P.op("dve", lambda v: v.tensor_tensor(out=mod[:, ch * 512:(ch + 1) * 512], in0=pmt[:],
                                                          in1=bada[:, ch * 512:(ch + 1) * 512], op=ALU.add),
                         r=[bp, B_bada], w=[bm])
            def stt(dst, a, b, op0):
                P.op("dve", lambda v: v.scalar_tensor_tensor(out=rows[:, dst, :], in0=a, scalar=(1.0 if op0 == ALU.add else 1.0),
                                                             in1=b, op0=op0, op1=ALU.mult),
                     r=[B_modl, B_modc, B_nrm], w=[B_rows])
            stt(0, modl[:, D:2 * D], nrm[:, 0:D], ALU.add)
            P.op("dve", lambda v: v.tensor_copy(out=rows[:, 1, :], in_=modl[:, 0:D]), r=[B_modl], w=[B_rows])
            stt(2, modc[:, D:2 * D], nrm[:, 0:D], ALU.add)
            P.op("dve", lambda v: v.tensor_copy(out=rows[:, 3, :], in_=modc[:, 0:D]), r=[B_modc], w=[B_rows])
            stt(4, modl[:, 2 * D:3 * D], nrm[:, D:2 * D], ALU.mult)
            stt(5, modl[:, 4 * D:5 * D], nrm[:, 2 * D:3 * D], ALU.add)
            P.op("dve", lambda v: v.tensor_copy(out=rows[:, 6, :], in_=modl[:, 3 * D:4 * D]), r=[B_modl], w=[B_rows])
            stt(7, modl[:, 5 * D:6 * D], nrm[:, 3 * D:4 * D], ALU.mult)
            P.dma("pool", lambda q: q.dma_start(out=modrows_d[:, :].rearrange("(o r) d -> o r d", o=1), in_=rows[:]),
                  r=[B_rows], w=[B_modrows])
            P.op("dve", lambda v: v.tensor_tensor(out=lamv[:, 0:64], in0=lamv[:, 0:64], in1=lamv[:, 64:128], op=ALU.mult),
                 r=[B_lam], w=[B_lam])
            P.op("dve", lambda v: v.tensor_tensor(out=lamv[:, 128:192], in0=lamv[:, 128:192], in1=lamv[:, 192:256], op=ALU.mult),
                 r=[B_lam], w=[B_lam])
            P.op("dve", lambda v: v.tensor_reduce(out=lamt[:, 0:1], in_=lamv[:, 0:64], axis=AX.X, op=ALU.add),
                 r=[B_lam], w=[B_lam])
            P.op("dve", lambda v: v.tensor_reduce(out=lamt[:, 1:2], in_=lamv[:, 128:192], axis=AX.X, op=ALU.add),
                 r=[B_lam], w=[B_lam])
            P.op("act", lambda a: a.activation(out=lamt[:, 2:4], in_=lamt[:, 0:2], func=AF.Exp), r=[B_lam], w=[B_lam])
            P.op("dve", lambda v: v.scalar_tensor_tensor(out=lamt[:, 4:5], in0=lamt[:, 3:4], scalar=-0.2, in1=lamt[:, 2:3],
                                                         op0=ALU.add, op1=ALU.subtract), r=[B_lam], w=[B_lam])
            P.mm(pl[:], [(ones_f[0:1, :], lamt[0:1, 4:5])], r=[B_lam, B_const], w=[B_pl])
            P.op("dve", lambda v: v.tensor_copy(out=neglam[:], in_=pl[:]), r=[B_pl], w=[B_const])
            P.op("dve", lambda v: v.tensor_scalar(out=subln8[:], in0=sl[:], scalar1=0.8, scalar2=None, op0=ALU.mult),
                 r=[B_hlb], w=[B_const])
            P.op("dve", lambda v: v.tensor_tensor(out=hlb[:, 0:8], in0=hlb[:, 8:16], in1=hlb[:, 0:8], op=ALU.subtract),
                 r=[B_hlb], w=[B_hlb])
            P.op("act", lambda a: a.activation(out=hlb[:, 0:8], in_=hlb[:, 0:8], func=AF.Exp), r=[B_hlb], w=[B_hlb])
            P.op("dve", lambda v: v.tensor_scalar(out=hlb[:, 0:8], in0=hlb[:, 0:8], scalar1=1.0, scalar2=None, op0=ALU.add),
                 r=[B_hlb], w=[B_hlb])
            P.op("dve", lambda v: v.reciprocal(out=lbt[:], in_=hlb[:, 0:8]), r=[B_hlb], w=[B_const])
            P.op("dve", lambda v: v.tensor_scalar(out=omlt[:], in0=lbt[:], scalar1=-1.0, scalar2=1.0, op0=ALU.mult, op1=ALU.add),
                 r=[B_const], w=[B_const])
            P.barrier()

        def load_bc(stack, name, row):
            t = sb(stack, name, [128, D], F32)
            b = Buf()
            P.dma("sp", lambda q: q.dma_start(out=t[:], in_=modrows_d[row:row + 1, :].partition_broadcast(128)),
                  r=[B_modrows], w=[b])
            return t, b

        def pe_group(fns, r, w):
            P._deps("pe", r, w)
            ins = None
            for fn in fns:
                ins = fn(nc.tensor)
            P.cnt["pe"] += 1
            ins.then_inc(P.sem["pe"], 1)
            ev = (P.sem["pe"], P.cnt["pe"])
            P._mark(ev, r, w)
            return ev

        SCS = [(0, 2)] + [(2 + 4 * k, 4) for k in range(16)]

        def phase_a0():
            with ExitStack() as st:
                wbf = sb(st, "wbf", [128, 8, WCOLS], BF16)
                B_w = [[Buf() for _ in range(4)] for _ in range(8)]
                for kc in range(8):
                    for pi in range(4):
                        c0 = pi * 1280
                        P.dma("pool", lambda q: q.dma_start(out=wbf[:, kc, c0:c0 + 1280], in_=win_d[:, kc, c0:c0 + 1280]),
                              w=[B_w[kc][pi]])
                B_wall = [b for l in B_w for b in l]
                gm_l, B_gml = load_bc(st, "gm_l", 0)
                sh_l, B_shl = load_bc(st, "sh_l", 1)
                gm_c, B_gmc = load_bc(st, "gm_c", 2)
                sh_c, B_shc = load_bc(st, "sh_c", 3)
                xb = [sb(st, "xb%d" % i, [128, D], F32) for i in range(2)]
                B_xb = [Buf(), Buf()]
                junk = sb(st, "junk", [128, D], BF16)
                B_junk = Buf()
                hn = [sb(st, "hn%d" % i, [128, D], F32) for i in range(2)]
                B_hn = [Buf(), Buf()]
                hb = [sb(st, "hb%d" % i, [128, D], BF16) for i in range(4)]
                B_hb = [Buf() for _ in range(4)]
                ssm = sb(st, "ssm", [128, 8], F32)
                B_ss = [Buf() for _ in range(4)]
                hT = [sb(st, "hT%d" % i, [128, 8, 512], BF16) for i in range(2)]
                B_hT = [Buf(), Buf()]
                cs = [sb(st, "cs%d" % i, [128, 2, 512], F32) for i in range(2)]
                B_cs = [Buf(), Buf()]
                qks = [sb(st, "qks%d" % i, [128, 2, 512], BF16) for i in range(2)]
                B_qks = [Buf(), Buf()]
                zs = [sb(st, "zs%d" % i, [128, 3, 512], F32) for i in range(2)]
                B_zs = [Buf(), Buf()]
                t1 = sb(st, "t1", [128, 512], F32)
                t2 = sb(st, "t2", [128, 512], F32)
                B_t1, B_t2 = Buf(), Buf()
                vis = [sb(st, "vis%d" % i, [128, NH, 2, 128], BF16) for i in range(2)]
                B_vis = [Buf(), Buf()]
                gs = [sb(st, "gs%d" % i, [128, NH, 128], F32) for i in range(2)]
                B_gs = [Buf(), Buf()]
                tp = [ps(st, "tp%d" % i, [128, 8, 128], BF16) for i in range(2)]
                B_tp = [Buf(), Buf()]
                fm = [ps(st, "fm%d" % i, [128, 512]) for i in range(3)]
                B_fm = [Buf() for _ in range(3)]
                tm = ps(st, "tm", [128, 1536])
                B_tm = Buf()
                fmi = [0]
                tile_ctr = [0]

                def stage1(k):
                    t0, nt = SCS[k]
                    for j in range(nt):
                        i = tile_ctr[0]
                        tile_ctr[0] += 1
                        x_ = xb[i % 2]
                        st_ = t0 + j
                        src = ctx_d[st_ * 128:(st_ + 1) * 128, :] if k == 0 else x_d[(st_ - 2) * 128:(st_ - 1) * 128, :]
                        gm, bgm, sh, bsh = (gm_c, B_gmc, sh_c, B_shc) if k == 0 else (gm_l, B_gml, sh_l, B_shl)
                        P.dma("sp", lambda q: q.dma_start(out=x_[:], in_=src), w=[B_xb[i % 2]])
                        ss = ssm[:, 2 * j:2 * j + 1]
                        rs = ssm[:, 2 * j + 1:2 * j + 2]
                        P.op("dve", lambda v: v.scalar_tensor_tensor(out=junk[:], in0=x_[:], scalar=1.0, in1=x_[:],
                                                                     op0=ALU.mult, op1=ALU.mult, accum_out=ss),
                             r=[B_xb[i % 2]], w=[B_junk, B_ss[j]])
                        rstd_from_ss(ss, D, rs, ss, [B_ss[j]], [B_ss[j]], B_ss[j])
                        h_ = hn[i % 2]
                        P.op("dve", lambda v: v.scalar_tensor_tensor(out=h_[:], in0=x_[:], scalar=rs, in1=gm[:],
                                                                     op0=ALU.mult, op1=ALU.mult),
                             r=[B_xb[i % 2], B_ss[j], bgm], w=[B_hn[i % 2]])
                        P.op("pool", lambda g: g.tensor_tensor(out=hb[j][:], in0=h_[:], in1=sh[:], op=ALU.add),
                             r=[B_hn[i % 2], bsh], w=[B_hb[j]])

                def stage2(k):
                    t0, nt = SCS[k]
                    for j in range(nt):
                        tpp = tp[j % 2]
                        pe_group([(lambda pe, kc=kc: pe.transpose(out=tpp[:, kc, :], in_=hb[j][:, kc * 128:(kc + 1) * 128],
                                                                   identity=ident_bf[:])) for kc in range(8)],
                                 r=[B_hb[j], B_const], w=[B_tp[j % 2]])
                        P.op("act", lambda a: a.copy(out=hT[k % 2][:, :, j * 128:(j + 1) * 128], in_=tpp[:]),
                             r=[B_tp[j % 2]], w=[B_hT[k % 2]])

                def fm_mm(k, col0, n):
                    bi = fmi[0] % 3
                    fmi[0] += 1
                    P.mm(fm[bi][:, 0:n], [(wbf[:, kc, col0:col0 + 128], hT[k % 2][:, kc, 0:n]) for kc in range(8)],
                         r=[B_hT[k % 2]] + B_wall, w=[B_fm[bi]])
                    return bi

                def stage3(k):
                    t0, nt = SCS[k]
                    n = nt * 128
                    s0 = t0 * 128
                    c_ = cs[k % 2]
                    P.dma("sp", lambda q: q.dma_start(out=c_[:, 0, 0:n], in_=cos_d[:, s0:s0 + n]), w=[B_cs[k % 2]])
                    P.dma("sp", lambda q: q.dma_start(out=c_[:, 1, 0:n], in_=sin_d[:, s0:s0 + n]), w=[B_cs[k % 2]])
                    for h in range(NH):
                        hi = k * NH + h
                        qk_ = qks[hi % 2]
                        z_ = zs[hi % 2]
                        base = h * FM_BLOCKS * 128
                        for t in range(2):
                            b0 = fm_mm(k, base + (2 * t) * 128, n)
                            b1 = fm_mm(k, base + (2 * t + 1) * 128, n)
                            P.op("dve", lambda v: v.tensor_tensor(out=t1[:, 0:n], in0=fm[b0][:, 0:n], in1=c_[:, 0, 0:n], op=ALU.mult),
                                 r=[B_fm[b0], B_cs[k % 2]], w=[B_t1])
                            P.op("dve", lambda v: v.tensor_tensor(out=t2[:, 0:n], in0=fm[b1][:, 0:n], in1=c_[:, 1, 0:n], op=ALU.mult),
                                 r=[B_fm[b1], B_cs[k % 2]], w=[B_t2])
                            P.op("pool", lambda g: g.tensor_tensor(out=qk_[:, t, 0:n], in0=t1[:, 0:n], in1=t2[:, 0:n], op=ALU.add),
                                 r=[B_t1, B_t2], w=[B_qks[hi % 2]])
                        for t in range(3):
                            b0 = fm_mm(k, base + (4 + t) * 128, n)
                            P.op("act", lambda a: a.copy(out=z_[:, t, 0:n], in_=fm[b0][:, 0:n]), r=[B_fm[b0]], w=[B_zs[hi % 2]])
                        P.dma("pool", lambda q: q.dma_start(out=qkt_d[h].rearrange("t p s -> p t s")[:, :, s0:s0 + n],
                                                            in_=qk_[:, :, 0:n]), r=[B_qks[hi % 2]], w=[B_qkt[h][k]])
                        P.dma("pool", lambda q: q.dma_start(out=zt_d[h].rearrange("t p s -> p t s")[:, :, s0:s0 + n],
                                                            in_=z_[:, :, 0:n]), r=[B_zs[hi % 2]], w=[B_zt[h][k]])
                    for j in range(nt):
                        ti = t0 + j
                        for nb in range(3):
                            P.mm(tm[:, nb * 512:(nb + 1) * 512],
                                 [(hT[k % 2][:, kc, j * 128:(j + 1) * 128], wbf[:, kc, FM_TOTAL + nb * 512:FM_TOTAL + (nb + 1) * 512])
                                  for kc in range(8)], r=[B_hT[k % 2]] + B_wall, w=[B_tm])
                        v_ = vis[ti % 2]
                        g_ = gs[ti % 2]
                        vflat = v_[:].rearrange("p h t c -> p (h t c)")
                        P.op("dve", lambda v: v.tensor_copy(out=vflat[:, 0:512], in_=tm[:, 0:512]),
                             r=[B_tm], w=[B_vis[ti % 2]])
                        P.op("dve", lambda v: v.tensor_copy(out=vflat[:, 512:1024], in_=tm[:, 512:1024]),
                             r=[B_tm], w=[B_vis[ti % 2]])
                        P.op("act", lambda a: a.activation(out=g_[:].rearrange("p h c -> p (h c)"), in_=tm[:, 1024:1536], func=AF.Silu),
                             r=[B_tm], w=[B_gs[ti % 2]])
                        P.dma("pool", lambda q: q.dma_start(out=vi_d[ti * 128:(ti + 1) * 128], in_=v_[:]),
                              r=[B_vis[ti % 2]], w=[B_vi[k]])
                        P.dma("pool", lambda q: q.dma_start(out=g_d[ti * 128:(ti + 1) * 128], in_=g_[:]),
                              r=[B_gs[ti % 2]], w=[B_g[k]])

                stage1(0)
                stage2(0)
                for k in range(17):
                    if k + 1 < 17:
                        stage1(k + 1)
                    stage3(k)
                    if k + 1 < 17:
                        stage2(k + 1)
                P.barrier()

        def mm1(out_ap, lhsT, rhs, start, stop, r, w):
            P.nops += 1
            if P.nops > P.limit:
                return None
            P._deps("pe", r, w)
            ins = nc.tensor.matmul(out_ap, lhsT=lhsT, rhs=rhs, start=start, stop=stop)
            P.cnt["pe"] += 1
            ins.then_inc(P.sem["pe"], 1)
            ev = (P.sem["pe"], P.cnt["pe"])
            P._mark(ev, r, w)
            return ev

        NKT = S // 128

        def phase_a1(heads=range(NH), qchunks=range(16)):
            with ExitStack() as st:
                kt = [sb(st, "kt%d" % i, [128, S], BF16) for i in range(2)]
                vv = [sb(st, "vv%d" % i, [128, NKT, 128], BF16) for i in range(2)]
                B_kt = [Buf(), Buf()]
                B_vv = [Buf(), Buf()]
                qt = [sb(st, "qt%d" % i, [128, 512], BF16) for i in range(2)]
                B_qt = [Buf(), Buf()]
                p1 = [sb(st, "p1_%d" % i, [128, 512], BF16) for i in range(3)]
                p2 = [sb(st, "p2_%d" % i, [128, 512], BF16) for i in range(3)]
                B_p1 = [Buf() for _ in range(3)]
                B_p2 = [Buf() for _ in range(3)]
                acc1 = sb(st, "acc1", [128, 512], F32)
                acc2 = sb(st, "acc2", [128, 512], F32)
                B_acc1, B_acc2 = Buf(), Buf()
                r1 = sb(st, "r1", [128, 512], F32)
                o1 = sb(st, "o1", [128, 512], F32)
                r2 = sb(st, "r2", [128, 512], F32)
                o2 = sb(st, "o2", [128, 512], F32)
                oo = sb(st, "oo", [128, 512], F32)
                sq = sb(st, "sq", [128, 512], F32)
                rs = sb(st, "rs", [128, 512], F32)
                ob = [sb(st, "ob%d" % i, [128, 512], BF16) for i in range(2)]
                B_r1, B_o1, B_r2, B_o2, B_oo, B_sq, B_rs = Buf(), Buf(), Buf(), Buf(), Buf(), Buf(), Buf()
                B_ob = [Buf(), Buf()]
                s1 = [ps(st, "s1_%d" % i, [128, 512]) for i in range(2)]
                s2 = [ps(st, "s2_%d" % i, [128, 512]) for i in range(2)]
                B_s1 = [Buf(), Buf()]
                B_s2 = [Buf(), Buf()]
                o1p = ps(st, "o1p", [128, 512])
                o2p = ps(st, "o2p", [128, 512])
                l1p = ps(st, "l1p", [128, 512])
                l2p = ps(st, "l2p", [128, 512])
                B_o1p, B_o2p, B_l1p, B_l2p = Buf(), Buf(), Buf(), Buf()
                cnt = 0
                for hi, h in enumerate(heads):
                    k_ = kt[hi % 2]
                    v_ = vv[hi % 2]
                    P.dma("sp", lambda q: q.dma_start(out=k_[:], in_=qkt_d[h, 1]), r=B_qkt[h], w=[B_kt[hi % 2]])
                    vsrc = vi_d[:, h, 0, :].rearrange("(t p) c -> p t c", p=128)
                    for part in range(3):
                        P.dma("sp", lambda q: q.dma_start(out=v_[:, part * 22:(part + 1) * 22, :],
                                                          in_=vsrc[:, part * 22:(part + 1) * 22, :]),
                              r=B_vi, w=[B_vv[hi % 2]])
                    for qc in qchunks:
                        q_ = qt[cnt % 2]
                        bq = B_qt[cnt % 2]
                        o_ = ob[cnt % 2]
                        bo = B_ob[cnt % 2]
                        cnt += 1
                        P.dma("sp", lambda q: q.dma_start(out=q_[:], in_=qkt_d[h, 0][:, TC + qc * 512:TC + (qc + 1) * 512]),
                              r=B_qkt[h], w=[bq])

                        def scores(i):
                            mm1(s1[i % 2][:], k_[0:64, i * 128:(i + 1) * 128], q_[0:64, :], True, True,
                                [B_kt[hi % 2], bq], [B_s1[i % 2]])
                            mm1(s2[i % 2][:], k_[64:128, i * 128:(i + 1) * 128], q_[64:128, :], True, True,
                                [B_kt[hi % 2], bq], [B_s2[i % 2]])

                        scores(0)
                        for i in range(NKT):
                            if i + 1 < NKT:
                                scores(i + 1)
                            pa, pb = p1[i % 3], p2[i % 3]
                            P.op("act", lambda a: a.activation(out=pa[:], in_=s1[i % 2][:], func=AF.Exp, scale=0.125),
                                 r=[B_s1[i % 2]], w=[B_p1[i % 3]])
                            P.op("act", lambda a: a.activation(out=pb[:], in_=s2[i % 2][:], func=AF.Exp, scale=0.125),
                                 r=[B_s2[i % 2]], w=[B_p2[i % 3]])
                            st_, sp_ = (i == 0), (i == NKT - 1)
                            mm1(o1p[:], v_[:, i, :], pa[:], st_, sp_, [B_vv[hi % 2], B_p1[i % 3]], [B_o1p])
                            mm1(l1p[:], ones_bf[:], pa[:], st_, sp_, [B_const, B_p1[i % 3]], [B_l1p])
                            mm1(o2p[:], v_[:, i, :], pb[:], st_, sp_, [B_vv[hi % 2], B_p2[i % 3]], [B_o2p])
                            mm1(l2p[:], ones_bf[:], pb[:], st_, sp_, [B_const, B_p2[i % 3]], [B_l2p])
                        P.op("dve", lambda v: v.reciprocal(out=r1[:], in_=l1p[:]), r=[B_l1p], w=[B_r1])
                        P.op("dve", lambda v: v.tensor_tensor(out=o1[:], in0=o1p[:], in1=r1[:], op=ALU.mult),
                             r=[B_o1p, B_r1], w=[B_o1])
                        P.op("dve", lambda v: v.reciprocal(out=r2[:], in_=l2p[:]), r=[B_l2p], w=[B_r2])
                        P.op("dve", lambda v: v.tensor_tensor(out=o2[:], in0=o2p[:], in1=r2[:], op=ALU.mult),
                             r=[B_o2p, B_r2], w=[B_o2])
                        P.op("dve", lambda v: v.scalar_tensor_tensor(out=oo[:], in0=o2[:], scalar=neglam[:, 0:1], in1=o1[:],
                                                                     op0=ALU.mult, op1=ALU.add),
                             r=[B_o2, B_o1, B_const], w=[B_oo])
                        P.op("pool", lambda g: g.tensor_tensor(out=sq[:], in0=oo[:], in1=oo[:], op=ALU.mult),
                             r=[B_oo], w=[B_sq])
                        P.mm(l1p[:], [(ones_f[:], sq[:])], r=[B_sq, B_const], w=[B_l1p])
                        P.op("dve", lambda v: v.tensor_scalar(out=sq[:], in0=l1p[:], scalar1=1.0 / 128, scalar2=EPS,
                                                              op0=ALU.mult, op1=ALU.add), r=[B_l1p], w=[B_sq])
                        P.op("pool", lambda g: g.tensor_tensor(out=rs[:], in0=sq[:], in1=mhalf[:], op=ALU.pow),
                             r=[B_sq, B_const], w=[B_rs])
                        P.op("dve", lambda v: v.scalar_tensor_tensor(out=o_[:], in0=oo[:], scalar=subln8[:, 0:1], in1=rs[:],
                                                                     op0=ALU.mult, op1=ALU.mult),
                             r=[B_oo, B_rs, B_const], w=[bo])
                        P.dma("pool", lambda q: q.dma_start(out=mixt_d[h * 128:(h + 1) * 128, qc * 512:(qc + 1) * 512], in_=o_[:]),
                              r=[bo], w=[B_mixt[qc]])
                P.barrier()

        def phase_a2(heads=range(NH)):
            with ExitStack() as st:
                o_acc = sb(st, "o_acc", [128, NTILE, 128], F32)
                B_oacc = [Buf() for _ in range(NTILE)]
                rm = sb(st, "rm", [128, 2, 512], F32)
                cmA = sb(st, "cmA", [128, 512], BF16)
                cmB = sb(st, "cmB", [128, 512], BF16)
                hgn = sb(st, "hgn_b", [128, 128], F32)
                B_c2 = Buf()
                P.dma("sp", lambda q: q.dma_start(out=rm[:], in_=rm_d[:, :, 0:512]), w=[B_c2])
                P.dma("sp", lambda q: q.dma_start(out=hgn[:], in_=hgn_d[0:1, :].partition_broadcast(128)), w=[B_c2])
                P.op("dve", lambda v: v.memset(cmA[:], 0.0), w=[B_c2])
                P.op("dve", lambda v: v.memset(cmB[:], 0.0), w=[B_c2])
                P.op("dve", lambda v: v.memset(cmA[:].rearrange("p (t c) -> p t c", c=128)[:, :, 0:64], 1.0), w=[B_c2])
                P.op("dve", lambda v: v.memset(cmB[:].rearrange("p (t c) -> p t c", c=128)[:, :, 64:128], 1.0), w=[B_c2])

                def dbl(name, shape, dt):
                    return [sb(st, "%s%d" % (name, i), shape, dt) for i in range(2)], [Buf(), Buf()]
                zin, B_zin = dbl("zin", [128, 2, 512], F32)
                vt, B_vt = dbl("vt", [128, 4, 128], BF16)
                gt, B_gt = dbl("gt", [128, 4, 128], F32)
                e_, B_e = dbl("e_", [128, 512], F32)
                f_, B_f_ = dbl("f_", [128, 512], F32)
                lf, B_lf = dbl("lf", [128, 512], F32)
                kk, B_kk = dbl("kk", [128, 512], F32)
                bc, B_bc = dbl("bc", [128, 512], F32)
                ep, B_ep = dbl("ep", [128, 512], F32)
                en, B_en = dbl("en", [128, 512], F32)
                kdf, B_kdf = dbl("kdf", [128, 512], F32)
                Qd, B_Qd = dbl("Qd", [128, 512], BF16)
                QdA, B_QdA = dbl("QdA", [128, 512], BF16)
                QdB, B_QdB = dbl("QdB", [128, 512], BF16)
                Kd, B_Kd = dbl("Kd", [128, 512], BF16)
                K2T, B_K2T = dbl("K2T", [128, 512], BF16)
                dec, B_dec = dbl("dec", [128, 8], F32)
                k2, B_k2 = dbl("k2", [128, 128], BF16)
                scm, B_scm = dbl("scm", [128, 128], BF16)
                Sbf, B_Sbf = dbl("Sbf", [128, 128], BF16)
                Sst = sb(st, "Sst", [128, 128], F32)
                B_S = Buf()
                ot, B_ot = dbl("ot", [128, 128], F32)
                ojunk = sb(st, "ojunk", [128, 128], F32)
                B_ojunk = Buf()
                osm = sb(st, "osm", [128, 8], F32)
                B_osm = [Buf(), Buf()]
                yb, B_yb = dbl("yb", [128, 128], BF16)
                mixs, B_mixs = dbl("mixs", [128, 512], BF16)
                tpb = [ps(st, "tpb%d" % i, [128, 1024], BF16) for i in range(2)]
                B_tpb = [Buf(), Buf()]
                scp = [ps(st, "scp%d" % i, [128, 512]) for i in range(2)]
                B_scp = [Buf(), Buf()]
                ops_ = [ps(st, "ops%d" % i, [128, 512]) for i in range(2)]
                B_ops = [Buf(), Buf()]
                ups = [ps(st, "ups%d" % i, [128, 512]) for i in range(2)]
                B_ups = [Buf(), Buf()]
                ctr = dict(sc=0, tile=0, ch=0, tp=0)

                for h in heads:
                    for d in range(2):
                        col = d * 4 + h
                        lb_ap = lbt[:, col:col + 1]
                        oml_ap = omlt[:, col:col + 1]
                        P.op("dve", lambda v: v.memset(Sst[:], 0.0), w=[B_S])
                        P.op("dve", lambda v: v.memset(Sbf[0][:], 0.0), w=[B_Sbf[0]])
                        P.op("dve", lambda v: v.memset(Sbf[1][:], 0.0), w=[B_Sbf[1]])
                        sbi = 0
                        order = list(range(17)) if d == 0 else [0] + list(range(16, 0, -1))
                        for k in order:
                            t0, nt = SCS[k]
                            n = nt * 128
                            s0 = t0 * 128
                            nch = n // 64
                            lat = k >= 1
                            i2 = ctr["sc"] % 2
                            ctr["sc"] += 1
                            z_ = zin[i2]
                            P.dma("sp", lambda q: q.dma_start(out=z_[:, 0, 0:n], in_=zt_d[h, d][:, s0:s0 + n]),
                                  r=B_zt[h], w=[B_zin[i2]])
                            P.dma("sp", lambda q: q.dma_start(out=z_[:, 1, 0:n], in_=zt_d[h, 2][:, s0:s0 + n]),
                                  r=B_zt[h], w=[B_zin[i2]])
                            P.dma("sp", lambda q: q.dma_start(
                                out=vt[i2][:, 0:nt, :], in_=vi_d[s0:s0 + n, h, 1, :].rearrange("(t p) c -> p t c", p=128)),
                                r=B_vi, w=[B_vt[i2]])
                            if lat and d == 1:
                                P.dma("sp", lambda q: q.dma_start(
                                    out=gt[i2][:, 0:nt, :], in_=g_d[s0:s0 + n, h, :].rearrange("(t p) c -> p t c", p=128)),
                                    r=B_g, w=[B_gt[i2]])
                            zz = z_[:, 0, 0:n]
                            hq = z_[:, 1, 0:n]
                            P.op("act", lambda a: a.activation(out=e_[i2][:, 0:n], in_=zz, func=AF.Exp, scale=-1.0),
                                 r=[B_zin[i2]], w=[B_e[i2]])
                            P.op("dve", lambda v: v.tensor_scalar(out=e_[i2][:, 0:n], in0=e_[i2][:, 0:n], scalar1=1.0, scalar2=None,
                                                                  op0=ALU.add), r=[B_e[i2]], w=[B_e[i2]])
                            P.op("dve", lambda v: v.reciprocal(out=e_[i2][:, 0:n], in_=e_[i2][:, 0:n]), r=[B_e[i2]], w=[B_e[i2]])
                            P.op("dve", lambda v: v.tensor_scalar(out=f_[i2][:, 0:n], in0=e_[i2][:, 0:n], scalar1=oml_ap, scalar2=lb_ap,
                                                                  op0=ALU.mult, op1=ALU.add), r=[B_e[i2], B_const], w=[B_f_[i2]])
                            P.op("act", lambda a: a.activation(out=lf[i2][:, 0:n], in_=f_[i2][:, 0:n], func=AF.Ln),
                                 r=[B_f_[i2]], w=[B_lf[i2]])
                            P.op("act", lambda a: a.activation(out=kk[i2][:, 0:n], in_=f_[i2][:, 0:n], func=AF.Copy, scale=-1.0, bias=1.0),
                                 r=[B_f_[i2]], w=[B_kk[i2]])
                            if d == 0:
                                P.op("dve", lambda v: v.tensor_tensor_scan(out=bc[i2][:, 0:n], data0=rm[:, 0, 0:n], data1=lf[i2][:, 0:n],
                                                                           initial=0.0, op0=ALU.mult, op1=ALU.add),
                                     r=[B_lf[i2], B_c2], w=[B_bc[i2]])
                            else:
                                P.op("dve", lambda v: v.tensor_tensor_scan(out=bc[i2][:, 0:n][:, ::-1], data0=rm[:, 1, 0:n][:, ::-1],
                                                                           data1=lf[i2][:, 0:n][:, ::-1],
                                                                           initial=0.0, op0=ALU.mult, op1=ALU.add),
                                     r=[B_lf[i2], B_c2], w=[B_bc[i2]])
                            P.op("act", lambda a: a.activation(out=ep[i2][:, 0:n], in_=bc[i2][:, 0:n], func=AF.Exp),
                                 r=[B_bc[i2]], w=[B_ep[i2]])
                            P.op("act", lambda a: a.activation(out=en[i2][:, 0:n], in_=bc[i2][:, 0:n], func=AF.Exp, scale=-1.0),
                                 r=[B_bc[i2]], w=[B_en[i2]])
                            if lat:
                                P.op("dve", lambda v: v.tensor_tensor(out=Qd[i2][:, 0:n], in0=hq, in1=ep[i2][:, 0:n], op=ALU.mult),
                                     r=[B_zin[i2], B_ep[i2]], w=[B_Qd[i2]])
                                P.op("pool", lambda g: g.tensor_tensor(out=QdA[i2][:, 0:n], in0=Qd[i2][:, 0:n], in1=cmA[:, 0:n], op=ALU.mult),
                                     r=[B_Qd[i2], B_c2], w=[B_QdA[i2]])
                                P.op("pool", lambda g: g.tensor_tensor(out=QdB[i2][:, 0:n], in0=Qd[i2][:, 0:n], in1=cmB[:, 0:n], op=ALU.mult),
                                     r=[B_Qd[i2], B_c2], w=[B_QdB[i2]])
                            P.op("pool", lambda g: g.tensor_tensor(out=kdf[i2][:, 0:n], in0=kk[i2][:, 0:n], in1=en[i2][:, 0:n], op=ALU.mult),
                                 r=[B_kk[i2], B_en[i2]], w=[B_kdf[i2]])
                            if lat:
                                P.op("pool", lambda g: g.tensor_copy(out=Kd[i2][:, 0:n], in_=kdf[i2][:, 0:n]),
                                     r=[B_kdf[i2]], w=[B_Kd[i2]])
                            endcol = 63 if d == 0 else 0
                            P.op("dve", lambda v: v.tensor_copy(out=dec[i2][:, 0:nch],
                                                                in_=ep[i2][:, 0:n].rearrange("p (c j) -> p c j", j=64)[:, :, endcol]),
                                 r=[B_ep[i2]], w=[B_dec[i2]])
                            P.op("dve", lambda v: v.tensor_tensor(
                                out=K2T[i2][:, 0:n].rearrange("p (c j) -> p c j", j=64),
                                in0=kdf[i2][:, 0:n].rearrange("p (c j) -> p c j", j=64),
                                in1=dec[i2][:, 0:nch].unsqueeze(2).to_broadcast([128, nch, 64]), op=ALU.mult),
                                r=[B_kdf[i2], B_dec[i2]], w=[B_K2T[i2]])
                            tiles = list(range(nt)) if d == 0 else list(range(nt - 1, -1, -1))
                            for j in tiles:
                                cs_ = slice(j * 128, (j + 1) * 128)
                                ti = ctr["tile"] % 2
                                ctr["tile"] += 1
                                tpi = ctr["tp"] % 2
                                ctr["tp"] += 1
                                pe_group([lambda pe: pe.transpose(out=tpb[tpi][:, 0:128], in_=K2T[i2][:, cs_], identity=ident_bf[:])],
                                         r=[B_K2T[i2], B_const], w=[B_tpb[tpi]])
                                P.op("act", lambda a: a.copy(out=k2[ti][:], in_=tpb[tpi][:, 0:128]), r=[B_tpb[tpi]], w=[B_k2[ti]])
                                if lat:
                                    gtile = (k - 1) * 4 + j
                                    mm1(scp[ti][:, 0:128], Kd[i2][:, cs_], Qd[i2][:, cs_], True, True,
                                        [B_Kd[i2], B_Qd[i2]], [B_scp[ti]])
                                    P.op("dve", lambda v: v.tensor_tensor(out=scm[ti][:], in0=scp[ti][:, 0:128], in1=cm_f[:, 1 + d, :],
                                                                          op=ALU.mult), r=[B_scp[ti], B_const], w=[B_scm[ti]])
                                    mm1(ops_[ti][:, 0:128], scm[ti][:], vt[i2][:, j, :], True, False,
                                        [B_scm[ti], B_vt[i2]], [B_ops[ti]])
                                chunks = (0, 1) if d == 0 else (1, 0)
                                for ci, c in enumerate(chunks):
                                    rows = slice(c * 64, (c + 1) * 64)
                                    gc = 2 * j + c
                                    ui = ctr["ch"] % 2
                                    ctr["ch"] += 1
                                    if lat:
                                        qsel = QdA if c == 0 else QdB
                                        bq = B_QdA if c == 0 else B_QdB
                                        mm1(ops_[ti][:, 0:128], qsel[i2][:, cs_], Sbf[sbi][:], False, ci == 1,
                                            [bq[i2], B_Sbf[sbi]], [B_ops[ti]])
                                    mm1(ups[ui][:, 0:128], k2[ti][rows, :], vt[i2][rows, j, :], True, True,
                                        [B_k2[ti], B_vt[i2]], [B_ups[ui]])
                                    P.op("dve", lambda v: v.scalar_tensor_tensor(out=Sst[:], in0=Sst[:], scalar=dec[i2][:, gc:gc + 1],
                                                                                 in1=ups[ui][:, 0:128], op0=ALU.mult, op1=ALU.add),
                                         r=[B_S, B_dec[i2], B_ups[ui]], w=[B_S])
                                    sbi = 1 - sbi
                                    P.op("act", lambda a: a.copy(out=Sbf[sbi][:], in_=Sst[:]), r=[B_S], w=[B_Sbf[sbi]])
                                if lat:
                                    if d == 0:
                                        P.op("act", lambda a: a.copy(out=o_acc[:, gtile, :], in_=ops_[ti][:, 0:128]),
                                             r=[B_ops[ti]], w=[B_oacc[gtile]])
                                    else:
                                        o_ = ot[ti]
                                        P.op("dve", lambda v: v.tensor_tensor(out=o_[:], in0=ops_[ti][:, 0:128], in1=o_acc[:, gtile, :],
                                                                              op=ALU.add), r=[B_ops[ti], B_oacc[gtile]], w=[B_ot[ti]])
                                        ss = osm[:, 2 * ti:2 * ti + 1]
                                        rsd = osm[:, 2 * ti + 1:2 * ti + 2]
                                        P.op("dve", lambda v: v.scalar_tensor_tensor(out=ojunk[:], in0=o_[:], scalar=1.0, in1=o_[:],
                                                                                     op0=ALU.mult, op1=ALU.mult, accum_out=ss),
                                             r=[B_ot[ti]], w=[B_ojunk, B_osm[ti]])
                                        rstd_from_ss(ss, 128, rsd, ss, [B_osm[ti]], [B_osm[ti]], B_osm[ti])
                                        P.op("dve", lambda v: v.scalar_tensor_tensor(out=o_[:], in0=o_[:], scalar=rsd, in1=hgn[:],
                                                                                     op0=ALU.mult, op1=ALU.mult),
                                             r=[B_ot[ti], B_osm[ti], B_c2], w=[B_ot[ti]])
                                        P.op("pool", lambda g: g.tensor_tensor(out=yb[ti][:], in0=o_[:], in1=gt[i2][:, j, :], op=ALU.mult),
                                             r=[B_ot[ti], B_gt[i2]], w=[B_yb[ti]])
                                        tpo = ctr["tp"] % 2
                                        ctr["tp"] += 1
                                        pe_group([lambda pe: pe.transpose(out=tpb[tpo][:, 0:128], in_=yb[ti][:], identity=ident_bf[:])],
                                                 r=[B_yb[ti], B_const], w=[B_tpb[tpo]])
                                        P.op("act", lambda a: a.copy(out=mixs[i2][:, cs_], in_=tpb[tpo][:, 0:128]),
                                             r=[B_tpb[tpo]], w=[B_mixs[i2]])
                            if lat and d == 1:
                                P.dma("pool", lambda q: q.dma_start(
                                    out=mixt_d[512 + h * 128:512 + (h + 1) * 128, (k - 1) * 512:k * 512], in_=mixs[i2][:]),
                                    r=[B_mixs[i2]], w=[B_mixt[k - 1]])
                P.barrier()

        AFF = sb(es, "AFF", [128, NTILE, NE], F32)
        B_AFF = [Buf() for _ in range(NTILE)]
        B_h2t = [Buf() for _ in range(NTILE)]
        B_afft = [Buf() for _ in range(NTILE)]

        def phase_b():
            with ExitStack() as st:
                wo = sb(st, "wo", [128, 8, D], BF16)
                B_wo = [Buf() for _ in range(4)]
                for pi in range(4):
                    P.dma("pool", lambda q: q.dma_start(out=wo[:, 2 * pi:2 * pi + 2, :], in_=wout_d[:, 2 * pi:2 * pi + 2, :]),
                          w=[B_wo[pi]])
                wr = sb(st, "wr", [128, 8, NE], F32)
                B_wr = Buf()
                P.dma("sp", lambda q: q.dma_start(out=wr[:], in_=wr_d[:]), w=[B_wr])
                gpm, B_gpm = load_bc(st, "gpm", 4)
                g2m, B_g2m = load_bc(st, "g2m", 5)
                sh2, B_sh2 = load_bc(st, "sh2", 6)
                mix = [sb(st, "mix%d" % i, [128, 8, 512], BF16) for i in range(2)]
                B_mix = [Buf(), Buf()]
                xb = [sb(st, "bxb%d" % i, [128, D], F32) for i in range(2)]
                B_xb = [Buf(), Buf()]
                tt = [sb(st, "btt%d" % i, [128, D], F32) for i in range(2)]
                B_tt = [Buf(), Buf()]
                x1 = [sb(st, "bx1%d" % i, [128, D], F32) for i in range(2)]
                B_x1s = [Buf(), Buf()]
                h2f = [sb(st, "h2f%d" % i, [128, D], F32) for i in range(2)]
                B_h2f = [Buf(), Buf()]
                h2b = [sb(st, "h2b%d" % i, [128, D], BF16) for i in range(2)]
                B_h2b = [Buf(), Buf()]
                junk = sb(st, "bjunk", [128, D], BF16)
                B_junk = Buf()
                h2T = [sb(st, "h2T%d" % i, [128, 8, 128], F32) for i in range(2)]
                B_h2T = [Buf(), Buf()]
                sm = sb(st, "bsm", [128, 2, 8], F32)
                B_sm = [Buf(), Buf()]
                ee = sb(st, "bee", [128, 2, NE], F32)
                yps = [ps(st, "yps%d" % i, [128, D]) for i in range(2)]
                B_yps = [Buf(), Buf()]
                trp = ps(st, "trp", [128, D])
                B_trp = Buf()
                lgp = ps(st, "lgp", [128, 512])
                B_lgp = Buf()
                for sc in range(16):
                    m_ = mix[sc % 2]
                    P.dma("sp", lambda q: q.dma_start(out=m_[:], in_=mixt_d[:, sc * 512:(sc + 1) * 512].rearrange("(kc p) t -> p kc t", p=128)),
                          r=[B_mixt[sc]], w=[B_mix[sc % 2]])
                    for j in range(4):
                        tl = sc * 4 + j
                        i2 = tl % 2
                        y_ = yps[i2]
                        for half in range(2):
                            P.mm(y_[:, half * 512:(half + 1) * 512],
                                 [(m_[:, kc, j * 128:(j + 1) * 128], wo[:, kc, half * 512:(half + 1) * 512]) for kc in range(8)],
                                 r=[B_mix[sc % 2]] + B_wo, w=[B_yps[i2]])
                        s_ = sm[:, i2, :]
                        for half in range(2):
                            P.op("act", lambda a: a.activation(out=junk[:, half * 512:(half + 1) * 512], in_=y_[:, half * 512:(half + 1) * 512],
                                                               func=AF.Square, accum_out=s_[:, half:half + 1]),
                                 r=[B_yps[i2]], w=[B_junk, B_sm[i2]])
                        P.op("dve", lambda v: v.tensor_tensor(out=s_[:, 2:3], in0=s_[:, 0:1], in1=s_[:, 1:2], op=ALU.add),
                             r=[B_sm[i2]], w=[B_sm[i2]])
                        rstd_from_ss(s_[:, 2:3], D, s_[:, 3:4], s_[:, 2:3], [B_sm[i2]], [B_sm[i2]], B_sm[i2])
                        P.dma("sp", lambda q: q.dma_start(out=xb[i2][:], in_=x_d[tl * 128:(tl + 1) * 128, :]), w=[B_xb[i2]])
                        for half in range(2):
                            hs = slice(half * 512, (half + 1) * 512)
                            P.op("dve", lambda v: v.scalar_tensor_tensor(out=tt[i2][:, hs], in0=y_[:, hs], scalar=s_[:, 3:4], in1=gpm[:, hs],
                                                                         op0=ALU.mult, op1=ALU.mult),
                                 r=[B_yps[i2], B_sm[i2], B_gpm], w=[B_tt[i2]])
                        P.op("pool", lambda g: g.tensor_tensor(out=x1[i2][:], in0=tt[i2][:], in1=xb[i2][:], op=ALU.add),
                             r=[B_tt[i2], B_xb[i2]], w=[B_x1s[i2]])
                        P.dma("pool", lambda q: q.dma_start(out=x1_d[tl * 128:(tl + 1) * 128, :], in_=x1[i2][:]),
                              r=[B_x1s[i2]], w=[B_x1[tl]])
                        P.op("dve", lambda v: v.scalar_tensor_tensor(out=junk[:], in0=x1[i2][:], scalar=1.0, in1=x1[i2][:],
                                                                     op0=ALU.mult, op1=ALU.mult, accum_out=s_[:, 4:5]),
                             r=[B_x1s[i2]], w=[B_junk, B_sm[i2]])
                        rstd_from_ss(s_[:, 4:5], D, s_[:, 5:6], s_[:, 4:5], [B_sm[i2]], [B_sm[i2]], B_sm[i2])
                        P.op("dve", lambda v: v.scalar_tensor_tensor(out=tt[i2][:], in0=x1[i2][:], scalar=s_[:, 5:6], in1=g2m[:],
                                                                     op0=ALU.mult, op1=ALU.mult),
                             r=[B_x1s[i2], B_sm[i2], B_g2m], w=[B_tt[i2]])
                        P.op("pool", lambda g: g.tensor_tensor(out=h2f[i2][:], in0=tt[i2][:], in1=sh2[:], op=ALU.add),
                             r=[B_tt[i2], B_sh2], w=[B_h2f[i2]])
                        P.op("act", lambda a: a.copy(out=h2b[i2][:], in_=h2f[i2][:]), r=[B_h2f[i2]], w=[B_h2b[i2]])
                        P.dma("pool", lambda q: q.dma_start(out=h2_d[tl * 128:(tl + 1) * 128, :], in_=h2b[i2][:]),
                              r=[B_h2b[i2]], w=[B_h2t[tl]])
                        pe_group([(lambda pe, kc=kc: pe.transpose(out=trp[:, kc * 128:(kc + 1) * 128],
                                                                   in_=h2f[i2][:, kc * 128:(kc + 1) * 128], identity=ident_f))
                                  for kc in range(8)], r=[B_h2f[i2], B_const], w=[B_trp])
                        P.op("act", lambda a: a.copy(out=h2T[i2][:, 0:4, :].rearrange("p k t -> p (k t)"), in_=trp[:, 0:512]),
                             r=[B_trp], w=[B_h2T[i2]])
                        P.op("dve", lambda v: v.tensor_copy(out=h2T[i2][:, 4:8, :].rearrange("p k t -> p (k t)"), in_=trp[:, 512:1024]),
                             r=[B_trp], w=[B_h2T[i2]])
                        P.mm(lgp[:, 0:NE], [(h2T[i2][:, kc, :], wr[:, kc, :]) for kc in range(8)],
                             r=[B_h2T[i2], B_wr], w=[B_lgp])
                        P.op("dve", lambda v: v.tensor_reduce(out=s_[:, 6:7], in_=lgp[:, 0:NE], axis=AX.X, op=ALU.max, negate=True),
                             r=[B_lgp], w=[B_sm[i2]])
                        P.op("act", lambda a: a.activation(out=ee[:, i2, :], in_=lgp[:, 0:NE], func=AF.Exp, bias=s_[:, 6:7],
                                                           accum_out=s_[:, 7:8]), r=[B_lgp, B_sm[i2]], w=[B_sm[i2]])
                        P.op("dve", lambda v: v.reciprocal(out=s_[:, 7:8], in_=s_[:, 7:8]), r=[B_sm[i2]], w=[B_sm[i2]])
                        P.op("dve", lambda v: v.tensor_scalar(out=AFF[:, tl, :], in0=ee[:, i2, :], scalar1=s_[:, 7:8], scalar2=None,
                                                              op0=ALU.mult), r=[B_sm[i2]], w=[B_AFF[tl]])
                        P.dma("pool", lambda q: q.dma_start(out=aff_d[tl * 128:(tl + 1) * 128, :], in_=AFF[:, tl, :]),
                              r=[B_AFF[tl]], w=[B_afft[tl]])
                P.barrier()

        posm = sb(es, "posm", [128, NE, NTILE], F32)
        B_posm = Buf()

        def phase_c():
            with ExitStack() as st:
                lo = sb(st, "c_lo", [128, NE], F32)
                hi = sb(st, "c_hi", [128, NE], F32)
                mid = sb(st, "c_mid", [128, NE], F32)
                ge = sb(st, "c_ge", [128, NTILE, NE], F32)
                cntp = sb(st, "c_cntp", [128, NE], F32)
                mge = sb(st, "c_mge", [128, NE], U32)
                mlt = sb(st, "c_mlt", [128, NE], U32)
                Mt = sb(st, "c_Mt", [128, NE, NTILE], F32)
                Psc = sb(st, "c_Psc", [128, NE, NTILE], F32)
                rmc = sb(st, "c_rmc", [128, 1024], F32)
                Tt = sb(st, "c_Tt", [128, NE], BF16)
                Lbf = sb(st, "c_Lbf", [128, 128], BF16)
                off = sb(st, "c_off", [128, NE], F32)
                cps = ps(st, "c_cps", [128, 512])
                B_lo, B_hi, B_mid, B_ge, B_cntp, B_m, B_cps, B_x = Buf(), Buf(), Buf(), Buf(), Buf(), Buf(), Buf(), Buf()
                P.dma("sp", lambda q: q.dma_start(out=rmc[:], in_=rm_d[:, 0, :]), w=[B_x])
                P.op("dve", lambda v: v.tensor_copy(out=Lbf[:], in_=cm_f[:, 3, :]), r=[B_const], w=[B_x])
                P.op("dve", lambda v: v.memset(lo[:], 0.0), w=[B_lo])
                P.op("dve", lambda v: v.memset(hi[:], 2.0), w=[B_hi])
                for it in range(34):
                    P.op("dve", lambda v: v.tensor_tensor(out=mid[:], in0=lo[:], in1=hi[:], op=ALU.add), r=[B_lo, B_hi], w=[B_mid])
                    P.op("dve", lambda v: v.tensor_scalar(out=mid[:], in0=mid[:], scalar1=0.5, scalar2=None, op0=ALU.mult),
                         r=[B_mid], w=[B_mid])
                    P.op("dve", lambda v: v.tensor_tensor(out=ge[:], in0=AFF[:], in1=mid[:].unsqueeze(1).to_broadcast([128, NTILE, NE]),
                                                          op=ALU.is_ge), r=B_AFF + [B_mid], w=[B_ge])
                    P.op("dve", lambda v: v.tensor_reduce(out=cntp[:], in_=ge[:].rearrange("p i e -> p e i"), axis=AX.X, op=ALU.add),
                         r=[B_ge], w=[B_cntp])
                    P.mm(cps[:, 0:NE], [(ones_f[:], cntp[:])], r=[B_cntp, B_const], w=[B_cps])
                    P.op("dve", lambda v: v.tensor_scalar(out=mge[:], in0=cps[:, 0:NE], scalar1=float(CAP), scalar2=None, op0=ALU.is_ge),
                         r=[B_cps], w=[B_m])
                    P.op("dve", lambda v: v.tensor_scalar(out=mlt[:], in0=cps[:, 0:NE], scalar1=float(CAP), scalar2=None, op0=ALU.is_lt),
                         r=[B_cps], w=[B_m])
                    P.op("dve", lambda v: v.copy_predicated(out=lo[:], mask=mge[:], data=mid[:]), r=[B_m, B_mid], w=[B_lo])
                    P.op("dve", lambda v: v.copy_predicated(out=hi[:], mask=mlt[:], data=mid[:]), r=[B_m, B_mid], w=[B_hi])
                P.op("dve", lambda v: v.tensor_tensor(out=ge[:], in0=AFF[:], in1=lo[:].unsqueeze(1).to_broadcast([128, NTILE, NE]),
                                                      op=ALU.is_ge), r=B_AFF + [B_lo], w=[B_ge])
                P.op("dve", lambda v: v.tensor_copy(out=Mt[:], in_=ge[:].rearrange("p i e -> p e i")), r=[B_ge], w=[B_x])
                P.op("dve", lambda v: v.tensor_tensor_scan(out=Psc[:].rearrange("p e i -> p (e i)"), data0=rmc[:],
                                                           data1=Mt[:].rearrange("p e i -> p (e i)"), initial=0.0,
                                                           op0=ALU.mult, op1=ALU.add), r=[B_x], w=[B_x])
                P.op("dve", lambda v: v.tensor_copy(out=Tt[:], in_=Psc[:, :, NTILE - 1]), r=[B_x], w=[B_x])
                P.mm(cps[:, 0:NE], [(Lbf[:], Tt[:])], r=[B_x], w=[B_cps])
                P.op("dve", lambda v: v.tensor_copy(out=off[:], in_=cps[:, 0:NE]), r=[B_cps], w=[B_x])
                P.op("dve", lambda v: v.tensor_tensor(out=Psc[:], in0=Psc[:], in1=off[:].unsqueeze(2).to_broadcast([128, NE, NTILE]),
                                                      op=ALU.add), r=[B_x], w=[B_x])
                P.op("dve", lambda v: v.tensor_tensor(out=Psc[:], in0=Psc[:], in1=Mt[:], op=ALU.mult), r=[B_x], w=[B_x])
                P.op("dve", lambda v: v.tensor_scalar(out=posm[:], in0=Psc[:], scalar1=-1.0, scalar2=None, op0=ALU.add),
                     r=[B_x], w=[B_posm])
                P.barrier()

        def idma(fn, r, w):
            return P.dma("pool", fn, r=r, w=w)

        def phase_d(experts=range(NE)):
            with ExitStack() as st:
                iota = sb(st, "d_iota", [128, 1024], F32)
                tokf = sb(st, "d_tokf", [128, NTILE, 2], F32)
                tokb = sb(st, "d_tokb", [128, NTILE, 2], BF16)
                zt_ = sb(st, "d_zero", [128, D], F32)
                B_dc = Buf()
                P.dma("sp", lambda q: q.dma_start(out=iota[:], in_=iota_d[:]), w=[B_dc])
                P.dma("sp", lambda q: q.dma_start(out=tokf[:], in_=tokhl_d[:]), w=[B_dc])
                P.op("dve", lambda v: v.tensor_copy(out=tokb[:], in_=tokf[:]), r=[B_dc], w=[B_dc])
                P.op("dve", lambda v: v.memset(zt_[:], 0.0), w=[B_dc])
                fview = f_d.rearrange("(t p) d -> p t d", p=128)
                for part in range(4):
                    P.dma("sp", lambda q: q.dma_start(out=fview[:, part * 16:(part + 1) * 16, :],
                                                      in_=zt_[:].unsqueeze(1).to_broadcast([128, 16, D])), r=[B_dc], w=[B_f])
                sel = [sb(st, "d_sel%d" % i, [128, 1024], BF16) for i in range(4)]
                B_sel = [Buf() for _ in range(4)]
                idxf = sb(st, "d_idxf", [2, 1024], F32)
                idx2 = sb(st, "d_idx2", [128, 8], F32)
                idxi = [sb(st, "d_idxi%d" % i, [128, 8], I32) for i in range(2)]
                B_idxf, B_idx2 = Buf(), Buf()
                B_idxi = [Buf(), Buf()]
                X = [sb(st, "d_X%d" % i, [128, D], BF16) for i in range(16)]
                B_X = [Buf() for _ in range(16)]
                gat = [sb(st, "d_gat%d" % i, [128, 8, NE], F32) for i in range(2)]
                B_gat = [Buf(), Buf()]
                XT = sb(st, "d_XT", [128, 8, 1024], BF16)
                B_XT = Buf()
                AT = sb(st, "d_AT", [128, 8, 1024], BF16)
                B_AT = Buf()
                W = [[sb(st, "d_w%d_%d" % (m, i), [128, 8, D], BF16) for m in range(3)] for i in range(2)]
                B_W = [[[Buf() for _ in range(4)] for _ in range(3)] for _ in range(2)]
                sg = [sb(st, "d_sg%d" % i, [128, 512], F32) for i in range(2)]
                B_sg = [Buf(), Buf()]
                Ysb = [sb(st, "d_Y%d" % i, [128, D], F32) for i in range(2)]
                B_Y = [Buf(), Buf()]
                ips = [ps(st, "d_ips%d" % i, [128, 512]) for i in range(2)]
                B_ips = [Buf(), Buf()]
                tpx = ps(st, "d_tpx", [128, 8, 128], BF16)
                B_tpx = Buf()
                itp = ps(st, "d_itp", [128, 512])
                B_itp = Buf()
                gps = [ps(st, "d_gps%d" % i, [128, 512]) for i in range(2)]
                B_gps = [Buf(), Buf()]
                ups = [ps(st, "d_ups%d" % i, [128, 512]) for i in range(2)]
                B_ups = [Buf(), Buf()]
                wsrc = (wg_d, wu_d, wd_d)

                def load_w(e, slot):
                    for m in range(3):
                        for pi in range(4):
                            P.dma("pool", lambda q: q.dma_start(out=W[slot][m][:, 2 * pi:2 * pi + 2, :],
                                                                in_=wsrc[m][e][:, 2 * pi:2 * pi + 2, :]), w=[B_W[slot][m][pi]])

                elist = list(experts)
                ctr = dict(sel=0, g=0, y=0)

                def compaction(e, slot):
                    for i in range(NTILE):
                        si = ctr["sel"] % 4
                        ctr["sel"] += 1
                        P.op("dve", lambda v: v.tensor_scalar(out=sel[si][:], in0=iota[:], scalar1=posm[:, e, i:i + 1], scalar2=None,
                                                            op0=ALU.is_equal), r=[B_dc, B_posm], w=[B_sel[si]])
                        for half in range(2):
                            mm1(ips[half][0:2, :], tokb[:, i, :], sel[si][:, half * 512:(half + 1) * 512], i == 0, i == NTILE - 1,
                                [B_dc, B_sel[si]], [B_ips[half]])
                        if i % 4 == 3 and i != NTILE - 1:
                            yield
                    for half in range(2):
                        P.op("act", lambda a: a.copy(out=idxf[:, half * 512:(half + 1) * 512], in_=ips[half][0:2, :]),
                             r=[B_ips[half]], w=[B_idxf])
                    pe_group([(lambda pe, jt=jt: pe.transpose(out=itp[:, 2 * jt:2 * jt + 2], in_=idxf[0:2, jt * 128:(jt + 1) * 128],
                                                               identity=ident_f[0:2, 0:2])) for jt in range(8)],
                             r=[B_idxf, B_const], w=[B_itp])
                    P.op("dve", lambda v: v.tensor_reduce(out=idx2[:], in_=itp[:, 0:16].rearrange("p (j t) -> p j t", t=2),
                                                          axis=AX.X, op=ALU.add), r=[B_itp], w=[B_idx2])
                    P.op("dve", lambda v: v.tensor_copy(out=idxi[slot][:], in_=idx2[:]), r=[B_idx2], w=[B_idxi[slot]])
                    yield

                def gather(e, slot):
                    ii = idxi[slot]
                    for jt in range(8):
                        xj = X[slot * 8 + jt]
                        idma(lambda q: q.indirect_dma_start(out=xj[:], out_offset=None, in_=h2_d[:, :],
                                                            in_offset=IndirectOffsetOnAxis(ap=ii[:, jt:jt + 1], axis=0)),
                             r=[B_idxi[slot]] + B_h2t, w=[B_X[slot * 8 + jt]])
                        idma(lambda q: q.indirect_dma_start(out=gat[slot][:, jt, :], out_offset=None, in_=aff_d[:, :],
                                                            in_offset=IndirectOffsetOnAxis(ap=ii[:, jt:jt + 1], axis=0)),
                             r=[B_idxi[slot]] + B_afft, w=[B_gat[slot]])

                load_w(elist[0], 0)
                for _ in compaction(elist[0], 0):
                    pass
                gather(elist[0], 0)
                for ei, e in enumerate(elist):
                    slot = ei % 2
                    ii = idxi[slot]
                    g_ = gat[slot]
                    nxt = None
                    if ei + 1 < len(elist):
                        load_w(elist[ei + 1], 1 - slot)
                        nxt = compaction(elist[ei + 1], 1 - slot)
                    for jt in range(8):
                        xj = X[slot * 8 + jt]
                        pe_group([(lambda pe, kc=kc: pe.transpose(out=tpx[:, kc, :], in_=xj[:, kc * 128:(kc + 1) * 128],
                                                                   identity=ident_bf[:])) for kc in range(8)],
                                 r=[B_X[slot * 8 + jt], B_const], w=[B_tpx])
                        if jt % 2 == 0:
                            P.op("act", lambda a: a.copy(out=XT[:, :, jt * 128:(jt + 1) * 128], in_=tpx[:]), r=[B_tpx], w=[B_XT])
                        else:
                            P.op("dve", lambda v: v.tensor_copy(out=XT[:, :, jt * 128:(jt + 1) * 128], in_=tpx[:]), r=[B_tpx], w=[B_XT])
                    wg_, wu_, wd_ = W[slot]
                    bwg, bwu, bwd = B_W[slot]
                    for fc in range(8):
                        for sh in range(2):
                            gi = ctr["g"] % 2
                            ctr["g"] += 1
                            cs_ = slice(sh * 512, (sh + 1) * 512)
                            P.mm(gps[gi][:], [(wg_[:, kc, fc * 128:(fc + 1) * 128], XT[:, kc, cs_]) for kc in range(8)],
                                 r=[B_XT] + bwg, w=[B_gps[gi]])
                            P.mm(ups[gi][:], [(wu_[:, kc, fc * 128:(fc + 1) * 128], XT[:, kc, cs_]) for kc in range(8)],
                                 r=[B_XT] + bwu, w=[B_ups[gi]])
                            P.op("act", lambda a: a.activation(out=sg[gi][:], in_=gps[gi][:], func=AF.Silu),
                                 r=[B_gps[gi]], w=[B_sg[gi]])
                            P.op("dve", lambda v: v.tensor_tensor(out=AT[:, fc, cs_], in0=ups[gi][:], in1=sg[gi][:], op=ALU.mult),
                                 r=[B_ups[gi], B_sg[gi]], w=[B_AT])
                            if nxt is not None:
                                next(nxt, None)
                    if nxt is not None:
                        for _ in nxt:
                            pass
                        gather(elist[ei + 1], 1 - slot)
                    for jt in range(8):
                        yi = ctr["y"] % 2
                        ctr["y"] += 1
                        for dh in range(2):
                            gi = ctr["g"] % 2
                            ctr["g"] += 1
                            P.mm(gps[gi][:], [(AT[:, fc, jt * 128:(jt + 1) * 128], wd_[:, fc, dh * 512:(dh + 1) * 512]) for fc in range(8)],
                                 r=[B_AT] + bwd, w=[B_gps[gi]])
                            P.op("dve", lambda v: v.tensor_scalar(out=Ysb[yi][:, dh * 512:(dh + 1) * 512], in0=gps[gi][:],
                                                                  scalar1=g_[:, jt, e:e + 1], scalar2=None, op0=ALU.mult),
                                 r=[B_gps[gi], B_gat[slot]], w=[B_Y[yi]])
                        idma(lambda q: q.indirect_dma_start(out=f_d[:, :], out_offset=IndirectOffsetOnAxis(ap=ii[:, jt:jt + 1], axis=0),
                                                            in_=Ysb[yi][:], in_offset=None, compute_op=ALU.add),
                             r=[B_Y[yi], B_idxi[slot]], w=[B_f])
                P.barrier()

        def phase_e():
            with ExitStack() as st:
                gpf, B_gpf = load_bc(st, "gpf", 7)
                fb = [sb(st, "e_f%d" % i, [128, D], F32) for i in range(2)]
                xb = [sb(st, "e_x%d" % i, [128, D], F32) for i in range(2)]
                tb = [sb(st, "e_t%d" % i, [128, D], F32) for i in range(2)]
                ob_ = [sb(st, "e_o%d" % i, [128, D], F32) for i in range(2)]
                junk = sb(st, "e_junk", [128, D], BF16)
                sm = sb(st, "e_sm", [128, 2, 2], F32)
                B_fb, B_xb, B_tb, B_ob, B_sm = [Buf(), Buf()], [Buf(), Buf()], [Buf(), Buf()], [Buf(), Buf()], [Buf(), Buf()]
                B_junk = Buf()
                B_out = [Buf() for _ in range(NTILE)]
                for tl in range(NTILE):
                    i2 = tl % 2
                    rows = slice(tl * 128, (tl + 1) * 128)
                    P.dma("sp", lambda q: q.dma_start(out=fb[i2][:], in_=f_d[rows, :]), r=[B_f], w=[B_fb[i2]])
                    P.dma("sp", lambda q: q.dma_start(out=xb[i2][:], in_=x1_d[rows, :]), r=[B_x1[tl]], w=[B_xb[i2]])
                    P.op("dve", lambda v: v.scalar_tensor_tensor(out=junk[:], in0=fb[i2][:], scalar=1.0, in1=fb[i2][:],
                                                                 op0=ALU.mult, op1=ALU.mult, accum_out=sm[:, i2, 0:1]),
                         r=[B_fb[i2]], w=[B_junk, B_sm[i2]])
                    rstd_from_ss(sm[:, i2, 0:1], D, sm[:, i2, 1:2], sm[:, i2, 0:1], [B_sm[i2]], [B_sm[i2]], B_sm[i2])
                    P.op("dve", lambda v: v.scalar_tensor_tensor(out=tb[i2][:], in0=fb[i2][:], scalar=sm[:, i2, 1:2], in1=gpf[:],
                                                                 op0=ALU.mult, op1=ALU.mult),
                         r=[B_fb[i2], B_sm[i2], B_gpf], w=[B_tb[i2]])
                    P.op("pool", lambda g: g.tensor_tensor(out=ob_[i2][:], in0=tb[i2][:], in1=xb[i2][:], op=ALU.add),
                         r=[B_tb[i2], B_xb[i2]], w=[B_ob[i2]])
                    P.dma("pool", lambda q: q.dma_start(out=out_d[rows, :], in_=ob_[i2][:]), r=[B_ob[i2]], w=[B_out[tl]])
                P.barrier()

        import os
        if stop_after == "0":
            return nc
        phase_a0()
        if stop_after == "A0":
            return nc
        if stop_after == "A1":
            phase_a1(heads=[int(x) for x in os.environ.get("A1_HEADS", "0").split(",")],
                     qchunks=[int(x) for x in os.environ.get("A1_QC", "0,9").split(",")])
            return nc
        if stop_after == "A2":
            phase_a2(heads=[int(x) for x in os.environ.get("A2_HEADS", "0").split(",")])
            return nc
        if not os.environ.get("SKIP_A1"):
            phase_a1()
        phase_a2()
        phase_b()
        if stop_after == "B":
            return nc
        phase_c()
        if stop_after == "C":
            return nc
        phase_d()
        phase_e()
        return nc


def _rope_tables():
    half = 32
    inv_freq = (1.0 / (10000.0 ** (np.arange(0, half, 2, dtype=np.float32) / np.float32(half)))).astype(np.float32)
    t = np.arange(T)
    r = (t // 64).astype(np.float32)
    c = (t % 64).astype(np.float32)
    ang_r = r[:, None] * inv_freq[None, :]
    ang_c = c[:, None] * inv_freq[None, :]
    ang = np.concatenate([ang_r, ang_r, ang_c, ang_c], axis=-1).astype(np.float32)
    cos = np.cos(ang).astype(np.float32)
    sin = np.sin(ang).astype(np.float32)
    sign = np.concatenate([-np.ones(16), np.ones(16), -np.ones(16), np.ones(16)]).astype(np.float32)
    sin = sin * sign[None, :]
    cosT = np.ones((128, S), np.float32)
    sinT = np.zeros((128, S), np.float32)
    cosT[:, TC:] = np.concatenate([cos.T, cos.T], axis=0)
    sinT[:, TC:] = np.concatenate([sin.T, sin.T], axis=0)
    return cosT, sinT


def _win_cols():
    rot = np.concatenate([np.arange(16, 32), np.arange(0, 16), np.arange(48, 64), np.arange(32, 48)])
    fm, tm, tg = [], [], []
    for h in range(NH):
        for off in (0, 512):
            base = off + h * 128
            fm.append(base + np.arange(128))
            fm.append(np.concatenate([base + rot, base + 64 + rot]))
        fm.append(1536 + h * 128 + np.arange(128))
        fm.append(2048 + h * 128 + np.arange(128))
        fm.append(3072 + h * 128 + np.arange(128))
        tm.append(1024 + h * 128 + np.arange(128))
        tm.append(2560 + h * 128 + np.arange(128))
        tg.append(3584 + h * 128 + np.arange(128))
    tm = tm + tg
    return np.concatenate(fm + tm)


def _kc(a):
    n = a.shape[-1]
    return np.ascontiguousarray(a.reshape(8, 128, n).transpose(1, 0, 2))


def prep_inputs(inp, n_cores):
    f = lambda k: np.asarray(inp[k], dtype=np.float32)
    x, c, ctx, c_ctx = f("x"), f("c"), f("ctx"), f("c_ctx")
    cosT, sinT = _rope_tables()
    p = np.arange(128)
    blk = p // 64
    same = blk[:, None] == blk[None, :]
    cm = np.zeros((128, 4, 128), np.float32)
    cm[:, 0, :] = np.eye(128)
    cm[:, 1, :] = same & (p[:, None] <= p[None, :])
    cm[:, 2, :] = same & (p[:, None] >= p[None, :])
    cm[:, 3, :] = p[:, None] < p[None, :]
    j = np.arange(1024)
    rm = np.ones((128, 2, 1024), np.float32)
    rm[:, 0, j % 64 == 0] = 0.0
    rm[:, 1, j % 64 == 63] = 0.0
    iota = np.broadcast_to(j.astype(np.float32), (128, 1024)).copy()
    tt = np.arange(NTILE)[None, :] * 128 + p[:, None]
    tokhl = np.stack([64 * (tt // 64), tt % 64], axis=-1).astype(np.float32)
    hlb = f("hg_lower_bound").reshape(2, 2, 4, 128).transpose(3, 0, 1, 2).reshape(128, 16)
    shared = {
        "w_ada": _kc(f("w_ada")[0]),
        "b_ada": f("b_ada")[0][None, :],
        "norms": np.concatenate([f("norm_pre_mix")[0], f("norm_post_mix")[0], f("norm_pre_ffn")[0],
                                 f("norm_post_ffn")[0]])[None, :],
        "w_in": _kc(f("w_in")[0][:, _win_cols()]),
        "lamv": np.concatenate([f("da_lambda_q1")[0], f("da_lambda_k1")[0], f("da_lambda_q2")[0],
                                f("da_lambda_k2")[0]])[None, :],
        "subln": f("da_subln")[0][:, None],
        "hgn": f("hg_norm")[0][None, :],
        "hlb": np.ascontiguousarray(hlb),
        "w_out": _kc(f("w_out")[0]),
        "w_r": _kc(f("w_router")[0]),
        "w_gate": np.ascontiguousarray(f("w_gate")[0].reshape(NE, 8, 128, D).transpose(0, 2, 1, 3)),
        "w_up": np.ascontiguousarray(f("w_up")[0].reshape(NE, 8, 128, D).transpose(0, 2, 1, 3)),
        "w_down": np.ascontiguousarray(f("w_down")[0].reshape(NE, 8, 128, D).transpose(0, 2, 1, 3)),
        "cosT": cosT, "sinT": sinT, "cmasks": cm, "rmask": rm, "iota": iota, "tokhl": tokhl,
    }
    maps = []
    for i in range(n_cores):
        b = i % 2
        m = dict(shared)
        m["x"] = np.ascontiguousarray(x[b])
        m["ctx"] = np.ascontiguousarray(ctx[b])
        m["cc"] = _kc(np.stack([c[b], c_ctx], axis=1))
        maps.append(m)
    return maps


N_CORES = 2
_NC_CACHE = {}


def kernel(**inputs):
    if "nc" not in _NC_CACHE:
        _NC_CACHE["nc"] = build()
    nc = _NC_CACHE["nc"]
    maps = prep_inputs(inputs, N_CORES)
    res = run_bass_kernel_spmd(nc, maps, core_ids=list(range(N_CORES)))
    out = np.stack([np.asarray(res.results[b]["out"], dtype=np.float32) for b in range(2)], axis=0)
    return out
```

```python
import numpy as np
from contextlib import ExitStack
import concourse.bass as bass
import concourse.mybir as mybir
from concourse.bass import IndirectOffsetOnAxis
from concourse.bass_utils import run_bass_kernel_spmd

F32 = mybir.dt.float32
BF16 = mybir.dt.bfloat16
I32 = mybir.dt.int32
U32 = mybir.dt.uint32
AF = mybir.ActivationFunctionType
ALU = mybir.AluOpType
AX = mybir.AxisListType

D = 1024
T = 8192
TC = 256
S = T + TC
NH = 4
NE = 16
CAP = 1024
EPS = 1e-6
NTILE = T // 128
FM_BLOCKS = 7
TM_COLS = 384
HEAD_COLS = FM_BLOCKS * 128 + TM_COLS
FM_TOTAL = NH * FM_BLOCKS * 128
WCOLS = NH * HEAD_COLS


class Buf:
    __slots__ = ("w", "r")

    def __init__(self):
        self.w = None
        self.r = {}


class Prog:
    def __init__(self, nc, es, ndma=12):
        self.nc = nc
        self.eng = dict(pe=nc.tensor, act=nc.scalar, dve=nc.vector, pool=nc.gpsimd, sp=nc.sync)
        self.sem = {k: es.enter_context(nc.semaphore("s_" + k)) for k in self.eng}
        self.cnt = {k: 0 for k in self.eng}
        self.waited = {k: {} for k in self.eng}
        self.dsem = {q: [[es.enter_context(nc.semaphore("d_%s%d" % (q, i))), 0] for i in range(ndma)]
                     for q in ("sp", "pool")}
        self.dnext = {"sp": 0, "pool": 0}
        self.nwait = 0
        self.nops = 0
        import os
        self.limit = int(os.environ.get('OPLIMIT', '1000000000'))

    def _wait(self, e, ev):
        s, v = ev
        w = self.waited[e]
        if w.get(s.num, 0) < v:
            self.eng[e].wait_ge(s, v)
            w[s.num] = v
            self.nwait += 1

    def _deps(self, e, reads, writes):
        own = self.sem[e].num
        for b in reads:
            if b.w is not None:
                if not (e == "pe" and b.w[0].num == own):
                    self._wait(e, b.w)
        for b in writes:
            if b.w is not None:
                if not (e == "pe" and b.w[0].num == own):
                    self._wait(e, b.w)
            for ev in b.r.values():
                if ev[0].num == own:
                    continue
                self._wait(e, ev)

    def _mark(self, ev, reads, writes):
        k = ev[0].num
        for b in reads:
            old = b.r.get(k)
            if old is None or old[1] < ev[1]:
                b.r[k] = ev
        for b in writes:
            b.w = ev
            b.r = {}

    def op(self, e, fn, r=(), w=()):
        self.nops += 1
        if self.nops > self.limit:
            return None
        if self.nops == self.limit:
            print('LAST OP', e, fn.__code__.co_firstlineno)
        self._deps(e, r, w)
        ins = fn(self.eng[e])
        self.cnt[e] += 1
        ins.then_inc(self.sem[e], 1)
        ev = (self.sem[e], self.cnt[e])
        self._mark(ev, r, w)
        return ev

    def mm(self, out_ap, pairs, r=(), w=()):
        self.nops += 1
        if self.nops > self.limit:
            return None
        self._deps("pe", r, w)
        n = len(pairs)
        ins = None
        for i, (l, rh) in enumerate(pairs):
            ins = self.nc.tensor.matmul(out_ap, lhsT=l, rhs=rh, start=(i == 0), stop=(i == n - 1))
        self.cnt["pe"] += 1
        ins.then_inc(self.sem["pe"], 1)
        ev = (self.sem["pe"], self.cnt["pe"])
        self._mark(ev, r, w)
        return ev

    def dma(self, q, fn, r=(), w=()):
        self.nops += 1
        if self.nops > self.limit:
            return None
        slots = self.dsem[q]
        i = self.dnext[q]
        self.dnext[q] = (i + 1) % len(slots)
        s, v = slots[i]
        if v > 0:
            self._wait(q, (s, v))
        self._deps(q, r, w)
        ins = fn(self.eng[q])
        slots[i][1] = v + 16
        ins.then_inc(s, 16)
        ev = (s, v + 16)
        self._mark(ev, r, w)
        return ev

    def all_events(self):
        evs = [(self.sem[k], self.cnt[k]) for k in self.eng if self.cnt[k] > 0]
        for q in self.dsem:
            for s, v in self.dsem[q]:
                if v > 0:
                    evs.append((s, v))
        return evs

    def barrier(self, engines=None):
        evs = self.all_events()
        for e in (engines or self.eng):
            for ev in evs:
                if ev[0].num != self.sem[e].num:
                    self._wait(e, ev)


def build(stop_after=None, dbg=()):
    nc = bass.Bass("TRN2", target_bir_lowering=False)
    dbg = set(dbg)

    def din(name, shape, dt=F32):
        return nc.dram_tensor(name, list(shape), dt, kind="ExternalInput").ap()

    def dscr(name, shape, dt):
        kind = "ExternalOutput" if name in dbg else "Internal"
        return nc.dram_tensor(name, list(shape), dt, kind=kind).ap()

    x_d = din("x", [T, D])
    ctx_d = din("ctx", [TC, D])
    cc_d = din("cc", [128, 8, 2])
    wada_d = din("w_ada", [128, 8, 6 * D])
    bada_d = din("b_ada", [1, 6 * D])
    norms_d = din("norms", [1, 4 * D])
    win_d = din("w_in", [128, 8, WCOLS])
    lamv_d = din("lamv", [1, 256])
    subln_d = din("subln", [128, 1])
    hgn_d = din("hgn", [1, 128])
    hlb_d = din("hlb", [128, 16])
    wout_d = din("w_out", [128, 8, D])
    wr_d = din("w_r", [128, 8, NE])
    wg_d = din("w_gate", [NE, 128, 8, D])
    wu_d = din("w_up", [NE, 128, 8, D])
    wd_d = din("w_down", [NE, 128, 8, D])
    cos_d = din("cosT", [128, S])
    sin_d = din("sinT", [128, S])
    cm_d = din("cmasks", [128, 4, 128])
    rm_d = din("rmask", [128, 2, 1024])
    iota_d = din("iota", [128, 1024])
    tokhl_d = din("tokhl", [128, NTILE, 2])
    out_d = nc.dram_tensor("out", [T, D], F32, kind="ExternalOutput").ap()

    modrows_d = dscr("modrows", [8, D], F32)
    qkt_d = dscr("qkt", [NH, 2, 128, S], BF16)
    zt_d = dscr("zt", [NH, 3, 128, S], F32)
    vi_d = dscr("vi", [S, NH, 2, 128], BF16)
    g_d = dscr("gsil", [S, NH, 128], F32)
    mixt_d = dscr("mixt", [D, T], BF16)
    x1_d = dscr("x1", [T, D], F32)
    h2_d = dscr("h2", [T, D], BF16)
    aff_d = dscr("aff", [T, NE], F32)
    f_d = dscr("facc", [T, D], F32)

    B_modrows = Buf()
    B_qkt = [[Buf() for _ in range(17)] for _ in range(NH)]
    B_zt = [[Buf() for _ in range(17)] for _ in range(NH)]
    B_vi = [Buf() for _ in range(17)]
    B_g = [Buf() for _ in range(17)]
    B_mixt = [Buf() for _ in range(16)]
    B_x1 = [Buf() for _ in range(NTILE)]
    B_h2 = Buf()
    B_aff = Buf()
    B_f = Buf()

    es = ExitStack()
    with es:
        P = Prog(nc, es)

        def sb(stack, name, shape, dt):
            return stack.enter_context(nc.sbuf_tensor("sb_" + name, list(shape), dt))

        def ps(stack, name, shape, dt=F32):
            return stack.enter_context(nc.psum_tensor("ps_" + name, list(shape), dt))

        cm_f = sb(es, "cm_f", [128, 4, 128], F32)
        ident_bf = sb(es, "ident_bf", [128, 128], BF16)
        ones_bf = sb(es, "ones_bf", [128, 128], BF16)
        ones_f = sb(es, "ones_f", [128, 128], F32)
        neglam = sb(es, "neglam", [128, 1], F32)
        subln8 = sb(es, "subln8", [128, 1], F32)
        lbt = sb(es, "lbt", [128, 8], F32)
        omlt = sb(es, "omlt", [128, 8], F32)
        mhalf = sb(es, "mhalf", [128, 512], F32)
        epsb = sb(es, "epsb", [128, 1], F32)
        B_const = Buf()
        ident_f = cm_f[:, 0, :]

        P.dma("sp", lambda q: q.dma_start(out=cm_f[:], in_=cm_d[:]), w=[B_const])
        P.op("dve", lambda v: v.tensor_copy(out=ident_bf[:], in_=cm_f[:, 0, :]), r=[B_const], w=[B_const])
        P.op("dve", lambda v: v.memset(ones_bf[:], 1.0), w=[B_const])
        P.op("dve", lambda v: v.memset(ones_f[:], 1.0), w=[B_const])
        P.op("dve", lambda v: v.memset(mhalf[:], -0.5), w=[B_const])
        P.op("dve", lambda v: v.memset(epsb[:], EPS), w=[B_const])

        def rstd_from_ss(ss_ap, n, out_ap, tmp_ap, bufs_r, bufs_w, tmpbuf):
            P.op("dve", lambda v: v.tensor_scalar(out=tmp_ap, in0=ss_ap, scalar1=1.0 / n, scalar2=EPS,
                                                  op0=ALU.mult, op1=ALU.add), r=bufs_r, w=[tmpbuf])
            shp = list(tmp_ap.shape)
            P.op("pool", lambda g: g.tensor_tensor(out=out_ap, in0=tmp_ap, in1=mhalf[0:shp[0], 0:shp[1]],
                                                   op=ALU.pow), r=[tmpbuf, B_const], w=bufs_w)

        with ExitStack() as p0:
            scf = sb(p0, "scf", [128, 8, 2], F32)
            sct = sb(p0, "sct", [128, 8, 2], F32)
            wa = [sb(p0, "wa%d" % i, [128, 8, 512], F32) for i in range(2)]
            modl = sb(p0, "modl", [1, 6 * D], F32)
            modc = sb(p0, "modc", [1, 6 * D], F32)
            bada = sb(p0, "bada", [1, 6 * D], F32)
            nrm = sb(p0, "nrm", [1, 4 * D], F32)
            rows = sb(p0, "rows", [1, 8, D], F32)
            lamv = sb(p0, "lamv", [1, 256], F32)
            lamt = sb(p0, "lamt", [1, 8], F32)
            hlb = sb(p0, "hlb", [128, 16], F32)
            sl = sb(p0, "sl", [128, 1], F32)
            pm = [ps(p0, "pm%d" % i, [1, 512]) for i in range(4)]
            pl = ps(p0, "pl", [128, 1])
            B_sc, B_wa, B_modl, B_modc, B_bada, B_nrm, B_rows, B_lam, B_hlb = (
                Buf(), [Buf(), Buf()], Buf(), Buf(), Buf(), Buf(), Buf(), Buf(), Buf())
            B_pm = [Buf() for _ in range(4)]
            B_pl = Buf()

            P.dma("sp", lambda q: q.dma_start(out=scf[:], in_=cc_d[:]), w=[B_sc])
            P.dma("sp", lambda q: q.dma_start(out=bada[:], in_=bada_d[:]), w=[B_bada])
            P.dma("sp", lambda q: q.dma_start(out=nrm[:], in_=norms_d[:]), w=[B_nrm])
            P.dma("sp", lambda q: q.dma_start(out=lamv[:], in_=lamv_d[:]), w=[B_lam])
            P.dma("sp", lambda q: q.dma_start(out=hlb[:], in_=hlb_d[:]), w=[B_hlb])
            P.dma("sp", lambda q: q.dma_start(out=sl[:], in_=subln_d[:]), w=[B_hlb])
            P.op("act", lambda a: a.activation(out=sct[:], in_=scf[:], func=AF.Exp, scale=-1.0), r=[B_sc], w=[B_rows])
            P.op("dve", lambda v: v.tensor_scalar(out=sct[:], in0=sct[:], scalar1=1.0, scalar2=None, op0=ALU.add),
                 r=[B_rows], w=[B_rows])
            P.op("dve", lambda v: v.reciprocal(out=sct[:], in_=sct[:]), r=[B_rows], w=[B_rows])
            P.op("dve", lambda v: v.tensor_tensor(out=scf[:], in0=scf[:], in1=sct[:], op=ALU.mult),
                 r=[B_rows, B_sc], w=[B_sc])
            for ch in range(12):
                wb_ = wa[ch % 2]
                P.dma("sp", lambda q: q.dma_start(out=wb_[:], in_=wada_d[:, :, ch * 512:(ch + 1) * 512]),
                      w=[B_wa[ch % 2]])
                for j, (mod, bm) in enumerate(((modl, B_modl), (modc, B_modc))):
                    pmt = pm[(2 * ch + j) % 4]
                    bp = B_pm[(2 * ch + j) % 4]
                    P.mm(pmt[:], [(scf[:, kc, j:j + 1], wb_[:, kc, :]) for kc in range(8)],
                         r=[B_sc, B_wa[ch % 2]], w=[bp])
                    P.op("dve", lambda v: v.tensor_tensor(out=mod[:, ch * 512:(ch + 1) * 512], in0=pmt[:],
                                                          in1=bada[:, ch * 512:(ch + 1) * 512], op=ALU.add),
                         r=[bp, B_bada], w=[bm])
            def stt(dst, a, b, op0):
                P.op("dve", lambda v: v.scalar_tensor_tensor(out=rows[:, dst, :], in0=a, scalar=(1.0 if op0 == ALU.add else 1.0),
                                                             in1=b, op0=op0, op1=ALU.mult),
                     r=[B_modl, B_modc, B_nrm], w=[B_rows])
            stt(0, modl[:, D:2 * D], nrm[:, 0:D], ALU.add)
            P.op("dve", lambda v: v.tensor_copy(out=rows[:, 1, :], in_=modl[:, 0:D]), r=[B_modl], w=[B_rows])
            stt(2, modc[:, D:2 * D], nrm[:, 0:D], ALU.add)
            P.op("dve", lambda v: v.tensor_copy(out=rows[:, 3, :], in_=modc[:, 0:D]), r=[B_modc], w=[B_rows])
            stt(4, modl[:, 2 * D:3 * D], nrm[:, D:2 * D], ALU.mult)
            stt(5, modl[:, 4 * D:5 * D], nrm[:, 2 * D:3 * D], ALU.add)
            P.op("dve", lambda v: v.tensor_copy(out=rows[:, 6, :], in_=modl[:, 3 * D:4 * D]), r=[B_modl], w=[B_rows])
            stt(7, modl[:, 5 * D:6 * D], nrm[:, 3 * D:4 * D], ALU.mult)
            P.dma("pool", lambda q: q.dma_start(out=modrows_d[:, :].rearrange("(o r) d -> o r d", o=1), in_=rows[:]),
                  r=[B_rows], w=[B_modrows])
            P.op("dve", lambda v: v.tensor_tensor(out=lamv[:, 0:64], in0=lamv[:, 0:64], in1=lamv[:, 64:128], op=ALU.mult),
                 r=[B_lam], w=[B_lam])
            P.op("dve", lambda v: v.tensor_tensor(out=lamv[:, 128:192], in0=lamv[:, 128:192], in1=lamv[:, 192:256], op=ALU.mult),
                 r=[B_lam], w=[B_lam])
            P.op("dve", lambda v: v.tensor_reduce(out=lamt[:, 0:1], in_=lamv[:, 0:64], axis=AX.X, op=ALU.add),
                 r=[B_lam], w=[B_lam])
            P.op("dve", lambda v: v.tensor_reduce(out=lamt[:, 1:2], in_=lamv[:, 128:192], axis=AX.X, op=ALU.add),
                 r=[B_lam], w=[B_lam])
            P.op("act", lambda a: a.activation(out=lamt[:, 2:4], in_=lamt[:, 0:2], func=AF.Exp), r=[B_lam], w=[B_lam])
            P.op("dve", lambda v: v.scalar_tensor_tensor(out=lamt[:, 4:5], in0=lamt[:, 3:4], scalar=-0.2, in1=lamt[:, 2:3],
                                                         op0=ALU.add, op1=ALU.subtract), r=[B_lam], w=[B_lam])
            P.mm(pl[:], [(ones_f[0:1, :], lamt[0:1, 4:5])], r=[B_lam, B_const], w=[B_pl])
            P.op("dve", lambda v: v.tensor_copy(out=neglam[:], in_=pl[:]), r=[B_pl], w=[B_const])
            P.op("dve", lambda v: v.tensor_scalar(out=subln8[:], in0=sl[:], scalar1=0.8, scalar2=None, op0=ALU.mult),
                 r=[B_hlb], w=[B_const])
            P.op("dve", lambda v: v.tensor_tensor(out=hlb[:, 0:8], in0=hlb[:, 8:16], in1=hlb[:, 0:8], op=ALU.subtract),
                 r=[B_hlb], w=[B_hlb])
            P.op("act", lambda a: a.activation(out=hlb[:, 0:8], in_=hlb[:, 0:8], func=AF.Exp), r=[B_hlb], w=[B_hlb])
            P.op("dve", lambda v: v.tensor_scalar(out=hlb[:, 0:8], in0=hlb[:, 0:8], scalar1=1.0, scalar2=None, op0=ALU.add),
                 r=[B_hlb], w=[B_hlb])
            P.op("dve", lambda v: v.reciprocal(out=lbt[:], in_=hlb[:, 0:8]), r=[B_hlb], w=[B_const])
            P.op("dve", lambda v: v.tensor_scalar(out=omlt[:], in0=lbt[:], scalar1=-1.0, scalar2=1.0, op0=ALU.mult, op1=ALU.add),
                 r=[B_const], w=[B_const])
            P.barrier()

        def load_bc(stack, name, row):
            t = sb(stack, name, [128, D], F32)
            b = Buf()
            P.dma("sp", lambda q: q.dma_start(out=t[:], in_=modrows_d[row:row + 1, :].partition_broadcast(128)),
                  r=[B_modrows], w=[b])
            return t, b

        def pe_group(fns, r, w):
            P._deps("pe", r, w)
            ins = None
            for fn in fns:
                ins = fn(nc.tensor)
            P.cnt["pe"] += 1
            ins.then_inc(P.sem["pe"], 1)
            ev = (P.sem["pe"], P.cnt["pe"])
            P._mark(ev, r, w)
            return ev

        SCS = [(0, 2)] + [(2 + 4 * k, 4) for k in range(16)]

        def phase_a0():
            with ExitStack() as st:
                wbf = sb(st, "wbf", [128, 8, WCOLS], BF16)
                B_w = [[Buf() for _ in range(4)] for _ in range(8)]
                for kc in range(8):
                    for pi in range(4):
                        c0 = pi * 1280
                        P.dma("pool", lambda q: q.dma_start(out=wbf[:, kc, c0:c0 + 1280], in_=win_d[:, kc, c0:c0 + 1280]),
                              w=[B_w[kc][pi]])
                B_wall = [b for l in B_w for b in l]
                gm_l, B_gml = load_bc(st, "gm_l", 0)
                sh_l, B_shl = load_bc(st, "sh_l", 1)
                gm_c, B_gmc = load_bc(st, "gm_c", 2)
                sh_c, B_shc = load_bc(st, "sh_c", 3)
                xb = [sb(st, "xb%d" % i, [128, D], F32) for i in range(2)]
                B_xb = [Buf(), Buf()]
                junk = sb(st, "junk", [128, D], BF16)
                B_junk = Buf()
                hn = [sb(st, "hn%d" % i, [128, D], F32) for i in range(2)]
                B_hn = [Buf(), Buf()]
                hb = [sb(st, "hb%d" % i, [128, D], BF16) for i in range(4)]
                B_hb = [Buf() for _ in range(4)]
                ssm = sb(st, "ssm", [128, 8], F32)
                B_ss = [Buf() for _ in range(4)]
                hT = [sb(st, "hT%d" % i, [128, 8, 512], BF16) for i in range(2)]
                B_hT = [Buf(), Buf()]
                cs = [sb(st, "cs%d" % i, [128, 2, 512], F32) for i in range(2)]
                B_cs = [Buf(), Buf()]
                qks = [sb(st, "qks%d" % i, [128, 2, 512], BF16) for i in range(2)]
                B_qks = [Buf(), Buf()]
                zs = [sb(st, "zs%d" % i, [128, 3, 512], F32) for i in range(2)]
                B_zs = [Buf(), Buf()]
                t1 = sb(st, "t1", [128, 512], F32)
                t2 = sb(st, "t2", [128, 512], F32)
                B_t1, B_t2 = Buf(), Buf()
                vis = [sb(st, "vis%d" % i, [128, NH, 2, 128], BF16) for i in range(2)]
                B_vis = [Buf(), Buf()]
                gs = [sb(st, "gs%d" % i, [128, NH, 128], F32) for i in range(2)]
                B_gs = [Buf(), Buf()]
                tp = [ps(st, "tp%d" % i, [128, 8, 128], BF16) for i in range(2)]
                B_tp = [Buf(), Buf()]
                fm = [ps(st, "fm%d" % i, [128, 512]) for i in range(3)]
                B_fm = [Buf() for _ in range(3)]
                tm = ps(st, "tm", [128, 1536])
                B_tm = Buf()
                fmi = [0]
                tile_ctr = [0]

                def stage1(k):
                    t0, nt = SCS[k]
                    for j in range(nt):
                        i = tile_ctr[0]
                        tile_ctr[0] += 1
                        x_ = xb[i % 2]
                        st_ = t0 + j
                        src = ctx_d[st_ * 128:(st_ + 1) * 128, :] if k == 0 else x_d[(st_ - 2) * 128:(st_ - 1) * 128, :]
                        gm, bgm, sh, bsh = (gm_c, B_gmc, sh_c, B_shc) if k == 0 else (gm_l, B_gml, sh_l, B_shl)
                        P.dma("sp", lambda q: q.dma_start(out=x_[:], in_=src), w=[B_xb[i % 2]])
                        ss = ssm[:, 2 * j:2 * j + 1]
                        rs = ssm[:, 2 * j + 1:2 * j + 2]
                        P.op("dve", lambda v: v.scalar_tensor_tensor(out=junk[:], in0=x_[:], scalar=1.0, in1=x_[:],
                                                                     op0=ALU.mult, op1=ALU.mult, accum_out=ss),
                             r=[B_xb[i % 2]], w=[B_junk, B_ss[j]])
                        rstd_from_ss(ss, D, rs, ss, [B_ss[j]], [B_ss[j]], B_ss[j])
                        h_ = hn[i % 2]
                        P.op("dve", lambda v: v.scalar_tensor_tensor(out=h_[:], in0=x_[:], scalar=rs, in1=gm[:],
                                                                     op0=ALU.mult, op1=ALU.mult),
                             r=[B_xb[i % 2], B_ss[j], bgm], w=[B_hn[i % 2]])
                        P.op("pool", lambda g: g.tensor_tensor(out=hb[j][:], in0=h_[:], in1=sh[:], op=ALU.add),
                             r=[B_hn[i % 2], bsh], w=[B_hb[j]])

                def stage2(k):
                    t0, nt = SCS[k]
                    for j in range(nt):
                        tpp = tp[j % 2]
                        pe_group([(lambda pe, kc=kc: pe.transpose(out=tpp[:, kc, :], in_=hb[j][:, kc * 128:(kc + 1) * 128],
                                                                   identity=ident_bf[:])) for kc in range(8)],
                                 r=[B_hb[j], B_const], w=[B_tp[j % 2]])
                        P.op("act", lambda a: a.copy(out=hT[k % 2][:, :, j * 128:(j + 1) * 128], in_=tpp[:]),
                             r=[B_tp[j % 2]], w=[B_hT[k % 2]])

                def fm_mm(k, col0, n):
                    bi = fmi[0] % 3
                    fmi[0] += 1
                    P.mm(fm[bi][:, 0:n], [(wbf[:, kc, col0:col0 + 128], hT[k % 2][:, kc, 0:n]) for kc in range(8)],
                         r=[B_hT[k % 2]] + B_wall, w=[B_fm[bi]])
                    return bi

                def stage3(k):
                    t0, nt = SCS[k]
                    n = nt * 128
                    s0 = t0 * 128
                    c_ = cs[k % 2]
                    P.dma("sp", lambda q: q.dma_start(out=c_[:, 0, 0:n], in_=cos_d[:, s0:s0 + n]), w=[B_cs[k % 2]])
                    P.dma("sp", lambda q: q.dma_start(out=c_[:, 1, 0:n], in_=sin_d[:, s0:s0 + n]), w=[B_cs[k % 2]])
                    for h in range(NH):
                        hi = k * NH + h
                        qk_ = qks[hi % 2]
                        z_ = zs[hi % 2]
                        base = h * FM_BLOCKS * 128
                        for t in range(2):
                            b0 = fm_mm(k, base + (2 * t) * 128, n)
                            b1 = fm_mm(k, base + (2 * t + 1) * 128, n)
                            P.op("dve", lambda v: v.tensor_tensor(out=t1[:, 0:n], in0=fm[b0][:, 0:n], in1=c_[:, 0, 0:n], op=ALU.mult),
                                 r=[B_fm[b0], B_cs[k % 2]], w=[B_t1])
                            P.op("dve", lambda v: v.tensor_tensor(out=t2[:, 0:n], in0=fm[b1][:, 0:n], in1=c_[:, 1, 0:n], op=ALU.mult),
                                 r=[B_fm[b1], B_cs[k % 2]], w=[B_t2])
                            P.op("pool", lambda g: g.tensor_tensor(out=qk_[:, t, 0:n], in0=t1[:, 0:n], in1=t2[:, 0:n], op=ALU.add),
                                 r=[B_t1, B_t2], w=[B_qks[hi % 2]])
                        for t in range(3):
                            b0 = fm_mm(k, base + (4 + t) * 128, n)
                            P.op("act", lambda a: a.copy(out=z_[:, t, 0:n], in_=fm[b0][:, 0:n]), r=[B_fm[b0]], w=[B_zs[hi % 2]])
                        P.dma("pool", lambda q: q.dma_start(out=qkt_d[h].rearrange("t p s -> p t s")[:, :, s0:s0 + n],
                                                            in_=qk_[:, :, 0:n]), r=[B_qks[hi % 2]], w=[B_qkt[h][k]])
                        P.dma("pool", lambda q: q.dma_start(out=zt_d[h].rearrange("t p s -> p t s")[:, :, s0:s0 + n],
                                                            in_=z_[:, :, 0:n]), r=[B_zs[hi % 2]], w=[B_zt[h][k]])
                    for j in range(nt):
                        ti = t0 + j
                        for nb in range(3):
                            P.mm(tm[:, nb * 512:(nb + 1) * 512],
                                 [(hT[k % 2][:, kc, j * 128:(j + 1) * 128], wbf[:, kc, FM_TOTAL + nb * 512:FM_TOTAL + (nb + 1) * 512])
                                  for kc in range(8)], r=[B_hT[k % 2]] + B_wall, w=[B_tm])
                        v_ = vis[ti % 2]
                        g_ = gs[ti % 2]
                        vflat = v_[:].rearrange("p h t c -> p (h t c)")
                        P.op("dve", lambda v: v.tensor_copy(out=vflat[:, 0:512], in_=tm[:, 0:512]),
                             r=[B_tm], w=[B_vis[ti % 2]])
                        P.op("dve", lambda v: v.tensor_copy(out=vflat[:, 512:1024], in_=tm[:, 512:1024]),
                             r=[B_tm], w=[B_vis[ti % 2]])
                        P.op("act", lambda a: a.activation(out=g_[:].rearrange("p h c -> p (h c)"), in_=tm[:, 1024:1536], func=AF.Silu),
                             r=[B_tm], w=[B_gs[ti % 2]])
                        P.dma("pool", lambda q: q.dma_start(out=vi_d[ti * 128:(ti + 1) * 128], in_=v_[:]),
                              r=[B_vis[ti % 2]], w=[B_vi[k]])
                        P.dma("pool", lambda q: q.dma_start(out=g_d[ti * 128:(ti + 1) * 128], in_=g_[:]),
                              r=[B_gs[ti % 2]], w=[B_g[k]])

                stage1(0)
                stage2(0)
                for k in range(17):
                    if k + 1 < 17:
                        stage1(k + 1)
                    stage3(k)
                    if k + 1 < 17:
                        stage2(k + 1)
                P.barrier()

        def mm1(out_ap, lhsT, rhs, start, stop, r, w):
            P.nops += 1
            if P.nops > P.limit:
                return None
            P._deps("pe", r, w)
            ins = nc.tensor.matmul(out_ap, lhsT=lhsT, rhs=rhs, start=start, stop=stop)
            P.cnt["pe"] += 1
            ins.then_inc(P.sem["pe"], 1)
            ev = (P.sem["pe"], P.cnt["pe"])
            P._mark(ev, r, w)
            return ev

        NKT = S // 128

        def phase_a1(heads=range(NH), qchunks=range(16)):
            with ExitStack() as st:
                kt = [sb(st, "kt%d" % i, [128, S], BF16) for i in range(2)]
                vv = [sb(st, "vv%d" % i, [128, NKT, 128], BF16) for i in range(2)]
                B_kt = [Buf(), Buf()]
                B_vv = [Buf(), Buf()]
                qt = [sb(st, "qt%d" % i, [128, 512], BF16) for i in range(2)]
                B_qt = [Buf(), Buf()]
                p1 = [sb(st, "p1_%d" % i, [128, 512], BF16) for i in range(3)]
                p2 = [sb(st, "p2_%d" % i, [128, 512], BF16) for i in range(3)]
                B_p1 = [Buf() for _ in range(3)]
                B_p2 = [Buf() for _ in range(3)]
                acc1 = sb(st, "acc1", [128, 512], F32)
                acc2 = sb(st, "acc2", [128, 512], F32)
                B_acc1, B_acc2 = Buf(), Buf()
                r1 = sb(st, "r1", [128, 512], F32)
                o1 = sb(st, "o1", [128, 512], F32)
                r2 = sb(st, "r2", [128, 512], F32)
                o2 = sb(st, "o2", [128, 512], F32)
                oo = sb(st, "oo", [128, 512], F32)
                sq = sb(st, "sq", [128, 512], F32)
                rs = sb(st, "rs", [128, 512], F32)
                ob = [sb(st, "ob%d" % i, [128, 512], BF16) for i in range(2)]
                B_r1, B_o1, B_r2, B_o2, B_oo, B_sq, B_rs = Buf(), Buf(), Buf(), Buf(), Buf(), Buf(), Buf()
                B_ob = [Buf(), Buf()]
                s1 = [ps(st, "s1_%d" % i, [128, 512]) for i in range(2)]
                s2 = [ps(st, "s2_%d" % i, [128, 512]) for i in range(2)]
                B_s1 = [Buf(), Buf()]
                B_s2 = [Buf(), Buf()]
                o1p = ps(st, "o1p", [128, 512])
                o2p = ps(st, "o2p", [128, 512])
                l1p = ps(st, "l1p", [128, 512])
                l2p = ps(st, "l2p", [128, 512])
                B_o1p, B_o2p, B_l1p, B_l2p = Buf(), Buf(), Buf(), Buf()
                cnt = 0
                for hi, h in enumerate(heads):
                    k_ = kt[hi % 2]
                    v_ = vv[hi % 2]
                    P.dma("sp", lambda q: q.dma_start(out=k_[:], in_=qkt_d[h, 1]), r=B_qkt[h], w=[B_kt[hi % 2]])
                    vsrc = vi_d[:, h, 0, :].rearrange("(t p) c -> p t c", p=128)
                    for part in range(3):
                        P.dma("sp", lambda q: q.dma_start(out=v_[:, part * 22:(part + 1) * 22, :],
                                                          in_=vsrc[:, part * 22:(part + 1) * 22, :]),
                              r=B_vi, w=[B_vv[hi % 2]])
                    for qc in qchunks:
                        q_ = qt[cnt % 2]
                        bq = B_qt[cnt % 2]
                        o_ = ob[cnt % 2]
                        bo = B_ob[cnt % 2]
                        cnt += 1
                        P.dma("sp", lambda q: q.dma_start(out=q_[:], in_=qkt_d[h, 0][:, TC + qc * 512:TC + (qc + 1) * 512]),
                              r=B_qkt[h], w=[bq])

                        def scores(i):
                            mm1(s1[i % 2][:], k_[0:64, i * 128:(i + 1) * 128], q_[0:64, :], True, True,
                                [B_kt[hi % 2], bq], [B_s1[i % 2]])
                            mm1(s2[i % 2][:], k_[64:128, i * 128:(i + 1) * 128], q_[64:128, :], True, True,
                                [B_kt[hi % 2], bq], [B_s2[i % 2]])

                        scores(0)
                        scores(1)
                        for i in range(NKT):
                            pa, pb = p1[i % 3], p2[i % 3]
                            P.op("act", lambda a: a.activation(out=pa[:], in_=s1[i % 2][:], func=AF.Exp, scale=0.125),
                                 r=[B_s1[i % 2]], w=[B_p1[i % 3]])
                            P.op("act", lambda a: a.activation(out=pb[:], in_=s2[i % 2][:], func=AF.Exp, scale=0.125),
                                 r=[B_s2[i % 2]], w=[B_p2[i % 3]])
                            st_, sp_ = (i == 0), (i == NKT - 1)
                            P._deps("pe", [B_p1[i % 3], B_p2[i % 3]], [])
                            mm1(o1p[:], v_[:, i, :], pa[:], st_, sp_, [B_vv[hi % 2], B_p1[i % 3]], [B_o1p])
                            mm1(o2p[:], v_[:, i, :], pb[:], st_, sp_, [B_vv[hi % 2], B_p2[i % 3]], [B_o2p])
                            if i == 0:
                                P.op("dve", lambda v: v.tensor_copy(out=acc1[:], in_=pa[:]), r=[B_p1[i % 3]], w=[B_acc1])
                                P.op("dve", lambda v: v.tensor_copy(out=acc2[:], in_=pb[:]), r=[B_p2[i % 3]], w=[B_acc2])
                            else:
                                P.op("dve", lambda v: v.tensor_tensor(out=acc1[:], in0=acc1[:], in1=pa[:], op=ALU.add),
                                     r=[B_p1[i % 3], B_acc1], w=[B_acc1])
                                P.op("dve", lambda v: v.tensor_tensor(out=acc2[:], in0=acc2[:], in1=pb[:], op=ALU.add),
                                     r=[B_p2[i % 3], B_acc2], w=[B_acc2])
                            if i + 2 < NKT:
                                scores(i + 2)
                        P.mm(l1p[:], [(ones_f[:], acc1[:])], r=[B_acc1, B_const], w=[B_l1p])
                        P.mm(l2p[:], [(ones_f[:], acc2[:])], r=[B_acc2, B_const], w=[B_l2p])
                        P.op("act", lambda a: a.activation(out=r1[:], in_=l1p[:], func=AF.Ln), r=[B_l1p], w=[B_r1])
                        P.op("act", lambda a: a.activation(out=r1[:], in_=r1[:], func=AF.Exp, scale=-1.0), r=[B_r1], w=[B_r1])
                        P.op("dve", lambda v: v.tensor_tensor(out=o1[:], in0=o1p[:], in1=r1[:], op=ALU.mult),
                             r=[B_o1p, B_r1], w=[B_o1])
                        P.op("act", lambda a: a.activation(out=r2[:], in_=l2p[:], func=AF.Ln), r=[B_l2p], w=[B_r2])
                        P.op("act", lambda a: a.activation(out=r2[:], in_=r2[:], func=AF.Exp, scale=-1.0), r=[B_r2], w=[B_r2])
                        P.op("dve", lambda v: v.tensor_tensor(out=o2[:], in0=o2p[:], in1=r2[:], op=ALU.mult),
                             r=[B_o2p, B_r2], w=[B_o2])
                        P.op("dve", lambda v: v.scalar_tensor_tensor(out=oo[:], in0=o2[:], scalar=neglam[:, 0:1], in1=o1[:],
                                                                     op0=ALU.mult, op1=ALU.add),
                             r=[B_o2, B_o1, B_const], w=[B_oo])
                        P.op("dve", lambda v: v.tensor_tensor(out=sq[:], in0=oo[:], in1=oo[:], op=ALU.mult),
                             r=[B_oo], w=[B_sq])
                        P.mm(l1p[:], [(ones_f[:], sq[:])], r=[B_sq, B_const], w=[B_l1p])
                        P.op("act", lambda a: a.activation(out=rs[:], in_=l1p[:], func=AF.Ln, scale=1.0 / 128, bias=epsb[:, 0:1]),
                             r=[B_l1p, B_const], w=[B_rs])
                        P.op("act", lambda a: a.activation(out=rs[:], in_=rs[:], func=AF.Exp, scale=-0.5), r=[B_rs], w=[B_rs])
                        P.op("dve", lambda v: v.scalar_tensor_tensor(out=o_[:], in0=oo[:], scalar=subln8[:, 0:1], in1=rs[:],
                                                                     op0=ALU.mult, op1=ALU.mult),
                             r=[B_oo, B_rs, B_const], w=[bo])
                        P.dma("pool", lambda q: q.dma_start(out=mixt_d[h * 128:(h + 1) * 128, qc * 512:(qc + 1) * 512], in_=o_[:]),
                              r=[bo], w=[B_mixt[qc]])
                P.barrier()

        def phase_a2(heads=range(NH)):
            with ExitStack() as st:
                o_accs = [sb(st, "o_acc%d" % i, [128, NTILE, 128], F32) for i in range(2)]
                B_oaccs = [[Buf() for _ in range(NTILE)] for _ in range(2)]
                rm = sb(st, "rm", [128, 2, 512], F32)
                cmA = sb(st, "cmA", [128, 512], BF16)
                cmB = sb(st, "cmB", [128, 512], BF16)
                hgn = sb(st, "hgn_b", [128, 128], F32)
                B_c2 = Buf()
                P.dma("sp", lambda q: q.dma_start(out=rm[:], in_=rm_d[:, :, 0:512]), w=[B_c2])
                P.dma("sp", lambda q: q.dma_start(out=hgn[:], in_=hgn_d[0:1, :].partition_broadcast(128)), w=[B_c2])
                P.op("dve", lambda v: v.memset(cmA[:], 0.0), w=[B_c2])
                P.op("dve", lambda v: v.memset(cmB[:], 0.0), w=[B_c2])
                P.op("dve", lambda v: v.memset(cmA[:].rearrange("p (t c) -> p t c", c=128)[:, :, 0:64], 1.0), w=[B_c2])
                P.op("dve", lambda v: v.memset(cmB[:].rearrange("p (t c) -> p t c", c=128)[:, :, 64:128], 1.0), w=[B_c2])

                def dbl(name, shape, dt):
                    return [sb(st, "%s%d" % (name, i), shape, dt) for i in range(2)], [Buf(), Buf()]
                zin, B_zin = dbl("zin", [128, 2, 512], F32)
                vt, B_vt = dbl("vt", [128, 4, 128], BF16)
                gt, B_gt = dbl("gt", [128, 4, 128], F32)
                e_, B_e = dbl("e_", [128, 512], F32)
                f_, B_f_ = dbl("f_", [128, 512], F32)
                lf, B_lf = dbl("lf", [128, 512], F32)
                kk, B_kk = dbl("kk", [128, 512], F32)
                bc, B_bc = dbl("bc", [128, 512], F32)
                ep, B_ep = dbl("ep", [128, 512], F32)
                en, B_en = dbl("en", [128, 512], F32)
                kdf, B_kdf = dbl("kdf", [128, 512], F32)
                Qd, B_Qd = dbl("Qd", [128, 512], BF16)
                QdA, B_QdA = dbl("QdA", [128, 512], BF16)
                QdB, B_QdB = dbl("QdB", [128, 512], BF16)
                Kd, B_Kd = dbl("Kd", [128, 512], BF16)
                K2T, B_K2T = dbl("K2T", [128, 512], BF16)
                dec, B_dec = dbl("dec", [128, 8], F32)
                k2, B_k2 = dbl("k2", [128, 128], BF16)
                scm, B_scm = dbl("scm", [128, 128], BF16)
                SbfA, B_SbfA = dbl("SbfA", [128, 128], BF16)
                SbfB, B_SbfB = dbl("SbfB", [128, 128], BF16)
                Sst2 = [sb(st, "Sst%d" % i, [128, 128], F32) for i in range(2)]
                B_S2 = [Buf(), Buf()]
                ot, B_ot = dbl("ot", [128, 128], F32)
                ojunk = sb(st, "ojunk", [128, 128], F32)
                B_ojunk = Buf()
                osm = sb(st, "osm", [128, 8], F32)
                B_osm = [Buf(), Buf()]
                yb, B_yb = dbl("yb", [128, 128], BF16)
                mixs, B_mixs = dbl("mixs", [128, 512], BF16)
                tpb = [ps(st, "tpb%d" % i, [128, 1024], BF16) for i in range(2)]
                B_tpb = [Buf(), Buf()]
                scp = [ps(st, "scp%d" % i, [128, 512]) for i in range(2)]
                B_scp = [Buf(), Buf()]
                ops_ = [ps(st, "ops%d" % i, [128, 512]) for i in range(2)]
                B_ops = [Buf(), Buf()]
                ups = [ps(st, "ups%d" % i, [128, 512]) for i in range(2)]
                B_ups = [Buf(), Buf()]
                ctr = dict(sc=0, tile=0, ch=0, tp=0)

                def chain(h, d):
                    if True:
                        Sst = Sst2[d]
                        B_S = B_S2[d]
                        Sbf, B_Sbf = (SbfA, B_SbfA) if d == 0 else (SbfB, B_SbfB)
                        o_acc = o_accs[d]
                        B_oacc = B_oaccs[d]
                        col = d * 4 + h
                        lb_ap = lbt[:, col:col + 1]
                        oml_ap = omlt[:, col:col + 1]
                        P.op("dve", lambda v: v.memset(Sst[:], 0.0), w=[B_S])
                        P.op("dve", lambda v: v.memset(Sbf[0][:], 0.0), w=[B_Sbf[0]])
                        P.op("dve", lambda v: v.memset(Sbf[1][:], 0.0), w=[B_Sbf[1]])
                        sbi = 0
                        order = list(range(17)) if d == 0 else [0] + list(range(16, 0, -1))
                        for k in order:
                            t0, nt = SCS[k]
                            n = nt * 128
                            s0 = t0 * 128
                            nch = n // 64
                            lat = k >= 1
                            i2 = d
                            z_ = zin[i2]
                            P.dma("sp", lambda q: q.dma_start(out=z_[:, 0, 0:n], in_=zt_d[h, d][:, s0:s0 + n]),
                                  r=B_zt[h], w=[B_zin[i2]])
                            P.dma("sp", lambda q: q.dma_start(out=z_[:, 1, 0:n], in_=zt_d[h, 2][:, s0:s0 + n]),
                                  r=B_zt[h], w=[B_zin[i2]])
                            P.dma("sp", lambda q: q.dma_start(
                                out=vt[i2][:, 0:nt, :], in_=vi_d[s0:s0 + n, h, 1, :].rearrange("(t p) c -> p t c", p=128)),
                                r=B_vi, w=[B_vt[i2]])
                            zz = z_[:, 0, 0:n]
                            hq = z_[:, 1, 0:n]
                            P.op("act", lambda a: a.activation(out=e_[i2][:, 0:n], in_=zz, func=AF.Exp, scale=-1.0),
                                 r=[B_zin[i2]], w=[B_e[i2]])
                            P.op("dve", lambda v: v.tensor_scalar(out=e_[i2][:, 0:n], in0=e_[i2][:, 0:n], scalar1=1.0, scalar2=None,
                                                                  op0=ALU.add), r=[B_e[i2]], w=[B_e[i2]])
                            P.op("act", lambda a: a.activation(out=e_[i2][:, 0:n], in_=e_[i2][:, 0:n], func=AF.Ln), r=[B_e[i2]], w=[B_e[i2]])
                            P.op("act", lambda a: a.activation(out=e_[i2][:, 0:n], in_=e_[i2][:, 0:n], func=AF.Exp, scale=-1.0),
                                 r=[B_e[i2]], w=[B_e[i2]])
                            P.op("dve", lambda v: v.tensor_scalar(out=f_[i2][:, 0:n], in0=e_[i2][:, 0:n], scalar1=oml_ap, scalar2=lb_ap,
                                                                  op0=ALU.mult, op1=ALU.add), r=[B_e[i2], B_const], w=[B_f_[i2]])
                            P.op("act", lambda a: a.activation(out=lf[i2][:, 0:n], in_=f_[i2][:, 0:n], func=AF.Ln),
                                 r=[B_f_[i2]], w=[B_lf[i2]])
                            P.op("act", lambda a: a.activation(out=kk[i2][:, 0:n], in_=f_[i2][:, 0:n], func=AF.Copy, scale=-1.0, bias=1.0),
                                 r=[B_f_[i2]], w=[B_kk[i2]])
                            if d == 0:
                                P.op("dve", lambda v: v.tensor_tensor_scan(out=bc[i2][:, 0:n], data0=rm[:, 0, 0:n], data1=lf[i2][:, 0:n],
                                                                           initial=0.0, op0=ALU.mult, op1=ALU.add),
                                     r=[B_lf[i2], B_c2], w=[B_bc[i2]])
                            else:
                                P.op("dve", lambda v: v.tensor_tensor_scan(out=bc[i2][:, 0:n][:, ::-1], data0=rm[:, 1, 0:n][:, ::-1],
                                                                           data1=lf[i2][:, 0:n][:, ::-1],
                                                                           initial=0.0, op0=ALU.mult, op1=ALU.add),
                                     r=[B_lf[i2], B_c2], w=[B_bc[i2]])
                            P.op("act", lambda a: a.activation(out=ep[i2][:, 0:n], in_=bc[i2][:, 0:n], func=AF.Exp),
                                 r=[B_bc[i2]], w=[B_ep[i2]])
                            P.op("act", lambda a: a.activation(out=en[i2][:, 0:n], in_=bc[i2][:, 0:n], func=AF.Exp, scale=-1.0),
                                 r=[B_bc[i2]], w=[B_en[i2]])
                            if lat:
                                P.op("dve", lambda v: v.tensor_tensor(out=Qd[i2][:, 0:n], in0=hq, in1=ep[i2][:, 0:n], op=ALU.mult),
                                     r=[B_zin[i2], B_ep[i2]], w=[B_Qd[i2]])
                                P.op("pool", lambda g: g.tensor_tensor(out=QdA[i2][:, 0:n], in0=Qd[i2][:, 0:n], in1=cmA[:, 0:n], op=ALU.mult),
                                     r=[B_Qd[i2], B_c2], w=[B_QdA[i2]])
                                P.op("pool", lambda g: g.tensor_tensor(out=QdB[i2][:, 0:n], in0=Qd[i2][:, 0:n], in1=cmB[:, 0:n], op=ALU.mult),
                                     r=[B_Qd[i2], B_c2], w=[B_QdB[i2]])
                            P.op("pool", lambda g: g.tensor_tensor(out=kdf[i2][:, 0:n], in0=kk[i2][:, 0:n], in1=en[i2][:, 0:n], op=ALU.mult),
                                 r=[B_kk[i2], B_en[i2]], w=[B_kdf[i2]])
                            if lat:
                                P.op("pool", lambda g: g.tensor_copy(out=Kd[i2][:, 0:n], in_=kdf[i2][:, 0:n]),
                                     r=[B_kdf[i2]], w=[B_Kd[i2]])
                            endcol = 63 if d == 0 else 0
                            P.op("dve", lambda v: v.tensor_copy(out=dec[i2][:, 0:nch],
                                                                in_=ep[i2][:, 0:n].rearrange("p (c j) -> p c j", j=64)[:, :, endcol]),
                                 r=[B_ep[i2]], w=[B_dec[i2]])
                            P.op("dve", lambda v: v.tensor_tensor(
                                out=K2T[i2][:, 0:n].rearrange("p (c j) -> p c j", j=64),
                                in0=kdf[i2][:, 0:n].rearrange("p (c j) -> p c j", j=64),
                                in1=dec[i2][:, 0:nch].unsqueeze(2).to_broadcast([128, nch, 64]), op=ALU.mult),
                                r=[B_kdf[i2], B_dec[i2]], w=[B_K2T[i2]])
                            tiles = list(range(nt)) if d == 0 else list(range(nt - 1, -1, -1))
                            for j in tiles:
                                cs_ = slice(j * 128, (j + 1) * 128)
                                ti = d
                                tpi = d
                                pe_group([lambda pe: pe.transpose(out=tpb[tpi][:, 0:128], in_=K2T[i2][:, cs_], identity=ident_bf[:])],
                                         r=[B_K2T[i2], B_const], w=[B_tpb[tpi]])
                                P.op("act", lambda a: a.copy(out=k2[ti][:], in_=tpb[tpi][:, 0:128]), r=[B_tpb[tpi]], w=[B_k2[ti]])
                                if lat:
                                    gtile = (k - 1) * 4 + j
                                    mm1(scp[ti][:, 0:128], Kd[i2][:, cs_], Qd[i2][:, cs_], True, True,
                                        [B_Kd[i2], B_Qd[i2]], [B_scp[ti]])
                                    P.op("dve", lambda v: v.tensor_tensor(out=scm[ti][:], in0=scp[ti][:, 0:128], in1=cm_f[:, 1 + d, :],
                                                                          op=ALU.mult), r=[B_scp[ti], B_const], w=[B_scm[ti]])
                                    mm1(ops_[ti][:, 0:128], scm[ti][:], vt[i2][:, j, :], True, False,
                                        [B_scm[ti], B_vt[i2]], [B_ops[ti]])
                                chunks = (0, 1) if d == 0 else (1, 0)
                                for ci, c in enumerate(chunks):
                                    rows = slice(c * 64, (c + 1) * 64)
                                    gc = 2 * j + c
                                    ui = ctr["ch"] % 2
                                    ctr["ch"] += 1
                                    if lat:
                                        qsel = QdA if c == 0 else QdB
                                        bq = B_QdA if c == 0 else B_QdB
                                        mm1(ops_[ti][:, 0:128], qsel[i2][:, cs_], Sbf[sbi][:], False, ci == 1,
                                            [bq[i2], B_Sbf[sbi]], [B_ops[ti]])
                                    mm1(ups[ui][:, 0:128], k2[ti][rows, :], vt[i2][rows, j, :], True, True,
                                        [B_k2[ti], B_vt[i2]], [B_ups[ui]])
                                    P.op("dve", lambda v: v.scalar_tensor_tensor(out=Sst[:], in0=Sst[:], scalar=dec[i2][:, gc:gc + 1],
                                                                                 in1=ups[ui][:, 0:128], op0=ALU.mult, op1=ALU.add),
                                         r=[B_S, B_dec[i2], B_ups[ui]], w=[B_S])
                                    sbi = 1 - sbi
                                    P.op("act", lambda a: a.copy(out=Sbf[sbi][:], in_=Sst[:]), r=[B_S], w=[B_Sbf[sbi]])
                                if lat:
                                    P.op("act", lambda a: a.copy(out=o_acc[:, gtile, :], in_=ops_[ti][:, 0:128]),
                                         r=[B_ops[ti]], w=[B_oacc[gtile]])
                                yield

                def combine(h):
                    for k in range(1, 17):
                        t0, nt = SCS[k]
                        s0 = t0 * 128
                        i2 = k % 2
                        P.dma("sp", lambda q: q.dma_start(
                            out=gt[i2][:, 0:4, :], in_=g_d[s0:s0 + 512, h, :].rearrange("(t p) c -> p t c", p=128)),
                            r=B_g, w=[B_gt[i2]])
                        for j in range(4):
                            gtile = (k - 1) * 4 + j
                            ti = gtile % 2
                            cs_ = slice(j * 128, (j + 1) * 128)
                            o_ = ot[ti]
                            P.op("dve", lambda v: v.tensor_tensor(out=o_[:], in0=o_accs[0][:, gtile, :], in1=o_accs[1][:, gtile, :],
                                                                  op=ALU.add), r=[B_oaccs[0][gtile], B_oaccs[1][gtile]], w=[B_ot[ti]])
                            ss = osm[:, 2 * ti:2 * ti + 1]
                            rsd = osm[:, 2 * ti + 1:2 * ti + 2]
                            P.op("dve", lambda v: v.scalar_tensor_tensor(out=ojunk[:], in0=o_[:], scalar=1.0, in1=o_[:],
                                                                         op0=ALU.mult, op1=ALU.mult, accum_out=ss),
                                 r=[B_ot[ti]], w=[B_ojunk, B_osm[ti]])
                            rstd_from_ss(ss, 128, rsd, ss, [B_osm[ti]], [B_osm[ti]], B_osm[ti])
                            P.op("dve", lambda v: v.scalar_tensor_tensor(out=o_[:], in0=o_[:], scalar=rsd, in1=hgn[:],
                                                                         op0=ALU.mult, op1=ALU.mult),
                                 r=[B_ot[ti], B_osm[ti], B_c2], w=[B_ot[ti]])
                            P.op("pool", lambda g: g.tensor_tensor(out=yb[ti][:], in0=o_[:], in1=gt[i2][:, j, :], op=ALU.mult),
                                 r=[B_ot[ti], B_gt[i2]], w=[B_yb[ti]])
                            pe_group([lambda pe: pe.transpose(out=tpb[ti][:, 0:128], in_=yb[ti][:], identity=ident_bf[:])],
                                     r=[B_yb[ti], B_const], w=[B_tpb[ti]])
                            P.op("act", lambda a: a.copy(out=mixs[i2][:, cs_], in_=tpb[ti][:, 0:128]),
                                 r=[B_tpb[ti]], w=[B_mixs[i2]])
                        P.dma("pool", lambda q: q.dma_start(
                            out=mixt_d[512 + h * 128:512 + (h + 1) * 128, (k - 1) * 512:k * 512], in_=mixs[i2][:]),
                            r=[B_mixs[i2]], w=[B_mixt[k - 1]])

                for h in heads:
                    alive = [chain(h, 0), chain(h, 1)]
                    while alive:
                        for g_ in list(alive):
                            try:
                                next(g_)
                            except StopIteration:
                                alive.remove(g_)
                    combine(h)
                P.barrier()

        AFF = sb(es, "AFF", [128, NTILE, NE], F32)
        B_AFF = [Buf() for _ in range(NTILE)]
        B_h2t = [Buf() for _ in range(NTILE)]
        B_afft = [Buf() for _ in range(NTILE)]

        def phase_b():
            with ExitStack() as st:
                wo = sb(st, "wo", [128, 8, D], BF16)
                B_wo = [Buf() for _ in range(4)]
                for pi in range(4):
                    P.dma("pool", lambda q: q.dma_start(out=wo[:, 2 * pi:2 * pi + 2, :], in_=wout_d[:, 2 * pi:2 * pi + 2, :]),
                          w=[B_wo[pi]])
                wr = sb(st, "wr", [128, 8, NE], F32)
                B_wr = Buf()
                P.dma("sp", lambda q: q.dma_start(out=wr[:], in_=wr_d[:]), w=[B_wr])
                gpm, B_gpm = load_bc(st, "gpm", 4)
                g2m, B_g2m = load_bc(st, "g2m", 5)
                sh2, B_sh2 = load_bc(st, "sh2", 6)
                mix = [sb(st, "mix%d" % i, [128, 8, 512], BF16) for i in range(2)]
                B_mix = [Buf(), Buf()]
                xb = [sb(st, "bxb%d" % i, [128, D], F32) for i in range(2)]
                B_xb = [Buf(), Buf()]
                tt = [sb(st, "btt%d" % i, [128, D], F32) for i in range(2)]
                B_tt = [Buf(), Buf()]
                x1 = [sb(st, "bx1%d" % i, [128, D], F32) for i in range(2)]
                B_x1s = [Buf(), Buf()]
                h2f = [sb(st, "h2f%d" % i, [128, D], F32) for i in range(2)]
                B_h2f = [Buf(), Buf()]
                h2b = [sb(st, "h2b%d" % i, [128, D], BF16) for i in range(2)]
                B_h2b = [Buf(), Buf()]
                junk = sb(st, "bjunk", [128, D], BF16)
                B_junk = Buf()
                h2T = [sb(st, "h2T%d" % i, [128, 8, 128], F32) for i in range(2)]
                B_h2T = [Buf(), Buf()]
                sm = sb(st, "bsm", [128, 2, 8], F32)
                B_sm = [Buf(), Buf()]
                ee = sb(st, "bee", [128, 2, NE], F32)
                yps = [ps(st, "yps%d" % i, [128, D]) for i in range(2)]
                B_yps = [Buf(), Buf()]
                trp = ps(st, "trp", [128, D])
                B_trp = Buf()
                lgp = ps(st, "lgp", [128, 512])
                B_lgp = Buf()
                for sc in range(16):
                    m_ = mix[sc % 2]
                    P.dma("sp", lambda q: q.dma_start(out=m_[:], in_=mixt_d[:, sc * 512:(sc + 1) * 512].rearrange("(kc p) t -> p kc t", p=128)),
                          r=[B_mixt[sc]], w=[B_mix[sc % 2]])
                    for j in range(4):
                        tl = sc * 4 + j
                        i2 = tl % 2
                        y_ = yps[i2]
                        for half in range(2):
                            P.mm(y_[:, half * 512:(half + 1) * 512],
                                 [(m_[:, kc, j * 128:(j + 1) * 128], wo[:, kc, half * 512:(half + 1) * 512]) for kc in range(8)],
                                 r=[B_mix[sc % 2]] + B_wo, w=[B_yps[i2]])
                        s_ = sm[:, i2, :]
                        for half in range(2):
                            P.op("act", lambda a: a.activation(out=junk[:, half * 512:(half + 1) * 512], in_=y_[:, half * 512:(half + 1) * 512],
                                                               func=AF.Square, accum_out=s_[:, half:half + 1]),
                                 r=[B_yps[i2]], w=[B_junk, B_sm[i2]])
                        P.op("dve", lambda v: v.tensor_tensor(out=s_[:, 2:3], in0=s_[:, 0:1], in1=s_[:, 1:2], op=ALU.add),
                             r=[B_sm[i2]], w=[B_sm[i2]])
                        rstd_from_ss(s_[:, 2:3], D, s_[:, 3:4], s_[:, 2:3], [B_sm[i2]], [B_sm[i2]], B_sm[i2])
                        P.dma("sp", lambda q: q.dma_start(out=xb[i2][:], in_=x_d[tl * 128:(tl + 1) * 128, :]), w=[B_xb[i2]])
                        for half in range(2):
                            hs = slice(half * 512, (half + 1) * 512)
                            P.op("dve", lambda v: v.scalar_tensor_tensor(out=tt[i2][:, hs], in0=y_[:, hs], scalar=s_[:, 3:4], in1=gpm[:, hs],
                                                                         op0=ALU.mult, op1=ALU.mult),
                                 r=[B_yps[i2], B_sm[i2], B_gpm], w=[B_tt[i2]])
                        P.op("pool", lambda g: g.tensor_tensor(out=x1[i2][:], in0=tt[i2][:], in1=xb[i2][:], op=ALU.add),
                             r=[B_tt[i2], B_xb[i2]], w=[B_x1s[i2]])
                        P.dma("pool", lambda q: q.dma_start(out=x1_d[tl * 128:(tl + 1) * 128, :], in_=x1[i2][:]),
                              r=[B_x1s[i2]], w=[B_x1[tl]])
                        P.op("dve", lambda v: v.scalar_tensor_tensor(out=junk[:], in0=x1[i2][:], scalar=1.0, in1=x1[i2][:],
                                                                     op0=ALU.mult, op1=ALU.mult, accum_out=s_[:, 4:5]),
                             r=[B_x1s[i2]], w=[B_junk, B_sm[i2]])
                        rstd_from_ss(s_[:, 4:5], D, s_[:, 5:6], s_[:, 4:5], [B_sm[i2]], [B_sm[i2]], B_sm[i2])
                        P.op("dve", lambda v: v.scalar_tensor_tensor(out=tt[i2][:], in0=x1[i2][:], scalar=s_[:, 5:6], in1=g2m[:],
                                                                     op0=ALU.mult, op1=ALU.mult),
                             r=[B_x1s[i2], B_sm[i2], B_g2m], w=[B_tt[i2]])
                        P.op("pool", lambda g: g.tensor_tensor(out=h2f[i2][:], in0=tt[i2][:], in1=sh2[:], op=ALU.add),
                             r=[B_tt[i2], B_sh2], w=[B_h2f[i2]])
                        P.op("act", lambda a: a.copy(out=h2b[i2][:], in_=h2f[i2][:]), r=[B_h2f[i2]], w=[B_h2b[i2]])
                        P.dma("pool", lambda q: q.dma_start(out=h2_d[tl * 128:(tl + 1) * 128, :], in_=h2b[i2][:]),
                              r=[B_h2b[i2]], w=[B_h2t[tl]])
                        pe_group([(lambda pe, kc=kc: pe.transpose(out=trp[:, kc * 128:(kc + 1) * 128],
                                                                   in_=h2f[i2][:, kc * 128:(kc + 1) * 128], identity=ident_f))
                                  for kc in range(8)], r=[B_h2f[i2], B_const], w=[B_trp])
                        P.op("act", lambda a: a.copy(out=h2T[i2][:, 0:4, :].rearrange("p k t -> p (k t)"), in_=trp[:, 0:512]),
                             r=[B_trp], w=[B_h2T[i2]])
                        P.op("dve", lambda v: v.tensor_copy(out=h2T[i2][:, 4:8, :].rearrange("p k t -> p (k t)"), in_=trp[:, 512:1024]),
                             r=[B_trp], w=[B_h2T[i2]])
                        P.mm(lgp[:, 0:NE], [(h2T[i2][:, kc, :], wr[:, kc, :]) for kc in range(8)],
                             r=[B_h2T[i2], B_wr], w=[B_lgp])
                        P.op("dve", lambda v: v.tensor_reduce(out=s_[:, 6:7], in_=lgp[:, 0:NE], axis=AX.X, op=ALU.max, negate=True),
                             r=[B_lgp], w=[B_sm[i2]])
                        P.op("act", lambda a: a.activation(out=ee[:, i2, :], in_=lgp[:, 0:NE], func=AF.Exp, bias=s_[:, 6:7],
                                                           accum_out=s_[:, 7:8]), r=[B_lgp, B_sm[i2]], w=[B_sm[i2]])
                        P.op("dve", lambda v: v.reciprocal(out=s_[:, 7:8], in_=s_[:, 7:8]), r=[B_sm[i2]], w=[B_sm[i2]])
                        P.op("dve", lambda v: v.tensor_scalar(out=AFF[:, tl, :], in0=ee[:, i2, :], scalar1=s_[:, 7:8], scalar2=None,
                                                              op0=ALU.mult), r=[B_sm[i2]], w=[B_AFF[tl]])
                        P.dma("pool", lambda q: q.dma_start(out=aff_d[tl * 128:(tl + 1) * 128, :], in_=AFF[:, tl, :]),
                              r=[B_AFF[tl]], w=[B_afft[tl]])
                P.barrier()

        posm = sb(es, "posm", [128, NE, NTILE], F32)
        B_posm = Buf()

        def phase_c():
            with ExitStack() as st:
                lo = sb(st, "c_lo", [128, NE], F32)
                hi = sb(st, "c_hi", [128, NE], F32)
                mid = sb(st, "c_mid", [128, NE], F32)
                ge = sb(st, "c_ge", [128, NTILE, NE], F32)
                cntp = sb(st, "c_cntp", [128, NE], F32)
                mge = sb(st, "c_mge", [128, NE], U32)
                mlt = sb(st, "c_mlt", [128, NE], U32)
                Mt = sb(st, "c_Mt", [128, NE, NTILE], F32)
                Psc = sb(st, "c_Psc", [128, NE, NTILE], F32)
                rmc = sb(st, "c_rmc", [128, 1024], F32)
                Tt = sb(st, "c_Tt", [128, NE], BF16)
                Lbf = sb(st, "c_Lbf", [128, 128], BF16)
                off = sb(st, "c_off", [128, NE], F32)
                cps = ps(st, "c_cps", [128, 512])
                B_lo, B_hi, B_mid, B_ge, B_cntp, B_m, B_cps, B_x = Buf(), Buf(), Buf(), Buf(), Buf(), Buf(), Buf(), Buf()
                P.dma("sp", lambda q: q.dma_start(out=rmc[:], in_=rm_d[:, 0, :]), w=[B_x])
                P.op("dve", lambda v: v.tensor_copy(out=Lbf[:], in_=cm_f[:, 3, :]), r=[B_const], w=[B_x])
                P.op("dve", lambda v: v.memset(lo[:], 0.0), w=[B_lo])
                P.op("dve", lambda v: v.memset(hi[:], 2.0), w=[B_hi])
                for it in range(34):
                    P.op("dve", lambda v: v.tensor_tensor(out=mid[:], in0=lo[:], in1=hi[:], op=ALU.add), r=[B_lo, B_hi], w=[B_mid])
                    P.op("dve", lambda v: v.tensor_scalar(out=mid[:], in0=mid[:], scalar1=0.5, scalar2=None, op0=ALU.mult),
                         r=[B_mid], w=[B_mid])
                    P.op("dve", lambda v: v.tensor_tensor(out=ge[:], in0=AFF[:], in1=mid[:].unsqueeze(1).to_broadcast([128, NTILE, NE]),
                                                          op=ALU.is_ge), r=B_AFF + [B_mid], w=[B_ge])
                    P.op("dve", lambda v: v.tensor_reduce(out=cntp[:], in_=ge[:].rearrange("p i e -> p e i"), axis=AX.X, op=ALU.add),
                         r=[B_ge], w=[B_cntp])
                    P.mm(cps[:, 0:NE], [(ones_f[:], cntp[:])], r=[B_cntp, B_const], w=[B_cps])
                    P.op("dve", lambda v: v.tensor_scalar(out=mge[:], in0=cps[:, 0:NE], scalar1=float(CAP), scalar2=None, op0=ALU.is_ge),
                         r=[B_cps], w=[B_m])
                    P.op("dve", lambda v: v.tensor_scalar(out=mlt[:], in0=cps[:, 0:NE], scalar1=float(CAP), scalar2=None, op0=ALU.is_lt),
                         r=[B_cps], w=[B_m])
                    P.op("dve", lambda v: v.copy_predicated(out=lo[:], mask=mge[:], data=mid[:]), r=[B_m, B_mid], w=[B_lo])
                    P.op("dve", lambda v: v.copy_predicated(out=hi[:], mask=mlt[:], data=mid[:]), r=[B_m, B_mid], w=[B_hi])
                P.op("dve", lambda v: v.tensor_tensor(out=ge[:], in0=AFF[:], in1=lo[:].unsqueeze(1).to_broadcast([128, NTILE, NE]),
                                                      op=ALU.is_ge), r=B_AFF + [B_lo], w=[B_ge])
                P.op("dve", lambda v: v.tensor_copy(out=Mt[:], in_=ge[:].rearrange("p i e -> p e i")), r=[B_ge], w=[B_x])
                P.op("dve", lambda v: v.tensor_tensor_scan(out=Psc[:].rearrange("p e i -> p (e i)"), data0=rmc[:],
                                                           data1=Mt[:].rearrange("p e i -> p (e i)"), initial=0.0,
                                                           op0=ALU.mult, op1=ALU.add), r=[B_x], w=[B_x])
                P.op("dve", lambda v: v.tensor_copy(out=Tt[:], in_=Psc[:, :, NTILE - 1]), r=[B_x], w=[B_x])
                P.mm(cps[:, 0:NE], [(Lbf[:], Tt[:])], r=[B_x], w=[B_cps])
                P.op("dve", lambda v: v.tensor_copy(out=off[:], in_=cps[:, 0:NE]), r=[B_cps], w=[B_x])
                P.op("dve", lambda v: v.tensor_tensor(out=Psc[:], in0=Psc[:], in1=off[:].unsqueeze(2).to_broadcast([128, NE, NTILE]),
                                                      op=ALU.add), r=[B_x], w=[B_x])
                P.op("dve", lambda v: v.tensor_tensor(out=Psc[:], in0=Psc[:], in1=Mt[:], op=ALU.mult), r=[B_x], w=[B_x])
                P.op("dve", lambda v: v.tensor_scalar(out=posm[:], in0=Psc[:], scalar1=-1.0, scalar2=None, op0=ALU.add),
                     r=[B_x], w=[B_posm])
                P.barrier()

        def idma(fn, r, w):
            return P.dma("pool", fn, r=r, w=w)

        def phase_d(experts=range(NE)):
            with ExitStack() as st:
                iota = sb(st, "d_iota", [128, 1024], F32)
                tokf = sb(st, "d_tokf", [128, NTILE, 2], F32)
                tokb = sb(st, "d_tokb", [128, NTILE, 2], BF16)
                zt_ = sb(st, "d_zero", [128, D], F32)
                B_dc = Buf()
                P.dma("sp", lambda q: q.dma_start(out=iota[:], in_=iota_d[:]), w=[B_dc])
                P.dma("sp", lambda q: q.dma_start(out=tokf[:], in_=tokhl_d[:]), w=[B_dc])
                P.op("dve", lambda v: v.tensor_copy(out=tokb[:], in_=tokf[:]), r=[B_dc], w=[B_dc])
                P.op("dve", lambda v: v.memset(zt_[:], 0.0), w=[B_dc])
                fview = f_d.rearrange("(t p) d -> p t d", p=128)
                for part in range(4):
                    P.dma("sp", lambda q: q.dma_start(out=fview[:, part * 16:(part + 1) * 16, :],
                                                      in_=zt_[:].unsqueeze(1).to_broadcast([128, 16, D])), r=[B_dc], w=[B_f])
                sel = [sb(st, "d_sel%d" % i, [128, 1024], BF16) for i in range(4)]
                B_sel = [Buf() for _ in range(4)]
                idxf = sb(st, "d_idxf", [2, 1024], F32)
                idx2 = sb(st, "d_idx2", [128, 8], F32)
                idxi = [sb(st, "d_idxi%d" % i, [128, 8], I32) for i in range(2)]
                B_idxf, B_idx2 = Buf(), Buf()
                B_idxi = [Buf(), Buf()]
                X = [sb(st, "d_X%d" % i, [128, D], BF16) for i in range(16)]
                B_X = [Buf() for _ in range(16)]
                gat = [sb(st, "d_gat%d" % i, [128, 8, NE], F32) for i in range(2)]
                B_gat = [Buf(), Buf()]
                XT = sb(st, "d_XT", [128, 8, 1024], BF16)
                B_XT = Buf()
                AT = sb(st, "d_AT", [128, 8, 1024], BF16)
                B_AT = Buf()
                W = [[sb(st, "d_w%d_%d" % (m, i), [128, 8, D], BF16) for m in range(3)] for i in range(2)]
                B_W = [[[Buf() for _ in range(4)] for _ in range(3)] for _ in range(2)]
                sg = [sb(st, "d_sg%d" % i, [128, 512], F32) for i in range(2)]
                B_sg = [Buf(), Buf()]
                Ysb = [sb(st, "d_Y%d" % i, [128, D], F32) for i in range(2)]
                B_Y = [Buf(), Buf()]
                ips = [ps(st, "d_ips%d" % i, [128, 512]) for i in range(2)]
                B_ips = [Buf(), Buf()]
                tpx = ps(st, "d_tpx", [128, 8, 128], BF16)
                B_tpx = Buf()
                itp = ps(st, "d_itp", [128, 512])
                B_itp = Buf()
                gps = [ps(st, "d_gps%d" % i, [128, 512]) for i in range(2)]
                B_gps = [Buf(), Buf()]
                ups = [ps(st, "d_ups%d" % i, [128, 512]) for i in range(2)]
                B_ups = [Buf(), Buf()]
                wsrc = (wg_d, wu_d, wd_d)

                def load_w(e, slot):
                    for m in range(3):
                        for pi in range(4):
                            P.dma("pool", lambda q: q.dma_start(out=W[slot][m][:, 2 * pi:2 * pi + 2, :],
                                                                in_=wsrc[m][e][:, 2 * pi:2 * pi + 2, :]), w=[B_W[slot][m][pi]])

                elist = list(experts)
                ctr = dict(sel=0, g=0, y=0)

                def compaction(e, slot):
                    for i in range(NTILE):
                        si = ctr["sel"] % 4
                        ctr["sel"] += 1
                        P.op("dve", lambda v: v.tensor_scalar(out=sel[si][:], in0=iota[:], scalar1=posm[:, e, i:i + 1], scalar2=None,
                                                            op0=ALU.is_equal), r=[B_dc, B_posm], w=[B_sel[si]])
                        for half in range(2):
                            mm1(ips[half][0:2, :], tokb[:, i, :], sel[si][:, half * 512:(half + 1) * 512], i == 0, i == NTILE - 1,
                                [B_dc, B_sel[si]], [B_ips[half]])
                        if i % 4 == 3 and i != NTILE - 1:
                            yield
                    for half in range(2):
                        P.op("act", lambda a: a.copy(out=idxf[:, half * 512:(half + 1) * 512], in_=ips[half][0:2, :]),
                             r=[B_ips[half]], w=[B_idxf])
                    pe_group([(lambda pe, jt=jt: pe.transpose(out=itp[:, 2 * jt:2 * jt + 2], in_=idxf[0:2, jt * 128:(jt + 1) * 128],
                                                               identity=ident_f[0:2, 0:2])) for jt in range(8)],
                             r=[B_idxf, B_const], w=[B_itp])
                    P.op("dve", lambda v: v.tensor_reduce(out=idx2[:], in_=itp[:, 0:16].rearrange("p (j t) -> p j t", t=2),
                                                          axis=AX.X, op=ALU.add), r=[B_itp], w=[B_idx2])
                    P.op("dve", lambda v: v.tensor_copy(out=idxi[slot][:], in_=idx2[:]), r=[B_idx2], w=[B_idxi[slot]])
                    yield

                def gather(e, slot):
                    ii = idxi[slot]
                    for jt in range(8):
                        xj = X[slot * 8 + jt]
                        idma(lambda q: q.indirect_dma_start(out=xj[:], out_offset=None, in_=h2_d[:, :],
                                                            in_offset=IndirectOffsetOnAxis(ap=ii[:, jt:jt + 1], axis=0)),
                             r=[B_idxi[slot]] + B_h2t, w=[B_X[slot * 8 + jt]])
                        idma(lambda q: q.indirect_dma_start(out=gat[slot][:, jt, :], out_offset=None, in_=aff_d[:, :],
                                                            in_offset=IndirectOffsetOnAxis(ap=ii[:, jt:jt + 1], axis=0)),
                             r=[B_idxi[slot]] + B_afft, w=[B_gat[slot]])

                load_w(elist[0], 0)
                for _ in compaction(elist[0], 0):
                    pass
                gather(elist[0], 0)
                for ei, e in enumerate(elist):
                    slot = ei % 2
                    ii = idxi[slot]
                    g_ = gat[slot]
                    nxt = None
                    if ei + 1 < len(elist):
                        load_w(elist[ei + 1], 1 - slot)
                        nxt = compaction(elist[ei + 1], 1 - slot)
                    for jt in range(8):
                        xj = X[slot * 8 + jt]
                        pe_group([(lambda pe, kc=kc: pe.transpose(out=tpx[:, kc, :], in_=xj[:, kc * 128:(kc + 1) * 128],
                                                                   identity=ident_bf[:])) for kc in range(8)],
                                 r=[B_X[slot * 8 + jt], B_const], w=[B_tpx])
                        if jt % 2 == 0:
                            P.op("act", lambda a: a.copy(out=XT[:, :, jt * 128:(jt + 1) * 128], in_=tpx[:]), r=[B_tpx], w=[B_XT])
                        else:
                            P.op("dve", lambda v: v.tensor_copy(out=XT[:, :, jt * 128:(jt + 1) * 128], in_=tpx[:]), r=[B_tpx], w=[B_XT])
                    wg_, wu_, wd_ = W[slot]
                    bwg, bwu, bwd = B_W[slot]
                    for fc in range(8):
                        for sh in range(2):
                            gi = ctr["g"] % 2
                            ctr["g"] += 1
                            cs_ = slice(sh * 512, (sh + 1) * 512)
                            P.mm(gps[gi][:], [(wg_[:, kc, fc * 128:(fc + 1) * 128], XT[:, kc, cs_]) for kc in range(8)],
                                 r=[B_XT] + bwg, w=[B_gps[gi]])
                            P.mm(ups[gi][:], [(wu_[:, kc, fc * 128:(fc + 1) * 128], XT[:, kc, cs_]) for kc in range(8)],
                                 r=[B_XT] + bwu, w=[B_ups[gi]])
                            P.op("act", lambda a: a.activation(out=sg[gi][:], in_=gps[gi][:], func=AF.Silu),
                                 r=[B_gps[gi]], w=[B_sg[gi]])
                            P.op("dve", lambda v: v.tensor_tensor(out=AT[:, fc, cs_], in0=ups[gi][:], in1=sg[gi][:], op=ALU.mult),
                                 r=[B_ups[gi], B_sg[gi]], w=[B_AT])
                            if nxt is not None:
                                next(nxt, None)
                    if nxt is not None:
                        for _ in nxt:
                            pass
                        gather(elist[ei + 1], 1 - slot)
                    for jt in range(8):
                        yi = ctr["y"] % 2
                        ctr["y"] += 1
                        for dh in range(2):
                            gi = ctr["g"] % 2
                            ctr["g"] += 1
                            P.mm(gps[gi][:], [(AT[:, fc, jt * 128:(jt + 1) * 128], wd_[:, fc, dh * 512:(dh + 1) * 512]) for fc in range(8)],
                                 r=[B_AT] + bwd, w=[B_gps[gi]])
                            P.op("dve", lambda v: v.tensor_scalar(out=Ysb[yi][:, dh * 512:(dh + 1) * 512], in0=gps[gi][:],
                                                                  scalar1=g_[:, jt, e:e + 1], scalar2=None, op0=ALU.mult),
                                 r=[B_gps[gi], B_gat[slot]], w=[B_Y[yi]])
                        idma(lambda q: q.indirect_dma_start(out=f_d[:, :], out_offset=IndirectOffsetOnAxis(ap=ii[:, jt:jt + 1], axis=0),
                                                            in_=Ysb[yi][:], in_offset=None, compute_op=ALU.add),
                             r=[B_Y[yi], B_idxi[slot]], w=[B_f])
                P.barrier()

        def phase_e():
            with ExitStack() as st:
                gpf, B_gpf = load_bc(st, "gpf", 7)
                fb = [sb(st, "e_f%d" % i, [128, D], F32) for i in range(2)]
                xb = [sb(st, "e_x%d" % i, [128, D], F32) for i in range(2)]
                tb = [sb(st, "e_t%d" % i, [128, D], F32) for i in range(2)]
                ob_ = [sb(st, "e_o%d" % i, [128, D], F32) for i in range(2)]
                junk = sb(st, "e_junk", [128, D], BF16)
                sm = sb(st, "e_sm", [128, 2, 2], F32)
                B_fb, B_xb, B_tb, B_ob, B_sm = [Buf(), Buf()], [Buf(), Buf()], [Buf(), Buf()], [Buf(), Buf()], [Buf(), Buf()]
                B_junk = Buf()
                B_out = [Buf() for _ in range(NTILE)]
                for tl in range(NTILE):
                    i2 = tl % 2
                    rows = slice(tl * 128, (tl + 1) * 128)
                    P.dma("sp", lambda q: q.dma_start(out=fb[i2][:], in_=f_d[rows, :]), r=[B_f], w=[B_fb[i2]])
                    P.dma("sp", lambda q: q.dma_start(out=xb[i2][:], in_=x1_d[rows, :]), r=[B_x1[tl]], w=[B_xb[i2]])
                    P.op("dve", lambda v: v.scalar_tensor_tensor(out=junk[:], in0=fb[i2][:], scalar=1.0, in1=fb[i2][:],
                                                                 op0=ALU.mult, op1=ALU.mult, accum_out=sm[:, i2, 0:1]),
                         r=[B_fb[i2]], w=[B_junk, B_sm[i2]])
                    rstd_from_ss(sm[:, i2, 0:1], D, sm[:, i2, 1:2], sm[:, i2, 0:1], [B_sm[i2]], [B_sm[i2]], B_sm[i2])
                    P.op("dve", lambda v: v.scalar_tensor_tensor(out=tb[i2][:], in0=fb[i2][:], scalar=sm[:, i2, 1:2], in1=gpf[:],
                                                                 op0=ALU.mult, op1=ALU.mult),
                         r=[B_fb[i2], B_sm[i2], B_gpf], w=[B_tb[i2]])
                    P.op("pool", lambda g: g.tensor_tensor(out=ob_[i2][:], in0=tb[i2][:], in1=xb[i2][:], op=ALU.add),
                         r=[B_tb[i2], B_xb[i2]], w=[B_ob[i2]])
                    P.dma("pool", lambda q: q.dma_start(out=out_d[rows, :], in_=ob_[i2][:]), r=[B_ob[i2]], w=[B_out[tl]])
                P.barrier()

        import os
        if stop_after == "0":
            return nc
        phase_a0()
        if stop_after == "A0":
            return nc
        if stop_after == "A1":
            phase_a1(heads=[int(x) for x in os.environ.get("A1_HEADS", "0").split(",")],
                     qchunks=[int(x) for x in os.environ.get("A1_QC", "0,9").split(",")])
            return nc
        if stop_after == "A2":
            phase_a2(heads=[int(x) for x in os.environ.get("A2_HEADS", "0").split(",")])
            return nc
        if not os.environ.get("SKIP_A1"):
            phase_a1()
        phase_a2()
        phase_b()
        if stop_after == "B":
            return nc
        phase_c()
        if stop_after == "C":
            return nc
        phase_d()
        phase_e()
        return nc


def _rope_tables():
    half = 32
    inv_freq = (1.0 / (10000.0 ** (np.arange(0, half, 2, dtype=np.float32) / np.float32(half)))).astype(np.float32)
    t = np.arange(T)
    r = (t // 64).astype(np.float32)
    c = (t % 64).astype(np.float32)
    ang_r = r[:, None] * inv_freq[None, :]
    ang_c = c[:, None] * inv_freq[None, :]
    ang = np.concatenate([ang_r, ang_r, ang_c, ang_c], axis=-1).astype(np.float32)
    cos = np.cos(ang).astype(np.float32)
    sin = np.sin(ang).astype(np.float32)
    sign = np.concatenate([-np.ones(16), np.ones(16), -np.ones(16), np.ones(16)]).astype(np.float32)
    sin = sin * sign[None, :]
    cosT = np.ones((128, S), np.float32)
    sinT = np.zeros((128, S), np.float32)
    cosT[:, TC:] = np.concatenate([cos.T, cos.T], axis=0)
    sinT[:, TC:] = np.concatenate([sin.T, sin.T], axis=0)
    return cosT, sinT


def _win_cols():
    rot = np.concatenate([np.arange(16, 32), np.arange(0, 16), np.arange(48, 64), np.arange(32, 48)])
    fm, tm, tg = [], [], []
    for h in range(NH):
        for off in (0, 512):
            base = off + h * 128
            fm.append(base + np.arange(128))
            fm.append(np.concatenate([base + rot, base + 64 + rot]))
        fm.append(1536 + h * 128 + np.arange(128))
        fm.append(2048 + h * 128 + np.arange(128))
        fm.append(3072 + h * 128 + np.arange(128))
        tm.append(1024 + h * 128 + np.arange(128))
        tm.append(2560 + h * 128 + np.arange(128))
        tg.append(3584 + h * 128 + np.arange(128))
    tm = tm + tg
    return np.concatenate(fm + tm)


def _kc(a):
    n = a.shape[-1]
    return np.ascontiguousarray(a.reshape(8, 128, n).transpose(1, 0, 2))


def prep_inputs(inp, n_cores):
    f = lambda k: np.asarray(inp[k], dtype=np.float32)
    x, c, ctx, c_ctx = f("x"), f("c"), f("ctx"), f("c_ctx")
    cosT, sinT = _rope_tables()
    p = np.arange(128)
    blk = p // 64
    same = blk[:, None] == blk[None, :]
    cm = np.zeros((128, 4, 128), np.float32)
    cm[:, 0, :] = np.eye(128)
    cm[:, 1, :] = same & (p[:, None] <= p[None, :])
    cm[:, 2, :] = same & (p[:, None] >= p[None, :])
    cm[:, 3, :] = p[:, None] < p[None, :]
    j = np.arange(1024)
    rm = np.ones((128, 2, 1024), np.float32)
    rm[:, 0, j % 64 == 0] = 0.0
    rm[:, 1, j % 64 == 63] = 0.0
    iota = np.broadcast_to(j.astype(np.float32), (128, 1024)).copy()
    tt = np.arange(NTILE)[None, :] * 128 + p[:, None]
    tokhl = np.stack([64 * (tt // 64), tt % 64], axis=-1).astype(np.float32)
    hlb = f("hg_lower_bound").reshape(2, 2, 4, 128).transpose(3, 0, 1, 2).reshape(128, 16)
    shared = {
        "w_ada": _kc(f("w_ada")[0]),
        "b_ada": f("b_ada")[0][None, :],
        "norms": np.concatenate([f("norm_pre_mix")[0], f("norm_post_mix")[0], f("norm_pre_ffn")[0],
                                 f("norm_post_ffn")[0]])[None, :],
        "w_in": _kc(f("w_in")[0][:, _win_cols()]),
        "lamv": np.concatenate([f("da_lambda_q1")[0], f("da_lambda_k1")[0], f("da_lambda_q2")[0],
                                f("da_lambda_k2")[0]])[None, :],
        "subln": f("da_subln")[0][:, None],
        "hgn": f("hg_norm")[0][None, :],
        "hlb": np.ascontiguousarray(hlb),
        "w_out": _kc(f("w_out")[0]),
        "w_r": _kc(f("w_router")[0]),
        "w_gate": np.ascontiguousarray(f("w_gate")[0].reshape(NE, 8, 128, D).transpose(0, 2, 1, 3)),
        "w_up": np.ascontiguousarray(f("w_up")[0].reshape(NE, 8, 128, D).transpose(0, 2, 1, 3)),
        "w_down": np.ascontiguousarray(f("w_down")[0].reshape(NE, 8, 128, D).transpose(0, 2, 1, 3)),
        "cosT": cosT, "sinT": sinT, "cmasks": cm, "rmask": rm, "iota": iota, "tokhl": tokhl,
    }
    maps = []
    for i in range(n_cores):
        b = i % 2
        m = dict(shared)
        m["x"] = np.ascontiguousarray(x[b])
        m["ctx"] = np.ascontiguousarray(ctx[b])
        m["cc"] = _kc(np.stack([c[b], c_ctx], axis=1))
        maps.append(m)
    return maps


N_CORES = 2
_NC_CACHE = {}


def kernel(**inputs):
    if "nc" not in _NC_CACHE:
        _NC_CACHE["nc"] = build()
    nc = _NC_CACHE["nc"]
    maps = prep_inputs(inputs, N_CORES)
    res = run_bass_kernel_spmd(nc, maps, core_ids=list(range(N_CORES)))
    out = np.stack([np.asarray(res.results[b]["out"], dtype=np.float32) for b in range(2)], axis=0)
    return out
```

```python
import numpy as np
from contextlib import ExitStack
import concourse.bass as bass
import concourse.mybir as mybir
from concourse.bass import IndirectOffsetOnAxis
from concourse.bass_utils import run_bass_kernel_spmd

F32 = mybir.dt.float32
BF16 = mybir.dt.bfloat16
I32 = mybir.dt.int32
U32 = mybir.dt.uint32
AF = mybir.ActivationFunctionType
ALU = mybir.AluOpType
AX = mybir.AxisListType

D = 1024
T = 8192
TC = 256
S = T + TC
NH = 4
NE = 16
CAP = 1024
EPS = 1e-6
NTILE = T // 128
FM_BLOCKS = 7
TM_COLS = 384
HEAD_COLS = FM_BLOCKS * 128 + TM_COLS
FM_TOTAL = NH * FM_BLOCKS * 128
WCOLS = NH * HEAD_COLS


class Buf:
    __slots__ = ("w", "r")

    def __init__(self):
        self.w = None
        self.r = {}


class Prog:
    def __init__(self, nc, es, ndma=12):
        self.nc = nc
        self.eng = dict(pe=nc.tensor, act=nc.scalar, dve=nc.vector, pool=nc.gpsimd, sp=nc.sync)
        self.sem = {k: es.enter_context(nc.semaphore("s_" + k)) for k in self.eng}
        self.cnt = {k: 0 for k in self.eng}
        self.waited = {k: {} for k in self.eng}
        self.dsem = {q: [[es.enter_context(nc.semaphore("d_%s%d" % (q, i))), 0] for i in range(ndma)]
                     for q in ("sp", "pool")}
        self.dnext = {"sp": 0, "pool": 0}
        self.nwait = 0
        self.nops = 0
        import os
        self.limit = int(os.environ.get('OPLIMIT', '1000000000'))

    def _wait(self, e, ev):
        s, v = ev
        w = self.waited[e]
        if w.get(s.num, 0) < v:
            self.eng[e].wait_ge(s, v)
            w[s.num] = v
            self.nwait += 1

    def _deps(self, e, reads, writes):
        own = self.sem[e].num
        for b in reads:
            if b.w is not None:
                if not (e == "pe" and b.w[0].num == own):
                    self._wait(e, b.w)
        for b in writes:
            if b.w is not None:
                if not (e == "pe" and b.w[0].num == own):
                    self._wait(e, b.w)
            for ev in b.r.values():
                if ev[0].num == own:
                    continue
                self._wait(e, ev)

    def _mark(self, ev, reads, writes):
        k = ev[0].num
        for b in reads:
            old = b.r.get(k)
            if old is None or old[1] < ev[1]:
                b.r[k] = ev
        for b in writes:
            b.w = ev
            b.r = {}

    def op(self, e, fn, r=(), w=()):
        self.nops += 1
        if self.nops > self.limit:
            return None
        if self.nops == self.limit:
            print('LAST OP', e, fn.__code__.co_firstlineno)
        self._deps(e, r, w)
        ins = fn(self.eng[e])
        self.cnt[e] += 1
        ins.then_inc(self.sem[e], 1)
        ev = (self.sem[e], self.cnt[e])
        self._mark(ev, r, w)
        return ev

    def mm(self, out_ap, pairs, r=(), w=()):
        self.nops += 1
        if self.nops > self.limit:
            return None
        self._deps("pe", r, w)
        n = len(pairs)
        ins = None
        for i, (l, rh) in enumerate(pairs):
            ins = self.nc.tensor.matmul(out_ap, lhsT=l, rhs=rh, start=(i == 0), stop=(i == n - 1))
        self.cnt["pe"] += 1
        ins.then_inc(self.sem["pe"], 1)
        ev = (self.sem["pe"], self.cnt["pe"])
        self._mark(ev, r, w)
        return ev

    def dma(self, q, fn, r=(), w=()):
        self.nops += 1
        if self.nops > self.limit:
            return None
        slots = self.dsem[q]
        i = self.dnext[q]
        self.dnext[q] = (i + 1) % len(slots)
        s, v = slots[i]
        if v > 0:
            self._wait(q, (s, v))
        self._deps(q, r, w)
        ins = fn(self.eng[q])
        slots[i][1] = v + 16
        ins.then_inc(s, 16)
        ev = (s, v + 16)
        self._mark(ev, r, w)
        return ev

    def all_events(self):
        evs = [(self.sem[k], self.cnt[k]) for k in self.eng if self.cnt[k] > 0]
        for q in self.dsem:
            for s, v in self.dsem[q]:
                if v > 0:
                    evs.append((s, v))
        return evs

    def barrier(self, engines=None):
        evs = self.all_events()
        for e in (engines or self.eng):
            for ev in evs:
                if ev[0].num != self.sem[e].num:
                    self._wait(e, ev)


def build(stop_after=None, dbg=()):
    nc = bass.Bass("TRN2", target_bir_lowering=False)
    dbg = set(dbg)

    def din(name, shape, dt=F32):
        return nc.dram_tensor(name, list(shape), dt, kind="ExternalInput").ap()

    def dscr(name, shape, dt):
        kind = "ExternalOutput" if name in dbg else "Internal"
        return nc.dram_tensor(name, list(shape), dt, kind=kind).ap()

    x_d = din("x", [T, D])
    ctx_d = din("ctx", [TC, D])
    cc_d = din("cc", [128, 8, 2])
    wada_d = din("w_ada", [128, 8, 6 * D])
    bada_d = din("b_ada", [1, 6 * D])
    norms_d = din("norms", [1, 4 * D])
    win_d = din("w_in", [128, 8, WCOLS])
    lamv_d = din("lamv", [1, 256])
    subln_d = din("subln", [128, 1])
    hgn_d = din("hgn", [1, 128])
    hlb_d = din("hlb", [128, 16])
    wout_d = din("w_out", [128, 8, D])
    wr_d = din("w_r", [128, 8, NE])
    wg_d = din("w_gate", [NE, 128, 8, D])
    wu_d = din("w_up", [NE, 128, 8, D])
    wd_d = din("w_down", [NE, 128, 8, D])
    cos_d = din("cosT", [128, S])
    sin_d = din("sinT", [128, S])
    cm_d = din("cmasks", [128, 4, 128])
    rm_d = din("rmask", [128, 2, 1024])
    iota_d = din("iota", [128, 1024])
    tokhl_d = din("tokhl", [128, NTILE, 2])
    out_d = nc.dram_tensor("out", [T, D], F32, kind="ExternalOutput").ap()

    modrows_d = dscr("modrows", [8, D], F32)
    qkt_d = dscr("qkt", [NH, 2, 128, S], BF16)
    zt_d = dscr("zt", [NH, 3, 128, S], F32)
    vi_d = dscr("vi", [S, NH, 2, 128], BF16)
    g_d = dscr("gsil", [S, NH, 128], F32)
    mixt_d = dscr("mixt", [D, T], BF16)
    x1_d = dscr("x1", [T, D], F32)
    h2_d = dscr("h2", [T, D], BF16)
    aff_d = dscr("aff", [T, NE], F32)
    f_d = dscr("facc", [T, D], F32)

    B_modrows = Buf()
    B_qkt = [[Buf() for _ in range(17)] for _ in range(NH)]
    B_zt = [[Buf() for _ in range(17)] for _ in range(NH)]
    B_vi = [Buf() for _ in range(17)]
    B_g = [Buf() for _ in range(17)]
    B_mixt = [Buf() for _ in range(16)]
    B_x1 = [Buf() for _ in range(NTILE)]
    B_h2 = Buf()
    B_aff = Buf()
    B_f = Buf()

    es = ExitStack()
    with es:
        P = Prog(nc, es)

        def sb(stack, name, shape, dt):
            return stack.enter_context(nc.sbuf_tensor("sb_" + name, list(shape), dt))

        def ps(stack, name, shape, dt=F32):
            return stack.enter_context(nc.psum_tensor("ps_" + name, list(shape), dt))

        cm_f = sb(es, "cm_f", [128, 4, 128], F32)
        ident_bf = sb(es, "ident_bf", [128, 128], BF16)
        ones_bf = sb(es, "ones_bf", [128, 128], BF16)
        ones_f = sb(es, "ones_f", [128, 128], F32)
        neglam = sb(es, "neglam", [128, 1], F32)
        subln8 = sb(es, "subln8", [128, 1], F32)
        lbt = sb(es, "lbt", [128, 8], F32)
        omlt = sb(es, "omlt", [128, 8], F32)
        mhalf = sb(es, "mhalf", [128, 512], F32)
        epsb = sb(es, "epsb", [128, 1], F32)
        B_const = Buf()
        ident_f = cm_f[:, 0, :]

        P.dma("sp", lambda q: q.dma_start(out=cm_f[:], in_=cm_d[:]), w=[B_const])
        P.op("dve", lambda v: v.tensor_copy(out=ident_bf[:], in_=cm_f[:, 0, :]), r=[B_const], w=[B_const])
        P.op("dve", lambda v: v.memset(ones_bf[:], 1.0), w=[B_const])
        P.op("dve", lambda v: v.memset(ones_f[:], 1.0), w=[B_const])
        P.op("dve", lambda v: v.memset(mhalf[:], -0.5), w=[B_const])
        P.op("dve", lambda v: v.memset(epsb[:], EPS), w=[B_const])

        def rstd_from_ss(ss_ap, n, out_ap, tmp_ap, bufs_r, bufs_w, tmpbuf):
            P.op("dve", lambda v: v.tensor_scalar(out=tmp_ap, in0=ss_ap, scalar1=1.0 / n, scalar2=EPS,
                                                  op0=ALU.mult, op1=ALU.add), r=bufs_r, w=[tmpbuf])
            shp = list(tmp_ap.shape)
            P.op("pool", lambda g: g.tensor_tensor(out=out_ap, in0=tmp_ap, in1=mhalf[0:shp[0], 0:shp[1]],
                                                   op=ALU.pow), r=[tmpbuf, B_const], w=bufs_w)

        with ExitStack() as p0:
            scf = sb(p0, "scf", [128, 8, 2], F32)
            sct = sb(p0, "sct", [128, 8, 2], F32)
            wa = [sb(p0, "wa%d" % i, [128, 8, 512], F32) for i in range(2)]
            modl = sb(p0, "modl", [1, 6 * D], F32)
            modc = sb(p0, "modc", [1, 6 * D], F32)
            bada = sb(p0, "bada", [1, 6 * D], F32)
            nrm = sb(p0, "nrm", [1, 4 * D], F32)
            rows = sb(p0, "rows", [1, 8, D], F32)
            lamv = sb(p0, "lamv", [1, 256], F32)
            lamt = sb(p0, "lamt", [1, 8], F32)
            hlb = sb(p0, "hlb", [128, 16], F32)
            sl = sb(p0, "sl", [128, 1], F32)
            pm = [ps(p0, "pm%d" % i, [1, 512]) for i in range(4)]
            pl = ps(p0, "pl", [128, 1])
            B_sc, B_wa, B_modl, B_modc, B_bada, B_nrm, B_rows, B_lam, B_hlb = (
                Buf(), [Buf(), Buf()], Buf(), Buf(), Buf(), Buf(), Buf(), Buf(), Buf())
            B_pm = [Buf() for _ in range(4)]
            B_pl = Buf()

            P.dma("sp", lambda q: q.dma_start(out=scf[:], in_=cc_d[:]), w=[B_sc])
            P.dma("sp", lambda q: q.dma_start(out=bada[:], in_=bada_d[:]), w=[B_bada])
            P.dma("sp", lambda q: q.dma_start(out=nrm[:], in_=norms_d[:]), w=[B_nrm])
            P.dma("sp", lambda q: q.dma_start(out=lamv[:], in_=lamv_d[:]), w=[B_lam])
            P.dma("sp", lambda q: q.dma_start(out=hlb[:], in_=hlb_d[:]), w=[B_hlb])
            P.dma("sp", lambda q: q.dma_start(out=sl[:], in_=subln_d[:]), w=[B_hlb])
            P.op("act", lambda a: a.activation(out=sct[:], in_=scf[:], func=AF.Exp, scale=-1.0), r=[B_sc], w=[B_rows])
            P.op("dve", lambda v: v.tensor_scalar(out=sct[:], in0=sct[:], scalar1=1.0, scalar2=None, op0=ALU.add),
                 r=[B_rows], w=[B_rows])
            P.op("dve", lambda v: v.reciprocal(out=sct[:], in_=sct[:]), r=[B_rows], w=[B_rows])
            P.op("dve", lambda v: v.tensor_tensor(out=scf[:], in0=scf[:], in1=sct[:], op=ALU.mult),
                 r=[B_rows, B_sc], w=[B_sc])
            for ch in range(12):
                wb_ = wa[ch % 2]
                P.dma("sp", lambda q: q.dma_start(out=wb_[:], in_=wada_d[:, :, ch * 512:(ch + 1) * 512]),
                      w=[B_wa[ch % 2]])
                for j, (mod, bm) in enumerate(((modl, B_modl), (modc, B_modc))):
                    pmt = pm[(2 * ch + j) % 4]
                    bp = B_pm[(2 * ch + j) % 4]
                    P.mm(pmt[:], [(scf[:, kc, j:j + 1], wb_[:, kc, :]) for kc in range(8)],
                         r=[B_sc, B_wa[ch % 2]], w=[bp])
                    P.op("dve", lambda v: v.tensor_tensor(out=mod[:, ch * 512:(ch + 1) * 512], in0=pmt[:],
                                                          in1=bada[:, ch * 512:(ch + 1) * 512], op=ALU.add),
                         r=[bp, B_bada], w=[bm])
            def stt(dst, a, b, op0):
                P.op("dve", lambda v: v.scalar_tensor_tensor(out=rows[:, dst, :], in0=a, scalar=(1.0 if op0 == ALU.add else 1.0),
                                                             in1=b, op0=op0, op1=ALU.mult),
                     r=[B_modl, B_modc, B_nrm], w=[B_rows])
            stt(0, modl[:, D:2 * D], nrm[:, 0:D], ALU.add)
            P.op("dve", lambda v: v.tensor_copy(out=rows[:, 1, :], in_=modl[:, 0:D]), r=[B_modl], w=[B_rows])
            stt(2, modc[:, D:2 * D], nrm[:, 0:D], ALU.add)
            P.op("dve", lambda v: v.tensor_copy(out=rows[:, 3, :], in_=modc[:, 0:D]), r=[B_modc], w=[B_rows])
            stt(4, modl[:, 2 * D:3 * D], nrm[:, D:2 * D], ALU.mult)
            stt(5, modl[:, 4 * D:5 * D], nrm[:, 2 * D:3 * D], ALU.add)
            P.op("dve", lambda v: v.tensor_copy(out=rows[:, 6, :], in_=modl[:, 3 * D:4 * D]), r=[B_modl], w=[B_rows])
            stt(7, modl[:, 5 * D:6 * D], nrm[:, 3 * D:4 * D], ALU.mult)
            P.dma("pool", lambda q: q.dma_start(out=modrows_d[:, :].rearrange("(o r) d -> o r d", o=1), in_=rows[:]),
                  r=[B_rows], w=[B_modrows])
            P.op("dve", lambda v: v.tensor_tensor(out=lamv[:, 0:64], in0=lamv[:, 0:64], in1=lamv[:, 64:128], op=ALU.mult),
                 r=[B_lam], w=[B_lam])
            P.op("dve", lambda v: v.tensor_tensor(out=lamv[:, 128:192], in0=lamv[:, 128:192], in1=lamv[:, 192:256], op=ALU.mult),
                 r=[B_lam], w=[B_lam])
            P.op("dve", lambda v: v.tensor_reduce(out=lamt[:, 0:1], in_=lamv[:, 0:64], axis=AX.X, op=ALU.add),
                 r=[B_lam], w=[B_lam])
            P.op("dve", lambda v: v.tensor_reduce(out=lamt[:, 1:2], in_=lamv[:, 128:192], axis=AX.X, op=ALU.add),
                 r=[B_lam], w=[B_lam])
            P.op("act", lambda a: a.activation(out=lamt[:, 2:4], in_=lamt[:, 0:2], func=AF.Exp), r=[B_lam], w=[B_lam])
            P.op("dve", lambda v: v.scalar_tensor_tensor(out=lamt[:, 4:5], in0=lamt[:, 3:4], scalar=-0.2, in1=lamt[:, 2:3],
                                                         op0=ALU.add, op1=ALU.subtract), r=[B_lam], w=[B_lam])
            P.mm(pl[:], [(ones_f[0:1, :], lamt[0:1, 4:5])], r=[B_lam, B_const], w=[B_pl])
            P.op("dve", lambda v: v.tensor_copy(out=neglam[:], in_=pl[:]), r=[B_pl], w=[B_const])
            P.op("dve", lambda v: v.tensor_scalar(out=subln8[:], in0=sl[:], scalar1=0.8, scalar2=None, op0=ALU.mult),
                 r=[B_hlb], w=[B_const])
            P.op("dve", lambda v: v.tensor_tensor(out=hlb[:, 0:8], in0=hlb[:, 8:16], in1=hlb[:, 0:8], op=ALU.subtract),
                 r=[B_hlb], w=[B_hlb])
            P.op("act", lambda a: a.activation(out=hlb[:, 0:8], in_=hlb[:, 0:8], func=AF.Exp), r=[B_hlb], w=[B_hlb])
            P.op("dve", lambda v: v.tensor_scalar(out=hlb[:, 0:8], in0=hlb[:, 0:8], scalar1=1.0, scalar2=None, op0=ALU.add),
                 r=[B_hlb], w=[B_hlb])
            P.op("dve", lambda v: v.reciprocal(out=lbt[:], in_=hlb[:, 0:8]), r=[B_hlb], w=[B_const])
            P.op("dve", lambda v: v.tensor_scalar(out=omlt[:], in0=lbt[:], scalar1=-1.0, scalar2=1.0, op0=ALU.mult, op1=ALU.add),
                 r=[B_const], w=[B_const])
            P.barrier()

        def load_bc(stack, name, row):
            t = sb(stack, name, [128, D], F32)
            b = Buf()
            P.dma("sp", lambda q: q.dma_start(out=t[:], in_=modrows_d[row:row + 1, :].partition_broadcast(128)),
                  r=[B_modrows], w=[b])
            return t, b

        def pe_group(fns, r, w):
            P._deps("pe", r, w)
            ins = None
            for fn in fns:
                ins = fn(nc.tensor)
            P.cnt["pe"] += 1
            ins.then_inc(P.sem["pe"], 1)
            ev = (P.sem["pe"], P.cnt["pe"])
            P._mark(ev, r, w)
            return ev

        SCS = [(0, 2)] + [(2 + 4 * k, 4) for k in range(16)]

        def phase_a0():
            with ExitStack() as st:
                wbf = sb(st, "wbf", [128, 8, WCOLS], BF16)
                B_w = [[Buf() for _ in range(4)] for _ in range(8)]
                for kc in range(8):
                    for pi in range(4):
                        c0 = pi * 1280
                        P.dma("pool", lambda q: q.dma_start(out=wbf[:, kc, c0:c0 + 1280], in_=win_d[:, kc, c0:c0 + 1280]),
                              w=[B_w[kc][pi]])
                B_wall = [b for l in B_w for b in l]
                gm_l, B_gml = load_bc(st, "gm_l", 0)
                sh_l, B_shl = load_bc(st, "sh_l", 1)
                gm_c, B_gmc = load_bc(st, "gm_c", 2)
                sh_c, B_shc = load_bc(st, "sh_c", 3)
                xb = [sb(st, "xb%d" % i, [128, D], F32) for i in range(2)]
                B_xb = [Buf(), Buf()]
                junk = sb(st, "junk", [128, D], BF16)
                B_junk = Buf()
                hn = [sb(st, "hn%d" % i, [128, D], F32) for i in range(2)]
                B_hn = [Buf(), Buf()]
                hb = [sb(st, "hb%d" % i, [128, D], BF16) for i in range(4)]
                B_hb = [Buf() for _ in range(4)]
                ssm = sb(st, "ssm", [128, 8], F32)
                B_ss = [Buf() for _ in range(4)]
                hT = [sb(st, "hT%d" % i, [128, 8, 512], BF16) for i in range(2)]
                B_hT = [Buf(), Buf()]
                cs = [sb(st, "cs%d" % i, [128, 2, 512], F32) for i in range(2)]
                B_cs = [Buf(), Buf()]
                qks = [sb(st, "qks%d" % i, [128, 2, 512], BF16) for i in range(2)]
                B_qks = [Buf(), Buf()]
                zs = [sb(st, "zs%d" % i, [128, 3, 512], F32) for i in range(2)]
                B_zs = [Buf(), Buf()]
                t1 = sb(st, "t1", [128, 512], F32)
                t2 = sb(st, "t2", [128, 512], F32)
                B_t1, B_t2 = Buf(), Buf()
                vis = [sb(st, "vis%d" % i, [128, NH, 2, 128], BF16) for i in range(2)]
                B_vis = [Buf(), Buf()]
                gs = [sb(st, "gs%d" % i, [128, NH, 128], F32) for i in range(2)]
                B_gs = [Buf(), Buf()]
                tp = [ps(st, "tp%d" % i, [128, 8, 128], BF16) for i in range(2)]
                B_tp = [Buf(), Buf()]
                fm = [ps(st, "fm%d" % i, [128, 512]) for i in range(3)]
                B_fm = [Buf() for _ in range(3)]
                tm = ps(st, "tm", [128, 1536])
                B_tm = Buf()
                fmi = [0]
                tile_ctr = [0]

                def stage1(k):
                    t0, nt = SCS[k]
                    for j in range(nt):
                        i = tile_ctr[0]
                        tile_ctr[0] += 1
                        x_ = xb[i % 2]
                        st_ = t0 + j
                        src = ctx_d[st_ * 128:(st_ + 1) * 128, :] if k == 0 else x_d[(st_ - 2) * 128:(st_ - 1) * 128, :]
                        gm, bgm, sh, bsh = (gm_c, B_gmc, sh_c, B_shc) if k == 0 else (gm_l, B_gml, sh_l, B_shl)
                        P.dma("sp", lambda q: q.dma_start(out=x_[:], in_=src), w=[B_xb[i % 2]])
                        ss = ssm[:, 2 * j:2 * j + 1]
                        rs = ssm[:, 2 * j + 1:2 * j + 2]
                        P.op("dve", lambda v: v.scalar_tensor_tensor(out=junk[:], in0=x_[:], scalar=1.0, in1=x_[:],
                                                                     op0=ALU.mult, op1=ALU.mult, accum_out=ss),
                             r=[B_xb[i % 2]], w=[B_junk, B_ss[j]])
                        rstd_from_ss(ss, D, rs, ss, [B_ss[j]], [B_ss[j]], B_ss[j])
                        h_ = hn[i % 2]
                        P.op("dve", lambda v: v.scalar_tensor_tensor(out=h_[:], in0=x_[:], scalar=rs, in1=gm[:],
                                                                     op0=ALU.mult, op1=ALU.mult),
                             r=[B_xb[i % 2], B_ss[j], bgm], w=[B_hn[i % 2]])
                        P.op("pool", lambda g: g.tensor_tensor(out=hb[j][:], in0=h_[:], in1=sh[:], op=ALU.add),
                             r=[B_hn[i % 2], bsh], w=[B_hb[j]])

                def stage2(k):
                    t0, nt = SCS[k]
                    for j in range(nt):
                        tpp = tp[j % 2]
                        pe_group([(lambda pe, kc=kc: pe.transpose(out=tpp[:, kc, :], in_=hb[j][:, kc * 128:(kc + 1) * 128],
                                                                   identity=ident_bf[:])) for kc in range(8)],
                                 r=[B_hb[j], B_const], w=[B_tp[j % 2]])
                        P.op("act", lambda a: a.copy(out=hT[k % 2][:, :, j * 128:(j + 1) * 128], in_=tpp[:]),
                             r=[B_tp[j % 2]], w=[B_hT[k % 2]])

                def fm_mm(k, col0, n):
                    bi = fmi[0] % 3
                    fmi[0] += 1
                    P.mm(fm[bi][:, 0:n], [(wbf[:, kc, col0:col0 + 128], hT[k % 2][:, kc, 0:n]) for kc in range(8)],
                         r=[B_hT[k % 2]] + B_wall, w=[B_fm[bi]])
                    return bi

                def stage3(k):
                    t0, nt = SCS[k]
                    n = nt * 128
                    s0 = t0 * 128
                    c_ = cs[k % 2]
                    P.dma("sp", lambda q: q.dma_start(out=c_[:, 0, 0:n], in_=cos_d[:, s0:s0 + n]), w=[B_cs[k % 2]])
                    P.dma("sp", lambda q: q.dma_start(out=c_[:, 1, 0:n], in_=sin_d[:, s0:s0 + n]), w=[B_cs[k % 2]])
                    for h in range(NH):
                        hi = k * NH + h
                        qk_ = qks[hi % 2]
                        z_ = zs[hi % 2]
                        base = h * FM_BLOCKS * 128
                        for t in range(2):
                            b0 = fm_mm(k, base + (2 * t) * 128, n)
                            b1 = fm_mm(k, base + (2 * t + 1) * 128, n)
                            P.op("dve", lambda v: v.tensor_tensor(out=t1[:, 0:n], in0=fm[b0][:, 0:n], in1=c_[:, 0, 0:n], op=ALU.mult),
                                 r=[B_fm[b0], B_cs[k % 2]], w=[B_t1])
                            P.op("dve", lambda v: v.tensor_tensor(out=t2[:, 0:n], in0=fm[b1][:, 0:n], in1=c_[:, 1, 0:n], op=ALU.mult),
                                 r=[B_fm[b1], B_cs[k % 2]], w=[B_t2])
                            P.op("pool", lambda g: g.tensor_tensor(out=qk_[:, t, 0:n], in0=t1[:, 0:n], in1=t2[:, 0:n], op=ALU.add),
                                 r=[B_t1, B_t2], w=[B_qks[hi % 2]])
                        for t in range(3):
                            b0 = fm_mm(k, base + (4 + t) * 128, n)
                            P.op("act", lambda a: a.copy(out=z_[:, t, 0:n], in_=fm[b0][:, 0:n]), r=[B_fm[b0]], w=[B_zs[hi % 2]])
                        P.dma("pool", lambda q: q.dma_start(out=qkt_d[h].rearrange("t p s -> p t s")[:, :, s0:s0 + n],
                                                            in_=qk_[:, :, 0:n]), r=[B_qks[hi % 2]], w=[B_qkt[h][k]])
                        P.dma("pool", lambda q: q.dma_start(out=zt_d[h].rearrange("t p s -> p t s")[:, :, s0:s0 + n],
                                                            in_=z_[:, :, 0:n]), r=[B_zs[hi % 2]], w=[B_zt[h][k]])
                    for j in range(nt):
                        ti = t0 + j
                        for nb in range(3):
                            P.mm(tm[:, nb * 512:(nb + 1) * 512],
                                 [(hT[k % 2][:, kc, j * 128:(j + 1) * 128], wbf[:, kc, FM_TOTAL + nb * 512:FM_TOTAL + (nb + 1) * 512])
                                  for kc in range(8)], r=[B_hT[k % 2]] + B_wall, w=[B_tm])
                        v_ = vis[ti % 2]
                        g_ = gs[ti % 2]
                        vflat = v_[:].rearrange("p h t c -> p (h t c)")
                        P.op("dve", lambda v: v.tensor_copy(out=vflat[:, 0:512], in_=tm[:, 0:512]),
                             r=[B_tm], w=[B_vis[ti % 2]])
                        P.op("dve", lambda v: v.tensor_copy(out=vflat[:, 512:1024], in_=tm[:, 512:1024]),
                             r=[B_tm], w=[B_vis[ti % 2]])
                        P.op("act", lambda a: a.activation(out=g_[:].rearrange("p h c -> p (h c)"), in_=tm[:, 1024:1536], func=AF.Silu),
                             r=[B_tm], w=[B_gs[ti % 2]])
                        P.dma("pool", lambda q: q.dma_start(out=vi_d[ti * 128:(ti + 1) * 128], in_=v_[:]),
                              r=[B_vis[ti % 2]], w=[B_vi[k]])
                        P.dma("pool", lambda q: q.dma_start(out=g_d[ti * 128:(ti + 1) * 128], in_=g_[:]),
                              r=[B_gs[ti % 2]], w=[B_g[k]])

                stage1(0)
                stage2(0)
                for k in range(17):
                    if k + 1 < 17:
                        stage1(k + 1)
                    stage3(k)
                    if k + 1 < 17:
                        stage2(k + 1)
                P.barrier()

        def mm1(out_ap, lhsT, rhs, start, stop, r, w):
            P.nops += 1
            if P.nops > P.limit:
                return None
            P._deps("pe", r, w)
            ins = nc.tensor.matmul(out_ap, lhsT=lhsT, rhs=rhs, start=start, stop=stop)
            P.cnt["pe"] += 1
            ins.then_inc(P.sem["pe"], 1)
            ev = (P.sem["pe"], P.cnt["pe"])
            P._mark(ev, r, w)
            return ev

        NKT = S // 128

        def phase_a1(heads=range(NH), qchunks=range(16)):
            with ExitStack() as st:
                kt = [sb(st, "kt%d" % i, [128, S], BF16) for i in range(2)]
                vv = [sb(st, "vv%d" % i, [128, NKT, 128], BF16) for i in range(2)]
                B_kt = [Buf(), Buf()]
                B_vv = [Buf(), Buf()]
                qt = [sb(st, "qt%d" % i, [128, 512], BF16) for i in range(2)]
                B_qt = [Buf(), Buf()]
                p1 = [sb(st, "p1_%d" % i, [128, 512], BF16) for i in range(3)]
                p2 = [sb(st, "p2_%d" % i, [128, 512], BF16) for i in range(3)]
                B_p1 = [Buf() for _ in range(3)]
                B_p2 = [Buf() for _ in range(3)]
                acc1s = [sb(st, "acc1_%d" % i, [128, 512], F32) for i in range(2)]
                acc2s = [sb(st, "acc2_%d" % i, [128, 512], F32) for i in range(2)]
                B_acc1s, B_acc2s = [Buf(), Buf()], [Buf(), Buf()]
                o1c = sb(st, "o1c", [128, 512], F32)
                o2c = sb(st, "o2c", [128, 512], F32)
                B_o1c, B_o2c = Buf(), Buf()
                r1 = sb(st, "r1", [128, 512], F32)
                o1 = sb(st, "o1", [128, 512], F32)
                r2 = sb(st, "r2", [128, 512], F32)
                o2 = sb(st, "o2", [128, 512], F32)
                oo = sb(st, "oo", [128, 512], F32)
                sq = sb(st, "sq", [128, 512], F32)
                rs = sb(st, "rs", [128, 512], F32)
                ob = [sb(st, "ob%d" % i, [128, 512], BF16) for i in range(2)]
                B_r1, B_o1, B_r2, B_o2, B_oo, B_sq, B_rs = Buf(), Buf(), Buf(), Buf(), Buf(), Buf(), Buf()
                B_ob = [Buf(), Buf()]
                s1 = [ps(st, "s1_%d" % i, [128, 512]) for i in range(2)]
                s2 = [ps(st, "s2_%d" % i, [128, 512]) for i in range(2)]
                B_s1 = [Buf(), Buf()]
                B_s2 = [Buf(), Buf()]
                o1p = ps(st, "o1p", [128, 512])
                o2p = ps(st, "o2p", [128, 512])
                l1p = ps(st, "l1p", [128, 512])
                l2p = ps(st, "l2p", [128, 512])
                B_o1p, B_o2p, B_l1p, B_l2p = Buf(), Buf(), Buf(), Buf()
                def finalize(h, qc, acc1, acc2, B_acc1, B_acc2, o_, bo):
                    P.mm(l1p[:], [(ones_f[:], acc1[:])], r=[B_acc1, B_const], w=[B_l1p])
                    P.mm(l2p[:], [(ones_f[:], acc2[:])], r=[B_acc2, B_const], w=[B_l2p])
                    P.op("act", lambda a: a.activation(out=r1[:], in_=l1p[:], func=AF.Ln), r=[B_l1p], w=[B_r1])
                    P.op("act", lambda a: a.activation(out=r1[:], in_=r1[:], func=AF.Exp, scale=-1.0), r=[B_r1], w=[B_r1])
                    P.op("dve", lambda v: v.tensor_tensor(out=o1[:], in0=o1c[:], in1=r1[:], op=ALU.mult),
                         r=[B_o1c, B_r1], w=[B_o1])
                    P.op("act", lambda a: a.activation(out=r2[:], in_=l2p[:], func=AF.Ln), r=[B_l2p], w=[B_r2])
                    P.op("act", lambda a: a.activation(out=r2[:], in_=r2[:], func=AF.Exp, scale=-1.0), r=[B_r2], w=[B_r2])
                    P.op("dve", lambda v: v.tensor_tensor(out=o2[:], in0=o2c[:], in1=r2[:], op=ALU.mult),
                         r=[B_o2c, B_r2], w=[B_o2])
                    P.op("dve", lambda v: v.scalar_tensor_tensor(out=oo[:], in0=o2[:], scalar=neglam[:, 0:1], in1=o1[:],
                                                                 op0=ALU.mult, op1=ALU.add),
                         r=[B_o2, B_o1, B_const], w=[B_oo])
                    P.op("dve", lambda v: v.tensor_tensor(out=sq[:], in0=oo[:], in1=oo[:], op=ALU.mult),
                         r=[B_oo], w=[B_sq])
                    P.mm(l1p[:], [(ones_f[:], sq[:])], r=[B_sq, B_const], w=[B_l1p])
                    P.op("act", lambda a: a.activation(out=rs[:], in_=l1p[:], func=AF.Ln, scale=1.0 / 128, bias=epsb[:, 0:1]),
                         r=[B_l1p, B_const], w=[B_rs])
                    P.op("act", lambda a: a.activation(out=rs[:], in_=rs[:], func=AF.Exp, scale=-0.5), r=[B_rs], w=[B_rs])
                    P.op("dve", lambda v: v.scalar_tensor_tensor(out=o_[:], in0=oo[:], scalar=subln8[:, 0:1], in1=rs[:],
                                                                 op0=ALU.mult, op1=ALU.mult),
                         r=[B_oo, B_rs, B_const], w=[bo])
                    P.dma("pool", lambda q: q.dma_start(out=mixt_d[h * 128:(h + 1) * 128, qc * 512:(qc + 1) * 512], in_=o_[:]),
                          r=[bo], w=[B_mixt[qc]])

                pending = None
                cnt = 0
                for hi, h in enumerate(heads):
                    k_ = kt[hi % 2]
                    v_ = vv[hi % 2]
                    P.dma("sp", lambda q: q.dma_start(out=k_[:], in_=qkt_d[h, 1]), r=B_qkt[h], w=[B_kt[hi % 2]])
                    vsrc = vi_d[:, h, 0, :].rearrange("(t p) c -> p t c", p=128)
                    for part in range(3):
                        P.dma("sp", lambda q: q.dma_start(out=v_[:, part * 22:(part + 1) * 22, :],
                                                          in_=vsrc[:, part * 22:(part + 1) * 22, :]),
                              r=B_vi, w=[B_vv[hi % 2]])
                    for qc in qchunks:
                        q_ = qt[cnt % 2]
                        bq = B_qt[cnt % 2]
                        o_ = ob[cnt % 2]
                        bo = B_ob[cnt % 2]
                        acc1, acc2 = acc1s[cnt % 2], acc2s[cnt % 2]
                        B_acc1, B_acc2 = B_acc1s[cnt % 2], B_acc2s[cnt % 2]
                        cnt += 1
                        P.dma("sp", lambda q: q.dma_start(out=q_[:], in_=qkt_d[h, 0][:, TC + qc * 512:TC + (qc + 1) * 512]),
                              r=B_qkt[h], w=[bq])

                        def scores(i):
                            mm1(s1[i % 2][:], k_[0:64, i * 128:(i + 1) * 128], q_[0:64, :], True, True,
                                [B_kt[hi % 2], bq], [B_s1[i % 2]])
                            mm1(s2[i % 2][:], k_[64:128, i * 128:(i + 1) * 128], q_[64:128, :], True, True,
                                [B_kt[hi % 2], bq], [B_s2[i % 2]])

                        scores(0)
                        scores(1)
                        for i in range(NKT):
                            if i == 8 and pending is not None:
                                finalize(*pending)
                                pending = None
                            pa, pb = p1[i % 3], p2[i % 3]
                            P.op("act", lambda a: a.activation(out=pa[:], in_=s1[i % 2][:], func=AF.Exp, scale=0.125),
                                 r=[B_s1[i % 2]], w=[B_p1[i % 3]])
                            P.op("act", lambda a: a.activation(out=pb[:], in_=s2[i % 2][:], func=AF.Exp, scale=0.125),
                                 r=[B_s2[i % 2]], w=[B_p2[i % 3]])
                            st_, sp_ = (i == 0), (i == NKT - 1)
                            P._deps("pe", [B_p1[i % 3], B_p2[i % 3]], [])
                            mm1(o1p[:], v_[:, i, :], pa[:], st_, sp_, [B_vv[hi % 2], B_p1[i % 3]], [B_o1p])
                            mm1(o2p[:], v_[:, i, :], pb[:], st_, sp_, [B_vv[hi % 2], B_p2[i % 3]], [B_o2p])
                            if i == 0:
                                P.op("dve", lambda v: v.tensor_copy(out=acc1[:], in_=pa[:]), r=[B_p1[i % 3]], w=[B_acc1])
                                P.op("dve", lambda v: v.tensor_copy(out=acc2[:], in_=pb[:]), r=[B_p2[i % 3]], w=[B_acc2])
                            else:
                                P.op("dve", lambda v: v.tensor_tensor(out=acc1[:], in0=acc1[:], in1=pa[:], op=ALU.add),
                                     r=[B_p1[i % 3], B_acc1], w=[B_acc1])
                                P.op("dve", lambda v: v.tensor_tensor(out=acc2[:], in0=acc2[:], in1=pb[:], op=ALU.add),
                                     r=[B_p2[i % 3], B_acc2], w=[B_acc2])
                            if i + 2 < NKT:
                                scores(i + 2)
                        P.op("act", lambda a: a.copy(out=o1c[:], in_=o1p[:]), r=[B_o1p], w=[B_o1c])
                        P.op("dve", lambda v: v.tensor_copy(out=o2c[:], in_=o2p[:]), r=[B_o2p], w=[B_o2c])
                        pending = (h, qc, acc1, acc2, B_acc1, B_acc2, o_, bo)
                if pending is not None:
                    finalize(*pending)
                P.barrier()

        def phase_a2(heads=range(NH)):
            with ExitStack() as st:
                o_accs = [sb(st, "o_acc%d" % i, [128, NTILE, 128], F32) for i in range(2)]
                B_oaccs = [[Buf() for _ in range(NTILE)] for _ in range(2)]
                rm = sb(st, "rm", [128, 2, 512], F32)
                cmA = sb(st, "cmA", [128, 512], BF16)
                cmB = sb(st, "cmB", [128, 512], BF16)
                hgn = sb(st, "hgn_b", [128, 128], F32)
                B_c2 = Buf()
                P.dma("sp", lambda q: q.dma_start(out=rm[:], in_=rm_d[:, :, 0:512]), w=[B_c2])
                P.dma("sp", lambda q: q.dma_start(out=hgn[:], in_=hgn_d[0:1, :].partition_broadcast(128)), w=[B_c2])
                P.op("dve", lambda v: v.memset(cmA[:], 0.0), w=[B_c2])
                P.op("dve", lambda v: v.memset(cmB[:], 0.0), w=[B_c2])
                P.op("dve", lambda v: v.memset(cmA[:].rearrange("p (t c) -> p t c", c=128)[:, :, 0:64], 1.0), w=[B_c2])
                P.op("dve", lambda v: v.memset(cmB[:].rearrange("p (t c) -> p t c", c=128)[:, :, 64:128], 1.0), w=[B_c2])

                def dbl(name, shape, dt):
                    return [sb(st, "%s%d" % (name, i), shape, dt) for i in range(2)], [Buf(), Buf()]
                zin, B_zin = dbl("zin", [128, 2, 512], F32)
                vt, B_vt = dbl("vt", [128, 4, 128], BF16)
                gt, B_gt = dbl("gt", [128, 4, 128], F32)
                e_, B_e = dbl("e_", [128, 512], F32)
                f_, B_f_ = dbl("f_", [128, 512], F32)
                lf, B_lf = dbl("lf", [128, 512], F32)
                kk, B_kk = dbl("kk", [128, 512], F32)
                bc, B_bc = dbl("bc", [128, 512], F32)
                ep, B_ep = dbl("ep", [128, 512], F32)
                en, B_en = dbl("en", [128, 512], F32)
                kdf, B_kdf = dbl("kdf", [128, 512], F32)
                Qd, B_Qd = dbl("Qd", [128, 512], BF16)
                QdA, B_QdA = dbl("QdA", [128, 512], BF16)
                QdB, B_QdB = dbl("QdB", [128, 512], BF16)
                Kd, B_Kd = dbl("Kd", [128, 512], BF16)
                K2T, B_K2T = dbl("K2T", [128, 512], BF16)
                dec, B_dec = dbl("dec", [128, 8], F32)
                k2, B_k2 = dbl("k2", [128, 128], BF16)
                scm, B_scm = dbl("scm", [128, 128], BF16)
                SbfA, B_SbfA = dbl("SbfA", [128, 128], BF16)
                SbfB, B_SbfB = dbl("SbfB", [128, 128], BF16)
                Sst2 = [sb(st, "Sst%d" % i, [128, 128], F32) for i in range(2)]
                B_S2 = [Buf(), Buf()]
                ot, B_ot = dbl("ot", [128, 128], F32)
                ojunk = sb(st, "ojunk", [128, 128], F32)
                B_ojunk = Buf()
                osm = sb(st, "osm", [128, 8], F32)
                B_osm = [Buf(), Buf()]
                yb, B_yb = dbl("yb", [128, 128], BF16)
                mixs, B_mixs = dbl("mixs", [128, 512], BF16)
                tpb = [ps(st, "tpb%d" % i, [128, 1024], BF16) for i in range(2)]
                B_tpb = [Buf(), Buf()]
                scp = [ps(st, "scp%d" % i, [128, 512]) for i in range(2)]
                B_scp = [Buf(), Buf()]
                ops_ = [ps(st, "ops%d" % i, [128, 512]) for i in range(2)]
                B_ops = [Buf(), Buf()]
                ups = [ps(st, "ups%d" % i, [128, 512]) for i in range(2)]
                B_ups = [Buf(), Buf()]
                ctr = dict(sc=0, tile=0, ch=0, tp=0)

                def chain(h, d):
                    if True:
                        Sst = Sst2[d]
                        B_S = B_S2[d]
                        Sbf, B_Sbf = (SbfA, B_SbfA) if d == 0 else (SbfB, B_SbfB)
                        o_acc = o_accs[d]
                        B_oacc = B_oaccs[d]
                        col = d * 4 + h
                        lb_ap = lbt[:, col:col + 1]
                        oml_ap = omlt[:, col:col + 1]
                        P.op("dve", lambda v: v.memset(Sst[:], 0.0), w=[B_S])
                        P.op("dve", lambda v: v.memset(Sbf[0][:], 0.0), w=[B_Sbf[0]])
                        P.op("dve", lambda v: v.memset(Sbf[1][:], 0.0), w=[B_Sbf[1]])
                        sbi = 0
                        order = list(range(17)) if d == 0 else [0] + list(range(16, 0, -1))
                        for k in order:
                            t0, nt = SCS[k]
                            n = nt * 128
                            s0 = t0 * 128
                            nch = n // 64
                            lat = k >= 1
                            i2 = d
                            z_ = zin[i2]
                            P.dma("sp", lambda q: q.dma_start(out=z_[:, 0, 0:n], in_=zt_d[h, d][:, s0:s0 + n]),
                                  r=B_zt[h], w=[B_zin[i2]])
                            P.dma("sp", lambda q: q.dma_start(out=z_[:, 1, 0:n], in_=zt_d[h, 2][:, s0:s0 + n]),
                                  r=B_zt[h], w=[B_zin[i2]])
                            P.dma("sp", lambda q: q.dma_start(
                                out=vt[i2][:, 0:nt, :], in_=vi_d[s0:s0 + n, h, 1, :].rearrange("(t p) c -> p t c", p=128)),
                                r=B_vi, w=[B_vt[i2]])
                            zz = z_[:, 0, 0:n]
                            hq = z_[:, 1, 0:n]
                            P.op("act", lambda a: a.activation(out=e_[i2][:, 0:n], in_=zz, func=AF.Exp, scale=-1.0),
                                 r=[B_zin[i2]], w=[B_e[i2]])
                            P.op("dve", lambda v: v.tensor_scalar(out=e_[i2][:, 0:n], in0=e_[i2][:, 0:n], scalar1=1.0, scalar2=None,
                                                                  op0=ALU.add), r=[B_e[i2]], w=[B_e[i2]])
                            P.op("act", lambda a: a.activation(out=e_[i2][:, 0:n], in_=e_[i2][:, 0:n], func=AF.Ln), r=[B_e[i2]], w=[B_e[i2]])
                            P.op("act", lambda a: a.activation(out=e_[i2][:, 0:n], in_=e_[i2][:, 0:n], func=AF.Exp, scale=-1.0),
                                 r=[B_e[i2]], w=[B_e[i2]])
                            P.op("dve", lambda v: v.tensor_scalar(out=f_[i2][:, 0:n], in0=e_[i2][:, 0:n], scalar1=oml_ap, scalar2=lb_ap,
                                                                  op0=ALU.mult, op1=ALU.add), r=[B_e[i2], B_const], w=[B_f_[i2]])
                            P.op("act", lambda a: a.activation(out=lf[i2][:, 0:n], in_=f_[i2][:, 0:n], func=AF.Ln),
                                 r=[B_f_[i2]], w=[B_lf[i2]])
                            P.op("act", lambda a: a.activation(out=kk[i2][:, 0:n], in_=f_[i2][:, 0:n], func=AF.Copy, scale=-1.0, bias=1.0),
                                 r=[B_f_[i2]], w=[B_kk[i2]])
                            if d == 0:
                                P.op("dve", lambda v: v.tensor_tensor_scan(out=bc[i2][:, 0:n], data0=rm[:, 0, 0:n], data1=lf[i2][:, 0:n],
                                                                           initial=0.0, op0=ALU.mult, op1=ALU.add),
                                     r=[B_lf[i2], B_c2], w=[B_bc[i2]])
                            else:
                                P.op("dve", lambda v: v.tensor_tensor_scan(out=bc[i2][:, 0:n][:, ::-1], data0=rm[:, 1, 0:n][:, ::-1],
                                                                           data1=lf[i2][:, 0:n][:, ::-1],
                                                                           initial=0.0, op0=ALU.mult, op1=ALU.add),
                                     r=[B_lf[i2], B_c2], w=[B_bc[i2]])
                            P.op("act", lambda a: a.activation(out=ep[i2][:, 0:n], in_=bc[i2][:, 0:n], func=AF.Exp),
                                 r=[B_bc[i2]], w=[B_ep[i2]])
                            P.op("act", lambda a: a.activation(out=en[i2][:, 0:n], in_=bc[i2][:, 0:n], func=AF.Exp, scale=-1.0),
                                 r=[B_bc[i2]], w=[B_en[i2]])
                            if lat:
                                P.op("dve", lambda v: v.tensor_tensor(out=Qd[i2][:, 0:n], in0=hq, in1=ep[i2][:, 0:n], op=ALU.mult),
                                     r=[B_zin[i2], B_ep[i2]], w=[B_Qd[i2]])
                                P.op("pool", lambda g: g.tensor_tensor(out=QdA[i2][:, 0:n], in0=Qd[i2][:, 0:n], in1=cmA[:, 0:n], op=ALU.mult),
                                     r=[B_Qd[i2], B_c2], w=[B_QdA[i2]])
                                P.op("pool", lambda g: g.tensor_tensor(out=QdB[i2][:, 0:n], in0=Qd[i2][:, 0:n], in1=cmB[:, 0:n], op=ALU.mult),
                                     r=[B_Qd[i2], B_c2], w=[B_QdB[i2]])
                            P.op("pool", lambda g: g.tensor_tensor(out=kdf[i2][:, 0:n], in0=kk[i2][:, 0:n], in1=en[i2][:, 0:n], op=ALU.mult),
                                 r=[B_kk[i2], B_en[i2]], w=[B_kdf[i2]])
                            if lat:
                                P.op("pool", lambda g: g.tensor_copy(out=Kd[i2][:, 0:n], in_=kdf[i2][:, 0:n]),
                                     r=[B_kdf[i2]], w=[B_Kd[i2]])
                            endcol = 63 if d == 0 else 0
                            P.op("dve", lambda v: v.tensor_copy(out=dec[i2][:, 0:nch],
                                                                in_=ep[i2][:, 0:n].rearrange("p (c j) -> p c j", j=64)[:, :, endcol]),
                                 r=[B_ep[i2]], w=[B_dec[i2]])
                            P.op("dve", lambda v: v.tensor_tensor(
                                out=K2T[i2][:, 0:n].rearrange("p (c j) -> p c j", j=64),
                                in0=kdf[i2][:, 0:n].rearrange("p (c j) -> p c j", j=64),
                                in1=dec[i2][:, 0:nch].unsqueeze(2).to_broadcast([128, nch, 64]), op=ALU.mult),
                                r=[B_kdf[i2], B_dec[i2]], w=[B_K2T[i2]])
                            tiles = list(range(nt)) if d == 0 else list(range(nt - 1, -1, -1))
                            for j in tiles:
                                cs_ = slice(j * 128, (j + 1) * 128)
                                ti = d
                                tpi = d
                                pe_group([lambda pe: pe.transpose(out=tpb[tpi][:, 0:128], in_=K2T[i2][:, cs_], identity=ident_bf[:])],
                                         r=[B_K2T[i2], B_const], w=[B_tpb[tpi]])
                                P.op("act", lambda a: a.copy(out=k2[ti][:], in_=tpb[tpi][:, 0:128]), r=[B_tpb[tpi]], w=[B_k2[ti]])
                                if lat:
                                    gtile = (k - 1) * 4 + j
                                    mm1(scp[ti][:, 0:128], Kd[i2][:, cs_], Qd[i2][:, cs_], True, True,
                                        [B_Kd[i2], B_Qd[i2]], [B_scp[ti]])
                                    P.op("dve", lambda v: v.tensor_tensor(out=scm[ti][:], in0=scp[ti][:, 0:128], in1=cm_f[:, 1 + d, :],
                                                                          op=ALU.mult), r=[B_scp[ti], B_const], w=[B_scm[ti]])
                                    mm1(ops_[ti][:, 0:128], scm[ti][:], vt[i2][:, j, :], True, False,
                                        [B_scm[ti], B_vt[i2]], [B_ops[ti]])
                                chunks = (0, 1) if d == 0 else (1, 0)
                                for ci, c in enumerate(chunks):
                                    rows = slice(c * 64, (c + 1) * 64)
                                    gc = 2 * j + c
                                    ui = ctr["ch"] % 2
                                    ctr["ch"] += 1
                                    if lat:
                                        qsel = QdA if c == 0 else QdB
                                        bq = B_QdA if c == 0 else B_QdB
                                        mm1(ops_[ti][:, 0:128], qsel[i2][:, cs_], Sbf[sbi][:], False, ci == 1,
                                            [bq[i2], B_Sbf[sbi]], [B_ops[ti]])
                                    mm1(ups[ui][:, 0:128], k2[ti][rows, :], vt[i2][rows, j, :], True, True,
                                        [B_k2[ti], B_vt[i2]], [B_ups[ui]])
                                    sbi = 1 - sbi
                                    P.op("dve", lambda v: v.scalar_tensor_tensor(out=Sbf[sbi][:], in0=Sst[:], scalar=dec[i2][:, gc:gc + 1],
                                                                                 in1=ups[ui][:, 0:128], op0=ALU.mult, op1=ALU.add),
                                         r=[B_S, B_dec[i2], B_ups[ui]], w=[B_Sbf[sbi]])
                                    P.op("dve", lambda v: v.scalar_tensor_tensor(out=Sst[:], in0=Sst[:], scalar=dec[i2][:, gc:gc + 1],
                                                                                 in1=ups[ui][:, 0:128], op0=ALU.mult, op1=ALU.add),
                                         r=[B_S, B_dec[i2], B_ups[ui]], w=[B_S])
                                if lat:
                                    P.op("act", lambda a: a.copy(out=o_acc[:, gtile, :], in_=ops_[ti][:, 0:128]),
                                         r=[B_ops[ti]], w=[B_oacc[gtile]])
                                yield

                def combine(h):
                    for k in range(1, 17):
                        t0, nt = SCS[k]
                        s0 = t0 * 128
                        i2 = k % 2
                        P.dma("sp", lambda q: q.dma_start(
                            out=gt[i2][:, 0:4, :], in_=g_d[s0:s0 + 512, h, :].rearrange("(t p) c -> p t c", p=128)),
                            r=B_g, w=[B_gt[i2]])
                        for j in range(4):
                            gtile = (k - 1) * 4 + j
                            ti = gtile % 2
                            cs_ = slice(j * 128, (j + 1) * 128)
                            o_ = ot[ti]
                            P.op("dve", lambda v: v.tensor_tensor(out=o_[:], in0=o_accs[0][:, gtile, :], in1=o_accs[1][:, gtile, :],
                                                                  op=ALU.add), r=[B_oaccs[0][gtile], B_oaccs[1][gtile]], w=[B_ot[ti]])
                            ss = osm[:, 2 * ti:2 * ti + 1]
                            rsd = osm[:, 2 * ti + 1:2 * ti + 2]
                            P.op("dve", lambda v: v.scalar_tensor_tensor(out=ojunk[:], in0=o_[:], scalar=1.0, in1=o_[:],
                                                                         op0=ALU.mult, op1=ALU.mult, accum_out=ss),
                                 r=[B_ot[ti]], w=[B_ojunk, B_osm[ti]])
                            rstd_from_ss(ss, 128, rsd, ss, [B_osm[ti]], [B_osm[ti]], B_osm[ti])
                            P.op("dve", lambda v: v.scalar_tensor_tensor(out=o_[:], in0=o_[:], scalar=rsd, in1=hgn[:],
                                                                         op0=ALU.mult, op1=ALU.mult),
                                 r=[B_ot[ti], B_osm[ti], B_c2], w=[B_ot[ti]])
                            P.op("pool", lambda g: g.tensor_tensor(out=yb[ti][:], in0=o_[:], in1=gt[i2][:, j, :], op=ALU.mult),
                                 r=[B_ot[ti], B_gt[i2]], w=[B_yb[ti]])
                            pe_group([lambda pe: pe.transpose(out=tpb[ti][:, 0:128], in_=yb[ti][:], identity=ident_bf[:])],
                                     r=[B_yb[ti], B_const], w=[B_tpb[ti]])
                            P.op("act", lambda a: a.copy(out=mixs[i2][:, cs_], in_=tpb[ti][:, 0:128]),
                                 r=[B_tpb[ti]], w=[B_mixs[i2]])
                        P.dma("pool", lambda q: q.dma_start(
                            out=mixt_d[512 + h * 128:512 + (h + 1) * 128, (k - 1) * 512:k * 512], in_=mixs[i2][:]),
                            r=[B_mixs[i2]], w=[B_mixt[k - 1]])

                for h in heads:
                    alive = [chain(h, 0), chain(h, 1)]
                    while alive:
                        for g_ in list(alive):
                            try:
                                next(g_)
                            except StopIteration:
                                alive.remove(g_)
                    combine(h)
                P.barrier()

        AFF = sb(es, "AFF", [128, NTILE, NE], F32)
        B_AFF = [Buf() for _ in range(NTILE)]
        B_h2t = [Buf() for _ in range(NTILE)]
        B_afft = [Buf() for _ in range(NTILE)]

        def phase_b():
            with ExitStack() as st:
                wo = sb(st, "wo", [128, 8, D], BF16)
                B_wo = [Buf() for _ in range(4)]
                for pi in range(4):
                    P.dma("pool", lambda q: q.dma_start(out=wo[:, 2 * pi:2 * pi + 2, :], in_=wout_d[:, 2 * pi:2 * pi + 2, :]),
                          w=[B_wo[pi]])
                wr = sb(st, "wr", [128, 8, NE], F32)
                B_wr = Buf()
                P.dma("sp", lambda q: q.dma_start(out=wr[:], in_=wr_d[:]), w=[B_wr])
                gpm, B_gpm = load_bc(st, "gpm", 4)
                g2m, B_g2m = load_bc(st, "g2m", 5)
                sh2, B_sh2 = load_bc(st, "sh2", 6)
                mix = [sb(st, "mix%d" % i, [128, 8, 512], BF16) for i in range(2)]
                B_mix = [Buf(), Buf()]
                xb = [sb(st, "bxb%d" % i, [128, D], F32) for i in range(2)]
                B_xb = [Buf(), Buf()]
                tt = [sb(st, "btt%d" % i, [128, D], F32) for i in range(2)]
                B_tt = [Buf(), Buf()]
                x1 = [sb(st, "bx1%d" % i, [128, D], F32) for i in range(2)]
                B_x1s = [Buf(), Buf()]
                h2f = [sb(st, "h2f%d" % i, [128, D], F32) for i in range(2)]
                B_h2f = [Buf(), Buf()]
                h2b = [sb(st, "h2b%d" % i, [128, D], BF16) for i in range(2)]
                B_h2b = [Buf(), Buf()]
                junk = sb(st, "bjunk", [128, D], BF16)
                B_junk = Buf()
                h2T = [sb(st, "h2T%d" % i, [128, 8, 128], F32) for i in range(2)]
                B_h2T = [Buf(), Buf()]
                sm = sb(st, "bsm", [128, 2, 8], F32)
                B_sm = [Buf(), Buf()]
                ee = sb(st, "bee", [128, 2, NE], F32)
                yps = [ps(st, "yps%d" % i, [128, D]) for i in range(2)]
                B_yps = [Buf(), Buf()]
                trp = ps(st, "trp", [128, D])
                B_trp = Buf()
                lgp = ps(st, "lgp", [128, 512])
                B_lgp = Buf()
                for sc in range(16):
                    m_ = mix[sc % 2]
                    P.dma("sp", lambda q: q.dma_start(out=m_[:], in_=mixt_d[:, sc * 512:(sc + 1) * 512].rearrange("(kc p) t -> p kc t", p=128)),
                          r=[B_mixt[sc]], w=[B_mix[sc % 2]])
                    for j in range(4):
                        tl = sc * 4 + j
                        i2 = tl % 2
                        y_ = yps[i2]
                        for half in range(2):
                            P.mm(y_[:, half * 512:(half + 1) * 512],
                                 [(m_[:, kc, j * 128:(j + 1) * 128], wo[:, kc, half * 512:(half + 1) * 512]) for kc in range(8)],
                                 r=[B_mix[sc % 2]] + B_wo, w=[B_yps[i2]])
                        s_ = sm[:, i2, :]
                        for half in range(2):
                            P.op("act", lambda a: a.activation(out=junk[:, half * 512:(half + 1) * 512], in_=y_[:, half * 512:(half + 1) * 512],
                                                               func=AF.Square, accum_out=s_[:, half:half + 1]),
                                 r=[B_yps[i2]], w=[B_junk, B_sm[i2]])
                        P.op("dve", lambda v: v.tensor_tensor(out=s_[:, 2:3], in0=s_[:, 0:1], in1=s_[:, 1:2], op=ALU.add),
                             r=[B_sm[i2]], w=[B_sm[i2]])
                        rstd_from_ss(s_[:, 2:3], D, s_[:, 3:4], s_[:, 2:3], [B_sm[i2]], [B_sm[i2]], B_sm[i2])
                        P.dma("sp", lambda q: q.dma_start(out=xb[i2][:], in_=x_d[tl * 128:(tl + 1) * 128, :]), w=[B_xb[i2]])
                        for half in range(2):
                            hs = slice(half * 512, (half + 1) * 512)
                            P.op("dve", lambda v: v.scalar_tensor_tensor(out=tt[i2][:, hs], in0=y_[:, hs], scalar=s_[:, 3:4], in1=gpm[:, hs],
                                                                         op0=ALU.mult, op1=ALU.mult),
                                 r=[B_yps[i2], B_sm[i2], B_gpm], w=[B_tt[i2]])
                        P.op("pool", lambda g: g.tensor_tensor(out=x1[i2][:], in0=tt[i2][:], in1=xb[i2][:], op=ALU.add),
                             r=[B_tt[i2], B_xb[i2]], w=[B_x1s[i2]])
                        P.dma("pool", lambda q: q.dma_start(out=x1_d[tl * 128:(tl + 1) * 128, :], in_=x1[i2][:]),
                              r=[B_x1s[i2]], w=[B_x1[tl]])
                        P.op("dve", lambda v: v.scalar_tensor_tensor(out=junk[:], in0=x1[i2][:], scalar=1.0, in1=x1[i2][:],
                                                                     op0=ALU.mult, op1=ALU.mult, accum_out=s_[:, 4:5]),
                             r=[B_x1s[i2]], w=[B_junk, B_sm[i2]])
                        rstd_from_ss(s_[:, 4:5], D, s_[:, 5:6], s_[:, 4:5], [B_sm[i2]], [B_sm[i2]], B_sm[i2])
                        P.op("dve", lambda v: v.scalar_tensor_tensor(out=tt[i2][:], in0=x1[i2][:], scalar=s_[:, 5:6], in1=g2m[:],
                                                                     op0=ALU.mult, op1=ALU.mult),
                             r=[B_x1s[i2], B_sm[i2], B_g2m], w=[B_tt[i2]])
                        P.op("pool", lambda g: g.tensor_tensor(out=h2f[i2][:], in0=tt[i2][:], in1=sh2[:], op=ALU.add),
                             r=[B_tt[i2], B_sh2], w=[B_h2f[i2]])
                        P.op("act", lambda a: a.copy(out=h2b[i2][:], in_=h2f[i2][:]), r=[B_h2f[i2]], w=[B_h2b[i2]])
                        P.dma("pool", lambda q: q.dma_start(out=h2_d[tl * 128:(tl + 1) * 128, :], in_=h2b[i2][:]),
                              r=[B_h2b[i2]], w=[B_h2t[tl]])
                        pe_group([(lambda pe, kc=kc: pe.transpose(out=trp[:, kc * 128:(kc + 1) * 128],
                                                                   in_=h2f[i2][:, kc * 128:(kc + 1) * 128], identity=ident_f))
                                  for kc in range(8)], r=[B_h2f[i2], B_const], w=[B_trp])
                        P.op("act", lambda a: a.copy(out=h2T[i2][:, 0:4, :].rearrange("p k t -> p (k t)"), in_=trp[:, 0:512]),
                             r=[B_trp], w=[B_h2T[i2]])
                        P.op("dve", lambda v: v.tensor_copy(out=h2T[i2][:, 4:8, :].rearrange("p k t -> p (k t)"), in_=trp[:, 512:1024]),
                             r=[B_trp], w=[B_h2T[i2]])
                        P.mm(lgp[:, 0:NE], [(h2T[i2][:, kc, :], wr[:, kc, :]) for kc in range(8)],
                             r=[B_h2T[i2], B_wr], w=[B_lgp])
                        P.op("dve", lambda v: v.tensor_reduce(out=s_[:, 6:7], in_=lgp[:, 0:NE], axis=AX.X, op=ALU.max, negate=True),
                             r=[B_lgp], w=[B_sm[i2]])
                        P.op("act", lambda a: a.activation(out=ee[:, i2, :], in_=lgp[:, 0:NE], func=AF.Exp, bias=s_[:, 6:7],
                                                           accum_out=s_[:, 7:8]), r=[B_lgp, B_sm[i2]], w=[B_sm[i2]])
                        P.op("dve", lambda v: v.reciprocal(out=s_[:, 7:8], in_=s_[:, 7:8]), r=[B_sm[i2]], w=[B_sm[i2]])
                        P.op("dve", lambda v: v.tensor_scalar(out=AFF[:, tl, :], in0=ee[:, i2, :], scalar1=s_[:, 7:8], scalar2=None,
                                                              op0=ALU.mult), r=[B_sm[i2]], w=[B_AFF[tl]])
                        P.dma("pool", lambda q: q.dma_start(out=aff_d[tl * 128:(tl + 1) * 128, :], in_=AFF[:, tl, :]),
                              r=[B_AFF[tl]], w=[B_afft[tl]])
                P.barrier()

        posm = sb(es, "posm", [128, NE, NTILE], F32)
        B_posm = Buf()

        def phase_c():
            with ExitStack() as st:
                lo = sb(st, "c_lo", [128, NE], F32)
                hi = sb(st, "c_hi", [128, NE], F32)
                mid = sb(st, "c_mid", [128, NE], F32)
                ge = sb(st, "c_ge", [128, NTILE, NE], F32)
                cntp = sb(st, "c_cntp", [128, NE], F32)
                mge = sb(st, "c_mge", [128, NE], U32)
                mlt = sb(st, "c_mlt", [128, NE], U32)
                Mt = sb(st, "c_Mt", [128, NE, NTILE], F32)
                Psc = sb(st, "c_Psc", [128, NE, NTILE], F32)
                rmc = sb(st, "c_rmc", [128, 1024], F32)
                Tt = sb(st, "c_Tt", [128, NE], BF16)
                Lbf = sb(st, "c_Lbf", [128, 128], BF16)
                off = sb(st, "c_off", [128, NE], F32)
                cps = ps(st, "c_cps", [128, 512])
                B_lo, B_hi, B_mid, B_ge, B_cntp, B_m, B_cps, B_x = Buf(), Buf(), Buf(), Buf(), Buf(), Buf(), Buf(), Buf()
                P.dma("sp", lambda q: q.dma_start(out=rmc[:], in_=rm_d[:, 0, :]), w=[B_x])
                P.op("dve", lambda v: v.tensor_copy(out=Lbf[:], in_=cm_f[:, 3, :]), r=[B_const], w=[B_x])
                P.op("dve", lambda v: v.memset(lo[:], 0.0), w=[B_lo])
                P.op("dve", lambda v: v.memset(hi[:], 2.0), w=[B_hi])
                for it in range(34):
                    P.op("dve", lambda v: v.tensor_tensor(out=mid[:], in0=lo[:], in1=hi[:], op=ALU.add), r=[B_lo, B_hi], w=[B_mid])
                    P.op("dve", lambda v: v.tensor_scalar(out=mid[:], in0=mid[:], scalar1=0.5, scalar2=None, op0=ALU.mult),
                         r=[B_mid], w=[B_mid])
                    P.op("dve", lambda v: v.tensor_tensor(out=ge[:], in0=AFF[:], in1=mid[:].unsqueeze(1).to_broadcast([128, NTILE, NE]),
                                                          op=ALU.is_ge), r=B_AFF + [B_mid], w=[B_ge])
                    P.op("dve", lambda v: v.tensor_reduce(out=cntp[:], in_=ge[:].rearrange("p i e -> p e i"), axis=AX.X, op=ALU.add),
                         r=[B_ge], w=[B_cntp])
                    P.mm(cps[:, 0:NE], [(ones_f[:], cntp[:])], r=[B_cntp, B_const], w=[B_cps])
                    P.op("dve", lambda v: v.tensor_scalar(out=mge[:], in0=cps[:, 0:NE], scalar1=float(CAP), scalar2=None, op0=ALU.is_ge),
                         r=[B_cps], w=[B_m])
                    P.op("dve", lambda v: v.tensor_scalar(out=mlt[:], in0=cps[:, 0:NE], scalar1=float(CAP), scalar2=None, op0=ALU.is_lt),
                         r=[B_cps], w=[B_m])
                    P.op("dve", lambda v: v.copy_predicated(out=lo[:], mask=mge[:], data=mid[:]), r=[B_m, B_mid], w=[B_lo])
                    P.op("dve", lambda v: v.copy_predicated(out=hi[:], mask=mlt[:], data=mid[:]), r=[B_m, B_mid], w=[B_hi])
                P.op("dve", lambda v: v.tensor_tensor(out=ge[:], in0=AFF[:], in1=lo[:].unsqueeze(1).to_broadcast([128, NTILE, NE]),
                                                      op=ALU.is_ge), r=B_AFF + [B_lo], w=[B_ge])
                P.op("dve", lambda v: v.tensor_copy(out=Mt[:], in_=ge[:].rearrange("p i e -> p e i")), r=[B_ge], w=[B_x])
                P.op("dve", lambda v: v.tensor_tensor_scan(out=Psc[:].rearrange("p e i -> p (e i)"), data0=rmc[:],
                                                           data1=Mt[:].rearrange("p e i -> p (e i)"), initial=0.0,
                                                           op0=ALU.mult, op1=ALU.add), r=[B_x], w=[B_x])
                P.op("dve", lambda v: v.tensor_copy(out=Tt[:], in_=Psc[:, :, NTILE - 1]), r=[B_x], w=[B_x])
                P.mm(cps[:, 0:NE], [(Lbf[:], Tt[:])], r=[B_x], w=[B_cps])
                P.op("dve", lambda v: v.tensor_copy(out=off[:], in_=cps[:, 0:NE]), r=[B_cps], w=[B_x])
                P.op("dve", lambda v: v.tensor_tensor(out=Psc[:], in0=Psc[:], in1=off[:].unsqueeze(2).to_broadcast([128, NE, NTILE]),
                                                      op=ALU.add), r=[B_x], w=[B_x])
                P.op("dve", lambda v: v.tensor_tensor(out=Psc[:], in0=Psc[:], in1=Mt[:], op=ALU.mult), r=[B_x], w=[B_x])
                P.op("dve", lambda v: v.tensor_scalar(out=posm[:], in0=Psc[:], scalar1=-1.0, scalar2=None, op0=ALU.add),
                     r=[B_x], w=[B_posm])
                P.barrier()

        def idma(fn, r, w):
            return P.dma("pool", fn, r=r, w=w)

        def phase_d(experts=range(NE)):
            with ExitStack() as st:
                iota = sb(st, "d_iota", [128, 1024], F32)
                tokf = sb(st, "d_tokf", [128, NTILE, 2], F32)
                tokb = sb(st, "d_tokb", [128, NTILE, 2], BF16)
                zt_ = sb(st, "d_zero", [128, D], F32)
                B_dc = Buf()
                P.dma("sp", lambda q: q.dma_start(out=iota[:], in_=iota_d[:]), w=[B_dc])
                P.dma("sp", lambda q: q.dma_start(out=tokf[:], in_=tokhl_d[:]), w=[B_dc])
                P.op("dve", lambda v: v.tensor_copy(out=tokb[:], in_=tokf[:]), r=[B_dc], w=[B_dc])
                P.op("dve", lambda v: v.memset(zt_[:], 0.0), w=[B_dc])
                fview = f_d.rearrange("(t p) d -> p t d", p=128)
                for part in range(4):
                    P.dma("sp", lambda q: q.dma_start(out=fview[:, part * 16:(part + 1) * 16, :],
                                                      in_=zt_[:].unsqueeze(1).to_broadcast([128, 16, D])), r=[B_dc], w=[B_f])
                sel = [sb(st, "d_sel%d" % i, [128, 1024], BF16) for i in range(4)]
                B_sel = [Buf() for _ in range(4)]
                idxf = sb(st, "d_idxf", [2, 1024], F32)
                idx2 = sb(st, "d_idx2", [128, 8], F32)
                idxi = [sb(st, "d_idxi%d" % i, [128, 8], I32) for i in range(2)]
                B_idxf, B_idx2 = Buf(), Buf()
                B_idxi = [Buf(), Buf()]
                X = [sb(st, "d_X%d" % i, [128, D], BF16) for i in range(16)]
                B_X = [Buf() for _ in range(16)]
                gat = [sb(st, "d_gat%d" % i, [128, 8, NE], F32) for i in range(2)]
                B_gat = [Buf(), Buf()]
                XT = sb(st, "d_XT", [128, 8, 1024], BF16)
                B_XT = Buf()
                AT = sb(st, "d_AT", [128, 8, 1024], BF16)
                B_AT = Buf()
                W = [[sb(st, "d_w%d_%d" % (m, i), [128, 8, D], BF16) for m in range(3)] for i in range(2)]
                B_W = [[[Buf() for _ in range(4)] for _ in range(3)] for _ in range(2)]
                sg = [sb(st, "d_sg%d" % i, [128, 512], F32) for i in range(2)]
                B_sg = [Buf(), Buf()]
                Ysb = [sb(st, "d_Y%d" % i, [128, D], F32) for i in range(2)]
                B_Y = [Buf(), Buf()]
                ips = [ps(st, "d_ips%d" % i, [128, 512]) for i in range(2)]
                B_ips = [Buf(), Buf()]
                tpx = ps(st, "d_tpx", [128, 8, 128], BF16)
                B_tpx = Buf()
                itp = ps(st, "d_itp", [128, 512])
                B_itp = Buf()
                gps = [ps(st, "d_gps%d" % i, [128, 512]) for i in range(2)]
                B_gps = [Buf(), Buf()]
                ups = [ps(st, "d_ups%d" % i, [128, 512]) for i in range(2)]
                B_ups = [Buf(), Buf()]
                wsrc = (wg_d, wu_d, wd_d)

                def load_w(e, slot):
                    for m in range(3):
                        for pi in range(4):
                            P.dma("pool", lambda q: q.dma_start(out=W[slot][m][:, 2 * pi:2 * pi + 2, :],
                                                                in_=wsrc[m][e][:, 2 * pi:2 * pi + 2, :]), w=[B_W[slot][m][pi]])

                elist = list(experts)
                ctr = dict(sel=0, g=0, y=0)

                def compaction(e, slot):
                    for i in range(NTILE):
                        si = ctr["sel"] % 4
                        ctr["sel"] += 1
                        P.op("dve", lambda v: v.tensor_scalar(out=sel[si][:], in0=iota[:], scalar1=posm[:, e, i:i + 1], scalar2=None,
                                                            op0=ALU.is_equal), r=[B_dc, B_posm], w=[B_sel[si]])
                        for half in range(2):
                            mm1(ips[half][0:2, :], tokb[:, i, :], sel[si][:, half * 512:(half + 1) * 512], i == 0, i == NTILE - 1,
                                [B_dc, B_sel[si]], [B_ips[half]])
                        if i % 4 == 3 and i != NTILE - 1:
                            yield
                    for half in range(2):
                        P.op("act", lambda a: a.copy(out=idxf[:, half * 512:(half + 1) * 512], in_=ips[half][0:2, :]),
                             r=[B_ips[half]], w=[B_idxf])
                    pe_group([(lambda pe, jt=jt: pe.transpose(out=itp[:, 2 * jt:2 * jt + 2], in_=idxf[0:2, jt * 128:(jt + 1) * 128],
                                                               identity=ident_f[0:2, 0:2])) for jt in range(8)],
                             r=[B_idxf, B_const], w=[B_itp])
                    P.op("dve", lambda v: v.tensor_reduce(out=idx2[:], in_=itp[:, 0:16].rearrange("p (j t) -> p j t", t=2),
                                                          axis=AX.X, op=ALU.add), r=[B_itp], w=[B_idx2])
                    P.op("dve", lambda v: v.tensor_copy(out=idxi[slot][:], in_=idx2[:]), r=[B_idx2], w=[B_idxi[slot]])
                    yield

                def gather(e, slot):
                    ii = idxi[slot]
                    for jt in range(8):
                        xj = X[slot * 8 + jt]
                        idma(lambda q: q.indirect_dma_start(out=xj[:], out_offset=None, in_=h2_d[:, :],
                                                            in_offset=IndirectOffsetOnAxis(ap=ii[:, jt:jt + 1], axis=0)),
                             r=[B_idxi[slot]] + B_h2t, w=[B_X[slot * 8 + jt]])
                        idma(lambda q: q.indirect_dma_start(out=gat[slot][:, jt, :], out_offset=None, in_=aff_d[:, :],
                                                            in_offset=IndirectOffsetOnAxis(ap=ii[:, jt:jt + 1], axis=0)),
                             r=[B_idxi[slot]] + B_afft, w=[B_gat[slot]])

                load_w(elist[0], 0)
                for _ in compaction(elist[0], 0):
                    pass
                gather(elist[0], 0)
                for ei, e in enumerate(elist):
                    slot = ei % 2
                    ii = idxi[slot]
                    g_ = gat[slot]
                    nxt = None
                    if ei + 1 < len(elist):
                        load_w(elist[ei + 1], 1 - slot)
                        nxt = compaction(elist[ei + 1], 1 - slot)
                    for jt in range(8):
                        xj = X[slot * 8 + jt]
                        pe_group([(lambda pe, kc=kc: pe.transpose(out=tpx[:, kc, :], in_=xj[:, kc * 128:(kc + 1) * 128],
                                                                   identity=ident_bf[:])) for kc in range(8)],
                                 r=[B_X[slot * 8 + jt], B_const], w=[B_tpx])
                        if jt % 2 == 0:
                            P.op("act", lambda a: a.copy(out=XT[:, :, jt * 128:(jt + 1) * 128], in_=tpx[:]), r=[B_tpx], w=[B_XT])
                        else:
                            P.op("dve", lambda v: v.tensor_copy(out=XT[:, :, jt * 128:(jt + 1) * 128], in_=tpx[:]), r=[B_tpx], w=[B_XT])
                    wg_, wu_, wd_ = W[slot]
                    bwg, bwu, bwd = B_W[slot]
                    for fc in range(8):
                        for sh in range(2):
                            gi = ctr["g"] % 2
                            ctr["g"] += 1
                            cs_ = slice(sh * 512, (sh + 1) * 512)
                            P.mm(gps[gi][:], [(wg_[:, kc, fc * 128:(fc + 1) * 128], XT[:, kc, cs_]) for kc in range(8)],
                                 r=[B_XT] + bwg, w=[B_gps[gi]])
                            P.mm(ups[gi][:], [(wu_[:, kc, fc * 128:(fc + 1) * 128], XT[:, kc, cs_]) for kc in range(8)],
                                 r=[B_XT] + bwu, w=[B_ups[gi]])
                            P.op("act", lambda a: a.activation(out=sg[gi][:], in_=gps[gi][:], func=AF.Silu),
                                 r=[B_gps[gi]], w=[B_sg[gi]])
                            P.op("dve", lambda v: v.tensor_tensor(out=AT[:, fc, cs_], in0=ups[gi][:], in1=sg[gi][:], op=ALU.mult),
                                 r=[B_ups[gi], B_sg[gi]], w=[B_AT])
                            if nxt is not None:
                                next(nxt, None)
                    if nxt is not None:
                        for _ in nxt:
                            pass
                        gather(elist[ei + 1], 1 - slot)
                    for jt in range(8):
                        yi = ctr["y"] % 2
                        ctr["y"] += 1
                        for dh in range(2):
                            gi = ctr["g"] % 2
                            ctr["g"] += 1
                            P.mm(gps[gi][:], [(AT[:, fc, jt * 128:(jt + 1) * 128], wd_[:, fc, dh * 512:(dh + 1) * 512]) for fc in range(8)],
                                 r=[B_AT] + bwd, w=[B_gps[gi]])
                            P.op("dve", lambda v: v.tensor_scalar(out=Ysb[yi][:, dh * 512:(dh + 1) * 512], in0=gps[gi][:],
                                                                  scalar1=g_[:, jt, e:e + 1], scalar2=None, op0=ALU.mult),
                                 r=[B_gps[gi], B_gat[slot]], w=[B_Y[yi]])
                        idma(lambda q: q.indirect_dma_start(out=f_d[:, :], out_offset=IndirectOffsetOnAxis(ap=ii[:, jt:jt + 1], axis=0),
                                                            in_=Ysb[yi][:], in_offset=None, compute_op=ALU.add),
                             r=[B_Y[yi], B_idxi[slot]], w=[B_f])
                P.barrier()

        def phase_e():
            with ExitStack() as st:
                gpf, B_gpf = load_bc(st, "gpf", 7)
                fb = [sb(st, "e_f%d" % i, [128, D], F32) for i in range(2)]
                xb = [sb(st, "e_x%d" % i, [128, D], F32) for i in range(2)]
                tb = [sb(st, "e_t%d" % i, [128, D], F32) for i in range(2)]
                ob_ = [sb(st, "e_o%d" % i, [128, D], F32) for i in range(2)]
                junk = sb(st, "e_junk", [128, D], BF16)
                sm = sb(st, "e_sm", [128, 2, 2], F32)
                B_fb, B_xb, B_tb, B_ob, B_sm = [Buf(), Buf()], [Buf(), Buf()], [Buf(), Buf()], [Buf(), Buf()], [Buf(), Buf()]
                B_junk = Buf()
                B_out = [Buf() for _ in range(NTILE)]
                for tl in range(NTILE):
                    i2 = tl % 2
                    rows = slice(tl * 128, (tl + 1) * 128)
                    P.dma("sp", lambda q: q.dma_start(out=fb[i2][:], in_=f_d[rows, :]), r=[B_f], w=[B_fb[i2]])
                    P.dma("sp", lambda q: q.dma_start(out=xb[i2][:], in_=x1_d[rows, :]), r=[B_x1[tl]], w=[B_xb[i2]])
                    P.op("dve", lambda v: v.scalar_tensor_tensor(out=junk[:], in0=fb[i2][:], scalar=1.0, in1=fb[i2][:],
                                                                 op0=ALU.mult, op1=ALU.mult, accum_out=sm[:, i2, 0:1]),
                         r=[B_fb[i2]], w=[B_junk, B_sm[i2]])
                    rstd_from_ss(sm[:, i2, 0:1], D, sm[:, i2, 1:2], sm[:, i2, 0:1], [B_sm[i2]], [B_sm[i2]], B_sm[i2])
                    P.op("dve", lambda v: v.scalar_tensor_tensor(out=tb[i2][:], in0=fb[i2][:], scalar=sm[:, i2, 1:2], in1=gpf[:],
                                                                 op0=ALU.mult, op1=ALU.mult),
                         r=[B_fb[i2], B_sm[i2], B_gpf], w=[B_tb[i2]])
                    P.op("pool", lambda g: g.tensor_tensor(out=ob_[i2][:], in0=tb[i2][:], in1=xb[i2][:], op=ALU.add),
                         r=[B_tb[i2], B_xb[i2]], w=[B_ob[i2]])
                    P.dma("pool", lambda q: q.dma_start(out=out_d[rows, :], in_=ob_[i2][:]), r=[B_ob[i2]], w=[B_out[tl]])
                P.barrier()

        import os
        if stop_after == "0":
            return nc
        phase_a0()
        if stop_after == "A0":
            return nc
        if stop_after == "A1":
            phase_a1(heads=[int(x) for x in os.environ.get("A1_HEADS", "0").split(",")],
                     qchunks=[int(x) for x in os.environ.get("A1_QC", "0,9").split(",")])
            return nc
        if stop_after == "A2":
            phase_a2(heads=[int(x) for x in os.environ.get("A2_HEADS", "0").split(",")])
            return nc
        if not os.environ.get("SKIP_A1"):
            phase_a1()
        phase_a2()
        phase_b()
        if stop_after == "B":
            return nc
        phase_c()
        if stop_after == "C":
            return nc
        phase_d()
        phase_e()
        return nc


def _rope_tables():
    half = 32
    inv_freq = (1.0 / (10000.0 ** (np.arange(0, half, 2, dtype=np.float32) / np.float32(half)))).astype(np.float32)
    t = np.arange(T)
    r = (t // 64).astype(np.float32)
    c = (t % 64).astype(np.float32)
    ang_r = r[:, None] * inv_freq[None, :]
    ang_c = c[:, None] * inv_freq[None, :]
    ang = np.concatenate([ang_r, ang_r, ang_c, ang_c], axis=-1).astype(np.float32)
    cos = np.cos(ang).astype(np.float32)
    sin = np.sin(ang).astype(np.float32)
    sign = np.concatenate([-np.ones(16), np.ones(16), -np.ones(16), np.ones(16)]).astype(np.float32)
    sin = sin * sign[None, :]
    cosT = np.ones((128, S), np.float32)
    sinT = np.zeros((128, S), np.float32)
    cosT[:, TC:] = np.concatenate([cos.T, cos.T], axis=0)
    sinT[:, TC:] = np.concatenate([sin.T, sin.T], axis=0)
    return cosT, sinT


def _win_cols():
    rot = np.concatenate([np.arange(16, 32), np.arange(0, 16), np.arange(48, 64), np.arange(32, 48)])
    fm, tm, tg = [], [], []
    for h in range(NH):
        for off in (0, 512):
            base = off + h * 128
            fm.append(base + np.arange(128))
            fm.append(np.concatenate([base + rot, base + 64 + rot]))
        fm.append(1536 + h * 128 + np.arange(128))
        fm.append(2048 + h * 128 + np.arange(128))
        fm.append(3072 + h * 128 + np.arange(128))
        tm.append(1024 + h * 128 + np.arange(128))
        tm.append(2560 + h * 128 + np.arange(128))
        tg.append(3584 + h * 128 + np.arange(128))
    tm = tm + tg
    return np.concatenate(fm + tm)


def _kc(a):
    n = a.shape[-1]
    return np.ascontiguousarray(a.reshape(8, 128, n).transpose(1, 0, 2))


def prep_inputs(inp, n_cores):
    f = lambda k: np.asarray(inp[k], dtype=np.float32)
    x, c, ctx, c_ctx = f("x"), f("c"), f("ctx"), f("c_ctx")
    cosT, sinT = _rope_tables()
    p = np.arange(128)
    blk = p // 64
    same = blk[:, None] == blk[None, :]
    cm = np.zeros((128, 4, 128), np.float32)
    cm[:, 0, :] = np.eye(128)
    cm[:, 1, :] = same & (p[:, None] <= p[None, :])
    cm[:, 2, :] = same & (p[:, None] >= p[None, :])
    cm[:, 3, :] = p[:, None] < p[None, :]
    j = np.arange(1024)
    rm = np.ones((128, 2, 1024), np.float32)
    rm[:, 0, j % 64 == 0] = 0.0
    rm[:, 1, j % 64 == 63] = 0.0
    iota = np.broadcast_to(j.astype(np.float32), (128, 1024)).copy()
    tt = np.arange(NTILE)[None, :] * 128 + p[:, None]
    tokhl = np.stack([64 * (tt // 64), tt % 64], axis=-1).astype(np.float32)
    hlb = f("hg_lower_bound").reshape(2, 2, 4, 128).transpose(3, 0, 1, 2).reshape(128, 16)
    shared = {
        "w_ada": _kc(f("w_ada")[0]),
        "b_ada": f("b_ada")[0][None, :],
        "norms": np.concatenate([f("norm_pre_mix")[0], f("norm_post_mix")[0], f("norm_pre_ffn")[0],
                                 f("norm_post_ffn")[0]])[None, :],
        "w_in": _kc(f("w_in")[0][:, _win_cols()]),
        "lamv": np.concatenate([f("da_lambda_q1")[0], f("da_lambda_k1")[0], f("da_lambda_q2")[0],
                                f("da_lambda_k2")[0]])[None, :],
        "subln": f("da_subln")[0][:, None],
        "hgn": f("hg_norm")[0][None, :],
        "hlb": np.ascontiguousarray(hlb),
        "w_out": _kc(f("w_out")[0]),
        "w_r": _kc(f("w_router")[0]),
        "w_gate": np.ascontiguousarray(f("w_gate")[0].reshape(NE, 8, 128, D).transpose(0, 2, 1, 3)),
        "w_up": np.ascontiguousarray(f("w_up")[0].reshape(NE, 8, 128, D).transpose(0, 2, 1, 3)),
        "w_down": np.ascontiguousarray(f("w_down")[0].reshape(NE, 8, 128, D).transpose(0, 2, 1, 3)),
        "cosT": cosT, "sinT": sinT, "cmasks": cm, "rmask": rm, "iota": iota, "tokhl": tokhl,
    }
    maps = []
    for i in range(n_cores):
        b = i % 2
        m = dict(shared)
        m["x"] = np.ascontiguousarray(x[b])
        m["ctx"] = np.ascontiguousarray(ctx[b])
        m["cc"] = _kc(np.stack([c[b], c_ctx], axis=1))
        maps.append(m)
    return maps


N_CORES = 2
_NC_CACHE = {}


def kernel(**inputs):
    if "nc" not in _NC_CACHE:
        _NC_CACHE["nc"] = build()
    nc = _NC_CACHE["nc"]
    maps = prep_inputs(inputs, N_CORES)
    res = run_bass_kernel_spmd(nc, maps, core_ids=list(range(N_CORES)))
    out = np.stack([np.asarray(res.results[b]["out"], dtype=np.float32) for b in range(2)], axis=0)
    return out
```

```python
import numpy as np
from contextlib import ExitStack
import concourse.bass as bass
import concourse.mybir as mybir
from concourse.bass import IndirectOffsetOnAxis
from concourse.bass_utils import run_bass_kernel_spmd

F32 = mybir.dt.float32
BF16 = mybir.dt.bfloat16
I32 = mybir.dt.int32
U32 = mybir.dt.uint32
AF = mybir.ActivationFunctionType
ALU = mybir.AluOpType
AX = mybir.AxisListType

D = 1024
T = 8192
TC = 256
S = T + TC
NH = 4
NE = 16
CAP = 1024
EPS = 1e-6
NTILE = T // 128
FM_BLOCKS = 7
TM_COLS = 384
HEAD_COLS = FM_BLOCKS * 128 + TM_COLS
FM_TOTAL = NH * FM_BLOCKS * 128
WCOLS = NH * HEAD_COLS


class Buf:
    __slots__ = ("w", "r")

    def __init__(self):
        self.w = None
        self.r = {}


class Prog:
    def __init__(self, nc, es, ndma=12):
        self.nc = nc
        self.eng = dict(pe=nc.tensor, act=nc.scalar, dve=nc.vector, pool=nc.gpsimd, sp=nc.sync)
        self.sem = {k: es.enter_context(nc.semaphore("s_" + k)) for k in self.eng}
        self.cnt = {k: 0 for k in self.eng}
        self.waited = {k: {} for k in self.eng}
        self.dsem = {q: [[es.enter_context(nc.semaphore("d_%s%d" % (q, i))), 0] for i in range(ndma)]
                     for q in ("sp", "pool")}
        self.dnext = {"sp": 0, "pool": 0}
        self.nwait = 0
        self.nops = 0
        import os
        self.limit = int(os.environ.get('OPLIMIT', '1000000000'))

    def _wait(self, e, ev):
        s, v = ev
        w = self.waited[e]
        if w.get(s.num, 0) < v:
            self.eng[e].wait_ge(s, v)
            w[s.num] = v
            self.nwait += 1

    def _deps(self, e, reads, writes):
        own = self.sem[e].num
        for b in reads:
            if b.w is not None:
                if not (e == "pe" and b.w[0].num == own):
                    self._wait(e, b.w)
        for b in writes:
            if b.w is not None:
                if not (e == "pe" and b.w[0].num == own):
                    self._wait(e, b.w)
            for ev in b.r.values():
                if ev[0].num == own:
                    continue
                self._wait(e, ev)

    def _mark(self, ev, reads, writes):
        k = ev[0].num
        for b in reads:
            old = b.r.get(k)
            if old is None or old[1] < ev[1]:
                b.r[k] = ev
        for b in writes:
            b.w = ev
            b.r = {}

    def op(self, e, fn, r=(), w=()):
        self.nops += 1
        if self.nops > self.limit:
            return None
        if self.nops == self.limit:
            print('LAST OP', e, fn.__code__.co_firstlineno)
        self._deps(e, r, w)
        ins = fn(self.eng[e])
        self.cnt[e] += 1
        ins.then_inc(self.sem[e], 1)
        ev = (self.sem[e], self.cnt[e])
        self._mark(ev, r, w)
        return ev

    def mm(self, out_ap, pairs, r=(), w=()):
        self.nops += 1
        if self.nops > self.limit:
            return None
        self._deps("pe", r, w)
        n = len(pairs)
        ins = None
        for i, (l, rh) in enumerate(pairs):
            ins = self.nc.tensor.matmul(out_ap, lhsT=l, rhs=rh, start=(i == 0), stop=(i == n - 1))
        self.cnt["pe"] += 1
        ins.then_inc(self.sem["pe"], 1)
        ev = (self.sem["pe"], self.cnt["pe"])
        self._mark(ev, r, w)
        return ev

    def dma(self, q, fn, r=(), w=()):
        self.nops += 1
        if self.nops > self.limit:
            return None
        slots = self.dsem[q]
        i = self.dnext[q]
        self.dnext[q] = (i + 1) % len(slots)
        s, v = slots[i]
        if v > 0:
            self._wait(q, (s, v))
        self._deps(q, r, w)
        ins = fn(self.eng[q])
        slots[i][1] = v + 16
        ins.then_inc(s, 16)
        ev = (s, v + 16)
        self._mark(ev, r, w)
        return ev

    def all_events(self):
        evs = [(self.sem[k], self.cnt[k]) for k in self.eng if self.cnt[k] > 0]
        for q in self.dsem:
            for s, v in self.dsem[q]:
                if v > 0:
                    evs.append((s, v))
        return evs

    def barrier(self, engines=None):
        evs = self.all_events()
        for e in (engines or self.eng):
            for ev in evs:
                if ev[0].num != self.sem[e].num:
                    self._wait(e, ev)


def build(stop_after=None, dbg=()):
    nc = bass.Bass("TRN2", target_bir_lowering=False)
    dbg = set(dbg)

    def din(name, shape, dt=F32):
        return nc.dram_tensor(name, list(shape), dt, kind="ExternalInput").ap()

    def dscr(name, shape, dt):
        kind = "ExternalOutput" if name in dbg else "Internal"
        return nc.dram_tensor(name, list(shape), dt, kind=kind).ap()

    x_d = din("x", [T, D])
    ctx_d = din("ctx", [TC, D])
    cc_d = din("cc", [128, 8, 2])
    wada_d = din("w_ada", [128, 8, 6 * D])
    bada_d = din("b_ada", [1, 6 * D])
    norms_d = din("norms", [1, 4 * D])
    win_d = din("w_in", [128, 8, WCOLS])
    lamv_d = din("lamv", [1, 256])
    subln_d = din("subln", [128, 1])
    hgn_d = din("hgn", [1, 128])
    hlb_d = din("hlb", [128, 16])
    wout_d = din("w_out", [128, 8, D])
    wr_d = din("w_r", [128, 8, NE])
    wg_d = din("w_gate", [NE, 128, 8, D])
    wu_d = din("w_up", [NE, 128, 8, D])
    wd_d = din("w_down", [NE, 128, 8, D])
    cos_d = din("cosT", [128, S])
    sin_d = din("sinT", [128, S])
    cm_d = din("cmasks", [128, 4, 128])
    rm_d = din("rmask", [128, 2, 1024])
    iota_d = din("iota", [128, 1024])
    tokhl_d = din("tokhl", [128, NTILE, 2])
    out_d = nc.dram_tensor("out", [T, D], F32, kind="ExternalOutput").ap()

    modrows_d = dscr("modrows", [8, D], F32)
    qkt_d = dscr("qkt", [NH, 2, 128, S], BF16)
    zt_d = dscr("zt", [NH, 3, 128, S], F32)
    vi_d = dscr("vi", [S, NH, 2, 128], BF16)
    g_d = dscr("gsil", [S, NH, 128], F32)
    mixt_d = dscr("mixt", [D, T], BF16)
    x1_d = dscr("x1", [T, D], F32)
    h2_d = dscr("h2", [T, D], BF16)
    aff_d = dscr("aff", [T, NE], F32)
    f_d = dscr("facc", [T, D], F32)

    B_modrows = Buf()
    B_qkt = [[Buf() for _ in range(17)] for _ in range(NH)]
    B_zt = [[Buf() for _ in range(17)] for _ in range(NH)]
    B_vi = [Buf() for _ in range(17)]
    B_g = [Buf() for _ in range(17)]
    B_mixt = [Buf() for _ in range(16)]
    B_x1 = [Buf() for _ in range(NTILE)]
    B_h2 = Buf()
    B_aff = Buf()
    B_f = Buf()

    es = ExitStack()
    with es:
        P = Prog(nc, es)

        def sb(stack, name, shape, dt):
            return stack.enter_context(nc.sbuf_tensor("sb_" + name, list(shape), dt))

        def ps(stack, name, shape, dt=F32):
            return stack.enter_context(nc.psum_tensor("ps_" + name, list(shape), dt))

        cm_f = sb(es, "cm_f", [128, 4, 128], F32)
        ident_bf = sb(es, "ident_bf", [128, 128], BF16)
        ones_bf = sb(es, "ones_bf", [128, 128], BF16)
        ones_f = sb(es, "ones_f", [128, 128], F32)
        neglam = sb(es, "neglam", [128, 1], F32)
        subln8 = sb(es, "subln8", [128, 1], F32)
        lbt = sb(es, "lbt", [128, 8], F32)
        omlt = sb(es, "omlt", [128, 8], F32)
        mhalf = sb(es, "mhalf", [128, 512], F32)
        epsb = sb(es, "epsb", [128, 1], F32)
        B_const = Buf()
        ident_f = cm_f[:, 0, :]

        P.dma("sp", lambda q: q.dma_start(out=cm_f[:], in_=cm_d[:]), w=[B_const])
        P.op("dve", lambda v: v.tensor_copy(out=ident_bf[:], in_=cm_f[:, 0, :]), r=[B_const], w=[B_const])
        P.op("dve", lambda v: v.memset(ones_bf[:], 1.0), w=[B_const])
        P.op("dve", lambda v: v.memset(ones_f[:], 1.0), w=[B_const])
        P.op("dve", lambda v: v.memset(mhalf[:], -0.5), w=[B_const])
        P.op("dve", lambda v: v.memset(epsb[:], EPS), w=[B_const])

        def rstd_from_ss(ss_ap, n, out_ap, tmp_ap, bufs_r, bufs_w, tmpbuf):
            P.op("dve", lambda v: v.tensor_scalar(out=tmp_ap, in0=ss_ap, scalar1=1.0 / n, scalar2=EPS,
                                                  op0=ALU.mult, op1=ALU.add), r=bufs_r, w=[tmpbuf])
            shp = list(tmp_ap.shape)
            P.op("pool", lambda g: g.tensor_tensor(out=out_ap, in0=tmp_ap, in1=mhalf[0:shp[0], 0:shp[1]],
                                                   op=ALU.pow), r=[tmpbuf, B_const], w=bufs_w)

        with ExitStack() as p0:
            scf = sb(p0, "scf", [128, 8, 2], F32)
            sct = sb(p0, "sct", [128, 8, 2], F32)
            wa = [sb(p0, "wa%d" % i, [128, 8, 512], F32) for i in range(2)]
            modl = sb(p0, "modl", [1, 6 * D], F32)
            modc = sb(p0, "modc", [1, 6 * D], F32)
            bada = sb(p0, "bada", [1, 6 * D], F32)
            nrm = sb(p0, "nrm", [1, 4 * D], F32)
            rows = sb(p0, "rows", [1, 8, D], F32)
            lamv = sb(p0, "lamv", [1, 256], F32)
            lamt = sb(p0, "lamt", [1, 8], F32)
            hlb = sb(p0, "hlb", [128, 16], F32)
            sl = sb(p0, "sl", [128, 1], F32)
            pm = [ps(p0, "pm%d" % i, [1, 512]) for i in range(4)]
            pl = ps(p0, "pl", [128, 1])
            B_sc, B_wa, B_modl, B_modc, B_bada, B_nrm, B_rows, B_lam, B_hlb = (
                Buf(), [Buf(), Buf()], Buf(), Buf(), Buf(), Buf(), Buf(), Buf(), Buf())
            B_pm = [Buf() for _ in range(4)]
            B_pl = Buf()

            P.dma("sp", lambda q: q.dma_start(out=scf[:], in_=cc_d[:]), w=[B_sc])
            P.dma("sp", lambda q: q.dma_start(out=bada[:], in_=bada_d[:]), w=[B_bada])
            P.dma("sp", lambda q: q.dma_start(out=nrm[:], in_=norms_d[:]), w=[B_nrm])
            P.dma("sp", lambda q: q.dma_start(out=lamv[:], in_=lamv_d[:]), w=[B_lam])
            P.dma("sp", lambda q: q.dma_start(out=hlb[:], in_=hlb_d[:]), w=[B_hlb])
            P.dma("sp", lambda q: q.dma_start(out=sl[:], in_=subln_d[:]), w=[B_hlb])
            P.op("act", lambda a: a.activation(out=sct[:], in_=scf[:], func=AF.Exp, scale=-1.0), r=[B_sc], w=[B_rows])
            P.op("dve", lambda v: v.tensor_scalar(out=sct[:], in0=sct[:], scalar1=1.0, scalar2=None, op0=ALU.add),
                 r=[B_rows], w=[B_rows])
            P.op("dve", lambda v: v.reciprocal(out=sct[:], in_=sct[:]), r=[B_rows], w=[B_rows])
            P.op("dve", lambda v: v.tensor_tensor(out=scf[:], in0=scf[:], in1=sct[:], op=ALU.mult),
                 r=[B_rows, B_sc], w=[B_sc])
            for ch in range(12):
                wb_ = wa[ch % 2]
                P.dma("sp", lambda q: q.dma_start(out=wb_[:], in_=wada_d[:, :, ch * 512:(ch + 1) * 512]),
                      w=[B_wa[ch % 2]])
                for j, (mod, bm) in enumerate(((modl, B_modl), (modc, B_modc))):
                    pmt = pm[(2 * ch + j) % 4]
                    bp = B_pm[(2 * ch + j) % 4]
                    P.mm(pmt[:], [(scf[:, kc, j:j + 1], wb_[:, kc, :]) for kc in range(8)],
                         r=[B_sc, B_wa[ch % 2]], w=[bp])
                    P.op("dve", lambda v: v.tensor_tensor(out=mod[:, ch * 512:(ch + 1) * 512], in0=pmt[:],
                                                          in1=bada[:, ch * 512:(ch + 1) * 512], op=ALU.add),
                         r=[bp, B_bada], w=[bm])
            def stt(dst, a, b, op0):
                P.op("dve", lambda v: v.scalar_tensor_tensor(out=rows[:, dst, :], in0=a, scalar=(1.0 if op0 == ALU.add else 1.0),
                                                             in1=b, op0=op0, op1=ALU.mult),
                     r=[B_modl, B_modc, B_nrm], w=[B_rows])
            stt(0, modl[:, D:2 * D], nrm[:, 0:D], ALU.add)
            P.op("dve", lambda v: v.tensor_copy(out=rows[:, 1, :], in_=modl[:, 0:D]), r=[B_modl], w=[B_rows])
            stt(2, modc[:, D:2 * D], nrm[:, 0:D], ALU.add)
            P.op("dve", lambda v: v.tensor_copy(out=rows[:, 3, :], in_=modc[:, 0:D]), r=[B_modc], w=[B_rows])
            stt(4, modl[:, 2 * D:3 * D], nrm[:, D:2 * D], ALU.mult)
            stt(5, modl[:, 4 * D:5 * D], nrm[:, 2 * D:3 * D], ALU.add)
            P.op("dve", lambda v: v.tensor_copy(out=rows[:, 6, :], in_=modl[:, 3 * D:4 * D]), r=[B_modl], w=[B_rows])
            stt(7, modl[:, 5 * D:6 * D], nrm[:, 3 * D:4 * D], ALU.mult)
            P.dma("pool", lambda q: q.dma_start(out=modrows_d[:, :].rearrange("(o r) d -> o r d", o=1), in_=rows[:]),
                  r=[B_rows], w=[B_modrows])
            P.op("dve", lambda v: v.tensor_tensor(out=lamv[:, 0:64], in0=lamv[:, 0:64], in1=lamv[:, 64:128], op=ALU.mult),
                 r=[B_lam], w=[B_lam])
            P.op("dve", lambda v: v.tensor_tensor(out=lamv[:, 128:192], in0=lamv[:, 128:192], in1=lamv[:, 192:256], op=ALU.mult),
                 r=[B_lam], w=[B_lam])
            P.op("dve", lambda v: v.tensor_reduce(out=lamt[:, 0:1], in_=lamv[:, 0:64], axis=AX.X, op=ALU.add),
                 r=[B_lam], w=[B_lam])
            P.op("dve", lambda v: v.tensor_reduce(out=lamt[:, 1:2], in_=lamv[:, 128:192], axis=AX.X, op=ALU.add),
                 r=[B_lam], w=[B_lam])
            P.op("act", lambda a: a.activation(out=lamt[:, 2:4], in_=lamt[:, 0:2], func=AF.Exp), r=[B_lam], w=[B_lam])
            P.op("dve", lambda v: v.scalar_tensor_tensor(out=lamt[:, 4:5], in0=lamt[:, 3:4], scalar=-0.2, in1=lamt[:, 2:3],
                                                         op0=ALU.add, op1=ALU.subtract), r=[B_lam], w=[B_lam])
            P.mm(pl[:], [(ones_f[0:1, :], lamt[0:1, 4:5])], r=[B_lam, B_const], w=[B_pl])
            P.op("dve", lambda v: v.tensor_copy(out=neglam[:], in_=pl[:]), r=[B_pl], w=[B_const])
            P.op("dve", lambda v: v.tensor_scalar(out=subln8[:], in0=sl[:], scalar1=0.8, scalar2=None, op0=ALU.mult),
                 r=[B_hlb], w=[B_const])
            P.op("dve", lambda v: v.tensor_tensor(out=hlb[:, 0:8], in0=hlb[:, 8:16], in1=hlb[:, 0:8], op=ALU.subtract),
                 r=[B_hlb], w=[B_hlb])
            P.op("act", lambda a: a.activation(out=hlb[:, 0:8], in_=hlb[:, 0:8], func=AF.Exp), r=[B_hlb], w=[B_hlb])
            P.op("dve", lambda v: v.tensor_scalar(out=hlb[:, 0:8], in0=hlb[:, 0:8], scalar1=1.0, scalar2=None, op0=ALU.add),
                 r=[B_hlb], w=[B_hlb])
            P.op("dve", lambda v: v.reciprocal(out=lbt[:], in_=hlb[:, 0:8]), r=[B_hlb], w=[B_const])
            P.op("dve", lambda v: v.tensor_scalar(out=omlt[:], in0=lbt[:], scalar1=-1.0, scalar2=1.0, op0=ALU.mult, op1=ALU.add),
                 r=[B_const], w=[B_const])
            P.barrier()

        def load_bc(stack, name, row):
            t = sb(stack, name, [128, D], F32)
            b = Buf()
            P.dma("sp", lambda q: q.dma_start(out=t[:], in_=modrows_d[row:row + 1, :].partition_broadcast(128)),
                  r=[B_modrows], w=[b])
            return t, b

        def pe_group(fns, r, w):
            P._deps("pe", r, w)
            ins = None
            for fn in fns:
                ins = fn(nc.tensor)
            P.cnt["pe"] += 1
            ins.then_inc(P.sem["pe"], 1)
            ev = (P.sem["pe"], P.cnt["pe"])
            P._mark(ev, r, w)
            return ev

        SCS = [(0, 2)] + [(2 + 4 * k, 4) for k in range(16)]

        def phase_a0():
            with ExitStack() as st:
                wbf = sb(st, "wbf", [128, 8, WCOLS], BF16)
                B_w = [[Buf() for _ in range(4)] for _ in range(8)]
                for kc in range(8):
                    for pi in range(4):
                        c0 = pi * 1280
                        P.dma("pool", lambda q: q.dma_start(out=wbf[:, kc, c0:c0 + 1280], in_=win_d[:, kc, c0:c0 + 1280]),
                              w=[B_w[kc][pi]])
                B_wall = [b for l in B_w for b in l]
                gm_l, B_gml = load_bc(st, "gm_l", 0)
                sh_l, B_shl = load_bc(st, "sh_l", 1)
                gm_c, B_gmc = load_bc(st, "gm_c", 2)
                sh_c, B_shc = load_bc(st, "sh_c", 3)
                xb = [sb(st, "xb%d" % i, [128, D], F32) for i in range(2)]
                B_xb = [Buf(), Buf()]
                junk = sb(st, "junk", [128, D], BF16)
                B_junk = Buf()
                hn = [sb(st, "hn%d" % i, [128, D], F32) for i in range(2)]
                B_hn = [Buf(), Buf()]
                hb = [sb(st, "hb%d" % i, [128, D], BF16) for i in range(4)]
                B_hb = [Buf() for _ in range(4)]
                ssm = sb(st, "ssm", [128, 8], F32)
                B_ss = [Buf() for _ in range(4)]
                hT = [sb(st, "hT%d" % i, [128, 8, 512], BF16) for i in range(2)]
                B_hT = [Buf(), Buf()]
                cs = [sb(st, "cs%d" % i, [128, 2, 512], F32) for i in range(2)]
                B_cs = [Buf(), Buf()]
                qks = [sb(st, "qks%d" % i, [128, 2, 512], BF16) for i in range(2)]
                B_qks = [Buf(), Buf()]
                zs = [sb(st, "zs%d" % i, [128, 3, 512], F32) for i in range(2)]
                B_zs = [Buf(), Buf()]
                t1 = sb(st, "t1", [128, 512], F32)
                t2 = sb(st, "t2", [128, 512], F32)
                B_t1, B_t2 = Buf(), Buf()
                vis = [sb(st, "vis%d" % i, [128, NH, 2, 128], BF16) for i in range(2)]
                B_vis = [Buf(), Buf()]
                gs = [sb(st, "gs%d" % i, [128, NH, 128], F32) for i in range(2)]
                B_gs = [Buf(), Buf()]
                tp = [ps(st, "tp%d" % i, [128, 8, 128], BF16) for i in range(2)]
                B_tp = [Buf(), Buf()]
                fm = [ps(st, "fm%d" % i, [128, 512]) for i in range(3)]
                B_fm = [Buf() for _ in range(3)]
                tm = ps(st, "tm", [128, 1536])
                B_tm = Buf()
                fmi = [0]
                tile_ctr = [0]

                def stage1(k):
                    t0, nt = SCS[k]
                    for j in range(nt):
                        i = tile_ctr[0]
                        tile_ctr[0] += 1
                        x_ = xb[i % 2]
                        st_ = t0 + j
                        src = ctx_d[st_ * 128:(st_ + 1) * 128, :] if k == 0 else x_d[(st_ - 2) * 128:(st_ - 1) * 128, :]
                        gm, bgm, sh, bsh = (gm_c, B_gmc, sh_c, B_shc) if k == 0 else (gm_l, B_gml, sh_l, B_shl)
                        P.dma("sp", lambda q: q.dma_start(out=x_[:], in_=src), w=[B_xb[i % 2]])
                        ss = ssm[:, 2 * j:2 * j + 1]
                        rs = ssm[:, 2 * j + 1:2 * j + 2]
                        P.op("dve", lambda v: v.scalar_tensor_tensor(out=junk[:], in0=x_[:], scalar=1.0, in1=x_[:],
                                                                     op0=ALU.mult, op1=ALU.mult, accum_out=ss),
                             r=[B_xb[i % 2]], w=[B_junk, B_ss[j]])
                        rstd_from_ss(ss, D, rs, ss, [B_ss[j]], [B_ss[j]], B_ss[j])
                        h_ = hn[i % 2]
                        P.op("dve", lambda v: v.scalar_tensor_tensor(out=h_[:], in0=x_[:], scalar=rs, in1=gm[:],
                                                                     op0=ALU.mult, op1=ALU.mult),
                             r=[B_xb[i % 2], B_ss[j], bgm], w=[B_hn[i % 2]])
                        P.op("pool", lambda g: g.tensor_tensor(out=hb[j][:], in0=h_[:], in1=sh[:], op=ALU.add),
                             r=[B_hn[i % 2], bsh], w=[B_hb[j]])

                def stage2(k):
                    t0, nt = SCS[k]
                    for j in range(nt):
                        tpp = tp[j % 2]
                        pe_group([(lambda pe, kc=kc: pe.transpose(out=tpp[:, kc, :], in_=hb[j][:, kc * 128:(kc + 1) * 128],
                                                                   identity=ident_bf[:])) for kc in range(8)],
                                 r=[B_hb[j], B_const], w=[B_tp[j % 2]])
                        P.op("act", lambda a: a.copy(out=hT[k % 2][:, :, j * 128:(j + 1) * 128], in_=tpp[:]),
                             r=[B_tp[j % 2]], w=[B_hT[k % 2]])

                def fm_mm(k, col0, n):
                    bi = fmi[0] % 3
                    fmi[0] += 1
                    P.mm(fm[bi][:, 0:n], [(wbf[:, kc, col0:col0 + 128], hT[k % 2][:, kc, 0:n]) for kc in range(8)],
                         r=[B_hT[k % 2]] + B_wall, w=[B_fm[bi]])
                    return bi

                def stage3(k):
                    t0, nt = SCS[k]
                    n = nt * 128
                    s0 = t0 * 128
                    c_ = cs[k % 2]
                    P.dma("sp", lambda q: q.dma_start(out=c_[:, 0, 0:n], in_=cos_d[:, s0:s0 + n]), w=[B_cs[k % 2]])
                    P.dma("sp", lambda q: q.dma_start(out=c_[:, 1, 0:n], in_=sin_d[:, s0:s0 + n]), w=[B_cs[k % 2]])
                    for h in range(NH):
                        hi = k * NH + h
                        qk_ = qks[hi % 2]
                        z_ = zs[hi % 2]
                        base = h * FM_BLOCKS * 128
                        for t in range(2):
                            b0 = fm_mm(k, base + (2 * t) * 128, n)
                            b1 = fm_mm(k, base + (2 * t + 1) * 128, n)
                            P.op("dve", lambda v: v.tensor_tensor(out=t1[:, 0:n], in0=fm[b0][:, 0:n], in1=c_[:, 0, 0:n], op=ALU.mult),
                                 r=[B_fm[b0], B_cs[k % 2]], w=[B_t1])
                            P.op("dve", lambda v: v.tensor_tensor(out=t2[:, 0:n], in0=fm[b1][:, 0:n], in1=c_[:, 1, 0:n], op=ALU.mult),
                                 r=[B_fm[b1], B_cs[k % 2]], w=[B_t2])
                            P.op("pool", lambda g: g.tensor_tensor(out=qk_[:, t, 0:n], in0=t1[:, 0:n], in1=t2[:, 0:n], op=ALU.add),
                                 r=[B_t1, B_t2], w=[B_qks[hi % 2]])
                        for t in range(3):
                            b0 = fm_mm(k, base + (4 + t) * 128, n)
                            P.op("act", lambda a: a.copy(out=z_[:, t, 0:n], in_=fm[b0][:, 0:n]), r=[B_fm[b0]], w=[B_zs[hi % 2]])
                        P.dma("pool", lambda q: q.dma_start(out=qkt_d[h].rearrange("t p s -> p t s")[:, :, s0:s0 + n],
                                                            in_=qk_[:, :, 0:n]), r=[B_qks[hi % 2]], w=[B_qkt[h][k]])
                        P.dma("pool", lambda q: q.dma_start(out=zt_d[h].rearrange("t p s -> p t s")[:, :, s0:s0 + n],
                                                            in_=z_[:, :, 0:n]), r=[B_zs[hi % 2]], w=[B_zt[h][k]])
                    for j in range(nt):
                        ti = t0 + j
                        for nb in range(3):
                            P.mm(tm[:, nb * 512:(nb + 1) * 512],
                                 [(hT[k % 2][:, kc, j * 128:(j + 1) * 128], wbf[:, kc, FM_TOTAL + nb * 512:FM_TOTAL + (nb + 1) * 512])
                                  for kc in range(8)], r=[B_hT[k % 2]] + B_wall, w=[B_tm])
                        v_ = vis[ti % 2]
                        g_ = gs[ti % 2]
                        vflat = v_[:].rearrange("p h t c -> p (h t c)")
                        P.op("dve", lambda v: v.tensor_copy(out=vflat[:, 0:512], in_=tm[:, 0:512]),
                             r=[B_tm], w=[B_vis[ti % 2]])
                        P.op("dve", lambda v: v.tensor_copy(out=vflat[:, 512:1024], in_=tm[:, 512:1024]),
                             r=[B_tm], w=[B_vis[ti % 2]])
                        P.op("act", lambda a: a.activation(out=g_[:].rearrange("p h c -> p (h c)"), in_=tm[:, 1024:1536], func=AF.Silu),
                             r=[B_tm], w=[B_gs[ti % 2]])
                        P.dma("pool", lambda q: q.dma_start(out=vi_d[ti * 128:(ti + 1) * 128], in_=v_[:]),
                              r=[B_vis[ti % 2]], w=[B_vi[k]])
                        P.dma("pool", lambda q: q.dma_start(out=g_d[ti * 128:(ti + 1) * 128], in_=g_[:]),
                              r=[B_gs[ti % 2]], w=[B_g[k]])

                stage1(0)
                stage2(0)
                for k in range(17):
                    if k + 1 < 17:
                        stage1(k + 1)
                    stage3(k)
                    if k + 1 < 17:
                        stage2(k + 1)
                P.barrier()

        def mm1(out_ap, lhsT, rhs, start, stop, r, w):
            P.nops += 1
            if P.nops > P.limit:
                return None
            P._deps("pe", r, w)
            ins = nc.tensor.matmul(out_ap, lhsT=lhsT, rhs=rhs, start=start, stop=stop)
            P.cnt["pe"] += 1
            ins.then_inc(P.sem["pe"], 1)
            ev = (P.sem["pe"], P.cnt["pe"])
            P._mark(ev, r, w)
            return ev

        NKT = S // 128

        def phase_a1(heads=range(NH), qchunks=range(16)):
            with ExitStack() as st:
                kt = [sb(st, "kt%d" % i, [128, S], BF16) for i in range(2)]
                vv = [sb(st, "vv%d" % i, [128, NKT, 128], BF16) for i in range(2)]
                B_kt = [Buf(), Buf()]
                B_vv = [Buf(), Buf()]
                qt = [sb(st, "qt%d" % i, [128, 512], BF16) for i in range(2)]
                B_qt = [Buf(), Buf()]
                p1 = [sb(st, "p1_%d" % i, [128, 512], BF16) for i in range(3)]
                p2 = [sb(st, "p2_%d" % i, [128, 512], BF16) for i in range(3)]
                B_p1 = [Buf() for _ in range(3)]
                B_p2 = [Buf() for _ in range(3)]
                acc1s = [sb(st, "acc1_%d" % i, [128, 512], F32) for i in range(2)]
                acc2s = [sb(st, "acc2_%d" % i, [128, 512], F32) for i in range(2)]
                B_acc1s, B_acc2s = [Buf(), Buf()], [Buf(), Buf()]
                o1c = sb(st, "o1c", [128, 512], F32)
                o2c = sb(st, "o2c", [128, 512], F32)
                B_o1c, B_o2c = Buf(), Buf()
                r1 = sb(st, "r1", [128, 512], F32)
                o1 = sb(st, "o1", [128, 512], F32)
                r2 = sb(st, "r2", [128, 512], F32)
                o2 = sb(st, "o2", [128, 512], F32)
                oo = sb(st, "oo", [128, 512], F32)
                sq = sb(st, "sq", [128, 512], F32)
                rs = sb(st, "rs", [128, 512], F32)
                ob = [sb(st, "ob%d" % i, [128, 512], BF16) for i in range(2)]
                B_r1, B_o1, B_r2, B_o2, B_oo, B_sq, B_rs = Buf(), Buf(), Buf(), Buf(), Buf(), Buf(), Buf()
                B_ob = [Buf(), Buf()]
                s1 = [ps(st, "s1_%d" % i, [128, 512]) for i in range(2)]
                s2 = [ps(st, "s2_%d" % i, [128, 512]) for i in range(2)]
                B_s1 = [Buf(), Buf()]
                B_s2 = [Buf(), Buf()]
                o1p = ps(st, "o1p", [128, 512])
                o2p = ps(st, "o2p", [128, 512])
                l1p = ps(st, "l1p", [128, 512])
                l2p = ps(st, "l2p", [128, 512])
                B_o1p, B_o2p, B_l1p, B_l2p = Buf(), Buf(), Buf(), Buf()
                def finalize(h, qc, acc1, acc2, B_acc1, B_acc2, o_, bo):
                    P.mm(l1p[:], [(ones_f[:], acc1[:])], r=[B_acc1, B_const], w=[B_l1p])
                    P.mm(l2p[:], [(ones_f[:], acc2[:])], r=[B_acc2, B_const], w=[B_l2p])
                    P.op("act", lambda a: a.activation(out=r1[:], in_=l1p[:], func=AF.Ln), r=[B_l1p], w=[B_r1])
                    P.op("act", lambda a: a.activation(out=r1[:], in_=r1[:], func=AF.Exp, scale=-1.0), r=[B_r1], w=[B_r1])
                    P.op("dve", lambda v: v.tensor_tensor(out=o1[:], in0=o1c[:], in1=r1[:], op=ALU.mult),
                         r=[B_o1c, B_r1], w=[B_o1])
                    P.op("act", lambda a: a.activation(out=r2[:], in_=l2p[:], func=AF.Ln), r=[B_l2p], w=[B_r2])
                    P.op("act", lambda a: a.activation(out=r2[:], in_=r2[:], func=AF.Exp, scale=-1.0), r=[B_r2], w=[B_r2])
                    P.op("dve", lambda v: v.tensor_tensor(out=o2[:], in0=o2c[:], in1=r2[:], op=ALU.mult),
                         r=[B_o2c, B_r2], w=[B_o2])
                    P.op("dve", lambda v: v.scalar_tensor_tensor(out=oo[:], in0=o2[:], scalar=neglam[:, 0:1], in1=o1[:],
                                                                 op0=ALU.mult, op1=ALU.add),
                         r=[B_o2, B_o1, B_const], w=[B_oo])
                    P.op("dve", lambda v: v.tensor_tensor(out=sq[:], in0=oo[:], in1=oo[:], op=ALU.mult),
                         r=[B_oo], w=[B_sq])
                    P.mm(l1p[:], [(ones_f[:], sq[:])], r=[B_sq, B_const], w=[B_l1p])
                    P.op("act", lambda a: a.activation(out=rs[:], in_=l1p[:], func=AF.Ln, scale=1.0 / 128, bias=epsb[:, 0:1]),
                         r=[B_l1p, B_const], w=[B_rs])
                    P.op("act", lambda a: a.activation(out=rs[:], in_=rs[:], func=AF.Exp, scale=-0.5), r=[B_rs], w=[B_rs])
                    P.op("dve", lambda v: v.scalar_tensor_tensor(out=o_[:], in0=oo[:], scalar=subln8[:, 0:1], in1=rs[:],
                                                                 op0=ALU.mult, op1=ALU.mult),
                         r=[B_oo, B_rs, B_const], w=[bo])
                    P.dma("pool", lambda q: q.dma_start(out=mixt_d[h * 128:(h + 1) * 128, qc * 512:(qc + 1) * 512], in_=o_[:]),
                          r=[bo], w=[B_mixt[qc]])

                pending = None
                cnt = 0
                for hi, h in enumerate(heads):
                    k_ = kt[hi % 2]
                    v_ = vv[hi % 2]
                    P.dma("sp", lambda q: q.dma_start(out=k_[:], in_=qkt_d[h, 1]), r=B_qkt[h], w=[B_kt[hi % 2]])
                    vsrc = vi_d[:, h, 0, :].rearrange("(t p) c -> p t c", p=128)
                    for part in range(3):
                        P.dma("sp", lambda q: q.dma_start(out=v_[:, part * 22:(part + 1) * 22, :],
                                                          in_=vsrc[:, part * 22:(part + 1) * 22, :]),
                              r=B_vi, w=[B_vv[hi % 2]])
                    for qc in qchunks:
                        q_ = qt[cnt % 2]
                        bq = B_qt[cnt % 2]
                        o_ = ob[cnt % 2]
                        bo = B_ob[cnt % 2]
                        acc1, acc2 = acc1s[cnt % 2], acc2s[cnt % 2]
                        B_acc1, B_acc2 = B_acc1s[cnt % 2], B_acc2s[cnt % 2]
                        cnt += 1
                        P.dma("sp", lambda q: q.dma_start(out=q_[:], in_=qkt_d[h, 0][:, TC + qc * 512:TC + (qc + 1) * 512]),
                              r=B_qkt[h], w=[bq])

                        def scores(i):
                            mm1(s1[i % 2][:], k_[0:64, i * 128:(i + 1) * 128], q_[0:64, :], True, True,
                                [B_kt[hi % 2], bq], [B_s1[i % 2]])
                            mm1(s2[i % 2][:], k_[64:128, i * 128:(i + 1) * 128], q_[64:128, :], True, True,
                                [B_kt[hi % 2], bq], [B_s2[i % 2]])

                        scores(0)
                        scores(1)
                        for i in range(NKT):
                            if i == 8 and pending is not None:
                                finalize(*pending)
                                pending = None
                            pa, pb = p1[i % 3], p2[i % 3]
                            P.op("act", lambda a: a.activation(out=pa[:], in_=s1[i % 2][:], func=AF.Exp, scale=0.125),
                                 r=[B_s1[i % 2]], w=[B_p1[i % 3]])
                            P.op("act", lambda a: a.activation(out=pb[:], in_=s2[i % 2][:], func=AF.Exp, scale=0.125),
                                 r=[B_s2[i % 2]], w=[B_p2[i % 3]])
                            st_, sp_ = (i == 0), (i == NKT - 1)
                            P._deps("pe", [B_p1[i % 3], B_p2[i % 3]], [])
                            mm1(o1p[:], v_[:, i, :], pa[:], st_, sp_, [B_vv[hi % 2], B_p1[i % 3]], [B_o1p])
                            mm1(o2p[:], v_[:, i, :], pb[:], st_, sp_, [B_vv[hi % 2], B_p2[i % 3]], [B_o2p])
                            if i == 0:
                                P.op("dve", lambda v: v.tensor_copy(out=acc1[:], in_=pa[:]), r=[B_p1[i % 3]], w=[B_acc1])
                                P.op("dve", lambda v: v.tensor_copy(out=acc2[:], in_=pb[:]), r=[B_p2[i % 3]], w=[B_acc2])
                            else:
                                P.op("dve", lambda v: v.tensor_tensor(out=acc1[:], in0=acc1[:], in1=pa[:], op=ALU.add),
                                     r=[B_p1[i % 3], B_acc1], w=[B_acc1])
                                P.op("dve", lambda v: v.tensor_tensor(out=acc2[:], in0=acc2[:], in1=pb[:], op=ALU.add),
                                     r=[B_p2[i % 3], B_acc2], w=[B_acc2])
                            if i + 2 < NKT:
                                scores(i + 2)
                        P.op("act", lambda a: a.copy(out=o1c[:], in_=o1p[:]), r=[B_o1p], w=[B_o1c])
                        P.op("dve", lambda v: v.tensor_copy(out=o2c[:], in_=o2p[:]), r=[B_o2p], w=[B_o2c])
                        pending = (h, qc, acc1, acc2, B_acc1, B_acc2, o_, bo)
                if pending is not None:
                    finalize(*pending)
                P.barrier()

        def phase_a2(heads=range(NH)):
            with ExitStack() as st:
                o_accs = [sb(st, "o_acc%d" % i, [128, NTILE, 128], F32) for i in range(2)]
                B_oaccs = [[Buf() for _ in range(NTILE)] for _ in range(2)]
                rm = sb(st, "rm", [128, 2, 512], F32)
                cmA = sb(st, "cmA", [128, 512], BF16)
                cmB = sb(st, "cmB", [128, 512], BF16)
                hgn = sb(st, "hgn_b", [128, 128], F32)
                B_c2 = Buf()
                P.dma("sp", lambda q: q.dma_start(out=rm[:], in_=rm_d[:, :, 0:512]), w=[B_c2])
                P.dma("sp", lambda q: q.dma_start(out=hgn[:], in_=hgn_d[0:1, :].partition_broadcast(128)), w=[B_c2])
                P.op("dve", lambda v: v.memset(cmA[:], 0.0), w=[B_c2])
                P.op("dve", lambda v: v.memset(cmB[:], 0.0), w=[B_c2])
                P.op("dve", lambda v: v.memset(cmA[:].rearrange("p (t c) -> p t c", c=128)[:, :, 0:64], 1.0), w=[B_c2])
                P.op("dve", lambda v: v.memset(cmB[:].rearrange("p (t c) -> p t c", c=128)[:, :, 64:128], 1.0), w=[B_c2])

                def dbl(name, shape, dt):
                    return [sb(st, "%s%d" % (name, i), shape, dt) for i in range(2)], [Buf(), Buf()]
                zin, B_zin = dbl("zin", [128, 2, 512], F32)
                vt, B_vt = dbl("vt", [128, 4, 128], BF16)
                gt, B_gt = dbl("gt", [128, 4, 128], F32)
                e_, B_e = dbl("e_", [128, 512], F32)
                f_, B_f_ = dbl("f_", [128, 512], F32)
                lf, B_lf = dbl("lf", [128, 512], F32)
                kk, B_kk = dbl("kk", [128, 512], F32)
                bc, B_bc = dbl("bc", [128, 512], F32)
                ep, B_ep = dbl("ep", [128, 512], F32)
                en, B_en = dbl("en", [128, 512], F32)
                kdf, B_kdf = dbl("kdf", [128, 512], F32)
                Qd, B_Qd = dbl("Qd", [128, 512], BF16)
                QdA, B_QdA = dbl("QdA", [128, 512], BF16)
                QdB, B_QdB = dbl("QdB", [128, 512], BF16)
                Kd, B_Kd = dbl("Kd", [128, 512], BF16)
                K2T, B_K2T = dbl("K2T", [128, 512], BF16)
                dec, B_dec = dbl("dec", [128, 8], F32)
                k2all, B_k2all = dbl("k2all", [128, 4, 128], BF16)
                scma, B_scma = dbl("scma", [128, 4, 128], BF16)
                Sball, B_Sball = dbl("Sball", [128, 8, 128], BF16)
                ScarA, B_ScarA = dbl("ScarA", [128, 128], BF16)
                ScarB, B_ScarB = dbl("ScarB", [128, 128], BF16)
                Sst2 = [sb(st, "Sst%d" % i, [128, 128], F32) for i in range(2)]
                B_S2 = [Buf(), Buf()]
                otot, B_otot = dbl("otot", [128, 4, 128], F32)
                osq, B_osq = dbl("osq", [128, 4, 128], F32)
                osm4, B_osm4 = dbl("osm4", [128, 8], F32)
                yb4, B_yb4 = dbl("yb4", [128, 4, 128], BF16)
                mixs, B_mixs = dbl("mixs", [128, 512], BF16)
                tpb = ps(st, "tpb", [128, 1024], BF16)
                B_tpb = Buf()
                scp = ps(st, "scp", [128, 512])
                B_scp = Buf()
                ups2 = [ps(st, "ups%d" % i, [128, 1024]) for i in range(2)]
                B_ups2 = [[Buf() for _ in range(8)] for _ in range(2)]
                ops2 = [ps(st, "ops%d" % i, [128, 512]) for i in range(2)]
                B_ops2 = [Buf(), Buf()]
                ctr = dict(sc=0, tile=0, ch=0, tp=0)

                def chain(h, d):
                    if True:
                        Sst = Sst2[d]
                        B_S = B_S2[d]
                        Scar, B_Scar = (ScarA, B_ScarA) if d == 0 else (ScarB, B_ScarB)
                        o_acc = o_accs[d]
                        B_oacc = B_oaccs[d]
                        cp = 0
                        col = d * 4 + h
                        lb_ap = lbt[:, col:col + 1]
                        oml_ap = omlt[:, col:col + 1]
                        P.op("dve", lambda v: v.memset(Sst[:], 0.0), w=[B_S])
                        P.op("dve", lambda v: v.memset(Scar[0][:], 0.0), w=[B_Scar[0]])
                        P.op("dve", lambda v: v.memset(Scar[1][:], 0.0), w=[B_Scar[1]])
                        order = list(range(17)) if d == 0 else [0] + list(range(16, 0, -1))
                        for k in order:
                            t0, nt = SCS[k]
                            n = nt * 128
                            s0 = t0 * 128
                            nch = n // 64
                            lat = k >= 1
                            i2 = d
                            z_ = zin[i2]
                            P.dma("sp", lambda q: q.dma_start(out=z_[:, 0, 0:n], in_=zt_d[h, d][:, s0:s0 + n]),
                                  r=B_zt[h], w=[B_zin[i2]])
                            P.dma("sp", lambda q: q.dma_start(out=z_[:, 1, 0:n], in_=zt_d[h, 2][:, s0:s0 + n]),
                                  r=B_zt[h], w=[B_zin[i2]])
                            P.dma("sp", lambda q: q.dma_start(
                                out=vt[i2][:, 0:nt, :], in_=vi_d[s0:s0 + n, h, 1, :].rearrange("(t p) c -> p t c", p=128)),
                                r=B_vi, w=[B_vt[i2]])
                            zz = z_[:, 0, 0:n]
                            hq = z_[:, 1, 0:n]
                            P.op("act", lambda a: a.activation(out=e_[i2][:, 0:n], in_=zz, func=AF.Exp, scale=-1.0),
                                 r=[B_zin[i2]], w=[B_e[i2]])
                            P.op("dve", lambda v: v.tensor_scalar(out=e_[i2][:, 0:n], in0=e_[i2][:, 0:n], scalar1=1.0, scalar2=None,
                                                                  op0=ALU.add), r=[B_e[i2]], w=[B_e[i2]])
                            P.op("act", lambda a: a.activation(out=e_[i2][:, 0:n], in_=e_[i2][:, 0:n], func=AF.Ln), r=[B_e[i2]], w=[B_e[i2]])
                            P.op("act", lambda a: a.activation(out=e_[i2][:, 0:n], in_=e_[i2][:, 0:n], func=AF.Exp, scale=-1.0),
                                 r=[B_e[i2]], w=[B_e[i2]])
                            P.op("dve", lambda v: v.tensor_scalar(out=f_[i2][:, 0:n], in0=e_[i2][:, 0:n], scalar1=oml_ap, scalar2=lb_ap,
                                                                  op0=ALU.mult, op1=ALU.add), r=[B_e[i2], B_const], w=[B_f_[i2]])
                            P.op("act", lambda a: a.activation(out=lf[i2][:, 0:n], in_=f_[i2][:, 0:n], func=AF.Ln),
                                 r=[B_f_[i2]], w=[B_lf[i2]])
                            P.op("act", lambda a: a.activation(out=kk[i2][:, 0:n], in_=f_[i2][:, 0:n], func=AF.Copy, scale=-1.0, bias=1.0),
                                 r=[B_f_[i2]], w=[B_kk[i2]])
                            if d == 0:
                                P.op("dve", lambda v: v.tensor_tensor_scan(out=bc[i2][:, 0:n], data0=rm[:, 0, 0:n], data1=lf[i2][:, 0:n],
                                                                           initial=0.0, op0=ALU.mult, op1=ALU.add),
                                     r=[B_lf[i2], B_c2], w=[B_bc[i2]])
                            else:
                                P.op("dve", lambda v: v.tensor_tensor_scan(out=bc[i2][:, 0:n][:, ::-1], data0=rm[:, 1, 0:n][:, ::-1],
                                                                           data1=lf[i2][:, 0:n][:, ::-1],
                                                                           initial=0.0, op0=ALU.mult, op1=ALU.add),
                                     r=[B_lf[i2], B_c2], w=[B_bc[i2]])
                            P.op("act", lambda a: a.activation(out=ep[i2][:, 0:n], in_=bc[i2][:, 0:n], func=AF.Exp),
                                 r=[B_bc[i2]], w=[B_ep[i2]])
                            P.op("act", lambda a: a.activation(out=en[i2][:, 0:n], in_=bc[i2][:, 0:n], func=AF.Exp, scale=-1.0),
                                 r=[B_bc[i2]], w=[B_en[i2]])
                            if lat:
                                P.op("dve", lambda v: v.tensor_tensor(out=Qd[i2][:, 0:n], in0=hq, in1=ep[i2][:, 0:n], op=ALU.mult),
                                     r=[B_zin[i2], B_ep[i2]], w=[B_Qd[i2]])
                                P.op("pool", lambda g: g.tensor_tensor(out=QdA[i2][:, 0:n], in0=Qd[i2][:, 0:n], in1=cmA[:, 0:n], op=ALU.mult),
                                     r=[B_Qd[i2], B_c2], w=[B_QdA[i2]])
                                P.op("pool", lambda g: g.tensor_tensor(out=QdB[i2][:, 0:n], in0=Qd[i2][:, 0:n], in1=cmB[:, 0:n], op=ALU.mult),
                                     r=[B_Qd[i2], B_c2], w=[B_QdB[i2]])
                            P.op("pool", lambda g: g.tensor_tensor(out=kdf[i2][:, 0:n], in0=kk[i2][:, 0:n], in1=en[i2][:, 0:n], op=ALU.mult),
                                 r=[B_kk[i2], B_en[i2]], w=[B_kdf[i2]])
                            if lat:
                                P.op("pool", lambda g: g.tensor_copy(out=Kd[i2][:, 0:n], in_=kdf[i2][:, 0:n]),
                                     r=[B_kdf[i2]], w=[B_Kd[i2]])
                            endcol = 63 if d == 0 else 0
                            P.op("dve", lambda v: v.tensor_copy(out=dec[i2][:, 0:nch],
                                                                in_=ep[i2][:, 0:n].rearrange("p (c j) -> p c j", j=64)[:, :, endcol]),
                                 r=[B_ep[i2]], w=[B_dec[i2]])
                            P.op("dve", lambda v: v.tensor_tensor(
                                out=K2T[i2][:, 0:n].rearrange("p (c j) -> p c j", j=64),
                                in0=kdf[i2][:, 0:n].rearrange("p (c j) -> p c j", j=64),
                                in1=dec[i2][:, 0:nch].unsqueeze(2).to_broadcast([128, nch, 64]), op=ALU.mult),
                                r=[B_kdf[i2], B_dec[i2]], w=[B_K2T[i2]])
                            yield
                            pe_group([(lambda pe, j=j: pe.transpose(out=tpb[:, j * 128:(j + 1) * 128], in_=K2T[i2][:, j * 128:(j + 1) * 128],
                                                                     identity=ident_bf[:])) for j in range(nt)],
                                     r=[B_K2T[i2], B_const], w=[B_tpb])
                            P.op("act", lambda a: a.copy(out=k2all[d][:, 0:nt, :].rearrange("p t c -> p (t c)"), in_=tpb[:, 0:n]),
                                 r=[B_tpb], w=[B_k2all[d]])
                            for gc in range(nch):
                                j, c = gc // 2, gc % 2
                                rows = slice(c * 64, (c + 1) * 64)
                                uo = c * 512 + j * 128
                                mm1(ups2[d][:, uo:uo + 128], k2all[d][rows, j, :], vt[i2][rows, j, :], True, True,
                                    [B_k2all[d], B_vt[i2]], [B_ups2[d][c]])
                            if lat:
                                for j in range(nt):
                                    cs_ = slice(j * 128, (j + 1) * 128)
                                    mm1(scp[:, cs_], Kd[i2][:, cs_], Qd[i2][:, cs_], True, True, [B_Kd[i2], B_Qd[i2]], [B_scp])
                                P.op("dve", lambda v: v.tensor_tensor(
                                    out=scma[d][:], in0=scp[:, :].rearrange("p (t c) -> p t c", c=128),
                                    in1=cm_f[:, 1 + d, :].unsqueeze(1).to_broadcast([128, 4, 128]), op=ALU.mult),
                                    r=[B_scp, B_const], w=[B_scma[d]])
                            yield
                            seq = list(range(nch)) if d == 0 else list(range(nch - 1, -1, -1))
                            for m, gc in enumerate(seq):
                                last = m == nch - 1
                                uo = (gc % 2) * 512 + (gc // 2) * 128
                                if last:
                                    dst, bdst = Scar[1 - cp][:], B_Scar[1 - cp]
                                else:
                                    dst, bdst = Sball[d][:, seq[m + 1], :], B_Sball[d]
                                if lat or last:
                                    P.op("dve", lambda v: v.scalar_tensor_tensor(out=dst, in0=Sst[:], scalar=dec[i2][:, gc:gc + 1],
                                                                                 in1=ups2[d][:, uo:uo + 128],
                                                                                 op0=ALU.mult, op1=ALU.add),
                                         r=[B_S, B_dec[i2], B_ups2[d][gc % 2]], w=[bdst])
                                P.op("dve", lambda v: v.scalar_tensor_tensor(out=Sst[:], in0=Sst[:], scalar=dec[i2][:, gc:gc + 1],
                                                                             in1=ups2[d][:, uo:uo + 128],
                                                                             op0=ALU.mult, op1=ALU.add),
                                     r=[B_S, B_dec[i2], B_ups2[d][gc % 2]], w=[B_S])
                            yield
                            if lat:
                                for j in range(nt):
                                    cs_ = slice(j * 128, (j + 1) * 128)
                                    mm1(ops2[d][:, cs_], scma[d][:, j, :], vt[i2][:, j, :], True, False,
                                        [B_scma[d], B_vt[i2]], [B_ops2[d]])
                                    for c in range(2):
                                        gc = 2 * j + c
                                        qsel = QdA if c == 0 else QdB
                                        bq = B_QdA if c == 0 else B_QdB
                                        if gc == seq[0]:
                                            sap, bs = Scar[cp][:], B_Scar[cp]
                                        else:
                                            sap, bs = Sball[d][:, gc, :], B_Sball[d]
                                        mm1(ops2[d][:, cs_], qsel[i2][:, cs_], sap, False, c == 1, [bq[i2], bs], [B_ops2[d]])
                                gt0 = (k - 1) * 4
                                P.op("act", lambda a: a.copy(out=o_acc[:, gt0:gt0 + 4, :].rearrange("p t c -> p (t c)"), in_=ops2[d][:, :]),
                                     r=[B_ops2[d]], w=[B_oacc[gt0 + jj] for jj in range(4)])
                            cp = 1 - cp
                            yield

                def combine(h):
                    for k in range(1, 17):
                        t0, nt = SCS[k]
                        s0 = t0 * 128
                        i2 = k % 2
                        gt0 = (k - 1) * 4
                        P.dma("sp", lambda q: q.dma_start(
                            out=gt[i2][:, 0:4, :], in_=g_d[s0:s0 + 512, h, :].rearrange("(t p) c -> p t c", p=128)),
                            r=B_g, w=[B_gt[i2]])
                        ro = [B_oaccs[dd][gt0 + jj] for dd in range(2) for jj in range(4)]
                        P.op("dve", lambda v: v.tensor_tensor(out=otot[i2][:], in0=o_accs[0][:, gt0:gt0 + 4, :],
                                                              in1=o_accs[1][:, gt0:gt0 + 4, :], op=ALU.add), r=ro, w=[B_otot[i2]])
                        P.op("dve", lambda v: v.tensor_tensor(out=osq[i2][:], in0=otot[i2][:], in1=otot[i2][:], op=ALU.mult),
                             r=[B_otot[i2]], w=[B_osq[i2]])
                        P.op("dve", lambda v: v.tensor_reduce(out=osm4[i2][:, 0:4], in_=osq[i2][:], axis=AX.X, op=ALU.add),
                             r=[B_osq[i2]], w=[B_osm4[i2]])
                        rstd_from_ss(osm4[i2][:, 0:4], 128, osm4[i2][:, 4:8], osm4[i2][:, 0:4], [B_osm4[i2]], [B_osm4[i2]], B_osm4[i2])
                        P.op("dve", lambda v: v.tensor_tensor(out=otot[i2][:], in0=otot[i2][:],
                                                              in1=osm4[i2][:, 4:8].unsqueeze(2).to_broadcast([128, 4, 128]), op=ALU.mult),
                             r=[B_otot[i2], B_osm4[i2]], w=[B_otot[i2]])
                        P.op("dve", lambda v: v.tensor_tensor(out=otot[i2][:], in0=otot[i2][:],
                                                              in1=hgn[:].unsqueeze(1).to_broadcast([128, 4, 128]), op=ALU.mult),
                             r=[B_otot[i2], B_c2], w=[B_otot[i2]])
                        P.op("pool", lambda g: g.tensor_tensor(out=yb4[i2][:], in0=otot[i2][:], in1=gt[i2][:, 0:4, :], op=ALU.mult),
                             r=[B_otot[i2], B_gt[i2]], w=[B_yb4[i2]])
                        pe_group([(lambda pe, j=j: pe.transpose(out=tpb[:, j * 128:(j + 1) * 128], in_=yb4[i2][:, j, :],
                                                                 identity=ident_bf[:])) for j in range(4)],
                                 r=[B_yb4[i2], B_const], w=[B_tpb])
                        P.op("act", lambda a: a.copy(out=mixs[i2][:], in_=tpb[:, 0:512]), r=[B_tpb], w=[B_mixs[i2]])
                        P.dma("pool", lambda q: q.dma_start(
                            out=mixt_d[512 + h * 128:512 + (h + 1) * 128, (k - 1) * 512:k * 512], in_=mixs[i2][:]),
                            r=[B_mixs[i2]], w=[B_mixt[k - 1]])

                for h in heads:
                    alive = [chain(h, 0), chain(h, 1)]
                    while alive:
                        for g_ in list(alive):
                            try:
                                next(g_)
                            except StopIteration:
                                alive.remove(g_)
                    combine(h)
                P.barrier()

        AFF = sb(es, "AFF", [128, NTILE, NE], F32)
        B_AFF = [Buf() for _ in range(NTILE)]
        B_h2t = [Buf() for _ in range(NTILE)]
        B_afft = [Buf() for _ in range(NTILE)]

        def phase_b():
            with ExitStack() as st:
                wo = sb(st, "wo", [128, 8, D], BF16)
                B_wo = [Buf() for _ in range(4)]
                for pi in range(4):
                    P.dma("pool", lambda q: q.dma_start(out=wo[:, 2 * pi:2 * pi + 2, :], in_=wout_d[:, 2 * pi:2 * pi + 2, :]),
                          w=[B_wo[pi]])
                wr = sb(st, "wr", [128, 8, NE], F32)
                B_wr = Buf()
                P.dma("sp", lambda q: q.dma_start(out=wr[:], in_=wr_d[:]), w=[B_wr])
                gpm, B_gpm = load_bc(st, "gpm", 4)
                g2m, B_g2m = load_bc(st, "g2m", 5)
                sh2, B_sh2 = load_bc(st, "sh2", 6)
                mix = [sb(st, "mix%d" % i, [128, 8, 512], BF16) for i in range(2)]
                B_mix = [Buf(), Buf()]
                xb = [sb(st, "bxb%d" % i, [128, D], F32) for i in range(2)]
                B_xb = [Buf(), Buf()]
                tt = [sb(st, "btt%d" % i, [128, D], F32) for i in range(2)]
                B_tt = [Buf(), Buf()]
                x1 = [sb(st, "bx1%d" % i, [128, D], F32) for i in range(2)]
                B_x1s = [Buf(), Buf()]
                h2f = [sb(st, "h2f%d" % i, [128, D], F32) for i in range(2)]
                B_h2f = [Buf(), Buf()]
                h2b = [sb(st, "h2b%d" % i, [128, D], BF16) for i in range(2)]
                B_h2b = [Buf(), Buf()]
                junk = sb(st, "bjunk", [128, D], BF16)
                B_junk = Buf()
                h2T = [sb(st, "h2T%d" % i, [128, 8, 128], F32) for i in range(2)]
                B_h2T = [Buf(), Buf()]
                sm = sb(st, "bsm", [128, 2, 8], F32)
                B_sm = [Buf(), Buf()]
                ee = sb(st, "bee", [128, 2, NE], F32)
                yps = [ps(st, "yps%d" % i, [128, D]) for i in range(2)]
                B_yps = [Buf(), Buf()]
                trp = ps(st, "trp", [128, D])
                B_trp = Buf()
                lgp = ps(st, "lgp", [128, 512])
                B_lgp = Buf()
                for sc in range(16):
                    m_ = mix[sc % 2]
                    P.dma("sp", lambda q: q.dma_start(out=m_[:], in_=mixt_d[:, sc * 512:(sc + 1) * 512].rearrange("(kc p) t -> p kc t", p=128)),
                          r=[B_mixt[sc]], w=[B_mix[sc % 2]])
                    for j in range(4):
                        tl = sc * 4 + j
                        i2 = tl % 2
                        y_ = yps[i2]
                        for half in range(2):
                            P.mm(y_[:, half * 512:(half + 1) * 512],
                                 [(m_[:, kc, j * 128:(j + 1) * 128], wo[:, kc, half * 512:(half + 1) * 512]) for kc in range(8)],
                                 r=[B_mix[sc % 2]] + B_wo, w=[B_yps[i2]])
                        s_ = sm[:, i2, :]
                        for half in range(2):
                            P.op("act", lambda a: a.activation(out=junk[:, half * 512:(half + 1) * 512], in_=y_[:, half * 512:(half + 1) * 512],
                                                               func=AF.Square, accum_out=s_[:, half:half + 1]),
                                 r=[B_yps[i2]], w=[B_junk, B_sm[i2]])
                        P.op("dve", lambda v: v.tensor_tensor(out=s_[:, 2:3], in0=s_[:, 0:1], in1=s_[:, 1:2], op=ALU.add),
                             r=[B_sm[i2]], w=[B_sm[i2]])
                        rstd_from_ss(s_[:, 2:3], D, s_[:, 3:4], s_[:, 2:3], [B_sm[i2]], [B_sm[i2]], B_sm[i2])
                        P.dma("sp", lambda q: q.dma_start(out=xb[i2][:], in_=x_d[tl * 128:(tl + 1) * 128, :]), w=[B_xb[i2]])
                        for half in range(2):
                            hs = slice(half * 512, (half + 1) * 512)
                            P.op("dve", lambda v: v.scalar_tensor_tensor(out=tt[i2][:, hs], in0=y_[:, hs], scalar=s_[:, 3:4], in1=gpm[:, hs],
                                                                         op0=ALU.mult, op1=ALU.mult),
                                 r=[B_yps[i2], B_sm[i2], B_gpm], w=[B_tt[i2]])
                        P.op("pool", lambda g: g.tensor_tensor(out=x1[i2][:], in0=tt[i2][:], in1=xb[i2][:], op=ALU.add),
                             r=[B_tt[i2], B_xb[i2]], w=[B_x1s[i2]])
                        P.dma("pool", lambda q: q.dma_start(out=x1_d[tl * 128:(tl + 1) * 128, :], in_=x1[i2][:]),
                              r=[B_x1s[i2]], w=[B_x1[tl]])
                        P.op("dve", lambda v: v.scalar_tensor_tensor(out=junk[:], in0=x1[i2][:], scalar=1.0, in1=x1[i2][:],
                                                                     op0=ALU.mult, op1=ALU.mult, accum_out=s_[:, 4:5]),
                             r=[B_x1s[i2]], w=[B_junk, B_sm[i2]])
                        rstd_from_ss(s_[:, 4:5], D, s_[:, 5:6], s_[:, 4:5], [B_sm[i2]], [B_sm[i2]], B_sm[i2])
                        P.op("dve", lambda v: v.scalar_tensor_tensor(out=tt[i2][:], in0=x1[i2][:], scalar=s_[:, 5:6], in1=g2m[:],
                                                                     op0=ALU.mult, op1=ALU.mult),
                             r=[B_x1s[i2], B_sm[i2], B_g2m], w=[B_tt[i2]])
                        P.op("pool", lambda g: g.tensor_tensor(out=h2f[i2][:], in0=tt[i2][:], in1=sh2[:], op=ALU.add),
                             r=[B_tt[i2], B_sh2], w=[B_h2f[i2]])
                        P.op("act", lambda a: a.copy(out=h2b[i2][:], in_=h2f[i2][:]), r=[B_h2f[i2]], w=[B_h2b[i2]])
                        P.dma("pool", lambda q: q.dma_start(out=h2_d[tl * 128:(tl + 1) * 128, :], in_=h2b[i2][:]),
                              r=[B_h2b[i2]], w=[B_h2t[tl]])
                        pe_group([(lambda pe, kc=kc: pe.transpose(out=trp[:, kc * 128:(kc + 1) * 128],
                                                                   in_=h2f[i2][:, kc * 128:(kc + 1) * 128], identity=ident_f))
                                  for kc in range(8)], r=[B_h2f[i2], B_const], w=[B_trp])
                        P.op("act", lambda a: a.copy(out=h2T[i2][:, 0:4, :].rearrange("p k t -> p (k t)"), in_=trp[:, 0:512]),
                             r=[B_trp], w=[B_h2T[i2]])
                        P.op("dve", lambda v: v.tensor_copy(out=h2T[i2][:, 4:8, :].rearrange("p k t -> p (k t)"), in_=trp[:, 512:1024]),
                             r=[B_trp], w=[B_h2T[i2]])
                        P.mm(lgp[:, 0:NE], [(h2T[i2][:, kc, :], wr[:, kc, :]) for kc in range(8)],
                             r=[B_h2T[i2], B_wr], w=[B_lgp])
                        P.op("dve", lambda v: v.tensor_reduce(out=s_[:, 6:7], in_=lgp[:, 0:NE], axis=AX.X, op=ALU.max, negate=True),
                             r=[B_lgp], w=[B_sm[i2]])
                        P.op("act", lambda a: a.activation(out=ee[:, i2, :], in_=lgp[:, 0:NE], func=AF.Exp, bias=s_[:, 6:7],
                                                           accum_out=s_[:, 7:8]), r=[B_lgp, B_sm[i2]], w=[B_sm[i2]])
                        P.op("dve", lambda v: v.reciprocal(out=s_[:, 7:8], in_=s_[:, 7:8]), r=[B_sm[i2]], w=[B_sm[i2]])
                        P.op("dve", lambda v: v.tensor_scalar(out=AFF[:, tl, :], in0=ee[:, i2, :], scalar1=s_[:, 7:8], scalar2=None,
                                                              op0=ALU.mult), r=[B_sm[i2]], w=[B_AFF[tl]])
                        P.dma("pool", lambda q: q.dma_start(out=aff_d[tl * 128:(tl + 1) * 128, :], in_=AFF[:, tl, :]),
                              r=[B_AFF[tl]], w=[B_afft[tl]])
                P.barrier()

        posm = sb(es, "posm", [128, NE, NTILE], F32)
        B_posm = Buf()

        def phase_c():
            with ExitStack() as st:
                lo = sb(st, "c_lo", [128, NE], F32)
                hi = sb(st, "c_hi", [128, NE], F32)
                mid = sb(st, "c_mid", [128, NE], F32)
                ge = sb(st, "c_ge", [128, NTILE, NE], F32)
                cntp = sb(st, "c_cntp", [128, NE], F32)
                mge = sb(st, "c_mge", [128, NE], U32)
                mlt = sb(st, "c_mlt", [128, NE], U32)
                Mt = sb(st, "c_Mt", [128, NE, NTILE], F32)
                Psc = sb(st, "c_Psc", [128, NE, NTILE], F32)
                rmc = sb(st, "c_rmc", [128, 1024], F32)
                Tt = sb(st, "c_Tt", [128, NE], BF16)
                Lbf = sb(st, "c_Lbf", [128, 128], BF16)
                off = sb(st, "c_off", [128, NE], F32)
                cps = ps(st, "c_cps", [128, 512])
                B_lo, B_hi, B_mid, B_ge, B_cntp, B_m, B_cps, B_x = Buf(), Buf(), Buf(), Buf(), Buf(), Buf(), Buf(), Buf()
                P.dma("sp", lambda q: q.dma_start(out=rmc[:], in_=rm_d[:, 0, :]), w=[B_x])
                P.op("dve", lambda v: v.tensor_copy(out=Lbf[:], in_=cm_f[:, 3, :]), r=[B_const], w=[B_x])
                P.op("dve", lambda v: v.memset(lo[:], 0.0), w=[B_lo])
                P.op("dve", lambda v: v.memset(hi[:], 2.0), w=[B_hi])
                for it in range(34):
                    P.op("dve", lambda v: v.tensor_tensor(out=mid[:], in0=lo[:], in1=hi[:], op=ALU.add), r=[B_lo, B_hi], w=[B_mid])
                    P.op("dve", lambda v: v.tensor_scalar(out=mid[:], in0=mid[:], scalar1=0.5, scalar2=None, op0=ALU.mult),
                         r=[B_mid], w=[B_mid])
                    P.op("dve", lambda v: v.tensor_tensor(out=ge[:], in0=AFF[:], in1=mid[:].unsqueeze(1).to_broadcast([128, NTILE, NE]),
                                                          op=ALU.is_ge), r=B_AFF + [B_mid], w=[B_ge])
                    P.op("dve", lambda v: v.tensor_reduce(out=cntp[:], in_=ge[:].rearrange("p i e -> p e i"), axis=AX.X, op=ALU.add),
                         r=[B_ge], w=[B_cntp])
                    P.mm(cps[:, 0:NE], [(ones_f[:], cntp[:])], r=[B_cntp, B_const], w=[B_cps])
                    P.op("dve", lambda v: v.tensor_scalar(out=mge[:], in0=cps[:, 0:NE], scalar1=float(CAP), scalar2=None, op0=ALU.is_ge),
                         r=[B_cps], w=[B_m])
                    P.op("dve", lambda v: v.tensor_scalar(out=mlt[:], in0=cps[:, 0:NE], scalar1=float(CAP), scalar2=None, op0=ALU.is_lt),
                         r=[B_cps], w=[B_m])
                    P.op("dve", lambda v: v.copy_predicated(out=lo[:], mask=mge[:], data=mid[:]), r=[B_m, B_mid], w=[B_lo])
                    P.op("dve", lambda v: v.copy_predicated(out=hi[:], mask=mlt[:], data=mid[:]), r=[B_m, B_mid], w=[B_hi])
                P.op("dve", lambda v: v.tensor_tensor(out=ge[:], in0=AFF[:], in1=lo[:].unsqueeze(1).to_broadcast([128, NTILE, NE]),
                                                      op=ALU.is_ge), r=B_AFF + [B_lo], w=[B_ge])
                P.op("dve", lambda v: v.tensor_copy(out=Mt[:], in_=ge[:].rearrange("p i e -> p e i")), r=[B_ge], w=[B_x])
                P.op("dve", lambda v: v.tensor_tensor_scan(out=Psc[:].rearrange("p e i -> p (e i)"), data0=rmc[:],
                                                           data1=Mt[:].rearrange("p e i -> p (e i)"), initial=0.0,
                                                           op0=ALU.mult, op1=ALU.add), r=[B_x], w=[B_x])
                P.op("dve", lambda v: v.tensor_copy(out=Tt[:], in_=Psc[:, :, NTILE - 1]), r=[B_x], w=[B_x])
                P.mm(cps[:, 0:NE], [(Lbf[:], Tt[:])], r=[B_x], w=[B_cps])
                P.op("dve", lambda v: v.tensor_copy(out=off[:], in_=cps[:, 0:NE]), r=[B_cps], w=[B_x])
                P.op("dve", lambda v: v.tensor_tensor(out=Psc[:], in0=Psc[:], in1=off[:].unsqueeze(2).to_broadcast([128, NE, NTILE]),
                                                      op=ALU.add), r=[B_x], w=[B_x])
                P.op("dve", lambda v: v.tensor_tensor(out=Psc[:], in0=Psc[:], in1=Mt[:], op=ALU.mult), r=[B_x], w=[B_x])
                P.op("dve", lambda v: v.tensor_scalar(out=posm[:], in0=Psc[:], scalar1=-1.0, scalar2=None, op0=ALU.add),
                     r=[B_x], w=[B_posm])
                P.barrier()

        def idma(fn, r, w):
            return P.dma("pool", fn, r=r, w=w)

        def phase_d(experts=range(NE)):
            with ExitStack() as st:
                iota = sb(st, "d_iota", [128, 1024], F32)
                tokf = sb(st, "d_tokf", [128, NTILE, 2], F32)
                tokb = sb(st, "d_tokb", [128, NTILE, 2], BF16)
                zt_ = sb(st, "d_zero", [128, D], F32)
                B_dc = Buf()
                P.dma("sp", lambda q: q.dma_start(out=iota[:], in_=iota_d[:]), w=[B_dc])
                P.dma("sp", lambda q: q.dma_start(out=tokf[:], in_=tokhl_d[:]), w=[B_dc])
                P.op("dve", lambda v: v.tensor_copy(out=tokb[:], in_=tokf[:]), r=[B_dc], w=[B_dc])
                P.op("dve", lambda v: v.memset(zt_[:], 0.0), w=[B_dc])
                fview = f_d.rearrange("(t p) d -> p t d", p=128)
                for part in range(4):
                    P.dma("sp", lambda q: q.dma_start(out=fview[:, part * 16:(part + 1) * 16, :],
                                                      in_=zt_[:].unsqueeze(1).to_broadcast([128, 16, D])), r=[B_dc], w=[B_f])
                sel = [sb(st, "d_sel%d" % i, [128, 1024], BF16) for i in range(4)]
                B_sel = [Buf() for _ in range(4)]
                idxf = sb(st, "d_idxf", [2, 1024], F32)
                idx2 = sb(st, "d_idx2", [128, 8], F32)
                idxi = [sb(st, "d_idxi%d" % i, [128, 8], I32) for i in range(2)]
                B_idxf, B_idx2 = Buf(), Buf()
                B_idxi = [Buf(), Buf()]
                X = [sb(st, "d_X%d" % i, [128, D], BF16) for i in range(16)]
                B_X = [Buf() for _ in range(16)]
                gat = [sb(st, "d_gat%d" % i, [128, 8, NE], F32) for i in range(2)]
                B_gat = [Buf(), Buf()]
                XT = sb(st, "d_XT", [128, 8, 1024], BF16)
                B_XT = Buf()
                AT = sb(st, "d_AT", [128, 8, 1024], BF16)
                B_AT = Buf()
                W = [[sb(st, "d_w%d_%d" % (m, i), [128, 8, D], BF16) for m in range(3)] for i in range(2)]
                B_W = [[[Buf() for _ in range(4)] for _ in range(3)] for _ in range(2)]
                sg = [sb(st, "d_sg%d" % i, [128, 512], F32) for i in range(2)]
                B_sg = [Buf(), Buf()]
                Ysb = [sb(st, "d_Y%d" % i, [128, D], F32) for i in range(2)]
                B_Y = [Buf(), Buf()]
                ips = [ps(st, "d_ips%d" % i, [128, 512]) for i in range(2)]
                B_ips = [Buf(), Buf()]
                tpx = ps(st, "d_tpx", [128, 8, 128], BF16)
                B_tpx = Buf()
                itp = ps(st, "d_itp", [128, 512])
                B_itp = Buf()
                gps = [ps(st, "d_gps%d" % i, [128, 512]) for i in range(2)]
                B_gps = [Buf(), Buf()]
                ups = [ps(st, "d_ups%d" % i, [128, 512]) for i in range(2)]
                B_ups = [Buf(), Buf()]
                wsrc = (wg_d, wu_d, wd_d)

                def load_w(e, slot):
                    for m in range(3):
                        for pi in range(4):
                            P.dma("pool", lambda q: q.dma_start(out=W[slot][m][:, 2 * pi:2 * pi + 2, :],
                                                                in_=wsrc[m][e][:, 2 * pi:2 * pi + 2, :]), w=[B_W[slot][m][pi]])

                elist = list(experts)
                ctr = dict(sel=0, g=0, y=0)

                def compaction(e, slot):
                    for i in range(NTILE):
                        si = ctr["sel"] % 4
                        ctr["sel"] += 1
                        P.op("dve", lambda v: v.tensor_scalar(out=sel[si][:], in0=iota[:], scalar1=posm[:, e, i:i + 1], scalar2=None,
                                                            op0=ALU.is_equal), r=[B_dc, B_posm], w=[B_sel[si]])
                        for half in range(2):
                            mm1(ips[half][0:2, :], tokb[:, i, :], sel[si][:, half * 512:(half + 1) * 512], i == 0, i == NTILE - 1,
                                [B_dc, B_sel[si]], [B_ips[half]])
                        if i % 4 == 3 and i != NTILE - 1:
                            yield
                    for half in range(2):
                        P.op("act", lambda a: a.copy(out=idxf[:, half * 512:(half + 1) * 512], in_=ips[half][0:2, :]),
                             r=[B_ips[half]], w=[B_idxf])
                    pe_group([(lambda pe, jt=jt: pe.transpose(out=itp[:, 2 * jt:2 * jt + 2], in_=idxf[0:2, jt * 128:(jt + 1) * 128],
                                                               identity=ident_f[0:2, 0:2])) for jt in range(8)],
                             r=[B_idxf, B_const], w=[B_itp])
                    P.op("dve", lambda v: v.tensor_reduce(out=idx2[:], in_=itp[:, 0:16].rearrange("p (j t) -> p j t", t=2),
                                                          axis=AX.X, op=ALU.add), r=[B_itp], w=[B_idx2])
                    P.op("dve", lambda v: v.tensor_copy(out=idxi[slot][:], in_=idx2[:]), r=[B_idx2], w=[B_idxi[slot]])
                    yield

                def gather(e, slot):
                    ii = idxi[slot]
                    for jt in range(8):
                        xj = X[slot * 8 + jt]
                        idma(lambda q: q.indirect_dma_start(out=xj[:], out_offset=None, in_=h2_d[:, :],
                                                            in_offset=IndirectOffsetOnAxis(ap=ii[:, jt:jt + 1], axis=0)),
                             r=[B_idxi[slot]] + B_h2t, w=[B_X[slot * 8 + jt]])
                        idma(lambda q: q.indirect_dma_start(out=gat[slot][:, jt, :], out_offset=None, in_=aff_d[:, :],
                                                            in_offset=IndirectOffsetOnAxis(ap=ii[:, jt:jt + 1], axis=0)),
                             r=[B_idxi[slot]] + B_afft, w=[B_gat[slot]])

                load_w(elist[0], 0)
                for _ in compaction(elist[0], 0):
                    pass
                gather(elist[0], 0)
                for ei, e in enumerate(elist):
                    slot = ei % 2
                    ii = idxi[slot]
                    g_ = gat[slot]
                    nxt = None
                    if ei + 1 < len(elist):
                        load_w(elist[ei + 1], 1 - slot)
                        nxt = compaction(elist[ei + 1], 1 - slot)
                    for jt in range(8):
                        xj = X[slot * 8 + jt]
                        pe_group([(lambda pe, kc=kc: pe.transpose(out=tpx[:, kc, :], in_=xj[:, kc * 128:(kc + 1) * 128],
                                                                   identity=ident_bf[:])) for kc in range(8)],
                                 r=[B_X[slot * 8 + jt], B_const], w=[B_tpx])
                        if jt % 2 == 0:
                            P.op("act", lambda a: a.copy(out=XT[:, :, jt * 128:(jt + 1) * 128], in_=tpx[:]), r=[B_tpx], w=[B_XT])
                        else:
                            P.op("dve", lambda v: v.tensor_copy(out=XT[:, :, jt * 128:(jt + 1) * 128], in_=tpx[:]), r=[B_tpx], w=[B_XT])
                    wg_, wu_, wd_ = W[slot]
                    bwg, bwu, bwd = B_W[slot]
                    for fc in range(8):
                        for sh in range(2):
                            gi = ctr["g"] % 2
                            ctr["g"] += 1
                            cs_ = slice(sh * 512, (sh + 1) * 512)
                            P.mm(gps[gi][:], [(wg_[:, kc, fc * 128:(fc + 1) * 128], XT[:, kc, cs_]) for kc in range(8)],
                                 r=[B_XT] + bwg, w=[B_gps[gi]])
                            P.mm(ups[gi][:], [(wu_[:, kc, fc * 128:(fc + 1) * 128], XT[:, kc, cs_]) for kc in range(8)],
                                 r=[B_XT] + bwu, w=[B_ups[gi]])
                            P.op("act", lambda a: a.activation(out=sg[gi][:], in_=gps[gi][:], func=AF.Silu),
                                 r=[B_gps[gi]], w=[B_sg[gi]])
                            P.op("dve", lambda v: v.tensor_tensor(out=AT[:, fc, cs_], in0=ups[gi][:], in1=sg[gi][:], op=ALU.mult),
                                 r=[B_ups[gi], B_sg[gi]], w=[B_AT])
                            if nxt is not None:
                                next(nxt, None)
                    if nxt is not None:
                        for _ in nxt:
                            pass
                        gather(elist[ei + 1], 1 - slot)
                    for jt in range(8):
                        yi = ctr["y"] % 2
                        ctr["y"] += 1
                        for dh in range(2):
                            gi = ctr["g"] % 2
                            ctr["g"] += 1
                            P.mm(gps[gi][:], [(AT[:, fc, jt * 128:(jt + 1) * 128], wd_[:, fc, dh * 512:(dh + 1) * 512]) for fc in range(8)],
                                 r=[B_AT] + bwd, w=[B_gps[gi]])
                            P.op("dve", lambda v: v.tensor_scalar(out=Ysb[yi][:, dh * 512:(dh + 1) * 512], in0=gps[gi][:],
                                                                  scalar1=g_[:, jt, e:e + 1], scalar2=None, op0=ALU.mult),
                                 r=[B_gps[gi], B_gat[slot]], w=[B_Y[yi]])
                        idma(lambda q: q.indirect_dma_start(out=f_d[:, :], out_offset=IndirectOffsetOnAxis(ap=ii[:, jt:jt + 1], axis=0),
                                                            in_=Ysb[yi][:], in_offset=None, compute_op=ALU.add),
                             r=[B_Y[yi], B_idxi[slot]], w=[B_f])
                P.barrier()

        def phase_e():
            with ExitStack() as st:
                gpf, B_gpf = load_bc(st, "gpf", 7)
                fb = [sb(st, "e_f%d" % i, [128, D], F32) for i in range(2)]
                xb = [sb(st, "e_x%d" % i, [128, D], F32) for i in range(2)]
                tb = [sb(st, "e_t%d" % i, [128, D], F32) for i in range(2)]
                ob_ = [sb(st, "e_o%d" % i, [128, D], F32) for i in range(2)]
                junk = sb(st, "e_junk", [128, D], BF16)
                sm = sb(st, "e_sm", [128, 2, 2], F32)
                B_fb, B_xb, B_tb, B_ob, B_sm = [Buf(), Buf()], [Buf(), Buf()], [Buf(), Buf()], [Buf(), Buf()], [Buf(), Buf()]
                B_junk = Buf()
                B_out = [Buf() for _ in range(NTILE)]
                for tl in range(NTILE):
                    i2 = tl % 2
                    rows = slice(tl * 128, (tl + 1) * 128)
                    P.dma("sp", lambda q: q.dma_start(out=fb[i2][:], in_=f_d[rows, :]), r=[B_f], w=[B_fb[i2]])
                    P.dma("sp", lambda q: q.dma_start(out=xb[i2][:], in_=x1_d[rows, :]), r=[B_x1[tl]], w=[B_xb[i2]])
                    P.op("dve", lambda v: v.scalar_tensor_tensor(out=junk[:], in0=fb[i2][:], scalar=1.0, in1=fb[i2][:],
                                                                 op0=ALU.mult, op1=ALU.mult, accum_out=sm[:, i2, 0:1]),
                         r=[B_fb[i2]], w=[B_junk, B_sm[i2]])
                    rstd_from_ss(sm[:, i2, 0:1], D, sm[:, i2, 1:2], sm[:, i2, 0:1], [B_sm[i2]], [B_sm[i2]], B_sm[i2])
                    P.op("dve", lambda v: v.scalar_tensor_tensor(out=tb[i2][:], in0=fb[i2][:], scalar=sm[:, i2, 1:2], in1=gpf[:],
                                                                 op0=ALU.mult, op1=ALU.mult),
                         r=[B_fb[i2], B_sm[i2], B_gpf], w=[B_tb[i2]])
                    P.op("pool", lambda g: g.tensor_tensor(out=ob_[i2][:], in0=tb[i2][:], in1=xb[i2][:], op=ALU.add),
                         r=[B_tb[i2], B_xb[i2]], w=[B_ob[i2]])
                    P.dma("pool", lambda q: q.dma_start(out=out_d[rows, :], in_=ob_[i2][:]), r=[B_ob[i2]], w=[B_out[tl]])
                P.barrier()

        import os
        if stop_after == "0":
            return nc
        phase_a0()
        if stop_after == "A0":
            return nc
        if stop_after == "A1":
            phase_a1(heads=[int(x) for x in os.environ.get("A1_HEADS", "0").split(",")],
                     qchunks=[int(x) for x in os.environ.get("A1_QC", "0,9").split(",")])
            return nc
        if stop_after == "A2":
            phase_a2(heads=[int(x) for x in os.environ.get("A2_HEADS", "0").split(",")])
            return nc
        if not os.environ.get("SKIP_A1"):
            phase_a1()
        phase_a2()
        phase_b()
        if stop_after == "B":
            return nc
        phase_c()
        if stop_after == "C":
            return nc
        phase_d()
        phase_e()
        return nc


def _rope_tables():
    half = 32
    inv_freq = (1.0 / (10000.0 ** (np.arange(0, half, 2, dtype=np.float32) / np.float32(half)))).astype(np.float32)
    t = np.arange(T)
    r = (t // 64).astype(np.float32)
    c = (t % 64).astype(np.float32)
    ang_r = r[:, None] * inv_freq[None, :]
    ang_c = c[:, None] * inv_freq[None, :]
    ang = np.concatenate([ang_r, ang_r, ang_c, ang_c], axis=-1).astype(np.float32)
    cos = np.cos(ang).astype(np.float32)
    sin = np.sin(ang).astype(np.float32)
    sign = np.concatenate([-np.ones(16), np.ones(16), -np.ones(16), np.ones(16)]).astype(np.float32)
    sin = sin * sign[None, :]
    cosT = np.ones((128, S), np.float32)
    sinT = np.zeros((128, S), np.float32)
    cosT[:, TC:] = np.concatenate([cos.T, cos.T], axis=0)
    sinT[:, TC:] = np.concatenate([sin.T, sin.T], axis=0)
    return cosT, sinT


def _win_cols():
    rot = np.concatenate([np.arange(16, 32), np.arange(0, 16), np.arange(48, 64), np.arange(32, 48)])
    fm, tm, tg = [], [], []
    for h in range(NH):
        for off in (0, 512):
            base = off + h * 128
            fm.append(base + np.arange(128))
            fm.append(np.concatenate([base + rot, base + 64 + rot]))
        fm.append(1536 + h * 128 + np.arange(128))
        fm.append(2048 + h * 128 + np.arange(128))
        fm.append(3072 + h * 128 + np.arange(128))
        tm.append(1024 + h * 128 + np.arange(128))
        tm.append(2560 + h * 128 + np.arange(128))
        tg.append(3584 + h * 128 + np.arange(128))
    tm = tm + tg
    return np.concatenate(fm + tm)


def _kc(a):
    n = a.shape[-1]
    return np.ascontiguousarray(a.reshape(8, 128, n).transpose(1, 0, 2))


def prep_inputs(inp, n_cores):
    f = lambda k: np.asarray(inp[k], dtype=np.float32)
    x, c, ctx, c_ctx = f("x"), f("c"), f("ctx"), f("c_ctx")
    cosT, sinT = _rope_tables()
    p = np.arange(128)
    blk = p // 64
    same = blk[:, None] == blk[None, :]
    cm = np.zeros((128, 4, 128), np.float32)
    cm[:, 0, :] = np.eye(128)
    cm[:, 1, :] = same & (p[:, None] <= p[None, :])
    cm[:, 2, :] = same & (p[:, None] >= p[None, :])
    cm[:, 3, :] = p[:, None] < p[None, :]
    j = np.arange(1024)
    rm = np.ones((128, 2, 1024), np.float32)
    rm[:, 0, j % 64 == 0] = 0.0
    rm[:, 1, j % 64 == 63] = 0.0
    iota = np.broadcast_to(j.astype(np.float32), (128, 1024)).copy()
    tt = np.arange(NTILE)[None, :] * 128 + p[:, None]
    tokhl = np.stack([64 * (tt // 64), tt % 64], axis=-1).astype(np.float32)
    hlb = f("hg_lower_bound").reshape(2, 2, 4, 128).transpose(3, 0, 1, 2).reshape(128, 16)
    shared = {
        "w_ada": _kc(f("w_ada")[0]),
        "b_ada": f("b_ada")[0][None, :],
        "norms": np.concatenate([f("norm_pre_mix")[0], f("norm_post_mix")[0], f("norm_pre_ffn")[0],
                                 f("norm_post_ffn")[0]])[None, :],
        "w_in": _kc(f("w_in")[0][:, _win_cols()]),
        "lamv": np.concatenate([f("da_lambda_q1")[0], f("da_lambda_k1")[0], f("da_lambda_q2")[0],
                                f("da_lambda_k2")[0]])[None, :],
        "subln": f("da_subln")[0][:, None],
        "hgn": f("hg_norm")[0][None, :],
        "hlb": np.ascontiguousarray(hlb),
        "w_out": _kc(f("w_out")[0]),
        "w_r": _kc(f("w_router")[0]),
        "w_gate": np.ascontiguousarray(f("w_gate")[0].reshape(NE, 8, 128, D).transpose(0, 2, 1, 3)),
        "w_up": np.ascontiguousarray(f("w_up")[0].reshape(NE, 8, 128, D).transpose(0, 2, 1, 3)),
        "w_down": np.ascontiguousarray(f("w_down")[0].reshape(NE, 8, 128, D).transpose(0, 2, 1, 3)),
        "cosT": cosT, "sinT": sinT, "cmasks": cm, "rmask": rm, "iota": iota, "tokhl": tokhl,
    }
    maps = []
    for i in range(n_cores):
        b = i % 2
        m = dict(shared)
        m["x"] = np.ascontiguousarray(x[b])
        m["ctx"] = np.ascontiguousarray(ctx[b])
        m["cc"] = _kc(np.stack([c[b], c_ctx], axis=1))
        maps.append(m)
    return maps


N_CORES = 2
_NC_CACHE = {}


def kernel(**inputs):
    if "nc" not in _NC_CACHE:
        _NC_CACHE["nc"] = build()
    nc = _NC_CACHE["nc"]
    maps = prep_inputs(inputs, N_CORES)
    res = run_bass_kernel_spmd(nc, maps, core_ids=list(range(N_CORES)))
    out = np.stack([np.asarray(res.results[b]["out"], dtype=np.float32) for b in range(2)], axis=0)
    return out
```

```python
import numpy as np
from contextlib import ExitStack
import concourse.bass as bass
import concourse.mybir as mybir
from concourse.bass import IndirectOffsetOnAxis
from concourse.bass_utils import run_bass_kernel_spmd

F32 = mybir.dt.float32
BF16 = mybir.dt.bfloat16
I32 = mybir.dt.int32
U32 = mybir.dt.uint32
AF = mybir.ActivationFunctionType
ALU = mybir.AluOpType
AX = mybir.AxisListType

D = 1024
T = 8192
TC = 256
S = T + TC
NH = 4
NE = 16
CAP = 1024
EPS = 1e-6
NTILE = T // 128
FM_BLOCKS = 7
TM_COLS = 384
HEAD_COLS = FM_BLOCKS * 128 + TM_COLS
FM_TOTAL = NH * FM_BLOCKS * 128
WCOLS = NH * HEAD_COLS


class Buf:
    __slots__ = ("w", "r")

    def __init__(self):
        self.w = None
        self.r = {}


class Prog:
    def __init__(self, nc, es, ndma=12):
        self.nc = nc
        self.eng = dict(pe=nc.tensor, act=nc.scalar, dve=nc.vector, pool=nc.gpsimd, sp=nc.sync)
        self.sem = {k: es.enter_context(nc.semaphore("s_" + k)) for k in self.eng}
        self.cnt = {k: 0 for k in self.eng}
        self.waited = {k: {} for k in self.eng}
        self.dsem = {q: [[es.enter_context(nc.semaphore("d_%s%d" % (q, i))), 0] for i in range(ndma)]
                     for q in ("sp", "pool")}
        self.dnext = {"sp": 0, "pool": 0}
        self.nwait = 0
        self.nops = 0
        import os
        self.limit = int(os.environ.get('OPLIMIT', '1000000000'))

    def _wait(self, e, ev):
        s, v = ev
        w = self.waited[e]
        if w.get(s.num, 0) < v:
            self.eng[e].wait_ge(s, v)
            w[s.num] = v
            self.nwait += 1

    def _deps(self, e, reads, writes):
        own = self.sem[e].num
        for b in reads:
            if b.w is not None:
                if not (e == "pe" and b.w[0].num == own):
                    self._wait(e, b.w)
        for b in writes:
            if b.w is not None:
                if not (e == "pe" and b.w[0].num == own):
                    self._wait(e, b.w)
            for ev in b.r.values():
                if ev[0].num == own:
                    continue
                self._wait(e, ev)

    def _mark(self, ev, reads, writes):
        k = ev[0].num
        for b in reads:
            old = b.r.get(k)
            if old is None or old[1] < ev[1]:
                b.r[k] = ev
        for b in writes:
            b.w = ev
            b.r = {}

    def op(self, e, fn, r=(), w=()):
        self.nops += 1
        if self.nops > self.limit:
            return None
        if self.nops == self.limit:
            print('LAST OP', e, fn.__code__.co_firstlineno)
        self._deps(e, r, w)
        ins = fn(self.eng[e])
        self.cnt[e] += 1
        ins.then_inc(self.sem[e], 1)
        ev = (self.sem[e], self.cnt[e])
        self._mark(ev, r, w)
        return ev

    def mm(self, out_ap, pairs, r=(), w=()):
        self.nops += 1
        if self.nops > self.limit:
            return None
        self._deps("pe", r, w)
        n = len(pairs)
        ins = None
        for i, (l, rh) in enumerate(pairs):
            ins = self.nc.tensor.matmul(out_ap, lhsT=l, rhs=rh, start=(i == 0), stop=(i == n - 1))
        self.cnt["pe"] += 1
        ins.then_inc(self.sem["pe"], 1)
        ev = (self.sem["pe"], self.cnt["pe"])
        self._mark(ev, r, w)
        return ev

    def dma(self, q, fn, r=(), w=()):
        self.nops += 1
        if self.nops > self.limit:
            return None
        slots = self.dsem[q]
        i = self.dnext[q]
        self.dnext[q] = (i + 1) % len(slots)
        s, v = slots[i]
        if v > 0:
            self._wait(q, (s, v))
        self._deps(q, r, w)
        ins = fn(self.eng[q])
        slots[i][1] = v + 16
        ins.then_inc(s, 16)
        ev = (s, v + 16)
        self._mark(ev, r, w)
        return ev

    def all_events(self):
        evs = [(self.sem[k], self.cnt[k]) for k in self.eng if self.cnt[k] > 0]
        for q in self.dsem:
            for s, v in self.dsem[q]:
                if v > 0:
                    evs.append((s, v))
        return evs

    def barrier(self, engines=None):
        evs = self.all_events()
        for e in (engines or self.eng):
            for ev in evs:
                if ev[0].num != self.sem[e].num:
                    self._wait(e, ev)


def build(stop_after=None, dbg=()):
    nc = bass.Bass("TRN2", target_bir_lowering=False)
    dbg = set(dbg)

    def din(name, shape, dt=F32):
        return nc.dram_tensor(name, list(shape), dt, kind="ExternalInput").ap()

    def dscr(name, shape, dt):
        kind = "ExternalOutput" if name in dbg else "Internal"
        return nc.dram_tensor(name, list(shape), dt, kind=kind).ap()

    x_d = din("x", [T, D])
    ctx_d = din("ctx", [TC, D])
    cc_d = din("cc", [128, 8, 2])
    wada_d = din("w_ada", [128, 8, 6 * D])
    bada_d = din("b_ada", [1, 6 * D])
    norms_d = din("norms", [1, 4 * D])
    win_d = din("w_in", [128, 8, WCOLS])
    lamv_d = din("lamv", [1, 256])
    subln_d = din("subln", [128, 1])
    hgn_d = din("hgn", [1, 128])
    hlb_d = din("hlb", [128, 16])
    wout_d = din("w_out", [128, 8, D])
    wr_d = din("w_r", [128, 8, NE])
    wg_d = din("w_gate", [NE, 128, 8, D])
    wu_d = din("w_up", [NE, 128, 8, D])
    wd_d = din("w_down", [NE, 128, 8, D])
    cos_d = din("cosT", [128, S])
    sin_d = din("sinT", [128, S])
    cm_d = din("cmasks", [128, 4, 128])
    rm_d = din("rmask", [128, 2, 1024])
    iota_d = din("iota", [128, 1024])
    tokhl_d = din("tokhl", [128, NTILE, 2])
    out_d = nc.dram_tensor("out", [T, D], F32, kind="ExternalOutput").ap()

    modrows_d = dscr("modrows", [8, D], F32)
    qkt_d = dscr("qkt", [NH, 2, 128, S], BF16)
    zt_d = dscr("zt", [NH, 3, 128, S], F32)
    vi_d = dscr("vi", [S, NH, 2, 128], BF16)
    g_d = dscr("gsil", [S, NH, 128], F32)
    mixt_d = dscr("mixt", [D, T], BF16)
    x1_d = dscr("x1", [T, D], F32)
    h2_d = dscr("h2", [T, D], BF16)
    aff_d = dscr("aff", [T, NE], F32)
    f_d = dscr("facc", [T, D], F32)

    B_modrows = Buf()
    B_qkt = [[Buf() for _ in range(17)] for _ in range(NH)]
    B_zt = [[Buf() for _ in range(17)] for _ in range(NH)]
    B_vi = [Buf() for _ in range(17)]
    B_g = [Buf() for _ in range(17)]
    B_mixt = [Buf() for _ in range(16)]
    B_x1 = [Buf() for _ in range(NTILE)]
    B_h2 = Buf()
    B_aff = Buf()
    B_f = Buf()

    es = ExitStack()
    with es:
        P = Prog(nc, es)

        def sb(stack, name, shape, dt):
            return stack.enter_context(nc.sbuf_tensor("sb_" + name, list(shape), dt))

        def ps(stack, name, shape, dt=F32):
            return stack.enter_context(nc.psum_tensor("ps_" + name, list(shape), dt))

        cm_f = sb(es, "cm_f", [128, 4, 128], F32)
        ident_bf = sb(es, "ident_bf", [128, 128], BF16)
        ones_bf = sb(es, "ones_bf", [128, 128], BF16)
        ones_f = sb(es, "ones_f", [128, 128], F32)
        neglam = sb(es, "neglam", [128, 1], F32)
        subln8 = sb(es, "subln8", [128, 1], F32)
        lbt = sb(es, "lbt", [128, 8], F32)
        omlt = sb(es, "omlt", [128, 8], F32)
        mhalf = sb(es, "mhalf", [128, 512], F32)
        epsb = sb(es, "epsb", [128, 1], F32)
        B_const = Buf()
        ident_f = cm_f[:, 0, :]

        P.dma("sp", lambda q: q.dma_start(out=cm_f[:], in_=cm_d[:]), w=[B_const])
        P.op("dve", lambda v: v.tensor_copy(out=ident_bf[:], in_=cm_f[:, 0, :]), r=[B_const], w=[B_const])
        P.op("dve", lambda v: v.memset(ones_bf[:], 1.0), w=[B_const])
        P.op("dve", lambda v: v.memset(ones_f[:], 1.0), w=[B_const])
        P.op("dve", lambda v: v.memset(mhalf[:], -0.5), w=[B_const])
        P.op("dve", lambda v: v.memset(epsb[:], EPS), w=[B_const])

        def rstd_from_ss(ss_ap, n, out_ap, tmp_ap, bufs_r, bufs_w, tmpbuf):
            P.op("dve", lambda v: v.tensor_scalar(out=tmp_ap, in0=ss_ap, scalar1=1.0 / n, scalar2=EPS,
                                                  op0=ALU.mult, op1=ALU.add), r=bufs_r, w=[tmpbuf])
            shp = list(tmp_ap.shape)
            P.op("pool", lambda g: g.tensor_tensor(out=out_ap, in0=tmp_ap, in1=mhalf[0:shp[0], 0:shp[1]],
                                                   op=ALU.pow), r=[tmpbuf, B_const], w=bufs_w)

        with ExitStack() as p0:
            scf = sb(p0, "scf", [128, 8, 2], F32)
            sct = sb(p0, "sct", [128, 8, 2], F32)
            wa = [sb(p0, "wa%d" % i, [128, 8, 512], F32) for i in range(2)]
            modl = sb(p0, "modl", [1, 6 * D], F32)
            modc = sb(p0, "modc", [1, 6 * D], F32)
            bada = sb(p0, "bada", [1, 6 * D], F32)
            nrm = sb(p0, "nrm", [1, 4 * D], F32)
            rows = sb(p0, "rows", [1, 8, D], F32)
            lamv = sb(p0, "lamv", [1, 256], F32)
            lamt = sb(p0, "lamt", [1, 8], F32)
            hlb = sb(p0, "hlb", [128, 16], F32)
            sl = sb(p0, "sl", [128, 1], F32)
            pm = [ps(p0, "pm%d" % i, [1, 512]) for i in range(4)]
            pl = ps(p0, "pl", [128, 1])
            B_sc, B_wa, B_modl, B_modc, B_bada, B_nrm, B_rows, B_lam, B_hlb = (
                Buf(), [Buf(), Buf()], Buf(), Buf(), Buf(), Buf(), Buf(), Buf(), Buf())
            B_pm = [Buf() for _ in range(4)]
            B_pl = Buf()

            P.dma("sp", lambda q: q.dma_start(out=scf[:], in_=cc_d[:]), w=[B_sc])
            P.dma("sp", lambda q: q.dma_start(out=bada[:], in_=bada_d[:]), w=[B_bada])
            P.dma("sp", lambda q: q.dma_start(out=nrm[:], in_=norms_d[:]), w=[B_nrm])
            P.dma("sp", lambda q: q.dma_start(out=lamv[:], in_=lamv_d[:]), w=[B_lam])
            P.dma("sp", lambda q: q.dma_start(out=hlb[:], in_=hlb_d[:]), w=[B_hlb])
            P.dma("sp", lambda q: q.dma_start(out=sl[:], in_=subln_d[:]), w=[B_hlb])
            P.op("act", lambda a: a.activation(out=sct[:], in_=scf[:], func=AF.Exp, scale=-1.0), r=[B_sc], w=[B_rows])
            P.op("dve", lambda v: v.tensor_scalar(out=sct[:], in0=sct[:], scalar1=1.0, scalar2=None, op0=ALU.add),
                 r=[B_rows], w=[B_rows])
            P.op("dve", lambda v: v.reciprocal(out=sct[:], in_=sct[:]), r=[B_rows], w=[B_rows])
            P.op("dve", lambda v: v.tensor_tensor(out=scf[:], in0=scf[:], in1=sct[:], op=ALU.mult),
                 r=[B_rows, B_sc], w=[B_sc])
            for ch in range(12):
                wb_ = wa[ch % 2]
                P.dma("sp", lambda q: q.dma_start(out=wb_[:], in_=wada_d[:, :, ch * 512:(ch + 1) * 512]),
                      w=[B_wa[ch % 2]])
                for j, (mod, bm) in enumerate(((modl, B_modl), (modc, B_modc))):
                    pmt = pm[(2 * ch + j) % 4]
                    bp = B_pm[(2 * ch + j) % 4]
                    P.mm(pmt[:], [(scf[:, kc, j:j + 1], wb_[:, kc, :]) for kc in range(8)],
                         r=[B_sc, B_wa[ch % 2]], w=[bp])
                    P.op("dve", lambda v: v.tensor_tensor(out=mod[:, ch * 512:(ch + 1) * 512], in0=pmt[:],
                                                          in1=bada[:, ch * 512:(ch + 1) * 512], op=ALU.add),
                         r=[bp, B_bada], w=[bm])
            def stt(dst, a, b, op0):
                P.op("dve", lambda v: v.scalar_tensor_tensor(out=rows[:, dst, :], in0=a, scalar=(1.0 if op0 == ALU.add else 1.0),
                                                             in1=b, op0=op0, op1=ALU.mult),
                     r=[B_modl, B_modc, B_nrm], w=[B_rows])
            stt(0, modl[:, D:2 * D], nrm[:, 0:D], ALU.add)
            P.op("dve", lambda v: v.tensor_copy(out=rows[:, 1, :], in_=modl[:, 0:D]), r=[B_modl], w=[B_rows])
            stt(2, modc[:, D:2 * D], nrm[:, 0:D], ALU.add)
            P.op("dve", lambda v: v.tensor_copy(out=rows[:, 3, :], in_=modc[:, 0:D]), r=[B_modc], w=[B_rows])
            stt(4, modl[:, 2 * D:3 * D], nrm[:, D:2 * D], ALU.mult)
            stt(5, modl[:, 4 * D:5 * D], nrm[:, 2 * D:3 * D], ALU.add)
            P.op("dve", lambda v: v.tensor_copy(out=rows[:, 6, :], in_=modl[:, 3 * D:4 * D]), r=[B_modl], w=[B_rows])
            stt(7, modl[:, 5 * D:6 * D], nrm[:, 3 * D:4 * D], ALU.mult)
            P.dma("pool", lambda q: q.dma_start(out=modrows_d[:, :].rearrange("(o r) d -> o r d", o=1), in_=rows[:]),
                  r=[B_rows], w=[B_modrows])
            P.op("dve", lambda v: v.tensor_tensor(out=lamv[:, 0:64], in0=lamv[:, 0:64], in1=lamv[:, 64:128], op=ALU.mult),
                 r=[B_lam], w=[B_lam])
            P.op("dve", lambda v: v.tensor_tensor(out=lamv[:, 128:192], in0=lamv[:, 128:192], in1=lamv[:, 192:256], op=ALU.mult),
                 r=[B_lam], w=[B_lam])
            P.op("dve", lambda v: v.tensor_reduce(out=lamt[:, 0:1], in_=lamv[:, 0:64], axis=AX.X, op=ALU.add),
                 r=[B_lam], w=[B_lam])
            P.op("dve", lambda v: v.tensor_reduce(out=lamt[:, 1:2], in_=lamv[:, 128:192], axis=AX.X, op=ALU.add),
                 r=[B_lam], w=[B_lam])
            P.op("act", lambda a: a.activation(out=lamt[:, 2:4], in_=lamt[:, 0:2], func=AF.Exp), r=[B_lam], w=[B_lam])
            P.op("dve", lambda v: v.scalar_tensor_tensor(out=lamt[:, 4:5], in0=lamt[:, 3:4], scalar=-0.2, in1=lamt[:, 2:3],
                                                         op0=ALU.add, op1=ALU.subtract), r=[B_lam], w=[B_lam])
            P.mm(pl[:], [(ones_f[0:1, :], lamt[0:1, 4:5])], r=[B_lam, B_const], w=[B_pl])
            P.op("dve", lambda v: v.tensor_copy(out=neglam[:], in_=pl[:]), r=[B_pl], w=[B_const])
            P.op("dve", lambda v: v.tensor_scalar(out=subln8[:], in0=sl[:], scalar1=0.8, scalar2=None, op0=ALU.mult),
                 r=[B_hlb], w=[B_const])
            P.op("dve", lambda v: v.tensor_tensor(out=hlb[:, 0:8], in0=hlb[:, 8:16], in1=hlb[:, 0:8], op=ALU.subtract),
                 r=[B_hlb], w=[B_hlb])
            P.op("act", lambda a: a.activation(out=hlb[:, 0:8], in_=hlb[:, 0:8], func=AF.Exp), r=[B_hlb], w=[B_hlb])
            P.op("dve", lambda v: v.tensor_scalar(out=hlb[:, 0:8], in0=hlb[:, 0:8], scalar1=1.0, scalar2=None, op0=ALU.add),
                 r=[B_hlb], w=[B_hlb])
            P.op("dve", lambda v: v.reciprocal(out=lbt[:], in_=hlb[:, 0:8]), r=[B_hlb], w=[B_const])
            P.op("dve", lambda v: v.tensor_scalar(out=omlt[:], in0=lbt[:], scalar1=-1.0, scalar2=1.0, op0=ALU.mult, op1=ALU.add),
                 r=[B_const], w=[B_const])
            P.barrier()

        def load_bc(stack, name, row):
            t = sb(stack, name, [128, D], F32)
            b = Buf()
            P.dma("sp", lambda q: q.dma_start(out=t[:], in_=modrows_d[row:row + 1, :].partition_broadcast(128)),
                  r=[B_modrows], w=[b])
            return t, b

        def pe_group(fns, r, w):
            P._deps("pe", r, w)
            ins = None
            for fn in fns:
                ins = fn(nc.tensor)
            P.cnt["pe"] += 1
            ins.then_inc(P.sem["pe"], 1)
            ev = (P.sem["pe"], P.cnt["pe"])
            P._mark(ev, r, w)
            return ev

        SCS = [(0, 2)] + [(2 + 4 * k, 4) for k in range(16)]

        def phase_a0():
            with ExitStack() as st:
                wbf = sb(st, "wbf", [128, 8, WCOLS], BF16)
                B_w = [[Buf() for _ in range(4)] for _ in range(8)]
                for kc in range(8):
                    for pi in range(4):
                        c0 = pi * 1280
                        P.dma("pool", lambda q: q.dma_start(out=wbf[:, kc, c0:c0 + 1280], in_=win_d[:, kc, c0:c0 + 1280]),
                              w=[B_w[kc][pi]])
                B_wall = [b for l in B_w for b in l]
                gm_l, B_gml = load_bc(st, "gm_l", 0)
                sh_l, B_shl = load_bc(st, "sh_l", 1)
                gm_c, B_gmc = load_bc(st, "gm_c", 2)
                sh_c, B_shc = load_bc(st, "sh_c", 3)
                xb = [sb(st, "xb%d" % i, [128, D], F32) for i in range(2)]
                B_xb = [Buf(), Buf()]
                junk = sb(st, "junk", [128, D], BF16)
                B_junk = Buf()
                hn = [sb(st, "hn%d" % i, [128, D], F32) for i in range(2)]
                B_hn = [Buf(), Buf()]
                hb = [sb(st, "hb%d" % i, [128, D], BF16) for i in range(4)]
                B_hb = [Buf() for _ in range(4)]
                ssm = sb(st, "ssm", [128, 8], F32)
                B_ss = [Buf() for _ in range(4)]
                hT = [sb(st, "hT%d" % i, [128, 8, 512], BF16) for i in range(2)]
                B_hT = [Buf(), Buf()]
                cs = [sb(st, "cs%d" % i, [128, 2, 512], F32) for i in range(2)]
                B_cs = [Buf(), Buf()]
                qks = [sb(st, "qks%d" % i, [128, 2, 512], BF16) for i in range(2)]
                B_qks = [Buf(), Buf()]
                zs = [sb(st, "zs%d" % i, [128, 3, 512], F32) for i in range(2)]
                B_zs = [Buf(), Buf()]
                t1 = sb(st, "t1", [128, 512], F32)
                t2 = sb(st, "t2", [128, 512], F32)
                B_t1, B_t2 = Buf(), Buf()
                vis = [sb(st, "vis%d" % i, [128, NH, 2, 128], BF16) for i in range(2)]
                B_vis = [Buf(), Buf()]
                gs = [sb(st, "gs%d" % i, [128, NH, 128], F32) for i in range(2)]
                B_gs = [Buf(), Buf()]
                tp = [ps(st, "tp%d" % i, [128, 8, 128], BF16) for i in range(2)]
                B_tp = [Buf(), Buf()]
                fm = [ps(st, "fm%d" % i, [128, 512]) for i in range(3)]
                B_fm = [Buf() for _ in range(3)]
                tm = ps(st, "tm", [128, 1536])
                B_tm = Buf()
                fmi = [0]
                tile_ctr = [0]

                def stage1(k):
                    t0, nt = SCS[k]
                    for j in range(nt):
                        i = tile_ctr[0]
                        tile_ctr[0] += 1
                        x_ = xb[i % 2]
                        st_ = t0 + j
                        src = ctx_d[st_ * 128:(st_ + 1) * 128, :] if k == 0 else x_d[(st_ - 2) * 128:(st_ - 1) * 128, :]
                        gm, bgm, sh, bsh = (gm_c, B_gmc, sh_c, B_shc) if k == 0 else (gm_l, B_gml, sh_l, B_shl)
                        P.dma("sp", lambda q: q.dma_start(out=x_[:], in_=src), w=[B_xb[i % 2]])
                        ss = ssm[:, 2 * j:2 * j + 1]
                        rs = ssm[:, 2 * j + 1:2 * j + 2]
                        P.op("dve", lambda v: v.scalar_tensor_tensor(out=junk[:], in0=x_[:], scalar=1.0, in1=x_[:],
                                                                     op0=ALU.mult, op1=ALU.mult, accum_out=ss),
                             r=[B_xb[i % 2]], w=[B_junk, B_ss[j]])
                        rstd_from_ss(ss, D, rs, ss, [B_ss[j]], [B_ss[j]], B_ss[j])
                        h_ = hn[i % 2]
                        P.op("dve", lambda v: v.scalar_tensor_tensor(out=h_[:], in0=x_[:], scalar=rs, in1=gm[:],
                                                                     op0=ALU.mult, op1=ALU.mult),
                             r=[B_xb[i % 2], B_ss[j], bgm], w=[B_hn[i % 2]])
                        P.op("pool", lambda g: g.tensor_tensor(out=hb[j][:], in0=h_[:], in1=sh[:], op=ALU.add),
                             r=[B_hn[i % 2], bsh], w=[B_hb[j]])

                def stage2(k):
                    t0, nt = SCS[k]
                    for j in range(nt):
                        tpp = tp[j % 2]
                        pe_group([(lambda pe, kc=kc: pe.transpose(out=tpp[:, kc, :], in_=hb[j][:, kc * 128:(kc + 1) * 128],
                                                                   identity=ident_bf[:])) for kc in range(8)],
                                 r=[B_hb[j], B_const], w=[B_tp[j % 2]])
                        P.op("act", lambda a: a.copy(out=hT[k % 2][:, :, j * 128:(j + 1) * 128], in_=tpp[:]),
                             r=[B_tp[j % 2]], w=[B_hT[k % 2]])

                def fm_mm(k, col0, n):
                    bi = fmi[0] % 3
                    fmi[0] += 1
                    P.mm(fm[bi][:, 0:n], [(wbf[:, kc, col0:col0 + 128], hT[k % 2][:, kc, 0:n]) for kc in range(8)],
                         r=[B_hT[k % 2]] + B_wall, w=[B_fm[bi]])
                    return bi

                def stage3(k):
                    t0, nt = SCS[k]
                    n = nt * 128
                    s0 = t0 * 128
                    c_ = cs[k % 2]
                    P.dma("sp", lambda q: q.dma_start(out=c_[:, 0, 0:n], in_=cos_d[:, s0:s0 + n]), w=[B_cs[k % 2]])
                    P.dma("sp", lambda q: q.dma_start(out=c_[:, 1, 0:n], in_=sin_d[:, s0:s0 + n]), w=[B_cs[k % 2]])
                    for h in range(NH):
                        hi = k * NH + h
                        qk_ = qks[hi % 2]
                        z_ = zs[hi % 2]
                        base = h * FM_BLOCKS * 128
                        for t in range(2):
                            b0 = fm_mm(k, base + (2 * t) * 128, n)
                            b1 = fm_mm(k, base + (2 * t + 1) * 128, n)
                            P.op("dve", lambda v: v.tensor_tensor(out=t1[:, 0:n], in0=fm[b0][:, 0:n], in1=c_[:, 0, 0:n], op=ALU.mult),
                                 r=[B_fm[b0], B_cs[k % 2]], w=[B_t1])
                            P.op("dve", lambda v: v.tensor_tensor(out=t2[:, 0:n], in0=fm[b1][:, 0:n], in1=c_[:, 1, 0:n], op=ALU.mult),
                                 r=[B_fm[b1], B_cs[k % 2]], w=[B_t2])
                            P.op("pool", lambda g: g.tensor_tensor(out=qk_[:, t, 0:n], in0=t1[:, 0:n], in1=t2[:, 0:n], op=ALU.add),
                                 r=[B_t1, B_t2], w=[B_qks[hi % 2]])
                        for t in range(3):
                            b0 = fm_mm(k, base + (4 + t) * 128, n)
                            P.op("act", lambda a: a.copy(out=z_[:, t, 0:n], in_=fm[b0][:, 0:n]), r=[B_fm[b0]], w=[B_zs[hi % 2]])
                        P.dma("pool", lambda q: q.dma_start(out=qkt_d[h].rearrange("t p s -> p t s")[:, :, s0:s0 + n],
                                                            in_=qk_[:, :, 0:n]), r=[B_qks[hi % 2]], w=[B_qkt[h][k]])
                        P.dma("pool", lambda q: q.dma_start(out=zt_d[h].rearrange("t p s -> p t s")[:, :, s0:s0 + n],
                                                            in_=z_[:, :, 0:n]), r=[B_zs[hi % 2]], w=[B_zt[h][k]])
                    for j in range(nt):
                        ti = t0 + j
                        for nb in range(3):
                            P.mm(tm[:, nb * 512:(nb + 1) * 512],
                                 [(hT[k % 2][:, kc, j * 128:(j + 1) * 128], wbf[:, kc, FM_TOTAL + nb * 512:FM_TOTAL + (nb + 1) * 512])
                                  for kc in range(8)], r=[B_hT[k % 2]] + B_wall, w=[B_tm])
                        v_ = vis[ti % 2]
                        g_ = gs[ti % 2]
                        vflat = v_[:].rearrange("p h t c -> p (h t c)")
                        P.op("dve", lambda v: v.tensor_copy(out=vflat[:, 0:512], in_=tm[:, 0:512]),
                             r=[B_tm], w=[B_vis[ti % 2]])
                        P.op("dve", lambda v: v.tensor_copy(out=vflat[:, 512:1024], in_=tm[:, 512:1024]),
                             r=[B_tm], w=[B_vis[ti % 2]])
                        P.op("act", lambda a: a.activation(out=g_[:].rearrange("p h c -> p (h c)"), in_=tm[:, 1024:1536], func=AF.Silu),
                             r=[B_tm], w=[B_gs[ti % 2]])
                        P.dma("pool", lambda q: q.dma_start(out=vi_d[ti * 128:(ti + 1) * 128], in_=v_[:]),
                              r=[B_vis[ti % 2]], w=[B_vi[k]])
                        P.dma("pool", lambda q: q.dma_start(out=g_d[ti * 128:(ti + 1) * 128], in_=g_[:]),
                              r=[B_gs[ti % 2]], w=[B_g[k]])

                stage1(0)
                stage2(0)
                for k in range(17):
                    if k + 1 < 17:
                        stage1(k + 1)
                    stage3(k)
                    if k + 1 < 17:
                        stage2(k + 1)
                P.barrier()

        def mm1(out_ap, lhsT, rhs, start, stop, r, w):
            P.nops += 1
            if P.nops > P.limit:
                return None
            P._deps("pe", r, w)
            ins = nc.tensor.matmul(out_ap, lhsT=lhsT, rhs=rhs, start=start, stop=stop)
            P.cnt["pe"] += 1
            ins.then_inc(P.sem["pe"], 1)
            ev = (P.sem["pe"], P.cnt["pe"])
            P._mark(ev, r, w)
            return ev

        NKT = S // 128

        def phase_a1(heads=range(NH), qchunks=range(16)):
            with ExitStack() as st:
                kt = [sb(st, "kt%d" % i, [128, S], BF16) for i in range(2)]
                vv = [sb(st, "vv%d" % i, [128, NKT, 128], BF16) for i in range(2)]
                B_kt = [Buf(), Buf()]
                B_vv = [Buf(), Buf()]
                qt = [sb(st, "qt%d" % i, [128, 512], BF16) for i in range(2)]
                B_qt = [Buf(), Buf()]
                p1 = [sb(st, "p1_%d" % i, [128, 512], BF16) for i in range(3)]
                p2 = [sb(st, "p2_%d" % i, [128, 512], BF16) for i in range(3)]
                B_p1 = [Buf() for _ in range(3)]
                B_p2 = [Buf() for _ in range(3)]
                acc1s = [sb(st, "acc1_%d" % i, [128, 512], F32) for i in range(2)]
                acc2s = [sb(st, "acc2_%d" % i, [128, 512], F32) for i in range(2)]
                B_acc1s, B_acc2s = [Buf(), Buf()], [Buf(), Buf()]
                o1c = sb(st, "o1c", [128, 512], F32)
                o2c = sb(st, "o2c", [128, 512], F32)
                B_o1c, B_o2c = Buf(), Buf()
                r1 = sb(st, "r1", [128, 512], F32)
                o1 = sb(st, "o1", [128, 512], F32)
                r2 = sb(st, "r2", [128, 512], F32)
                o2 = sb(st, "o2", [128, 512], F32)
                oo = sb(st, "oo", [128, 512], F32)
                sq = sb(st, "sq", [128, 512], F32)
                rs = sb(st, "rs", [128, 512], F32)
                ob = [sb(st, "ob%d" % i, [128, 512], BF16) for i in range(2)]
                B_r1, B_o1, B_r2, B_o2, B_oo, B_sq, B_rs = Buf(), Buf(), Buf(), Buf(), Buf(), Buf(), Buf()
                B_ob = [Buf(), Buf()]
                s1 = [ps(st, "s1_%d" % i, [128, 512]) for i in range(2)]
                s2 = [ps(st, "s2_%d" % i, [128, 512]) for i in range(2)]
                B_s1 = [Buf(), Buf()]
                B_s2 = [Buf(), Buf()]
                o1p = ps(st, "o1p", [128, 512])
                o2p = ps(st, "o2p", [128, 512])
                l1p = ps(st, "l1p", [128, 512])
                l2p = ps(st, "l2p", [128, 512])
                B_o1p, B_o2p, B_l1p, B_l2p = Buf(), Buf(), Buf(), Buf()
                def finalize(h, qc, acc1, acc2, B_acc1, B_acc2, o_, bo):
                    P.mm(l1p[:], [(ones_f[:], acc1[:])], r=[B_acc1, B_const], w=[B_l1p])
                    P.mm(l2p[:], [(ones_f[:], acc2[:])], r=[B_acc2, B_const], w=[B_l2p])
                    P.op("act", lambda a: a.activation(out=r1[:], in_=l1p[:], func=AF.Ln), r=[B_l1p], w=[B_r1])
                    P.op("act", lambda a: a.activation(out=r1[:], in_=r1[:], func=AF.Exp, scale=-1.0), r=[B_r1], w=[B_r1])
                    P.op("dve", lambda v: v.tensor_tensor(out=o1[:], in0=o1c[:], in1=r1[:], op=ALU.mult),
                         r=[B_o1c, B_r1], w=[B_o1])
                    P.op("act", lambda a: a.activation(out=r2[:], in_=l2p[:], func=AF.Ln), r=[B_l2p], w=[B_r2])
                    P.op("act", lambda a: a.activation(out=r2[:], in_=r2[:], func=AF.Exp, scale=-1.0), r=[B_r2], w=[B_r2])
                    P.op("dve", lambda v: v.tensor_tensor(out=o2[:], in0=o2c[:], in1=r2[:], op=ALU.mult),
                         r=[B_o2c, B_r2], w=[B_o2])
                    P.op("dve", lambda v: v.scalar_tensor_tensor(out=oo[:], in0=o2[:], scalar=neglam[:, 0:1], in1=o1[:],
                                                                 op0=ALU.mult, op1=ALU.add),
                         r=[B_o2, B_o1, B_const], w=[B_oo])
                    P.op("dve", lambda v: v.tensor_tensor(out=sq[:], in0=oo[:], in1=oo[:], op=ALU.mult),
                         r=[B_oo], w=[B_sq])
                    P.mm(l1p[:], [(ones_f[:], sq[:])], r=[B_sq, B_const], w=[B_l1p])
                    P.op("act", lambda a: a.activation(out=rs[:], in_=l1p[:], func=AF.Ln, scale=1.0 / 128, bias=epsb[:, 0:1]),
                         r=[B_l1p, B_const], w=[B_rs])
                    P.op("act", lambda a: a.activation(out=rs[:], in_=rs[:], func=AF.Exp, scale=-0.5), r=[B_rs], w=[B_rs])
                    P.op("dve", lambda v: v.scalar_tensor_tensor(out=o_[:], in0=oo[:], scalar=subln8[:, 0:1], in1=rs[:],
                                                                 op0=ALU.mult, op1=ALU.mult),
                         r=[B_oo, B_rs, B_const], w=[bo])
                    P.dma("pool", lambda q: q.dma_start(out=mixt_d[h * 128:(h + 1) * 128, qc * 512:(qc + 1) * 512], in_=o_[:]),
                          r=[bo], w=[B_mixt[qc]])

                pending = None
                cnt = 0
                hl = list(heads)

                def load_kv(hi):
                    hh = hl[hi]
                    P.dma("sp", lambda q: q.dma_start(out=kt[hi % 2][:], in_=qkt_d[hh, 1]), r=B_qkt[hh], w=[B_kt[hi % 2]])
                    vsrc = vi_d[:, hh, 0, :].rearrange("(t p) c -> p t c", p=128)
                    for part in range(3):
                        P.dma("sp", lambda q: q.dma_start(out=vv[hi % 2][:, part * 22:(part + 1) * 22, :],
                                                          in_=vsrc[:, part * 22:(part + 1) * 22, :]),
                              r=B_vi, w=[B_vv[hi % 2]])

                load_kv(0)
                for hi, h in enumerate(hl):
                    k_ = kt[hi % 2]
                    v_ = vv[hi % 2]
                    if hi + 1 < len(hl):
                        load_kv(hi + 1)
                    for qc in qchunks:
                        q_ = qt[cnt % 2]
                        bq = B_qt[cnt % 2]
                        o_ = ob[cnt % 2]
                        bo = B_ob[cnt % 2]
                        acc1, acc2 = acc1s[cnt % 2], acc2s[cnt % 2]
                        B_acc1, B_acc2 = B_acc1s[cnt % 2], B_acc2s[cnt % 2]
                        cnt += 1
                        P.dma("sp", lambda q: q.dma_start(out=q_[:], in_=qkt_d[h, 0][:, TC + qc * 512:TC + (qc + 1) * 512]),
                              r=B_qkt[h], w=[bq])

                        def scores(i):
                            mm1(s1[i % 2][:], k_[0:64, i * 128:(i + 1) * 128], q_[0:64, :], True, True,
                                [B_kt[hi % 2], bq], [B_s1[i % 2]])
                            mm1(s2[i % 2][:], k_[64:128, i * 128:(i + 1) * 128], q_[64:128, :], True, True,
                                [B_kt[hi % 2], bq], [B_s2[i % 2]])

                        scores(0)
                        scores(1)
                        for i in range(NKT):
                            if i == 8 and pending is not None:
                                finalize(*pending)
                                pending = None
                            pa, pb = p1[i % 3], p2[i % 3]
                            P.op("act", lambda a: a.activation(out=pa[:], in_=s1[i % 2][:], func=AF.Exp, scale=0.125),
                                 r=[B_s1[i % 2]], w=[B_p1[i % 3]])
                            P.op("act", lambda a: a.activation(out=pb[:], in_=s2[i % 2][:], func=AF.Exp, scale=0.125),
                                 r=[B_s2[i % 2]], w=[B_p2[i % 3]])
                            st_, sp_ = (i == 0), (i == NKT - 1)
                            P._deps("pe", [B_p1[i % 3], B_p2[i % 3]], [])
                            mm1(o1p[:], v_[:, i, :], pa[:], st_, sp_, [B_vv[hi % 2], B_p1[i % 3]], [B_o1p])
                            mm1(o2p[:], v_[:, i, :], pb[:], st_, sp_, [B_vv[hi % 2], B_p2[i % 3]], [B_o2p])
                            if i == 0:
                                P.op("dve", lambda v: v.tensor_copy(out=acc1[:], in_=pa[:]), r=[B_p1[i % 3]], w=[B_acc1])
                                P.op("dve", lambda v: v.tensor_copy(out=acc2[:], in_=pb[:]), r=[B_p2[i % 3]], w=[B_acc2])
                            else:
                                P.op("dve", lambda v: v.tensor_tensor(out=acc1[:], in0=acc1[:], in1=pa[:], op=ALU.add),
                                     r=[B_p1[i % 3], B_acc1], w=[B_acc1])
                                P.op("dve", lambda v: v.tensor_tensor(out=acc2[:], in0=acc2[:], in1=pb[:], op=ALU.add),
                                     r=[B_p2[i % 3], B_acc2], w=[B_acc2])
                            if i + 2 < NKT:
                                scores(i + 2)
                        P.op("act", lambda a: a.copy(out=o1c[:], in_=o1p[:]), r=[B_o1p], w=[B_o1c])
                        P.op("dve", lambda v: v.tensor_copy(out=o2c[:], in_=o2p[:]), r=[B_o2p], w=[B_o2c])
                        pending = (h, qc, acc1, acc2, B_acc1, B_acc2, o_, bo)
                if pending is not None:
                    finalize(*pending)
                P.barrier()

        def phase_a2(heads=range(NH)):
            with ExitStack() as st:
                o_accs = [sb(st, "o_acc%d" % i, [128, NTILE, 128], F32) for i in range(2)]
                B_oaccs = [[Buf() for _ in range(NTILE)] for _ in range(2)]
                rm = sb(st, "rm", [128, 2, 512], F32)
                cmA = sb(st, "cmA", [128, 512], BF16)
                cmB = sb(st, "cmB", [128, 512], BF16)
                hgn = sb(st, "hgn_b", [128, 128], F32)
                B_c2 = Buf()
                P.dma("sp", lambda q: q.dma_start(out=rm[:], in_=rm_d[:, :, 0:512]), w=[B_c2])
                P.dma("sp", lambda q: q.dma_start(out=hgn[:], in_=hgn_d[0:1, :].partition_broadcast(128)), w=[B_c2])
                P.op("dve", lambda v: v.memset(cmA[:], 0.0), w=[B_c2])
                P.op("dve", lambda v: v.memset(cmB[:], 0.0), w=[B_c2])
                P.op("dve", lambda v: v.memset(cmA[:].rearrange("p (t c) -> p t c", c=128)[:, :, 0:64], 1.0), w=[B_c2])
                P.op("dve", lambda v: v.memset(cmB[:].rearrange("p (t c) -> p t c", c=128)[:, :, 64:128], 1.0), w=[B_c2])

                def dbl(name, shape, dt):
                    return [sb(st, "%s%d" % (name, i), shape, dt) for i in range(2)], [Buf(), Buf()]
                zin, B_zin = dbl("zin", [128, 2, 512], F32)
                vt, B_vt = dbl("vt", [128, 4, 128], BF16)
                gt, B_gt = dbl("gt", [128, 4, 128], F32)
                e_, B_e = dbl("e_", [128, 512], F32)
                f_, B_f_ = dbl("f_", [128, 512], F32)
                lf, B_lf = dbl("lf", [128, 512], F32)
                kk, B_kk = dbl("kk", [128, 512], F32)
                bc, B_bc = dbl("bc", [128, 512], F32)
                ep, B_ep = dbl("ep", [128, 512], F32)
                en, B_en = dbl("en", [128, 512], F32)
                kdf, B_kdf = dbl("kdf", [128, 512], F32)
                Qd, B_Qd = dbl("Qd", [128, 512], BF16)
                QdA, B_QdA = dbl("QdA", [128, 512], BF16)
                QdB, B_QdB = dbl("QdB", [128, 512], BF16)
                Kd, B_Kd = dbl("Kd", [128, 512], BF16)
                K2T, B_K2T = dbl("K2T", [128, 512], BF16)
                dec, B_dec = dbl("dec", [128, 8], F32)
                k2all, B_k2all = dbl("k2all", [128, 4, 128], BF16)
                scma, B_scma = dbl("scma", [128, 4, 128], BF16)
                Sball, B_Sball = dbl("Sball", [128, 8, 128], BF16)
                ScarA, B_ScarA = dbl("ScarA", [128, 128], BF16)
                ScarB, B_ScarB = dbl("ScarB", [128, 128], BF16)
                Sst2 = [sb(st, "Sst%d" % i, [128, 128], F32) for i in range(2)]
                B_S2 = [Buf(), Buf()]
                otot, B_otot = dbl("otot", [128, 4, 128], F32)
                osq, B_osq = dbl("osq", [128, 4, 128], F32)
                osm4, B_osm4 = dbl("osm4", [128, 8], F32)
                yb4, B_yb4 = dbl("yb4", [128, 4, 128], BF16)
                mixs, B_mixs = dbl("mixs", [128, 512], BF16)
                tpb = ps(st, "tpb", [128, 1024], BF16)
                B_tpb = Buf()
                scp = ps(st, "scp", [128, 512])
                B_scp = Buf()
                ups2 = [ps(st, "ups%d" % i, [128, 1024]) for i in range(2)]
                B_ups2 = [[Buf() for _ in range(8)] for _ in range(2)]
                ops2 = [ps(st, "ops%d" % i, [128, 512]) for i in range(2)]
                B_ops2 = [Buf(), Buf()]
                ctr = dict(sc=0, tile=0, ch=0, tp=0)

                def chain(h, d):
                    if True:
                        Sst = Sst2[d]
                        B_S = B_S2[d]
                        Scar, B_Scar = (ScarA, B_ScarA) if d == 0 else (ScarB, B_ScarB)
                        o_acc = o_accs[d]
                        B_oacc = B_oaccs[d]
                        cp = 0
                        col = d * 4 + h
                        lb_ap = lbt[:, col:col + 1]
                        oml_ap = omlt[:, col:col + 1]
                        P.op("dve", lambda v: v.memset(Sst[:], 0.0), w=[B_S])
                        P.op("dve", lambda v: v.memset(Scar[0][:], 0.0), w=[B_Scar[0]])
                        P.op("dve", lambda v: v.memset(Scar[1][:], 0.0), w=[B_Scar[1]])
                        order = list(range(17)) if d == 0 else [0] + list(range(16, 0, -1))
                        for k in order:
                            t0, nt = SCS[k]
                            n = nt * 128
                            s0 = t0 * 128
                            nch = n // 64
                            lat = k >= 1
                            i2 = d
                            z_ = zin[i2]
                            P.dma("sp", lambda q: q.dma_start(out=z_[:, 0, 0:n], in_=zt_d[h, d][:, s0:s0 + n]),
                                  r=B_zt[h], w=[B_zin[i2]])
                            P.dma("sp", lambda q: q.dma_start(out=z_[:, 1, 0:n], in_=zt_d[h, 2][:, s0:s0 + n]),
                                  r=B_zt[h], w=[B_zin[i2]])
                            P.dma("sp", lambda q: q.dma_start(
                                out=vt[i2][:, 0:nt, :], in_=vi_d[s0:s0 + n, h, 1, :].rearrange("(t p) c -> p t c", p=128)),
                                r=B_vi, w=[B_vt[i2]])
                            zz = z_[:, 0, 0:n]
                            hq = z_[:, 1, 0:n]
                            P.op("act", lambda a: a.activation(out=e_[i2][:, 0:n], in_=zz, func=AF.Exp, scale=-1.0),
                                 r=[B_zin[i2]], w=[B_e[i2]])
                            yield
                            P.op("dve", lambda v: v.tensor_scalar(out=e_[i2][:, 0:n], in0=e_[i2][:, 0:n], scalar1=1.0, scalar2=None,
                                                                  op0=ALU.add), r=[B_e[i2]], w=[B_e[i2]])
                            yield
                            P.op("act", lambda a: a.activation(out=e_[i2][:, 0:n], in_=e_[i2][:, 0:n], func=AF.Ln), r=[B_e[i2]], w=[B_e[i2]])
                            P.op("act", lambda a: a.activation(out=e_[i2][:, 0:n], in_=e_[i2][:, 0:n], func=AF.Exp, scale=-1.0),
                                 r=[B_e[i2]], w=[B_e[i2]])
                            yield
                            P.op("dve", lambda v: v.tensor_scalar(out=f_[i2][:, 0:n], in0=e_[i2][:, 0:n], scalar1=oml_ap, scalar2=lb_ap,
                                                                  op0=ALU.mult, op1=ALU.add), r=[B_e[i2], B_const], w=[B_f_[i2]])
                            yield
                            P.op("act", lambda a: a.activation(out=lf[i2][:, 0:n], in_=f_[i2][:, 0:n], func=AF.Ln),
                                 r=[B_f_[i2]], w=[B_lf[i2]])
                            P.op("act", lambda a: a.activation(out=kk[i2][:, 0:n], in_=f_[i2][:, 0:n], func=AF.Copy, scale=-1.0, bias=1.0),
                                 r=[B_f_[i2]], w=[B_kk[i2]])
                            yield
                            if d == 0:
                                P.op("dve", lambda v: v.tensor_tensor_scan(out=bc[i2][:, 0:n], data0=rm[:, 0, 0:n], data1=lf[i2][:, 0:n],
                                                                           initial=0.0, op0=ALU.mult, op1=ALU.add),
                                     r=[B_lf[i2], B_c2], w=[B_bc[i2]])
                            else:
                                P.op("dve", lambda v: v.tensor_tensor_scan(out=bc[i2][:, 0:n][:, ::-1], data0=rm[:, 1, 0:n][:, ::-1],
                                                                           data1=lf[i2][:, 0:n][:, ::-1],
                                                                           initial=0.0, op0=ALU.mult, op1=ALU.add),
                                     r=[B_lf[i2], B_c2], w=[B_bc[i2]])
                            yield
                            P.op("act", lambda a: a.activation(out=ep[i2][:, 0:n], in_=bc[i2][:, 0:n], func=AF.Exp),
                                 r=[B_bc[i2]], w=[B_ep[i2]])
                            P.op("act", lambda a: a.activation(out=en[i2][:, 0:n], in_=bc[i2][:, 0:n], func=AF.Exp, scale=-1.0),
                                 r=[B_bc[i2]], w=[B_en[i2]])
                            yield
                            if lat:
                                P.op("dve", lambda v: v.tensor_tensor(out=Qd[i2][:, 0:n], in0=hq, in1=ep[i2][:, 0:n], op=ALU.mult),
                                     r=[B_zin[i2], B_ep[i2]], w=[B_Qd[i2]])
                                P.op("pool", lambda g: g.tensor_tensor(out=QdA[i2][:, 0:n], in0=Qd[i2][:, 0:n], in1=cmA[:, 0:n], op=ALU.mult),
                                     r=[B_Qd[i2], B_c2], w=[B_QdA[i2]])
                                P.op("pool", lambda g: g.tensor_tensor(out=QdB[i2][:, 0:n], in0=Qd[i2][:, 0:n], in1=cmB[:, 0:n], op=ALU.mult),
                                     r=[B_Qd[i2], B_c2], w=[B_QdB[i2]])
                            P.op("pool", lambda g: g.tensor_tensor(out=kdf[i2][:, 0:n], in0=kk[i2][:, 0:n], in1=en[i2][:, 0:n], op=ALU.mult),
                                 r=[B_kk[i2], B_en[i2]], w=[B_kdf[i2]])
                            if lat:
                                P.op("pool", lambda g: g.tensor_copy(out=Kd[i2][:, 0:n], in_=kdf[i2][:, 0:n]),
                                     r=[B_kdf[i2]], w=[B_Kd[i2]])
                            endcol = 63 if d == 0 else 0
                            P.op("dve", lambda v: v.tensor_copy(out=dec[i2][:, 0:nch],
                                                                in_=ep[i2][:, 0:n].rearrange("p (c j) -> p c j", j=64)[:, :, endcol]),
                                 r=[B_ep[i2]], w=[B_dec[i2]])
                            P.op("dve", lambda v: v.tensor_tensor(
                                out=K2T[i2][:, 0:n].rearrange("p (c j) -> p c j", j=64),
                                in0=kdf[i2][:, 0:n].rearrange("p (c j) -> p c j", j=64),
                                in1=dec[i2][:, 0:nch].unsqueeze(2).to_broadcast([128, nch, 64]), op=ALU.mult),
                                r=[B_kdf[i2], B_dec[i2]], w=[B_K2T[i2]])
                            yield
                            pe_group([(lambda pe, j=j: pe.transpose(out=tpb[:, j * 128:(j + 1) * 128], in_=K2T[i2][:, j * 128:(j + 1) * 128],
                                                                     identity=ident_bf[:])) for j in range(nt)],
                                     r=[B_K2T[i2], B_const], w=[B_tpb])
                            P.op("act", lambda a: a.copy(out=k2all[d][:, 0:nt, :].rearrange("p t c -> p (t c)"), in_=tpb[:, 0:n]),
                                 r=[B_tpb], w=[B_k2all[d]])
                            for gc in range(nch):
                                j, c = gc // 2, gc % 2
                                rows = slice(c * 64, (c + 1) * 64)
                                uo = c * 512 + j * 128
                                mm1(ups2[d][:, uo:uo + 128], k2all[d][rows, j, :], vt[i2][rows, j, :], True, True,
                                    [B_k2all[d], B_vt[i2]], [B_ups2[d][c]])
                            if lat:
                                for j in range(nt):
                                    cs_ = slice(j * 128, (j + 1) * 128)
                                    mm1(scp[:, cs_], Kd[i2][:, cs_], Qd[i2][:, cs_], True, True, [B_Kd[i2], B_Qd[i2]], [B_scp])
                                P.op("dve", lambda v: v.tensor_tensor(
                                    out=scma[d][:], in0=scp[:, :].rearrange("p (t c) -> p t c", c=128),
                                    in1=cm_f[:, 1 + d, :].unsqueeze(1).to_broadcast([128, 4, 128]), op=ALU.mult),
                                    r=[B_scp, B_const], w=[B_scma[d]])
                            yield
                            seq = list(range(nch)) if d == 0 else list(range(nch - 1, -1, -1))
                            for m, gc in enumerate(seq):
                                last = m == nch - 1
                                uo = (gc % 2) * 512 + (gc // 2) * 128
                                if last:
                                    dst, bdst = Scar[1 - cp][:], B_Scar[1 - cp]
                                else:
                                    dst, bdst = Sball[d][:, seq[m + 1], :], B_Sball[d]
                                if lat or last:
                                    P.op("dve", lambda v: v.scalar_tensor_tensor(out=dst, in0=Sst[:], scalar=dec[i2][:, gc:gc + 1],
                                                                                 in1=ups2[d][:, uo:uo + 128],
                                                                                 op0=ALU.mult, op1=ALU.add),
                                         r=[B_S, B_dec[i2], B_ups2[d][gc % 2]], w=[bdst])
                                P.op("dve", lambda v: v.scalar_tensor_tensor(out=Sst[:], in0=Sst[:], scalar=dec[i2][:, gc:gc + 1],
                                                                             in1=ups2[d][:, uo:uo + 128],
                                                                             op0=ALU.mult, op1=ALU.add),
                                     r=[B_S, B_dec[i2], B_ups2[d][gc % 2]], w=[B_S])
                            yield
                            if lat:
                                for j in range(nt):
                                    cs_ = slice(j * 128, (j + 1) * 128)
                                    mm1(ops2[d][:, cs_], scma[d][:, j, :], vt[i2][:, j, :], True, False,
                                        [B_scma[d], B_vt[i2]], [B_ops2[d]])
                                    for c in range(2):
                                        gc = 2 * j + c
                                        qsel = QdA if c == 0 else QdB
                                        bq = B_QdA if c == 0 else B_QdB
                                        if gc == seq[0]:
                                            sap, bs = Scar[cp][:], B_Scar[cp]
                                        else:
                                            sap, bs = Sball[d][:, gc, :], B_Sball[d]
                                        mm1(ops2[d][:, cs_], qsel[i2][:, cs_], sap, False, c == 1, [bq[i2], bs], [B_ops2[d]])
                                gt0 = (k - 1) * 4
                                P.op("act", lambda a: a.copy(out=o_acc[:, gt0:gt0 + 4, :].rearrange("p t c -> p (t c)"), in_=ops2[d][:, :]),
                                     r=[B_ops2[d]], w=[B_oacc[gt0 + jj] for jj in range(4)])
                            cp = 1 - cp
                            yield

                def combine(h):
                    for k in range(1, 17):
                        t0, nt = SCS[k]
                        s0 = t0 * 128
                        i2 = k % 2
                        gt0 = (k - 1) * 4
                        P.dma("sp", lambda q: q.dma_start(
                            out=gt[i2][:, 0:4, :], in_=g_d[s0:s0 + 512, h, :].rearrange("(t p) c -> p t c", p=128)),
                            r=B_g, w=[B_gt[i2]])
                        ro = [B_oaccs[dd][gt0 + jj] for dd in range(2) for jj in range(4)]
                        P.op("dve", lambda v: v.tensor_tensor(out=otot[i2][:], in0=o_accs[0][:, gt0:gt0 + 4, :],
                                                              in1=o_accs[1][:, gt0:gt0 + 4, :], op=ALU.add), r=ro, w=[B_otot[i2]])
                        P.op("dve", lambda v: v.tensor_tensor(out=osq[i2][:], in0=otot[i2][:], in1=otot[i2][:], op=ALU.mult),
                             r=[B_otot[i2]], w=[B_osq[i2]])
                        P.op("dve", lambda v: v.tensor_reduce(out=osm4[i2][:, 0:4], in_=osq[i2][:], axis=AX.X, op=ALU.add),
                             r=[B_osq[i2]], w=[B_osm4[i2]])
                        rstd_from_ss(osm4[i2][:, 0:4], 128, osm4[i2][:, 4:8], osm4[i2][:, 0:4], [B_osm4[i2]], [B_osm4[i2]], B_osm4[i2])
                        P.op("dve", lambda v: v.tensor_tensor(out=otot[i2][:], in0=otot[i2][:],
                                                              in1=osm4[i2][:, 4:8].unsqueeze(2).to_broadcast([128, 4, 128]), op=ALU.mult),
                             r=[B_otot[i2], B_osm4[i2]], w=[B_otot[i2]])
                        P.op("dve", lambda v: v.tensor_tensor(out=otot[i2][:], in0=otot[i2][:],
                                                              in1=hgn[:].unsqueeze(1).to_broadcast([128, 4, 128]), op=ALU.mult),
                             r=[B_otot[i2], B_c2], w=[B_otot[i2]])
                        P.op("pool", lambda g: g.tensor_tensor(out=yb4[i2][:], in0=otot[i2][:], in1=gt[i2][:, 0:4, :], op=ALU.mult),
                             r=[B_otot[i2], B_gt[i2]], w=[B_yb4[i2]])
                        pe_group([(lambda pe, j=j: pe.transpose(out=tpb[:, j * 128:(j + 1) * 128], in_=yb4[i2][:, j, :],
                                                                 identity=ident_bf[:])) for j in range(4)],
                                 r=[B_yb4[i2], B_const], w=[B_tpb])
                        P.op("act", lambda a: a.copy(out=mixs[i2][:], in_=tpb[:, 0:512]), r=[B_tpb], w=[B_mixs[i2]])
                        P.dma("pool", lambda q: q.dma_start(
                            out=mixt_d[512 + h * 128:512 + (h + 1) * 128, (k - 1) * 512:k * 512], in_=mixs[i2][:]),
                            r=[B_mixs[i2]], w=[B_mixt[k - 1]])

                for h in heads:
                    alive = [chain(h, 0), chain(h, 1)]
                    while alive:
                        for g_ in list(alive):
                            try:
                                next(g_)
                            except StopIteration:
                                alive.remove(g_)
                    combine(h)
                P.barrier()

        AFF = sb(es, "AFF", [128, NTILE, NE], F32)
        B_AFF = [Buf() for _ in range(NTILE)]
        B_h2t = [Buf() for _ in range(NTILE)]
        B_afft = [Buf() for _ in range(NTILE)]

        def phase_b():
            with ExitStack() as st:
                wo = sb(st, "wo", [128, 8, D], BF16)
                B_wo = [Buf() for _ in range(4)]
                for pi in range(4):
                    P.dma("pool", lambda q: q.dma_start(out=wo[:, 2 * pi:2 * pi + 2, :], in_=wout_d[:, 2 * pi:2 * pi + 2, :]),
                          w=[B_wo[pi]])
                wr = sb(st, "wr", [128, 8, NE], F32)
                B_wr = Buf()
                P.dma("sp", lambda q: q.dma_start(out=wr[:], in_=wr_d[:]), w=[B_wr])
                gpm, B_gpm = load_bc(st, "gpm", 4)
                g2m, B_g2m = load_bc(st, "g2m", 5)
                sh2, B_sh2 = load_bc(st, "sh2", 6)
                mix = [sb(st, "mix%d" % i, [128, 8, 512], BF16) for i in range(2)]
                B_mix = [Buf(), Buf()]
                xb = [sb(st, "bxb%d" % i, [128, D], F32) for i in range(2)]
                B_xb = [Buf(), Buf()]
                tt = [sb(st, "btt%d" % i, [128, D], F32) for i in range(2)]
                B_tt = [Buf(), Buf()]
                x1 = [sb(st, "bx1%d" % i, [128, D], F32) for i in range(2)]
                B_x1s = [Buf(), Buf()]
                h2f = [sb(st, "h2f%d" % i, [128, D], F32) for i in range(2)]
                B_h2f = [Buf(), Buf()]
                h2b = [sb(st, "h2b%d" % i, [128, D], BF16) for i in range(2)]
                B_h2b = [Buf(), Buf()]
                junk = sb(st, "bjunk", [128, D], BF16)
                B_junk = Buf()
                h2T = [sb(st, "h2T%d" % i, [128, 8, 128], F32) for i in range(2)]
                B_h2T = [Buf(), Buf()]
                sm = sb(st, "bsm", [128, 2, 8], F32)
                B_sm = [Buf(), Buf()]
                ee = sb(st, "bee", [128, 2, NE], F32)
                yps = [ps(st, "yps%d" % i, [128, D]) for i in range(2)]
                B_yps = [Buf(), Buf()]
                trp = ps(st, "trp", [128, D])
                B_trp = Buf()
                lgp = ps(st, "lgp", [128, 512])
                B_lgp = Buf()
                for sc in range(16):
                    m_ = mix[sc % 2]
                    P.dma("sp", lambda q: q.dma_start(out=m_[:], in_=mixt_d[:, sc * 512:(sc + 1) * 512].rearrange("(kc p) t -> p kc t", p=128)),
                          r=[B_mixt[sc]], w=[B_mix[sc % 2]])
                    for j in range(4):
                        tl = sc * 4 + j
                        i2 = tl % 2
                        y_ = yps[i2]
                        for half in range(2):
                            P.mm(y_[:, half * 512:(half + 1) * 512],
                                 [(m_[:, kc, j * 128:(j + 1) * 128], wo[:, kc, half * 512:(half + 1) * 512]) for kc in range(8)],
                                 r=[B_mix[sc % 2]] + B_wo, w=[B_yps[i2]])
                        s_ = sm[:, i2, :]
                        for half in range(2):
                            P.op("act", lambda a: a.activation(out=junk[:, half * 512:(half + 1) * 512], in_=y_[:, half * 512:(half + 1) * 512],
                                                               func=AF.Square, accum_out=s_[:, half:half + 1]),
                                 r=[B_yps[i2]], w=[B_junk, B_sm[i2]])
                        P.op("dve", lambda v: v.tensor_tensor(out=s_[:, 2:3], in0=s_[:, 0:1], in1=s_[:, 1:2], op=ALU.add),
                             r=[B_sm[i2]], w=[B_sm[i2]])
                        rstd_from_ss(s_[:, 2:3], D, s_[:, 3:4], s_[:, 2:3], [B_sm[i2]], [B_sm[i2]], B_sm[i2])
                        P.dma("sp", lambda q: q.dma_start(out=xb[i2][:], in_=x_d[tl * 128:(tl + 1) * 128, :]), w=[B_xb[i2]])
                        for half in range(2):
                            hs = slice(half * 512, (half + 1) * 512)
                            P.op("dve", lambda v: v.scalar_tensor_tensor(out=tt[i2][:, hs], in0=y_[:, hs], scalar=s_[:, 3:4], in1=gpm[:, hs],
                                                                         op0=ALU.mult, op1=ALU.mult),
                                 r=[B_yps[i2], B_sm[i2], B_gpm], w=[B_tt[i2]])
                        P.op("pool", lambda g: g.tensor_tensor(out=x1[i2][:], in0=tt[i2][:], in1=xb[i2][:], op=ALU.add),
                             r=[B_tt[i2], B_xb[i2]], w=[B_x1s[i2]])
                        P.dma("pool", lambda q: q.dma_start(out=x1_d[tl * 128:(tl + 1) * 128, :], in_=x1[i2][:]),
                              r=[B_x1s[i2]], w=[B_x1[tl]])
                        P.op("dve", lambda v: v.scalar_tensor_tensor(out=junk[:], in0=x1[i2][:], scalar=1.0, in1=x1[i2][:],
                                                                     op0=ALU.mult, op1=ALU.mult, accum_out=s_[:, 4:5]),
                             r=[B_x1s[i2]], w=[B_junk, B_sm[i2]])
                        rstd_from_ss(s_[:, 4:5], D, s_[:, 5:6], s_[:, 4:5], [B_sm[i2]], [B_sm[i2]], B_sm[i2])
                        P.op("dve", lambda v: v.scalar_tensor_tensor(out=tt[i2][:], in0=x1[i2][:], scalar=s_[:, 5:6], in1=g2m[:],
                                                                     op0=ALU.mult, op1=ALU.mult),
                             r=[B_x1s[i2], B_sm[i2], B_g2m], w=[B_tt[i2]])
                        P.op("pool", lambda g: g.tensor_tensor(out=h2f[i2][:], in0=tt[i2][:], in1=sh2[:], op=ALU.add),
                             r=[B_tt[i2], B_sh2], w=[B_h2f[i2]])
                        P.op("act", lambda a: a.copy(out=h2b[i2][:], in_=h2f[i2][:]), r=[B_h2f[i2]], w=[B_h2b[i2]])
                        P.dma("pool", lambda q: q.dma_start(out=h2_d[tl * 128:(tl + 1) * 128, :], in_=h2b[i2][:]),
                              r=[B_h2b[i2]], w=[B_h2t[tl]])
                        pe_group([(lambda pe, kc=kc: pe.transpose(out=trp[:, kc * 128:(kc + 1) * 128],
                                                                   in_=h2f[i2][:, kc * 128:(kc + 1) * 128], identity=ident_f))
                                  for kc in range(8)], r=[B_h2f[i2], B_const], w=[B_trp])
                        P.op("act", lambda a: a.copy(out=h2T[i2][:, 0:4, :].rearrange("p k t -> p (k t)"), in_=trp[:, 0:512]),
                             r=[B_trp], w=[B_h2T[i2]])
                        P.op("dve", lambda v: v.tensor_copy(out=h2T[i2][:, 4:8, :].rearrange("p k t -> p (k t)"), in_=trp[:, 512:1024]),
                             r=[B_trp], w=[B_h2T[i2]])
                        P.mm(lgp[:, 0:NE], [(h2T[i2][:, kc, :], wr[:, kc, :]) for kc in range(8)],
                             r=[B_h2T[i2], B_wr], w=[B_lgp])
                        P.op("dve", lambda v: v.tensor_reduce(out=s_[:, 6:7], in_=lgp[:, 0:NE], axis=AX.X, op=ALU.max, negate=True),
                             r=[B_lgp], w=[B_sm[i2]])
                        P.op("act", lambda a: a.activation(out=ee[:, i2, :], in_=lgp[:, 0:NE], func=AF.Exp, bias=s_[:, 6:7],
                                                           accum_out=s_[:, 7:8]), r=[B_lgp, B_sm[i2]], w=[B_sm[i2]])
                        P.op("dve", lambda v: v.reciprocal(out=s_[:, 7:8], in_=s_[:, 7:8]), r=[B_sm[i2]], w=[B_sm[i2]])
                        P.op("dve", lambda v: v.tensor_scalar(out=AFF[:, tl, :], in0=ee[:, i2, :], scalar1=s_[:, 7:8], scalar2=None,
                                                              op0=ALU.mult), r=[B_sm[i2]], w=[B_AFF[tl]])
                        P.dma("pool", lambda q: q.dma_start(out=aff_d[tl * 128:(tl + 1) * 128, :], in_=AFF[:, tl, :]),
                              r=[B_AFF[tl]], w=[B_afft[tl]])
                P.barrier()

        posm = sb(es, "posm", [128, NE, NTILE], F32)
        B_posm = Buf()

        def phase_c():
            with ExitStack() as st:
                lo = sb(st, "c_lo", [128, NE], F32)
                hi = sb(st, "c_hi", [128, NE], F32)
                mid = sb(st, "c_mid", [128, NE], F32)
                ge = sb(st, "c_ge", [128, NTILE, NE], F32)
                cntp = sb(st, "c_cntp", [128, NE], F32)
                mge = sb(st, "c_mge", [128, NE], U32)
                mlt = sb(st, "c_mlt", [128, NE], U32)
                Mt = sb(st, "c_Mt", [128, NE, NTILE], F32)
                Psc = sb(st, "c_Psc", [128, NE, NTILE], F32)
                rmc = sb(st, "c_rmc", [128, 1024], F32)
                Tt = sb(st, "c_Tt", [128, NE], BF16)
                Lbf = sb(st, "c_Lbf", [128, 128], BF16)
                off = sb(st, "c_off", [128, NE], F32)
                cps = ps(st, "c_cps", [128, 512])
                B_lo, B_hi, B_mid, B_ge, B_cntp, B_m, B_cps, B_x = Buf(), Buf(), Buf(), Buf(), Buf(), Buf(), Buf(), Buf()
                P.dma("sp", lambda q: q.dma_start(out=rmc[:], in_=rm_d[:, 0, :]), w=[B_x])
                P.op("dve", lambda v: v.tensor_copy(out=Lbf[:], in_=cm_f[:, 3, :]), r=[B_const], w=[B_x])
                P.op("dve", lambda v: v.memset(lo[:], 0.0), w=[B_lo])
                P.op("dve", lambda v: v.memset(hi[:], 2.0), w=[B_hi])
                for it in range(34):
                    P.op("dve", lambda v: v.tensor_tensor(out=mid[:], in0=lo[:], in1=hi[:], op=ALU.add), r=[B_lo, B_hi], w=[B_mid])
                    P.op("dve", lambda v: v.tensor_scalar(out=mid[:], in0=mid[:], scalar1=0.5, scalar2=None, op0=ALU.mult),
                         r=[B_mid], w=[B_mid])
                    P.op("dve", lambda v: v.tensor_tensor(out=ge[:], in0=AFF[:], in1=mid[:].unsqueeze(1).to_broadcast([128, NTILE, NE]),
                                                          op=ALU.is_ge), r=B_AFF + [B_mid], w=[B_ge])
                    P.op("dve", lambda v: v.tensor_reduce(out=cntp[:], in_=ge[:].rearrange("p i e -> p e i"), axis=AX.X, op=ALU.add),
                         r=[B_ge], w=[B_cntp])
                    P.mm(cps[:, 0:NE], [(ones_f[:], cntp[:])], r=[B_cntp, B_const], w=[B_cps])
                    P.op("dve", lambda v: v.tensor_scalar(out=mge[:], in0=cps[:, 0:NE], scalar1=float(CAP), scalar2=None, op0=ALU.is_ge),
                         r=[B_cps], w=[B_m])
                    P.op("dve", lambda v: v.tensor_scalar(out=mlt[:], in0=cps[:, 0:NE], scalar1=float(CAP), scalar2=None, op0=ALU.is_lt),
                         r=[B_cps], w=[B_m])
                    P.op("dve", lambda v: v.copy_predicated(out=lo[:], mask=mge[:], data=mid[:]), r=[B_m, B_mid], w=[B_lo])
                    P.op("dve", lambda v: v.copy_predicated(out=hi[:], mask=mlt[:], data=mid[:]), r=[B_m, B_mid], w=[B_hi])
                P.op("dve", lambda v: v.tensor_tensor(out=ge[:], in0=AFF[:], in1=lo[:].unsqueeze(1).to_broadcast([128, NTILE, NE]),
                                                      op=ALU.is_ge), r=B_AFF + [B_lo], w=[B_ge])
                P.op("dve", lambda v: v.tensor_copy(out=Mt[:], in_=ge[:].rearrange("p i e -> p e i")), r=[B_ge], w=[B_x])
                P.op("dve", lambda v: v.tensor_tensor_scan(out=Psc[:].rearrange("p e i -> p (e i)"), data0=rmc[:],
                                                           data1=Mt[:].rearrange("p e i -> p (e i)"), initial=0.0,
                                                           op0=ALU.mult, op1=ALU.add), r=[B_x], w=[B_x])
                P.op("dve", lambda v: v.tensor_copy(out=Tt[:], in_=Psc[:, :, NTILE - 1]), r=[B_x], w=[B_x])
                P.mm(cps[:, 0:NE], [(Lbf[:], Tt[:])], r=[B_x], w=[B_cps])
                P.op("dve", lambda v: v.tensor_copy(out=off[:], in_=cps[:, 0:NE]), r=[B_cps], w=[B_x])
                P.op("dve", lambda v: v.tensor_tensor(out=Psc[:], in0=Psc[:], in1=off[:].unsqueeze(2).to_broadcast([128, NE, NTILE]),
                                                      op=ALU.add), r=[B_x], w=[B_x])
                P.op("dve", lambda v: v.tensor_tensor(out=Psc[:], in0=Psc[:], in1=Mt[:], op=ALU.mult), r=[B_x], w=[B_x])
                P.op("dve", lambda v: v.tensor_scalar(out=posm[:], in0=Psc[:], scalar1=-1.0, scalar2=None, op0=ALU.add),
                     r=[B_x], w=[B_posm])
                P.barrier()

        def idma(fn, r, w):
            return P.dma("pool", fn, r=r, w=w)

        def phase_d(experts=range(NE)):
            with ExitStack() as st:
                iota = sb(st, "d_iota", [128, 1024], F32)
                tokf = sb(st, "d_tokf", [128, NTILE, 2], F32)
                tokb = sb(st, "d_tokb", [128, NTILE, 2], BF16)
                zt_ = sb(st, "d_zero", [128, D], F32)
                B_dc = Buf()
                P.dma("sp", lambda q: q.dma_start(out=iota[:], in_=iota_d[:]), w=[B_dc])
                P.dma("sp", lambda q: q.dma_start(out=tokf[:], in_=tokhl_d[:]), w=[B_dc])
                P.op("dve", lambda v: v.tensor_copy(out=tokb[:], in_=tokf[:]), r=[B_dc], w=[B_dc])
                P.op("dve", lambda v: v.memset(zt_[:], 0.0), w=[B_dc])
                fview = f_d.rearrange("(t p) d -> p t d", p=128)
                for part in range(4):
                    P.dma("sp", lambda q: q.dma_start(out=fview[:, part * 16:(part + 1) * 16, :],
                                                      in_=zt_[:].unsqueeze(1).to_broadcast([128, 16, D])), r=[B_dc], w=[B_f])
                sel = [sb(st, "d_sel%d" % i, [128, 1024], BF16) for i in range(4)]
                B_sel = [Buf() for _ in range(4)]
                idxf = sb(st, "d_idxf", [2, 1024], F32)
                idx2 = sb(st, "d_idx2", [128, 8], F32)
                idxi = [sb(st, "d_idxi%d" % i, [128, 8], I32) for i in range(2)]
                B_idxf, B_idx2 = Buf(), Buf()
                B_idxi = [Buf(), Buf()]
                X = [sb(st, "d_X%d" % i, [128, D], BF16) for i in range(16)]
                B_X = [Buf() for _ in range(16)]
                gat = [sb(st, "d_gat%d" % i, [128, 8, NE], F32) for i in range(2)]
                B_gat = [Buf(), Buf()]
                XT = sb(st, "d_XT", [128, 8, 1024], BF16)
                B_XT = Buf()
                AT = sb(st, "d_AT", [128, 8, 1024], BF16)
                B_AT = Buf()
                W = [[sb(st, "d_w%d_%d" % (m, i), [128, 8, D], BF16) for m in range(3)] for i in range(2)]
                B_W = [[[Buf() for _ in range(4)] for _ in range(3)] for _ in range(2)]
                sg = [sb(st, "d_sg%d" % i, [128, 512], F32) for i in range(2)]
                B_sg = [Buf(), Buf()]
                Ysb = [sb(st, "d_Y%d" % i, [128, D], F32) for i in range(2)]
                B_Y = [Buf(), Buf()]
                ips = [ps(st, "d_ips%d" % i, [128, 512]) for i in range(2)]
                B_ips = [Buf(), Buf()]
                tpx = ps(st, "d_tpx", [128, 8, 128], BF16)
                B_tpx = Buf()
                itp = ps(st, "d_itp", [128, 512])
                B_itp = Buf()
                gps = [ps(st, "d_gps%d" % i, [128, 512]) for i in range(2)]
                B_gps = [Buf(), Buf()]
                ups = [ps(st, "d_ups%d" % i, [128, 512]) for i in range(2)]
                B_ups = [Buf(), Buf()]
                wsrc = (wg_d, wu_d, wd_d)

                def load_w(e, slot):
                    for m in range(3):
                        for pi in range(4):
                            P.dma("pool", lambda q: q.dma_start(out=W[slot][m][:, 2 * pi:2 * pi + 2, :],
                                                                in_=wsrc[m][e][:, 2 * pi:2 * pi + 2, :]), w=[B_W[slot][m][pi]])

                elist = list(experts)
                ctr = dict(sel=0, g=0, y=0)

                def compaction(e, slot):
                    for i in range(NTILE):
                        si = ctr["sel"] % 4
                        ctr["sel"] += 1
                        P.op("dve", lambda v: v.tensor_scalar(out=sel[si][:], in0=iota[:], scalar1=posm[:, e, i:i + 1], scalar2=None,
                                                            op0=ALU.is_equal), r=[B_dc, B_posm], w=[B_sel[si]])
                        for half in range(2):
                            mm1(ips[half][0:2, :], tokb[:, i, :], sel[si][:, half * 512:(half + 1) * 512], i == 0, i == NTILE - 1,
                                [B_dc, B_sel[si]], [B_ips[half]])
                        if i % 4 == 3 and i != NTILE - 1:
                            yield
                    for half in range(2):
                        P.op("act", lambda a: a.copy(out=idxf[:, half * 512:(half + 1) * 512], in_=ips[half][0:2, :]),
                             r=[B_ips[half]], w=[B_idxf])
                    pe_group([(lambda pe, jt=jt: pe.transpose(out=itp[:, 2 * jt:2 * jt + 2], in_=idxf[0:2, jt * 128:(jt + 1) * 128],
                                                               identity=ident_f[0:2, 0:2])) for jt in range(8)],
                             r=[B_idxf, B_const], w=[B_itp])
                    P.op("dve", lambda v: v.tensor_reduce(out=idx2[:], in_=itp[:, 0:16].rearrange("p (j t) -> p j t", t=2),
                                                          axis=AX.X, op=ALU.add), r=[B_itp], w=[B_idx2])
                    P.op("dve", lambda v: v.tensor_copy(out=idxi[slot][:], in_=idx2[:]), r=[B_idx2], w=[B_idxi[slot]])
                    yield

                def gather(e, slot):
                    ii = idxi[slot]
                    for jt in range(8):
                        xj = X[slot * 8 + jt]
                        idma(lambda q: q.indirect_dma_start(out=xj[:], out_offset=None, in_=h2_d[:, :],
                                                            in_offset=IndirectOffsetOnAxis(ap=ii[:, jt:jt + 1], axis=0)),
                             r=[B_idxi[slot]] + B_h2t, w=[B_X[slot * 8 + jt]])
                        idma(lambda q: q.indirect_dma_start(out=gat[slot][:, jt, :], out_offset=None, in_=aff_d[:, :],
                                                            in_offset=IndirectOffsetOnAxis(ap=ii[:, jt:jt + 1], axis=0)),
                             r=[B_idxi[slot]] + B_afft, w=[B_gat[slot]])

                load_w(elist[0], 0)
                for _ in compaction(elist[0], 0):
                    pass
                gather(elist[0], 0)
                for ei, e in enumerate(elist):
                    slot = ei % 2
                    ii = idxi[slot]
                    g_ = gat[slot]
                    nxt = None
                    if ei + 1 < len(elist):
                        load_w(elist[ei + 1], 1 - slot)
                        nxt = compaction(elist[ei + 1], 1 - slot)
                    for jt in range(8):
                        xj = X[slot * 8 + jt]
                        pe_group([(lambda pe, kc=kc: pe.transpose(out=tpx[:, kc, :], in_=xj[:, kc * 128:(kc + 1) * 128],
                                                                   identity=ident_bf[:])) for kc in range(8)],
                                 r=[B_X[slot * 8 + jt], B_const], w=[B_tpx])
                        if jt % 2 == 0:
                            P.op("act", lambda a: a.copy(out=XT[:, :, jt * 128:(jt + 1) * 128], in_=tpx[:]), r=[B_tpx], w=[B_XT])
                        else:
                            P.op("dve", lambda v: v.tensor_copy(out=XT[:, :, jt * 128:(jt + 1) * 128], in_=tpx[:]), r=[B_tpx], w=[B_XT])
                    wg_, wu_, wd_ = W[slot]
                    bwg, bwu, bwd = B_W[slot]
                    for fc in range(8):
                        for sh in range(2):
                            gi = ctr["g"] % 2
                            ctr["g"] += 1
                            cs_ = slice(sh * 512, (sh + 1) * 512)
                            P.mm(gps[gi][:], [(wg_[:, kc, fc * 128:(fc + 1) * 128], XT[:, kc, cs_]) for kc in range(8)],
                                 r=[B_XT] + bwg, w=[B_gps[gi]])
                            P.mm(ups[gi][:], [(wu_[:, kc, fc * 128:(fc + 1) * 128], XT[:, kc, cs_]) for kc in range(8)],
                                 r=[B_XT] + bwu, w=[B_ups[gi]])
                            P.op("act", lambda a: a.activation(out=sg[gi][:], in_=gps[gi][:], func=AF.Silu),
                                 r=[B_gps[gi]], w=[B_sg[gi]])
                            P.op("dve", lambda v: v.tensor_tensor(out=AT[:, fc, cs_], in0=ups[gi][:], in1=sg[gi][:], op=ALU.mult),
                                 r=[B_ups[gi], B_sg[gi]], w=[B_AT])
                            if nxt is not None:
                                next(nxt, None)
                    if nxt is not None:
                        for _ in nxt:
                            pass
                        gather(elist[ei + 1], 1 - slot)
                    for jt in range(8):
                        yi = ctr["y"] % 2
                        ctr["y"] += 1
                        for dh in range(2):
                            gi = ctr["g"] % 2
                            ctr["g"] += 1
                            P.mm(gps[gi][:], [(AT[:, fc, jt * 128:(jt + 1) * 128], wd_[:, fc, dh * 512:(dh + 1) * 512]) for fc in range(8)],
                                 r=[B_AT] + bwd, w=[B_gps[gi]])
                            P.op("dve", lambda v: v.tensor_scalar(out=Ysb[yi][:, dh * 512:(dh + 1) * 512], in0=gps[gi][:],
                                                                  scalar1=g_[:, jt, e:e + 1], scalar2=None, op0=ALU.mult),
                                 r=[B_gps[gi], B_gat[slot]], w=[B_Y[yi]])
                        idma(lambda q: q.indirect_dma_start(out=f_d[:, :], out_offset=IndirectOffsetOnAxis(ap=ii[:, jt:jt + 1], axis=0),
                                                            in_=Ysb[yi][:], in_offset=None, compute_op=ALU.add),
                             r=[B_Y[yi], B_idxi[slot]], w=[B_f])
                P.barrier()

        def phase_e():
            with ExitStack() as st:
                gpf, B_gpf = load_bc(st, "gpf", 7)
                fb = [sb(st, "e_f%d" % i, [128, D], F32) for i in range(2)]
                xb = [sb(st, "e_x%d" % i, [128, D], F32) for i in range(2)]
                tb = [sb(st, "e_t%d" % i, [128, D], F32) for i in range(2)]
                ob_ = [sb(st, "e_o%d" % i, [128, D], F32) for i in range(2)]
                junk = sb(st, "e_junk", [128, D], BF16)
                sm = sb(st, "e_sm", [128, 2, 2], F32)
                B_fb, B_xb, B_tb, B_ob, B_sm = [Buf(), Buf()], [Buf(), Buf()], [Buf(), Buf()], [Buf(), Buf()], [Buf(), Buf()]
                B_junk = Buf()
                B_out = [Buf() for _ in range(NTILE)]
                for tl in range(NTILE):
                    i2 = tl % 2
                    rows = slice(tl * 128, (tl + 1) * 128)
                    P.dma("sp", lambda q: q.dma_start(out=fb[i2][:], in_=f_d[rows, :]), r=[B_f], w=[B_fb[i2]])
                    P.dma("sp", lambda q: q.dma_start(out=xb[i2][:], in_=x1_d[rows, :]), r=[B_x1[tl]], w=[B_xb[i2]])
                    P.op("dve", lambda v: v.scalar_tensor_tensor(out=junk[:], in0=fb[i2][:], scalar=1.0, in1=fb[i2][:],
                                                                 op0=ALU.mult, op1=ALU.mult, accum_out=sm[:, i2, 0:1]),
                         r=[B_fb[i2]], w=[B_junk, B_sm[i2]])
                    rstd_from_ss(sm[:, i2, 0:1], D, sm[:, i2, 1:2], sm[:, i2, 0:1], [B_sm[i2]], [B_sm[i2]], B_sm[i2])
                    P.op("dve", lambda v: v.scalar_tensor_tensor(out=tb[i2][:], in0=fb[i2][:], scalar=sm[:, i2, 1:2], in1=gpf[:],
                                                                 op0=ALU.mult, op1=ALU.mult),
                         r=[B_fb[i2], B_sm[i2], B_gpf], w=[B_tb[i2]])
                    P.op("pool", lambda g: g.tensor_tensor(out=ob_[i2][:], in0=tb[i2][:], in1=xb[i2][:], op=ALU.add),
                         r=[B_tb[i2], B_xb[i2]], w=[B_ob[i2]])
                    P.dma("pool", lambda q: q.dma_start(out=out_d[rows, :], in_=ob_[i2][:]), r=[B_ob[i2]], w=[B_out[tl]])
                P.barrier()

        import os
        if stop_after == "0":
            return nc
        phase_a0()
        if stop_after == "A0":
            return nc
        if stop_after == "A1":
            phase_a1(heads=[int(x) for x in os.environ.get("A1_HEADS", "0").split(",")],
                     qchunks=[int(x) for x in os.environ.get("A1_QC", "0,9").split(",")])
            return nc
        if stop_after == "A2":
            phase_a2(heads=[int(x) for x in os.environ.get("A2_HEADS", "0").split(",")])
            return nc
        if not os.environ.get("SKIP_A1"):
            phase_a1()
        phase_a2()
        phase_b()
        if stop_after == "B":
            return nc
        phase_c()
        if stop_after == "C":
            return nc
        phase_d()
        phase_e()
        return nc


def _rope_tables():
    half = 32
    inv_freq = (1.0 / (10000.0 ** (np.arange(0, half, 2, dtype=np.float32) / np.float32(half)))).astype(np.float32)
    t = np.arange(T)
    r = (t // 64).astype(np.float32)
    c = (t % 64).astype(np.float32)
    ang_r = r[:, None] * inv_freq[None, :]
    ang_c = c[:, None] * inv_freq[None, :]
    ang = np.concatenate([ang_r, ang_r, ang_c, ang_c], axis=-1).astype(np.float32)
    cos = np.cos(ang).astype(np.float32)
    sin = np.sin(ang).astype(np.float32)
    sign = np.concatenate([-np.ones(16), np.ones(16), -np.ones(16), np.ones(16)]).astype(np.float32)
    sin = sin * sign[None, :]
    cosT = np.ones((128, S), np.float32)
    sinT = np.zeros((128, S), np.float32)
    cosT[:, TC:] = np.concatenate([cos.T, cos.T], axis=0)
    sinT[:, TC:] = np.concatenate([sin.T, sin.T], axis=0)
    return cosT, sinT


def _win_cols():
    rot = np.concatenate([np.arange(16, 32), np.arange(0, 16), np.arange(48, 64), np.arange(32, 48)])
    fm, tm, tg = [], [], []
    for h in range(NH):
        for off in (0, 512):
            base = off + h * 128
            fm.append(base + np.arange(128))
            fm.append(np.concatenate([base + rot, base + 64 + rot]))
        fm.append(1536 + h * 128 + np.arange(128))
        fm.append(2048 + h * 128 + np.arange(128))
        fm.append(3072 + h * 128 + np.arange(128))
        tm.append(1024 + h * 128 + np.arange(128))
        tm.append(2560 + h * 128 + np.arange(128))
        tg.append(3584 + h * 128 + np.arange(128))
    tm = tm + tg
    return np.concatenate(fm + tm)


def _kc(a):
    n = a.shape[-1]
    return np.ascontiguousarray(a.reshape(8, 128, n).transpose(1, 0, 2))


def prep_inputs(inp, n_cores):
    f = lambda k: np.asarray(inp[k], dtype=np.float32)
    x, c, ctx, c_ctx = f("x"), f("c"), f("ctx"), f("c_ctx")
    cosT, sinT = _rope_tables()
    p = np.arange(128)
    blk = p // 64
    same = blk[:, None] == blk[None, :]
    cm = np.zeros((128, 4, 128), np.float32)
    cm[:, 0, :] = np.eye(128)
    cm[:, 1, :] = same & (p[:, None] <= p[None, :])
    cm[:, 2, :] = same & (p[:, None] >= p[None, :])
    cm[:, 3, :] = p[:, None] < p[None, :]
    j = np.arange(1024)
    rm = np.ones((128, 2, 1024), np.float32)
    rm[:, 0, j % 64 == 0] = 0.0
    rm[:, 1, j % 64 == 63] = 0.0
    iota = np.broadcast_to(j.astype(np.float32), (128, 1024)).copy()
    tt = np.arange(NTILE)[None, :] * 128 + p[:, None]
    tokhl = np.stack([64 * (tt // 64), tt % 64], axis=-1).astype(np.float32)
    hlb = f("hg_lower_bound").reshape(2, 2, 4, 128).transpose(3, 0, 1, 2).reshape(128, 16)
    shared = {
        "w_ada": _kc(f("w_ada")[0]),
        "b_ada": f("b_ada")[0][None, :],
        "norms": np.concatenate([f("norm_pre_mix")[0], f("norm_post_mix")[0], f("norm_pre_ffn")[0],
                                 f("norm_post_ffn")[0]])[None, :],
        "w_in": _kc(f("w_in")[0][:, _win_cols()]),
        "lamv": np.concatenate([f("da_lambda_q1")[0], f("da_lambda_k1")[0], f("da_lambda_q2")[0],
                                f("da_lambda_k2")[0]])[None, :],
        "subln": f("da_subln")[0][:, None],
        "hgn": f("hg_norm")[0][None, :],
        "hlb": np.ascontiguousarray(hlb),
        "w_out": _kc(f("w_out")[0]),
        "w_r": _kc(f("w_router")[0]),
        "w_gate": np.ascontiguousarray(f("w_gate")[0].reshape(NE, 8, 128, D).transpose(0, 2, 1, 3)),
        "w_up": np.ascontiguousarray(f("w_up")[0].reshape(NE, 8, 128, D).transpose(0, 2, 1, 3)),
        "w_down": np.ascontiguousarray(f("w_down")[0].reshape(NE, 8, 128, D).transpose(0, 2, 1, 3)),
        "cosT": cosT, "sinT": sinT, "cmasks": cm, "rmask": rm, "iota": iota, "tokhl": tokhl,
    }
    maps = []
    for i in range(n_cores):
        b = i % 2
        m = dict(shared)
        m["x"] = np.ascontiguousarray(x[b])
        m["ctx"] = np.ascontiguousarray(ctx[b])
        m["cc"] = _kc(np.stack([c[b], c_ctx], axis=1))
        maps.append(m)
    return maps


N_CORES = 2
_NC_CACHE = {}


def kernel(**inputs):
    if "nc" not in _NC_CACHE:
        _NC_CACHE["nc"] = build()
    nc = _NC_CACHE["nc"]
    maps = prep_inputs(inputs, N_CORES)
    res = run_bass_kernel_spmd(nc, maps, core_ids=list(range(N_CORES)))
    out = np.stack([np.asarray(res.results[b]["out"], dtype=np.float32) for b in range(2)], axis=0)
    return out
```

```python
import numpy as np
from contextlib import ExitStack
import concourse.bass as bass
import concourse.mybir as mybir
from concourse.bass import IndirectOffsetOnAxis
from concourse.bass_utils import run_bass_kernel_spmd

F32 = mybir.dt.float32
BF16 = mybir.dt.bfloat16
I32 = mybir.dt.int32
U32 = mybir.dt.uint32
AF = mybir.ActivationFunctionType
ALU = mybir.AluOpType
AX = mybir.AxisListType

D = 1024
T = 8192
TC = 256
S = T + TC
NH = 4
NE = 16
CAP = 1024
EPS = 1e-6
NTILE = T // 128
FM_BLOCKS = 7
TM_COLS = 384
HEAD_COLS = FM_BLOCKS * 128 + TM_COLS
FM_TOTAL = NH * FM_BLOCKS * 128
WCOLS = NH * HEAD_COLS


class Buf:
    __slots__ = ("w", "r")

    def __init__(self):
        self.w = None
        self.r = {}


class Prog:
    def __init__(self, nc, es, ndma=12):
        self.nc = nc
        self.eng = dict(pe=nc.tensor, act=nc.scalar, dve=nc.vector, pool=nc.gpsimd, sp=nc.sync)
        self.sem = {k: es.enter_context(nc.semaphore("s_" + k)) for k in self.eng}
        self.cnt = {k: 0 for k in self.eng}
        self.waited = {k: {} for k in self.eng}
        self.dsem = {q: [[es.enter_context(nc.semaphore("d_%s%d" % (q, i))), 0] for i in range(ndma)]
                     for q in ("sp", "pool")}
        self.dnext = {"sp": 0, "pool": 0}
        self.nwait = 0
        self.nops = 0
        import os
        self.limit = int(os.environ.get('OPLIMIT', '1000000000'))

    def _wait(self, e, ev):
        s, v = ev
        w = self.waited[e]
        if w.get(s.num, 0) < v:
            self.eng[e].wait_ge(s, v)
            w[s.num] = v
            self.nwait += 1

    def _deps(self, e, reads, writes):
        own = self.sem[e].num
        for b in reads:
            if b.w is not None:
                if not (e == "pe" and b.w[0].num == own):
                    self._wait(e, b.w)
        for b in writes:
            if b.w is not None:
                if not (e == "pe" and b.w[0].num == own):
                    self._wait(e, b.w)
            for ev in b.r.values():
                if ev[0].num == own:
                    continue
                self._wait(e, ev)

    def _mark(self, ev, reads, writes):
        k = ev[0].num
        for b in reads:
            old = b.r.get(k)
            if old is None or old[1] < ev[1]:
                b.r[k] = ev
        for b in writes:
            b.w = ev
            b.r = {}

    def op(self, e, fn, r=(), w=()):
        self.nops += 1
        if self.nops > self.limit:
            return None
        if self.nops == self.limit:
            print('LAST OP', e, fn.__code__.co_firstlineno)
        self._deps(e, r, w)
        ins = fn(self.eng[e])
        self.cnt[e] += 1
        ins.then_inc(self.sem[e], 1)
        ev = (self.sem[e], self.cnt[e])
        self._mark(ev, r, w)
        return ev

    def mm(self, out_ap, pairs, r=(), w=()):
        self.nops += 1
        if self.nops > self.limit:
            return None
        self._deps("pe", r, w)
        n = len(pairs)
        ins = None
        for i, (l, rh) in enumerate(pairs):
            ins = self.nc.tensor.matmul(out_ap, lhsT=l, rhs=rh, start=(i == 0), stop=(i == n - 1))
        self.cnt["pe"] += 1
        ins.then_inc(self.sem["pe"], 1)
        ev = (self.sem["pe"], self.cnt["pe"])
        self._mark(ev, r, w)
        return ev

    def dma(self, q, fn, r=(), w=()):
        self.nops += 1
        if self.nops > self.limit:
            return None
        slots = self.dsem[q]
        i = self.dnext[q]
        self.dnext[q] = (i + 1) % len(slots)
        s, v = slots[i]
        if v > 0:
            self._wait(q, (s, v))
        self._deps(q, r, w)
        ins = fn(self.eng[q])
        slots[i][1] = v + 16
        ins.then_inc(s, 16)
        ev = (s, v + 16)
        self._mark(ev, r, w)
        return ev

    def all_events(self):
        evs = [(self.sem[k], self.cnt[k]) for k in self.eng if self.cnt[k] > 0]
        for q in self.dsem:
            for s, v in self.dsem[q]:
                if v > 0:
                    evs.append((s, v))
        return evs

    def barrier(self, engines=None):
        evs = self.all_events()
        for e in (engines or self.eng):
            for ev in evs:
                if ev[0].num != self.sem[e].num:
                    self._wait(e, ev)


def build(stop_after=None, dbg=()):
    nc = bass.Bass("TRN2", target_bir_lowering=False)
    dbg = set(dbg)

    def din(name, shape, dt=F32):
        return nc.dram_tensor(name, list(shape), dt, kind="ExternalInput").ap()

    def dscr(name, shape, dt):
        kind = "ExternalOutput" if name in dbg else "Internal"
        return nc.dram_tensor(name, list(shape), dt, kind=kind).ap()

    x_d = din("x", [T, D])
    ctx_d = din("ctx", [TC, D])
    cc_d = din("cc", [128, 8, 2])
    wada_d = din("w_ada", [128, 8, 6 * D])
    bada_d = din("b_ada", [1, 6 * D])
    norms_d = din("norms", [1, 4 * D])
    win_d = din("w_in", [128, 8, WCOLS])
    lamv_d = din("lamv", [1, 256])
    subln_d = din("subln", [128, 1])
    hgn_d = din("hgn", [1, 128])
    hlb_d = din("hlb", [128, 16])
    wout_d = din("w_out", [128, 8, D])
    wr_d = din("w_r", [128, 8, NE])
    wg_d = din("w_gate", [NE, 128, 8, D])
    wu_d = din("w_up", [NE, 128, 8, D])
    wd_d = din("w_down", [NE, 128, 8, D])
    cos_d = din("cosT", [128, S])
    sin_d = din("sinT", [128, S])
    cm_d = din("cmasks", [128, 4, 128])
    rm_d = din("rmask", [128, 2, 1024])
    iota_d = din("iota", [128, 1024])
    tokhl_d = din("tokhl", [128, NTILE, 2])
    out_d = nc.dram_tensor("out", [T, D], F32, kind="ExternalOutput").ap()

    modrows_d = dscr("modrows", [8, D], F32)
    qkt_d = dscr("qkt", [NH, 2, 128, S], BF16)
    zt_d = dscr("zt", [NH, 3, 128, S], F32)
    vi_d = dscr("vi", [S, NH, 2, 128], BF16)
    g_d = dscr("gsil", [S, NH, 128], F32)
    mixt_d = dscr("mixt", [D, T], BF16)
    x1_d = dscr("x1", [T, D], F32)
    h2_d = dscr("h2", [T, D], BF16)
    aff_d = dscr("aff", [T, NE], F32)
    f_d = dscr("facc", [T, D], F32)

    B_modrows = Buf()
    B_qkt = [[Buf() for _ in range(17)] for _ in range(NH)]
    B_zt = [[Buf() for _ in range(17)] for _ in range(NH)]
    B_vi = [Buf() for _ in range(17)]
    B_g = [Buf() for _ in range(17)]
    B_mixt = [Buf() for _ in range(16)]
    B_x1 = [Buf() for _ in range(NTILE)]
    B_h2 = Buf()
    B_aff = Buf()
    B_f = Buf()

    es = ExitStack()
    with es:
        P = Prog(nc, es)

        def sb(stack, name, shape, dt):
            return stack.enter_context(nc.sbuf_tensor("sb_" + name, list(shape), dt))

        def ps(stack, name, shape, dt=F32):
            return stack.enter_context(nc.psum_tensor("ps_" + name, list(shape), dt))

        cm_f = sb(es, "cm_f", [128, 4, 128], F32)
        ident_bf = sb(es, "ident_bf", [128, 128], BF16)
        ones_bf = sb(es, "ones_bf", [128, 128], BF16)
        ones_f = sb(es, "ones_f", [128, 128], F32)
        neglam = sb(es, "neglam", [128, 1], F32)
        subln8 = sb(es, "subln8", [128, 1], F32)
        lbt = sb(es, "lbt", [128, 8], F32)
        omlt = sb(es, "omlt", [128, 8], F32)
        mhalf = sb(es, "mhalf", [128, 512], F32)
        epsb = sb(es, "epsb", [128, 1], F32)
        B_const = Buf()
        ident_f = cm_f[:, 0, :]

        P.dma("sp", lambda q: q.dma_start(out=cm_f[:], in_=cm_d[:]), w=[B_const])
        P.op("dve", lambda v: v.tensor_copy(out=ident_bf[:], in_=cm_f[:, 0, :]), r=[B_const], w=[B_const])
        P.op("dve", lambda v: v.memset(ones_bf[:], 1.0), w=[B_const])
        P.op("dve", lambda v: v.memset(ones_f[:], 1.0), w=[B_const])
        P.op("dve", lambda v: v.memset(mhalf[:], -0.5), w=[B_const])
        P.op("dve", lambda v: v.memset(epsb[:], EPS), w=[B_const])

        def rstd_from_ss(ss_ap, n, out_ap, tmp_ap, bufs_r, bufs_w, tmpbuf):
            P.op("dve", lambda v: v.tensor_scalar(out=tmp_ap, in0=ss_ap, scalar1=1.0 / n, scalar2=EPS,
                                                  op0=ALU.mult, op1=ALU.add), r=bufs_r, w=[tmpbuf])
            shp = list(tmp_ap.shape)
            P.op("pool", lambda g: g.tensor_tensor(out=out_ap, in0=tmp_ap, in1=mhalf[0:shp[0], 0:shp[1]],
                                                   op=ALU.pow), r=[tmpbuf, B_const], w=bufs_w)

        with ExitStack() as p0:
            scf = sb(p0, "scf", [128, 8, 2], F32)
            sct = sb(p0, "sct", [128, 8, 2], F32)
            wa = [sb(p0, "wa%d" % i, [128, 8, 512], F32) for i in range(2)]
            modl = sb(p0, "modl", [1, 6 * D], F32)
            modc = sb(p0, "modc", [1, 6 * D], F32)
            bada = sb(p0, "bada", [1, 6 * D], F32)
            nrm = sb(p0, "nrm", [1, 4 * D], F32)
            rows = sb(p0, "rows", [1, 8, D], F32)
            lamv = sb(p0, "lamv", [1, 256], F32)
            lamt = sb(p0, "lamt", [1, 8], F32)
            hlb = sb(p0, "hlb", [128, 16], F32)
            sl = sb(p0, "sl", [128, 1], F32)
            pm = [ps(p0, "pm%d" % i, [1, 512]) for i in range(4)]
            pl = ps(p0, "pl", [128, 1])
            B_sc, B_wa, B_modl, B_modc, B_bada, B_nrm, B_rows, B_lam, B_hlb = (
                Buf(), [Buf(), Buf()], Buf(), Buf(), Buf(), Buf(), Buf(), Buf(), Buf())
            B_pm = [Buf() for _ in range(4)]
            B_pl = Buf()

            P.dma("sp", lambda q: q.dma_start(out=scf[:], in_=cc_d[:]), w=[B_sc])
            P.dma("sp", lambda q: q.dma_start(out=bada[:], in_=bada_d[:]), w=[B_bada])
            P.dma("sp", lambda q: q.dma_start(out=nrm[:], in_=norms_d[:]), w=[B_nrm])
            P.dma("sp", lambda q: q.dma_start(out=lamv[:], in_=lamv_d[:]), w=[B_lam])
            P.dma("sp", lambda q: q.dma_start(out=hlb[:], in_=hlb_d[:]), w=[B_hlb])
            P.dma("sp", lambda q: q.dma_start(out=sl[:], in_=subln_d[:]), w=[B_hlb])
            P.op("act", lambda a: a.activation(out=sct[:], in_=scf[:], func=AF.Exp, scale=-1.0), r=[B_sc], w=[B_rows])
            P.op("dve", lambda v: v.tensor_scalar(out=sct[:], in0=sct[:], scalar1=1.0, scalar2=None, op0=ALU.add),
                 r=[B_rows], w=[B_rows])
            P.op("dve", lambda v: v.reciprocal(out=sct[:], in_=sct[:]), r=[B_rows], w=[B_rows])
            P.op("dve", lambda v: v.tensor_tensor(out=scf[:], in0=scf[:], in1=sct[:], op=ALU.mult),
                 r=[B_rows, B_sc], w=[B_sc])
            for ch in range(12):
                wb_ = wa[ch % 2]
                P.dma("sp", lambda q: q.dma_start(out=wb_[:], in_=wada_d[:, :, ch * 512:(ch + 1) * 512]),
                      w=[B_wa[ch % 2]])
                for j, (mod, bm) in enumerate(((modl, B_modl), (modc, B_modc))):
                    pmt = pm[(2 * ch + j) % 4]
                    bp = B_pm[(2 * ch + j) % 4]
                    P.mm(pmt[:], [(scf[:, kc, j:j + 1], wb_[:, kc, :]) for kc in range(8)],
                         r=[B_sc, B_wa[ch % 2]], w=[bp])
                    P.op("dve", lambda v: v.tensor_tensor(out=mod[:, ch * 512:(ch + 1) * 512], in0=pmt[:],
                                                          in1=bada[:, ch * 512:(ch + 1) * 512], op=ALU.add),
                         r=[bp, B_bada], w=[bm])
            def stt(dst, a, b, op0):
                P.op("dve", lambda v: v.scalar_tensor_tensor(out=rows[:, dst, :], in0=a, scalar=(1.0 if op0 == ALU.add else 1.0),
                                                             in1=b, op0=op0, op1=ALU.mult),
                     r=[B_modl, B_modc, B_nrm], w=[B_rows])
            stt(0, modl[:, D:2 * D], nrm[:, 0:D], ALU.add)
            P.op("dve", lambda v: v.tensor_copy(out=rows[:, 1, :], in_=modl[:, 0:D]), r=[B_modl], w=[B_rows])
            stt(2, modc[:, D:2 * D], nrm[:, 0:D], ALU.add)
            P.op("dve", lambda v: v.tensor_copy(out=rows[:, 3, :], in_=modc[:, 0:D]), r=[B_modc], w=[B_rows])
            stt(4, modl[:, 2 * D:3 * D], nrm[:, D:2 * D], ALU.mult)
            stt(5, modl[:, 4 * D:5 * D], nrm[:, 2 * D:3 * D], ALU.add)
            P.op("dve", lambda v: v.tensor_copy(out=rows[:, 6, :], in_=modl[:, 3 * D:4 * D]), r=[B_modl], w=[B_rows])
            stt(7, modl[:, 5 * D:6 * D], nrm[:, 3 * D:4 * D], ALU.mult)
            P.dma("pool", lambda q: q.dma_start(out=modrows_d[:, :].rearrange("(o r) d -> o r d", o=1), in_=rows[:]),
                  r=[B_rows], w=[B_modrows])
            P.op("dve", lambda v: v.tensor_tensor(out=lamv[:, 0:64], in0=lamv[:, 0:64], in1=lamv[:, 64:128], op=ALU.mult),
                 r=[B_lam], w=[B_lam])
            P.op("dve", lambda v: v.tensor_tensor(out=lamv[:, 128:192], in0=lamv[:, 128:192], in1=lamv[:, 192:256], op=ALU.mult),
                 r=[B_lam], w=[B_lam])
            P.op("dve", lambda v: v.tensor_reduce(out=lamt[:, 0:1], in_=lamv[:, 0:64], axis=AX.X, op=ALU.add),
                 r=[B_lam], w=[B_lam])
            P.op("dve", lambda v: v.tensor_reduce(out=lamt[:, 1:2], in_=lamv[:, 128:192], axis=AX.X, op=ALU.add),
                 r=[B_lam], w=[B_lam])
            P.op("act", lambda a: a.activation(out=lamt[:, 2:4], in_=lamt[:, 0:2], func=AF.Exp), r=[B_lam], w=[B_lam])
            P.op("dve", lambda v: v.scalar_tensor_tensor(out=lamt[:, 4:5], in0=lamt[:, 3:4], scalar=-0.2, in1=lamt[:, 2:3],
                                                         op0=ALU.add, op1=ALU.subtract), r=[B_lam], w=[B_lam])
            P.mm(pl[:], [(ones_f[0:1, :], lamt[0:1, 4:5])], r=[B_lam, B_const], w=[B_pl])
            P.op("dve", lambda v: v.tensor_copy(out=neglam[:], in_=pl[:]), r=[B_pl], w=[B_const])
            P.op("dve", lambda v: v.tensor_scalar(out=subln8[:], in0=sl[:], scalar1=0.8, scalar2=None, op0=ALU.mult),
                 r=[B_hlb], w=[B_const])
            P.op("dve", lambda v: v.tensor_tensor(out=hlb[:, 0:8], in0=hlb[:, 8:16], in1=hlb[:, 0:8], op=ALU.subtract),
                 r=[B_hlb], w=[B_hlb])
            P.op("act", lambda a: a.activation(out=hlb[:, 0:8], in_=hlb[:, 0:8], func=AF.Exp), r=[B_hlb], w=[B_hlb])
            P.op("dve", lambda v: v.tensor_scalar(out=hlb[:, 0:8], in0=hlb[:, 0:8], scalar1=1.0, scalar2=None, op0=ALU.add),
                 r=[B_hlb], w=[B_hlb])
            P.op("dve", lambda v: v.reciprocal(out=lbt[:], in_=hlb[:, 0:8]), r=[B_hlb], w=[B_const])
            P.op("dve", lambda v: v.tensor_scalar(out=omlt[:], in0=lbt[:], scalar1=-1.0, scalar2=1.0, op0=ALU.mult, op1=ALU.add),
                 r=[B_const], w=[B_const])
            P.barrier()

        def load_bc(stack, name, row):
            t = sb(stack, name, [128, D], F32)
            b = Buf()
            P.dma("sp", lambda q: q.dma_start(out=t[:], in_=modrows_d[row:row + 1, :].partition_broadcast(128)),
                  r=[B_modrows], w=[b])
            return t, b

        def pe_group(fns, r, w):
            P._deps("pe", r, w)
            ins = None
            for fn in fns:
                ins = fn(nc.tensor)
            P.cnt["pe"] += 1
            ins.then_inc(P.sem["pe"], 1)
            ev = (P.sem["pe"], P.cnt["pe"])
            P._mark(ev, r, w)
            return ev

        SCS = [(0, 2)] + [(2 + 4 * k, 4) for k in range(16)]

        def phase_a0():
            with ExitStack() as st:
                wbf = sb(st, "wbf", [128, 8, WCOLS], BF16)
                B_w = [[Buf() for _ in range(4)] for _ in range(8)]
                for kc in range(8):
                    for pi in range(4):
                        c0 = pi * 1280
                        P.dma("pool", lambda q: q.dma_start(out=wbf[:, kc, c0:c0 + 1280], in_=win_d[:, kc, c0:c0 + 1280]),
                              w=[B_w[kc][pi]])
                B_wall = [b for l in B_w for b in l]
                gm_l, B_gml = load_bc(st, "gm_l", 0)
                sh_l, B_shl = load_bc(st, "sh_l", 1)
                gm_c, B_gmc = load_bc(st, "gm_c", 2)
                sh_c, B_shc = load_bc(st, "sh_c", 3)
                xb = [sb(st, "xb%d" % i, [128, D], F32) for i in range(2)]
                B_xb = [Buf(), Buf()]
                junk = sb(st, "junk", [128, D], BF16)
                B_junk = Buf()
                hn = [sb(st, "hn%d" % i, [128, D], F32) for i in range(2)]
                B_hn = [Buf(), Buf()]
                hb = [sb(st, "hb%d" % i, [128, D], BF16) for i in range(4)]
                B_hb = [Buf() for _ in range(4)]
                ssm = sb(st, "ssm", [128, 8], F32)
                B_ss = [Buf() for _ in range(4)]
                hT = [sb(st, "hT%d" % i, [128, 8, 512], BF16) for i in range(2)]
                B_hT = [Buf(), Buf()]
                cs = [sb(st, "cs%d" % i, [128, 2, 512], F32) for i in range(2)]
                B_cs = [Buf(), Buf()]
                qks = [sb(st, "qks%d" % i, [128, 2, 512], BF16) for i in range(2)]
                B_qks = [Buf(), Buf()]
                zs = [sb(st, "zs%d" % i, [128, 3, 512], F32) for i in range(2)]
                B_zs = [Buf(), Buf()]
                t1 = sb(st, "t1", [128, 512], F32)
                t2 = sb(st, "t2", [128, 512], F32)
                B_t1, B_t2 = Buf(), Buf()
                vis = [sb(st, "vis%d" % i, [128, NH, 2, 128], BF16) for i in range(2)]
                B_vis = [Buf(), Buf()]
                gs = [sb(st, "gs%d" % i, [128, NH, 128], F32) for i in range(2)]
                B_gs = [Buf(), Buf()]
                tp = [ps(st, "tp%d" % i, [128, 8, 128], BF16) for i in range(2)]
                B_tp = [Buf(), Buf()]
                fm = [ps(st, "fm%d" % i, [128, 512]) for i in range(3)]
                B_fm = [Buf() for _ in range(3)]
                tm = ps(st, "tm", [128, 1536])
                B_tm = Buf()
                fmi = [0]
                tile_ctr = [0]

                def stage1(k):
                    t0, nt = SCS[k]
                    for j in range(nt):
                        i = tile_ctr[0]
                        tile_ctr[0] += 1
                        x_ = xb[i % 2]
                        st_ = t0 + j
                        src = ctx_d[st_ * 128:(st_ + 1) * 128, :] if k == 0 else x_d[(st_ - 2) * 128:(st_ - 1) * 128, :]
                        gm, bgm, sh, bsh = (gm_c, B_gmc, sh_c, B_shc) if k == 0 else (gm_l, B_gml, sh_l, B_shl)
                        P.dma("sp", lambda q: q.dma_start(out=x_[:], in_=src), w=[B_xb[i % 2]])
                        ss = ssm[:, 2 * j:2 * j + 1]
                        rs = ssm[:, 2 * j + 1:2 * j + 2]
                        P.op("dve", lambda v: v.scalar_tensor_tensor(out=junk[:], in0=x_[:], scalar=1.0, in1=x_[:],
                                                                     op0=ALU.mult, op1=ALU.mult, accum_out=ss),
                             r=[B_xb[i % 2]], w=[B_junk, B_ss[j]])
                        rstd_from_ss(ss, D, rs, ss, [B_ss[j]], [B_ss[j]], B_ss[j])
                        h_ = hn[i % 2]
                        P.op("dve", lambda v: v.scalar_tensor_tensor(out=h_[:], in0=x_[:], scalar=rs, in1=gm[:],
                                                                     op0=ALU.mult, op1=ALU.mult),
                             r=[B_xb[i % 2], B_ss[j], bgm], w=[B_hn[i % 2]])
                        P.op("pool", lambda g: g.tensor_tensor(out=hb[j][:], in0=h_[:], in1=sh[:], op=ALU.add),
                             r=[B_hn[i % 2], bsh], w=[B_hb[j]])

                def stage2(k):
                    t0, nt = SCS[k]
                    for j in range(nt):
                        tpp = tp[j % 2]
                        pe_group([(lambda pe, kc=kc: pe.transpose(out=tpp[:, kc, :], in_=hb[j][:, kc * 128:(kc + 1) * 128],
                                                                   identity=ident_bf[:])) for kc in range(8)],
                                 r=[B_hb[j], B_const], w=[B_tp[j % 2]])
                        P.op("act", lambda a: a.copy(out=hT[k % 2][:, :, j * 128:(j + 1) * 128], in_=tpp[:]),
                             r=[B_tp[j % 2]], w=[B_hT[k % 2]])

                def fm_mm(k, col0, n):
                    bi = fmi[0] % 3
                    fmi[0] += 1
                    P.mm(fm[bi][:, 0:n], [(wbf[:, kc, col0:col0 + 128], hT[k % 2][:, kc, 0:n]) for kc in range(8)],
                         r=[B_hT[k % 2]] + B_wall, w=[B_fm[bi]])
                    return bi

                def stage3(k):
                    t0, nt = SCS[k]
                    n = nt * 128
                    s0 = t0 * 128
                    c_ = cs[k % 2]
                    P.dma("sp", lambda q: q.dma_start(out=c_[:, 0, 0:n], in_=cos_d[:, s0:s0 + n]), w=[B_cs[k % 2]])
                    P.dma("sp", lambda q: q.dma_start(out=c_[:, 1, 0:n], in_=sin_d[:, s0:s0 + n]), w=[B_cs[k % 2]])
                    for h in range(NH):
                        hi = k * NH + h
                        qk_ = qks[hi % 2]
                        z_ = zs[hi % 2]
                        base = h * FM_BLOCKS * 128
                        for t in range(2):
                            b0 = fm_mm(k, base + (2 * t) * 128, n)
                            b1 = fm_mm(k, base + (2 * t + 1) * 128, n)
                            P.op("dve", lambda v: v.tensor_tensor(out=t1[:, 0:n], in0=fm[b0][:, 0:n], in1=c_[:, 0, 0:n], op=ALU.mult),
                                 r=[B_fm[b0], B_cs[k % 2]], w=[B_t1])
                            P.op("dve", lambda v: v.tensor_tensor(out=t2[:, 0:n], in0=fm[b1][:, 0:n], in1=c_[:, 1, 0:n], op=ALU.mult),
                                 r=[B_fm[b1], B_cs[k % 2]], w=[B_t2])
                            P.op("pool", lambda g: g.tensor_tensor(out=qk_[:, t, 0:n], in0=t1[:, 0:n], in1=t2[:, 0:n], op=ALU.add),
                                 r=[B_t1, B_t2], w=[B_qks[hi % 2]])
                        for t in range(3):
                            b0 = fm_mm(k, base + (4 + t) * 128, n)
                            P.op("act", lambda a: a.copy(out=z_[:, t, 0:n], in_=fm[b0][:, 0:n]), r=[B_fm[b0]], w=[B_zs[hi % 2]])
                        P.dma("pool", lambda q: q.dma_start(out=qkt_d[h].rearrange("t p s -> p t s")[:, :, s0:s0 + n],
                                                            in_=qk_[:, :, 0:n]), r=[B_qks[hi % 2]], w=[B_qkt[h][k]])
                        P.dma("pool", lambda q: q.dma_start(out=zt_d[h].rearrange("t p s -> p t s")[:, :, s0:s0 + n],
                                                            in_=z_[:, :, 0:n]), r=[B_zs[hi % 2]], w=[B_zt[h][k]])
                    for j in range(nt):
                        ti = t0 + j
                        for nb in range(3):
                            P.mm(tm[:, nb * 512:(nb + 1) * 512],
                                 [(hT[k % 2][:, kc, j * 128:(j + 1) * 128], wbf[:, kc, FM_TOTAL + nb * 512:FM_TOTAL + (nb + 1) * 512])
                                  for kc in range(8)], r=[B_hT[k % 2]] + B_wall, w=[B_tm])
                        v_ = vis[ti % 2]
                        g_ = gs[ti % 2]
                        vflat = v_[:].rearrange("p h t c -> p (h t c)")
                        P.op("dve", lambda v: v.tensor_copy(out=vflat[:, 0:512], in_=tm[:, 0:512]),
                             r=[B_tm], w=[B_vis[ti % 2]])
                        P.op("dve", lambda v: v.tensor_copy(out=vflat[:, 512:1024], in_=tm[:, 512:1024]),
                             r=[B_tm], w=[B_vis[ti % 2]])
                        P.op("act", lambda a: a.activation(out=g_[:].rearrange("p h c -> p (h c)"), in_=tm[:, 1024:1536], func=AF.Silu),
                             r=[B_tm], w=[B_gs[ti % 2]])
                        P.dma("pool", lambda q: q.dma_start(out=vi_d[ti * 128:(ti + 1) * 128], in_=v_[:]),
                              r=[B_vis[ti % 2]], w=[B_vi[k]])
                        P.dma("pool", lambda q: q.dma_start(out=g_d[ti * 128:(ti + 1) * 128], in_=g_[:]),
                              r=[B_gs[ti % 2]], w=[B_g[k]])

                stage1(0)
                stage2(0)
                for k in range(17):
                    if k + 1 < 17:
                        stage1(k + 1)
                    stage3(k)
                    if k + 1 < 17:
                        stage2(k + 1)
                P.barrier()

        def mm1(out_ap, lhsT, rhs, start, stop, r, w):
            P.nops += 1
            if P.nops > P.limit:
                return None
            P._deps("pe", r, w)
            ins = nc.tensor.matmul(out_ap, lhsT=lhsT, rhs=rhs, start=start, stop=stop)
            P.cnt["pe"] += 1
            ins.then_inc(P.sem["pe"], 1)
            ev = (P.sem["pe"], P.cnt["pe"])
            P._mark(ev, r, w)
            return ev

        NKT = S // 128

        def phase_a1(heads=range(NH), qchunks=range(16)):
            with ExitStack() as st:
                kt = [sb(st, "kt%d" % i, [128, S], BF16) for i in range(2)]
                vv = [sb(st, "vv%d" % i, [128, NKT, 128], BF16) for i in range(2)]
                B_kt = [Buf(), Buf()]
                B_vv = [Buf(), Buf()]
                qt = [sb(st, "qt%d" % i, [128, 512], BF16) for i in range(2)]
                B_qt = [Buf(), Buf()]
                p1 = [sb(st, "p1_%d" % i, [128, 512], BF16) for i in range(3)]
                p2 = [sb(st, "p2_%d" % i, [128, 512], BF16) for i in range(3)]
                B_p1 = [Buf() for _ in range(3)]
                B_p2 = [Buf() for _ in range(3)]
                acc1s = [sb(st, "acc1_%d" % i, [128, 512], F32) for i in range(2)]
                acc2s = [sb(st, "acc2_%d" % i, [128, 512], F32) for i in range(2)]
                B_acc1s, B_acc2s = [Buf(), Buf()], [Buf(), Buf()]
                o1c = sb(st, "o1c", [128, 512], F32)
                o2c = sb(st, "o2c", [128, 512], F32)
                B_o1c, B_o2c = Buf(), Buf()
                r1 = sb(st, "r1", [128, 512], F32)
                o1 = sb(st, "o1", [128, 512], F32)
                r2 = sb(st, "r2", [128, 512], F32)
                o2 = sb(st, "o2", [128, 512], F32)
                oo = sb(st, "oo", [128, 512], F32)
                sq = sb(st, "sq", [128, 512], F32)
                rs = sb(st, "rs", [128, 512], F32)
                ob = [sb(st, "ob%d" % i, [128, 512], BF16) for i in range(2)]
                B_r1, B_o1, B_r2, B_o2, B_oo, B_sq, B_rs = Buf(), Buf(), Buf(), Buf(), Buf(), Buf(), Buf()
                B_ob = [Buf(), Buf()]
                s1 = [ps(st, "s1_%d" % i, [128, 512]) for i in range(2)]
                s2 = [ps(st, "s2_%d" % i, [128, 512]) for i in range(2)]
                B_s1 = [Buf(), Buf()]
                B_s2 = [Buf(), Buf()]
                o1p = ps(st, "o1p", [128, 512])
                o2p = ps(st, "o2p", [128, 512])
                l1p = ps(st, "l1p", [128, 512])
                l2p = ps(st, "l2p", [128, 512])
                B_o1p, B_o2p, B_l1p, B_l2p = Buf(), Buf(), Buf(), Buf()
                def finalize(h, qc, acc1, acc2, B_acc1, B_acc2, o_, bo):
                    P.mm(l1p[:], [(ones_f[:], acc1[:])], r=[B_acc1, B_const], w=[B_l1p])
                    P.mm(l2p[:], [(ones_f[:], acc2[:])], r=[B_acc2, B_const], w=[B_l2p])
                    P.op("act", lambda a: a.activation(out=r1[:], in_=l1p[:], func=AF.Ln), r=[B_l1p], w=[B_r1])
                    P.op("act", lambda a: a.activation(out=r1[:], in_=r1[:], func=AF.Exp, scale=-1.0), r=[B_r1], w=[B_r1])
                    P.op("dve", lambda v: v.tensor_tensor(out=o1[:], in0=o1c[:], in1=r1[:], op=ALU.mult),
                         r=[B_o1c, B_r1], w=[B_o1])
                    P.op("act", lambda a: a.activation(out=r2[:], in_=l2p[:], func=AF.Ln), r=[B_l2p], w=[B_r2])
                    P.op("act", lambda a: a.activation(out=r2[:], in_=r2[:], func=AF.Exp, scale=-1.0), r=[B_r2], w=[B_r2])
                    P.op("dve", lambda v: v.tensor_tensor(out=o2[:], in0=o2c[:], in1=r2[:], op=ALU.mult),
                         r=[B_o2c, B_r2], w=[B_o2])
                    P.op("dve", lambda v: v.scalar_tensor_tensor(out=oo[:], in0=o2[:], scalar=neglam[:, 0:1], in1=o1[:],
                                                                 op0=ALU.mult, op1=ALU.add),
                         r=[B_o2, B_o1, B_const], w=[B_oo])
                    P.op("dve", lambda v: v.tensor_tensor(out=sq[:], in0=oo[:], in1=oo[:], op=ALU.mult),
                         r=[B_oo], w=[B_sq])
                    P.mm(l1p[:], [(ones_f[:], sq[:])], r=[B_sq, B_const], w=[B_l1p])
                    P.op("act", lambda a: a.activation(out=rs[:], in_=l1p[:], func=AF.Ln, scale=1.0 / 128, bias=epsb[:, 0:1]),
                         r=[B_l1p, B_const], w=[B_rs])
                    P.op("act", lambda a: a.activation(out=rs[:], in_=rs[:], func=AF.Exp, scale=-0.5), r=[B_rs], w=[B_rs])
                    P.op("dve", lambda v: v.scalar_tensor_tensor(out=o_[:], in0=oo[:], scalar=subln8[:, 0:1], in1=rs[:],
                                                                 op0=ALU.mult, op1=ALU.mult),
                         r=[B_oo, B_rs, B_const], w=[bo])
                    P.dma("pool", lambda q: q.dma_start(out=mixt_d[h * 128:(h + 1) * 128, qc * 512:(qc + 1) * 512], in_=o_[:]),
                          r=[bo], w=[B_mixt[qc]])

                pending = None
                cnt = 0
                hl = list(heads)

                def load_kv(hi):
                    hh = hl[hi]
                    P.dma("sp", lambda q: q.dma_start(out=kt[hi % 2][:], in_=qkt_d[hh, 1]), r=B_qkt[hh], w=[B_kt[hi % 2]])
                    vsrc = vi_d[:, hh, 0, :].rearrange("(t p) c -> p t c", p=128)
                    for part in range(3):
                        P.dma("sp", lambda q: q.dma_start(out=vv[hi % 2][:, part * 22:(part + 1) * 22, :],
                                                          in_=vsrc[:, part * 22:(part + 1) * 22, :]),
                              r=B_vi, w=[B_vv[hi % 2]])

                load_kv(0)
                for hi, h in enumerate(hl):
                    k_ = kt[hi % 2]
                    v_ = vv[hi % 2]
                    if hi + 1 < len(hl):
                        load_kv(hi + 1)
                    for qc in qchunks:
                        q_ = qt[cnt % 2]
                        bq = B_qt[cnt % 2]
                        o_ = ob[cnt % 2]
                        bo = B_ob[cnt % 2]
                        acc1, acc2 = acc1s[cnt % 2], acc2s[cnt % 2]
                        B_acc1, B_acc2 = B_acc1s[cnt % 2], B_acc2s[cnt % 2]
                        cnt += 1
                        P.dma("sp", lambda q: q.dma_start(out=q_[:], in_=qkt_d[h, 0][:, TC + qc * 512:TC + (qc + 1) * 512]),
                              r=B_qkt[h], w=[bq])

                        def scores(i):
                            mm1(s1[i % 2][:], k_[0:64, i * 128:(i + 1) * 128], q_[0:64, :], True, True,
                                [B_kt[hi % 2], bq], [B_s1[i % 2]])
                            mm1(s2[i % 2][:], k_[64:128, i * 128:(i + 1) * 128], q_[64:128, :], True, True,
                                [B_kt[hi % 2], bq], [B_s2[i % 2]])

                        scores(0)
                        scores(1)
                        for i in range(NKT):
                            if i == 8 and pending is not None:
                                finalize(*pending)
                                pending = None
                            pa, pb = p1[i % 3], p2[i % 3]
                            P.op("act", lambda a: a.activation(out=pa[:], in_=s1[i % 2][:], func=AF.Exp, scale=0.125),
                                 r=[B_s1[i % 2]], w=[B_p1[i % 3]])
                            P.op("act", lambda a: a.activation(out=pb[:], in_=s2[i % 2][:], func=AF.Exp, scale=0.125),
                                 r=[B_s2[i % 2]], w=[B_p2[i % 3]])
                            st_, sp_ = (i == 0), (i == NKT - 1)
                            P._deps("pe", [B_p1[i % 3], B_p2[i % 3]], [])
                            mm1(o1p[:], v_[:, i, :], pa[:], st_, sp_, [B_vv[hi % 2], B_p1[i % 3]], [B_o1p])
                            mm1(o2p[:], v_[:, i, :], pb[:], st_, sp_, [B_vv[hi % 2], B_p2[i % 3]], [B_o2p])
                            if i == 0:
                                P.op("dve", lambda v: v.tensor_copy(out=acc1[:], in_=pa[:]), r=[B_p1[i % 3]], w=[B_acc1])
                                P.op("dve", lambda v: v.tensor_copy(out=acc2[:], in_=pb[:]), r=[B_p2[i % 3]], w=[B_acc2])
                            else:
                                P.op("dve", lambda v: v.tensor_tensor(out=acc1[:], in0=acc1[:], in1=pa[:], op=ALU.add),
                                     r=[B_p1[i % 3], B_acc1], w=[B_acc1])
                                P.op("dve", lambda v: v.tensor_tensor(out=acc2[:], in0=acc2[:], in1=pb[:], op=ALU.add),
                                     r=[B_p2[i % 3], B_acc2], w=[B_acc2])
                            if i + 2 < NKT:
                                scores(i + 2)
                        P.op("act", lambda a: a.copy(out=o1c[:], in_=o1p[:]), r=[B_o1p], w=[B_o1c])
                        P.op("dve", lambda v: v.tensor_copy(out=o2c[:], in_=o2p[:]), r=[B_o2p], w=[B_o2c])
                        pending = (h, qc, acc1, acc2, B_acc1, B_acc2, o_, bo)
                if pending is not None:
                    finalize(*pending)
                P.barrier()

        def phase_a2(heads=range(NH)):
            with ExitStack() as st:
                o_accs = [sb(st, "o_acc%d" % i, [128, NTILE, 128], F32) for i in range(2)]
                B_oaccs = [[Buf() for _ in range(NTILE)] for _ in range(2)]
                rm = sb(st, "rm", [128, 2, 512], F32)
                cmA = sb(st, "cmA", [128, 512], BF16)
                cmB = sb(st, "cmB", [128, 512], BF16)
                hgn = sb(st, "hgn_b", [128, 128], F32)
                B_c2 = Buf()
                P.dma("sp", lambda q: q.dma_start(out=rm[:], in_=rm_d[:, :, 0:512]), w=[B_c2])
                P.dma("sp", lambda q: q.dma_start(out=hgn[:], in_=hgn_d[0:1, :].partition_broadcast(128)), w=[B_c2])
                P.op("dve", lambda v: v.memset(cmA[:], 0.0), w=[B_c2])
                P.op("dve", lambda v: v.memset(cmB[:], 0.0), w=[B_c2])
                P.op("dve", lambda v: v.memset(cmA[:].rearrange("p (t c) -> p t c", c=128)[:, :, 0:64], 1.0), w=[B_c2])
                P.op("dve", lambda v: v.memset(cmB[:].rearrange("p (t c) -> p t c", c=128)[:, :, 64:128], 1.0), w=[B_c2])

                def dbl(name, shape, dt):
                    return [sb(st, "%s%d" % (name, i), shape, dt) for i in range(2)], [Buf(), Buf()]
                zin, B_zin = dbl("zin", [128, 2, 512], F32)
                vt, B_vt = dbl("vt", [128, 4, 128], BF16)
                gt, B_gt = dbl("gt", [128, 4, 128], F32)
                e_, B_e = dbl("e_", [128, 512], F32)
                f_, B_f_ = dbl("f_", [128, 512], F32)
                lf, B_lf = dbl("lf", [128, 512], F32)
                kk, B_kk = dbl("kk", [128, 512], F32)
                bc, B_bc = dbl("bc", [128, 512], F32)
                ep, B_ep = dbl("ep", [128, 512], F32)
                en, B_en = dbl("en", [128, 512], F32)
                kdf, B_kdf = dbl("kdf", [128, 512], F32)
                Qd, B_Qd = dbl("Qd", [128, 512], BF16)
                QdA, B_QdA = dbl("QdA", [128, 512], BF16)
                QdB, B_QdB = dbl("QdB", [128, 512], BF16)
                Kd, B_Kd = dbl("Kd", [128, 512], BF16)
                K2T, B_K2T = dbl("K2T", [128, 512], BF16)
                dec, B_dec = dbl("dec", [128, 8], F32)
                k2all, B_k2all = dbl("k2all", [128, 4, 128], BF16)
                scma, B_scma = dbl("scma", [128, 4, 128], BF16)
                Sball, B_Sball = dbl("Sball", [128, 8, 128], BF16)
                ScarA, B_ScarA = dbl("ScarA", [128, 128], BF16)
                ScarB, B_ScarB = dbl("ScarB", [128, 128], BF16)
                Sst2 = [sb(st, "Sst%d" % i, [128, 128], F32) for i in range(2)]
                B_S2 = [Buf(), Buf()]
                otot, B_otot = dbl("otot", [128, 4, 128], F32)
                osq, B_osq = dbl("osq", [128, 4, 128], F32)
                osm4, B_osm4 = dbl("osm4", [128, 8], F32)
                yb4, B_yb4 = dbl("yb4", [128, 4, 128], BF16)
                mixs, B_mixs = dbl("mixs", [128, 512], BF16)
                tpb = ps(st, "tpb", [128, 1024], BF16)
                B_tpb = Buf()
                scp = ps(st, "scp", [128, 512])
                B_scp = Buf()
                ups2 = [ps(st, "ups%d" % i, [128, 1024]) for i in range(2)]
                B_ups2 = [[Buf() for _ in range(8)] for _ in range(2)]
                ops2 = [ps(st, "ops%d" % i, [128, 512]) for i in range(2)]
                B_ops2 = [Buf(), Buf()]
                ctr = dict(sc=0, tile=0, ch=0, tp=0)
                for i_ in range(2):
                    P.op("dve", lambda v: v.memset(QdA[i_][:], 0.0), w=[B_QdA[i_]])
                    P.op("dve", lambda v: v.memset(QdB[i_][:], 0.0), w=[B_QdB[i_]])

                def chain(h, d):
                    if True:
                        Sst = Sst2[d]
                        B_S = B_S2[d]
                        Scar, B_Scar = (ScarA, B_ScarA) if d == 0 else (ScarB, B_ScarB)
                        o_acc = o_accs[d]
                        B_oacc = B_oaccs[d]
                        cp = 0
                        col = d * 4 + h
                        lb_ap = lbt[:, col:col + 1]
                        oml_ap = omlt[:, col:col + 1]
                        P.op("dve", lambda v: v.memset(Sst[:], 0.0), w=[B_S])
                        P.op("dve", lambda v: v.memset(Scar[0][:], 0.0), w=[B_Scar[0]])
                        P.op("dve", lambda v: v.memset(Scar[1][:], 0.0), w=[B_Scar[1]])
                        order = list(range(17)) if d == 0 else [0] + list(range(16, 0, -1))
                        for k in order:
                            t0, nt = SCS[k]
                            n = nt * 128
                            s0 = t0 * 128
                            nch = n // 64
                            lat = k >= 1
                            i2 = d
                            z_ = zin[i2]
                            P.dma("sp", lambda q: q.dma_start(out=z_[:, 0, 0:n], in_=zt_d[h, d][:, s0:s0 + n]),
                                  r=B_zt[h], w=[B_zin[i2]])
                            P.dma("sp", lambda q: q.dma_start(out=z_[:, 1, 0:n], in_=zt_d[h, 2][:, s0:s0 + n]),
                                  r=B_zt[h], w=[B_zin[i2]])
                            P.dma("sp", lambda q: q.dma_start(
                                out=vt[i2][:, 0:nt, :], in_=vi_d[s0:s0 + n, h, 1, :].rearrange("(t p) c -> p t c", p=128)),
                                r=B_vi, w=[B_vt[i2]])
                            zz = z_[:, 0, 0:n]
                            hq = z_[:, 1, 0:n]
                            P.op("act", lambda a: a.activation(out=e_[i2][:, 0:n], in_=zz, func=AF.Exp, scale=-1.0),
                                 r=[B_zin[i2]], w=[B_e[i2]])
                            yield
                            P.op("dve", lambda v: v.tensor_scalar(out=e_[i2][:, 0:n], in0=e_[i2][:, 0:n], scalar1=1.0, scalar2=None,
                                                                  op0=ALU.add), r=[B_e[i2]], w=[B_e[i2]])
                            yield
                            P.op("act", lambda a: a.activation(out=e_[i2][:, 0:n], in_=e_[i2][:, 0:n], func=AF.Ln), r=[B_e[i2]], w=[B_e[i2]])
                            P.op("act", lambda a: a.activation(out=e_[i2][:, 0:n], in_=e_[i2][:, 0:n], func=AF.Exp, scale=-1.0),
                                 r=[B_e[i2]], w=[B_e[i2]])
                            yield
                            P.op("dve", lambda v: v.tensor_scalar(out=f_[i2][:, 0:n], in0=e_[i2][:, 0:n], scalar1=oml_ap, scalar2=lb_ap,
                                                                  op0=ALU.mult, op1=ALU.add), r=[B_e[i2], B_const], w=[B_f_[i2]])
                            yield
                            P.op("act", lambda a: a.activation(out=lf[i2][:, 0:n], in_=f_[i2][:, 0:n], func=AF.Ln),
                                 r=[B_f_[i2]], w=[B_lf[i2]])
                            P.op("act", lambda a: a.activation(out=kk[i2][:, 0:n], in_=f_[i2][:, 0:n], func=AF.Copy, scale=-1.0, bias=1.0),
                                 r=[B_f_[i2]], w=[B_kk[i2]])
                            yield
                            if d == 0:
                                P.op("dve", lambda v: v.tensor_tensor_scan(out=bc[i2][:, 0:n], data0=rm[:, 0, 0:n], data1=lf[i2][:, 0:n],
                                                                           initial=0.0, op0=ALU.mult, op1=ALU.add),
                                     r=[B_lf[i2], B_c2], w=[B_bc[i2]])
                            else:
                                P.op("dve", lambda v: v.tensor_tensor_scan(out=bc[i2][:, 0:n][:, ::-1], data0=rm[:, 1, 0:n][:, ::-1],
                                                                           data1=lf[i2][:, 0:n][:, ::-1],
                                                                           initial=0.0, op0=ALU.mult, op1=ALU.add),
                                     r=[B_lf[i2], B_c2], w=[B_bc[i2]])
                            yield
                            P.op("act", lambda a: a.activation(out=ep[i2][:, 0:n], in_=bc[i2][:, 0:n], func=AF.Exp),
                                 r=[B_bc[i2]], w=[B_ep[i2]])
                            P.op("act", lambda a: a.activation(out=en[i2][:, 0:n], in_=bc[i2][:, 0:n], func=AF.Exp, scale=-1.0),
                                 r=[B_bc[i2]], w=[B_en[i2]])
                            yield
                            if lat:
                                P.op("dve", lambda v: v.tensor_tensor(out=Qd[i2][:, 0:n], in0=hq, in1=ep[i2][:, 0:n], op=ALU.mult),
                                     r=[B_zin[i2], B_ep[i2]], w=[B_Qd[i2]])
                                P.op("act", lambda a: a.copy(out=QdA[i2][:, 0:n].rearrange("p (t c) -> p t c", c=128)[:, :, 0:64],
                                                             in_=Qd[i2][:, 0:n].rearrange("p (t c) -> p t c", c=128)[:, :, 0:64]),
                                     r=[B_Qd[i2]], w=[B_QdA[i2]])
                                P.op("act", lambda a: a.copy(out=QdB[i2][:, 0:n].rearrange("p (t c) -> p t c", c=128)[:, :, 64:128],
                                                             in_=Qd[i2][:, 0:n].rearrange("p (t c) -> p t c", c=128)[:, :, 64:128]),
                                     r=[B_Qd[i2]], w=[B_QdB[i2]])
                            P.op("pool", lambda g: g.tensor_tensor(out=kdf[i2][:, 0:n], in0=kk[i2][:, 0:n], in1=en[i2][:, 0:n], op=ALU.mult),
                                 r=[B_kk[i2], B_en[i2]], w=[B_kdf[i2]])
                            if lat:
                                P.op("act", lambda a: a.copy(out=Kd[i2][:, 0:n], in_=kdf[i2][:, 0:n]),
                                     r=[B_kdf[i2]], w=[B_Kd[i2]])
                            endcol = 63 if d == 0 else 0
                            P.op("dve", lambda v: v.tensor_copy(out=dec[i2][:, 0:nch],
                                                                in_=ep[i2][:, 0:n].rearrange("p (c j) -> p c j", j=64)[:, :, endcol]),
                                 r=[B_ep[i2]], w=[B_dec[i2]])
                            P.op("dve", lambda v: v.tensor_tensor(
                                out=K2T[i2][:, 0:n].rearrange("p (c j) -> p c j", j=64),
                                in0=kdf[i2][:, 0:n].rearrange("p (c j) -> p c j", j=64),
                                in1=dec[i2][:, 0:nch].unsqueeze(2).to_broadcast([128, nch, 64]), op=ALU.mult),
                                r=[B_kdf[i2], B_dec[i2]], w=[B_K2T[i2]])
                            yield
                            pe_group([(lambda pe, j=j: pe.transpose(out=tpb[:, j * 128:(j + 1) * 128], in_=K2T[i2][:, j * 128:(j + 1) * 128],
                                                                     identity=ident_bf[:])) for j in range(nt)],
                                     r=[B_K2T[i2], B_const], w=[B_tpb])
                            P.op("act", lambda a: a.copy(out=k2all[d][:, 0:nt, :].rearrange("p t c -> p (t c)"), in_=tpb[:, 0:n]),
                                 r=[B_tpb], w=[B_k2all[d]])
                            for gc in range(nch):
                                j, c = gc // 2, gc % 2
                                rows = slice(c * 64, (c + 1) * 64)
                                uo = c * 512 + j * 128
                                mm1(ups2[d][:, uo:uo + 128], k2all[d][rows, j, :], vt[i2][rows, j, :], True, True,
                                    [B_k2all[d], B_vt[i2]], [B_ups2[d][c]])
                            if lat:
                                for j in range(nt):
                                    cs_ = slice(j * 128, (j + 1) * 128)
                                    mm1(scp[:, cs_], Kd[i2][:, cs_], Qd[i2][:, cs_], True, True, [B_Kd[i2], B_Qd[i2]], [B_scp])
                                P.op("dve", lambda v: v.tensor_tensor(
                                    out=scma[d][:], in0=scp[:, :].rearrange("p (t c) -> p t c", c=128),
                                    in1=cm_f[:, 1 + d, :].unsqueeze(1).to_broadcast([128, 4, 128]), op=ALU.mult),
                                    r=[B_scp, B_const], w=[B_scma[d]])
                            yield
                            seq = list(range(nch)) if d == 0 else list(range(nch - 1, -1, -1))
                            for m, gc in enumerate(seq):
                                last = m == nch - 1
                                uo = (gc % 2) * 512 + (gc // 2) * 128
                                if last:
                                    dst, bdst = Scar[1 - cp][:], B_Scar[1 - cp]
                                else:
                                    dst, bdst = Sball[d][:, seq[m + 1], :], B_Sball[d]
                                if lat or last:
                                    P.op("dve", lambda v: v.scalar_tensor_tensor(out=dst, in0=Sst[:], scalar=dec[i2][:, gc:gc + 1],
                                                                                 in1=ups2[d][:, uo:uo + 128],
                                                                                 op0=ALU.mult, op1=ALU.add),
                                         r=[B_S, B_dec[i2], B_ups2[d][gc % 2]], w=[bdst])
                                P.op("dve", lambda v: v.scalar_tensor_tensor(out=Sst[:], in0=Sst[:], scalar=dec[i2][:, gc:gc + 1],
                                                                             in1=ups2[d][:, uo:uo + 128],
                                                                             op0=ALU.mult, op1=ALU.add),
                                     r=[B_S, B_dec[i2], B_ups2[d][gc % 2]], w=[B_S])
                            yield
                            if lat:
                                for j in range(nt):
                                    cs_ = slice(j * 128, (j + 1) * 128)
                                    mm1(ops2[d][:, cs_], scma[d][:, j, :], vt[i2][:, j, :], True, False,
                                        [B_scma[d], B_vt[i2]], [B_ops2[d]])
                                    for c in range(2):
                                        gc = 2 * j + c
                                        qsel = QdA if c == 0 else QdB
                                        bq = B_QdA if c == 0 else B_QdB
                                        if gc == seq[0]:
                                            sap, bs = Scar[cp][:], B_Scar[cp]
                                        else:
                                            sap, bs = Sball[d][:, gc, :], B_Sball[d]
                                        mm1(ops2[d][:, cs_], qsel[i2][:, cs_], sap, False, c == 1, [bq[i2], bs], [B_ops2[d]])
                                gt0 = (k - 1) * 4
                                P.op("act", lambda a: a.copy(out=o_acc[:, gt0:gt0 + 4, :].rearrange("p t c -> p (t c)"), in_=ops2[d][:, :]),
                                     r=[B_ops2[d]], w=[B_oacc[gt0 + jj] for jj in range(4)])
                            cp = 1 - cp
                            yield

                def combine(h):
                    for k in range(1, 17):
                        t0, nt = SCS[k]
                        s0 = t0 * 128
                        i2 = k % 2
                        gt0 = (k - 1) * 4
                        P.dma("sp", lambda q: q.dma_start(
                            out=gt[i2][:, 0:4, :], in_=g_d[s0:s0 + 512, h, :].rearrange("(t p) c -> p t c", p=128)),
                            r=B_g, w=[B_gt[i2]])
                        ro = [B_oaccs[dd][gt0 + jj] for dd in range(2) for jj in range(4)]
                        P.op("dve", lambda v: v.tensor_tensor(out=otot[i2][:], in0=o_accs[0][:, gt0:gt0 + 4, :],
                                                              in1=o_accs[1][:, gt0:gt0 + 4, :], op=ALU.add), r=ro, w=[B_otot[i2]])
                        P.op("dve", lambda v: v.tensor_tensor(out=osq[i2][:], in0=otot[i2][:], in1=otot[i2][:], op=ALU.mult),
                             r=[B_otot[i2]], w=[B_osq[i2]])
                        P.op("dve", lambda v: v.tensor_reduce(out=osm4[i2][:, 0:4], in_=osq[i2][:], axis=AX.X, op=ALU.add),
                             r=[B_osq[i2]], w=[B_osm4[i2]])
                        rstd_from_ss(osm4[i2][:, 0:4], 128, osm4[i2][:, 4:8], osm4[i2][:, 0:4], [B_osm4[i2]], [B_osm4[i2]], B_osm4[i2])
                        P.op("dve", lambda v: v.tensor_tensor(out=otot[i2][:], in0=otot[i2][:],
                                                              in1=osm4[i2][:, 4:8].unsqueeze(2).to_broadcast([128, 4, 128]), op=ALU.mult),
                             r=[B_otot[i2], B_osm4[i2]], w=[B_otot[i2]])
                        P.op("dve", lambda v: v.tensor_tensor(out=otot[i2][:], in0=otot[i2][:],
                                                              in1=hgn[:].unsqueeze(1).to_broadcast([128, 4, 128]), op=ALU.mult),
                             r=[B_otot[i2], B_c2], w=[B_otot[i2]])
                        P.op("pool", lambda g: g.tensor_tensor(out=yb4[i2][:], in0=otot[i2][:], in1=gt[i2][:, 0:4, :], op=ALU.mult),
                             r=[B_otot[i2], B_gt[i2]], w=[B_yb4[i2]])
                        pe_group([(lambda pe, j=j: pe.transpose(out=tpb[:, j * 128:(j + 1) * 128], in_=yb4[i2][:, j, :],
                                                                 identity=ident_bf[:])) for j in range(4)],
                                 r=[B_yb4[i2], B_const], w=[B_tpb])
                        P.op("act", lambda a: a.copy(out=mixs[i2][:], in_=tpb[:, 0:512]), r=[B_tpb], w=[B_mixs[i2]])
                        P.dma("pool", lambda q: q.dma_start(
                            out=mixt_d[512 + h * 128:512 + (h + 1) * 128, (k - 1) * 512:k * 512], in_=mixs[i2][:]),
                            r=[B_mixs[i2]], w=[B_mixt[k - 1]])

                for h in heads:
                    alive = [chain(h, 0), chain(h, 1)]
                    while alive:
                        for g_ in list(alive):
                            try:
                                next(g_)
                            except StopIteration:
                                alive.remove(g_)
                    combine(h)
                P.barrier()

        AFF = sb(es, "AFF", [128, NTILE, NE], F32)
        B_AFF = [Buf() for _ in range(NTILE)]
        B_h2t = [Buf() for _ in range(NTILE)]
        B_afft = [Buf() for _ in range(NTILE)]

        def phase_b():
            with ExitStack() as st:
                wo = sb(st, "wo", [128, 8, D], BF16)
                B_wo = [Buf() for _ in range(4)]
                for pi in range(4):
                    P.dma("pool", lambda q: q.dma_start(out=wo[:, 2 * pi:2 * pi + 2, :], in_=wout_d[:, 2 * pi:2 * pi + 2, :]),
                          w=[B_wo[pi]])
                wr = sb(st, "wr", [128, 8, NE], F32)
                B_wr = Buf()
                P.dma("sp", lambda q: q.dma_start(out=wr[:], in_=wr_d[:]), w=[B_wr])
                gpm, B_gpm = load_bc(st, "gpm", 4)
                g2m, B_g2m = load_bc(st, "g2m", 5)
                sh2, B_sh2 = load_bc(st, "sh2", 6)
                mix = [sb(st, "mix%d" % i, [128, 8, 512], BF16) for i in range(2)]
                B_mix = [Buf(), Buf()]
                xb = [sb(st, "bxb%d" % i, [128, D], F32) for i in range(2)]
                B_xb = [Buf(), Buf()]
                tt = [sb(st, "btt%d" % i, [128, D], F32) for i in range(2)]
                B_tt = [Buf(), Buf()]
                x1 = [sb(st, "bx1%d" % i, [128, D], F32) for i in range(2)]
                B_x1s = [Buf(), Buf()]
                h2f = [sb(st, "h2f%d" % i, [128, D], F32) for i in range(2)]
                B_h2f = [Buf(), Buf()]
                h2b = [sb(st, "h2b%d" % i, [128, D], BF16) for i in range(2)]
                B_h2b = [Buf(), Buf()]
                junk = sb(st, "bjunk", [128, D], BF16)
                B_junk = Buf()
                h2T = [sb(st, "h2T%d" % i, [128, 8, 128], F32) for i in range(2)]
                B_h2T = [Buf(), Buf()]
                sm = sb(st, "bsm", [128, 2, 8], F32)
                B_sm = [Buf(), Buf()]
                ee = sb(st, "bee", [128, 2, NE], F32)
                yps = [ps(st, "yps%d" % i, [128, D]) for i in range(2)]
                B_yps = [Buf(), Buf()]
                trp = ps(st, "trp", [128, D])
                B_trp = Buf()
                lgp = ps(st, "lgp", [128, 512])
                B_lgp = Buf()
                for sc in range(16):
                    m_ = mix[sc % 2]
                    P.dma("sp", lambda q: q.dma_start(out=m_[:], in_=mixt_d[:, sc * 512:(sc + 1) * 512].rearrange("(kc p) t -> p kc t", p=128)),
                          r=[B_mixt[sc]], w=[B_mix[sc % 2]])
                    for j in range(4):
                        tl = sc * 4 + j
                        i2 = tl % 2
                        y_ = yps[i2]
                        for half in range(2):
                            P.mm(y_[:, half * 512:(half + 1) * 512],
                                 [(m_[:, kc, j * 128:(j + 1) * 128], wo[:, kc, half * 512:(half + 1) * 512]) for kc in range(8)],
                                 r=[B_mix[sc % 2]] + B_wo, w=[B_yps[i2]])
                        s_ = sm[:, i2, :]
                        for half in range(2):
                            P.op("act", lambda a: a.activation(out=junk[:, half * 512:(half + 1) * 512], in_=y_[:, half * 512:(half + 1) * 512],
                                                               func=AF.Square, accum_out=s_[:, half:half + 1]),
                                 r=[B_yps[i2]], w=[B_junk, B_sm[i2]])
                        P.op("dve", lambda v: v.tensor_tensor(out=s_[:, 2:3], in0=s_[:, 0:1], in1=s_[:, 1:2], op=ALU.add),
                             r=[B_sm[i2]], w=[B_sm[i2]])
                        rstd_from_ss(s_[:, 2:3], D, s_[:, 3:4], s_[:, 2:3], [B_sm[i2]], [B_sm[i2]], B_sm[i2])
                        P.dma("sp", lambda q: q.dma_start(out=xb[i2][:], in_=x_d[tl * 128:(tl + 1) * 128, :]), w=[B_xb[i2]])
                        for half in range(2):
                            hs = slice(half * 512, (half + 1) * 512)
                            P.op("dve", lambda v: v.scalar_tensor_tensor(out=tt[i2][:, hs], in0=y_[:, hs], scalar=s_[:, 3:4], in1=gpm[:, hs],
                                                                         op0=ALU.mult, op1=ALU.mult),
                                 r=[B_yps[i2], B_sm[i2], B_gpm], w=[B_tt[i2]])
                        P.op("pool", lambda g: g.tensor_tensor(out=x1[i2][:], in0=tt[i2][:], in1=xb[i2][:], op=ALU.add),
                             r=[B_tt[i2], B_xb[i2]], w=[B_x1s[i2]])
                        P.dma("pool", lambda q: q.dma_start(out=x1_d[tl * 128:(tl + 1) * 128, :], in_=x1[i2][:]),
                              r=[B_x1s[i2]], w=[B_x1[tl]])
                        P.op("dve", lambda v: v.scalar_tensor_tensor(out=junk[:], in0=x1[i2][:], scalar=1.0, in1=x1[i2][:],
                                                                     op0=ALU.mult, op1=ALU.mult, accum_out=s_[:, 4:5]),
                             r=[B_x1s[i2]], w=[B_junk, B_sm[i2]])
                        rstd_from_ss(s_[:, 4:5], D, s_[:, 5:6], s_[:, 4:5], [B_sm[i2]], [B_sm[i2]], B_sm[i2])
                        P.op("dve", lambda v: v.scalar_tensor_tensor(out=tt[i2][:], in0=x1[i2][:], scalar=s_[:, 5:6], in1=g2m[:],
                                                                     op0=ALU.mult, op1=ALU.mult),
                             r=[B_x1s[i2], B_sm[i2], B_g2m], w=[B_tt[i2]])
                        P.op("pool", lambda g: g.tensor_tensor(out=h2f[i2][:], in0=tt[i2][:], in1=sh2[:], op=ALU.add),
                             r=[B_tt[i2], B_sh2], w=[B_h2f[i2]])
                        P.op("act", lambda a: a.copy(out=h2b[i2][:], in_=h2f[i2][:]), r=[B_h2f[i2]], w=[B_h2b[i2]])
                        P.dma("pool", lambda q: q.dma_start(out=h2_d[tl * 128:(tl + 1) * 128, :], in_=h2b[i2][:]),
                              r=[B_h2b[i2]], w=[B_h2t[tl]])
                        pe_group([(lambda pe, kc=kc: pe.transpose(out=trp[:, kc * 128:(kc + 1) * 128],
                                                                   in_=h2f[i2][:, kc * 128:(kc + 1) * 128], identity=ident_f))
                                  for kc in range(8)], r=[B_h2f[i2], B_const], w=[B_trp])
                        P.op("act", lambda a: a.copy(out=h2T[i2][:, 0:4, :].rearrange("p k t -> p (k t)"), in_=trp[:, 0:512]),
                             r=[B_trp], w=[B_h2T[i2]])
                        P.op("dve", lambda v: v.tensor_copy(out=h2T[i2][:, 4:8, :].rearrange("p k t -> p (k t)"), in_=trp[:, 512:1024]),
                             r=[B_trp], w=[B_h2T[i2]])
                        P.mm(lgp[:, 0:NE], [(h2T[i2][:, kc, :], wr[:, kc, :]) for kc in range(8)],
                             r=[B_h2T[i2], B_wr], w=[B_lgp])
                        P.op("dve", lambda v: v.tensor_reduce(out=s_[:, 6:7], in_=lgp[:, 0:NE], axis=AX.X, op=ALU.max, negate=True),
                             r=[B_lgp], w=[B_sm[i2]])
                        P.op("act", lambda a: a.activation(out=ee[:, i2, :], in_=lgp[:, 0:NE], func=AF.Exp, bias=s_[:, 6:7],
                                                           accum_out=s_[:, 7:8]), r=[B_lgp, B_sm[i2]], w=[B_sm[i2]])
                        P.op("dve", lambda v: v.reciprocal(out=s_[:, 7:8], in_=s_[:, 7:8]), r=[B_sm[i2]], w=[B_sm[i2]])
                        P.op("dve", lambda v: v.tensor_scalar(out=AFF[:, tl, :], in0=ee[:, i2, :], scalar1=s_[:, 7:8], scalar2=None,
                                                              op0=ALU.mult), r=[B_sm[i2]], w=[B_AFF[tl]])
                        P.dma("pool", lambda q: q.dma_start(out=aff_d[tl * 128:(tl + 1) * 128, :], in_=AFF[:, tl, :]),
                              r=[B_AFF[tl]], w=[B_afft[tl]])
                P.barrier()

        posm = sb(es, "posm", [128, NE, NTILE], F32)
        B_posm = Buf()

        def phase_c():
            with ExitStack() as st:
                lo = sb(st, "c_lo", [128, NE], F32)
                hi = sb(st, "c_hi", [128, NE], F32)
                mid = sb(st, "c_mid", [128, NE], F32)
                ge = sb(st, "c_ge", [128, NTILE, NE], F32)
                cntp = sb(st, "c_cntp", [128, NE], F32)
                mge = sb(st, "c_mge", [128, NE], U32)
                mlt = sb(st, "c_mlt", [128, NE], U32)
                Mt = sb(st, "c_Mt", [128, NE, NTILE], F32)
                Psc = sb(st, "c_Psc", [128, NE, NTILE], F32)
                rmc = sb(st, "c_rmc", [128, 1024], F32)
                Tt = sb(st, "c_Tt", [128, NE], BF16)
                Lbf = sb(st, "c_Lbf", [128, 128], BF16)
                off = sb(st, "c_off", [128, NE], F32)
                cps = ps(st, "c_cps", [128, 512])
                B_lo, B_hi, B_mid, B_ge, B_cntp, B_m, B_cps, B_x = Buf(), Buf(), Buf(), Buf(), Buf(), Buf(), Buf(), Buf()
                P.dma("sp", lambda q: q.dma_start(out=rmc[:], in_=rm_d[:, 0, :]), w=[B_x])
                P.op("dve", lambda v: v.tensor_copy(out=Lbf[:], in_=cm_f[:, 3, :]), r=[B_const], w=[B_x])
                P.op("dve", lambda v: v.memset(lo[:], 0.0), w=[B_lo])
                P.op("dve", lambda v: v.memset(hi[:], 2.0), w=[B_hi])
                for it in range(34):
                    P.op("dve", lambda v: v.tensor_tensor(out=mid[:], in0=lo[:], in1=hi[:], op=ALU.add), r=[B_lo, B_hi], w=[B_mid])
                    P.op("dve", lambda v: v.tensor_scalar(out=mid[:], in0=mid[:], scalar1=0.5, scalar2=None, op0=ALU.mult),
                         r=[B_mid], w=[B_mid])
                    P.op("dve", lambda v: v.tensor_tensor(out=ge[:], in0=AFF[:], in1=mid[:].unsqueeze(1).to_broadcast([128, NTILE, NE]),
                                                          op=ALU.is_ge), r=B_AFF + [B_mid], w=[B_ge])
                    P.op("dve", lambda v: v.tensor_reduce(out=cntp[:], in_=ge[:].rearrange("p i e -> p e i"), axis=AX.X, op=ALU.add),
                         r=[B_ge], w=[B_cntp])
                    P.mm(cps[:, 0:NE], [(ones_f[:], cntp[:])], r=[B_cntp, B_const], w=[B_cps])
                    P.op("dve", lambda v: v.tensor_scalar(out=mge[:], in0=cps[:, 0:NE], scalar1=float(CAP), scalar2=None, op0=ALU.is_ge),
                         r=[B_cps], w=[B_m])
                    P.op("dve", lambda v: v.tensor_scalar(out=mlt[:], in0=cps[:, 0:NE], scalar1=float(CAP), scalar2=None, op0=ALU.is_lt),
                         r=[B_cps], w=[B_m])
                    P.op("dve", lambda v: v.copy_predicated(out=lo[:], mask=mge[:], data=mid[:]), r=[B_m, B_mid], w=[B_lo])
                    P.op("dve", lambda v: v.copy_predicated(out=hi[:], mask=mlt[:], data=mid[:]), r=[B_m, B_mid], w=[B_hi])
                P.op("dve", lambda v: v.tensor_tensor(out=ge[:], in0=AFF[:], in1=lo[:].unsqueeze(1).to_broadcast([128, NTILE, NE]),
                                                      op=ALU.is_ge), r=B_AFF + [B_lo], w=[B_ge])
                P.op("dve", lambda v: v.tensor_copy(out=Mt[:], in_=ge[:].rearrange("p i e -> p e i")), r=[B_ge], w=[B_x])
                P.op("dve", lambda v: v.tensor_tensor_scan(out=Psc[:].rearrange("p e i -> p (e i)"), data0=rmc[:],
                                                           data1=Mt[:].rearrange("p e i -> p (e i)"), initial=0.0,
                                                           op0=ALU.mult, op1=ALU.add), r=[B_x], w=[B_x])
                P.op("dve", lambda v: v.tensor_copy(out=Tt[:], in_=Psc[:, :, NTILE - 1]), r=[B_x], w=[B_x])
                P.mm(cps[:, 0:NE], [(Lbf[:], Tt[:])], r=[B_x], w=[B_cps])
                P.op("dve", lambda v: v.tensor_copy(out=off[:], in_=cps[:, 0:NE]), r=[B_cps], w=[B_x])
                P.op("dve", lambda v: v.tensor_tensor(out=Psc[:], in0=Psc[:], in1=off[:].unsqueeze(2).to_broadcast([128, NE, NTILE]),
                                                      op=ALU.add), r=[B_x], w=[B_x])
                P.op("dve", lambda v: v.tensor_tensor(out=Psc[:], in0=Psc[:], in1=Mt[:], op=ALU.mult), r=[B_x], w=[B_x])
                P.op("dve", lambda v: v.tensor_scalar(out=posm[:], in0=Psc[:], scalar1=-1.0, scalar2=None, op0=ALU.add),
                     r=[B_x], w=[B_posm])
                P.barrier()

        def idma(fn, r, w):
            return P.dma("pool", fn, r=r, w=w)

        def phase_d(experts=range(NE)):
            with ExitStack() as st:
                iota = sb(st, "d_iota", [128, 1024], F32)
                tokf = sb(st, "d_tokf", [128, NTILE, 2], F32)
                tokb = sb(st, "d_tokb", [128, NTILE, 2], BF16)
                zt_ = sb(st, "d_zero", [128, D], F32)
                B_dc = Buf()
                P.dma("sp", lambda q: q.dma_start(out=iota[:], in_=iota_d[:]), w=[B_dc])
                P.dma("sp", lambda q: q.dma_start(out=tokf[:], in_=tokhl_d[:]), w=[B_dc])
                P.op("dve", lambda v: v.tensor_copy(out=tokb[:], in_=tokf[:]), r=[B_dc], w=[B_dc])
                P.op("dve", lambda v: v.memset(zt_[:], 0.0), w=[B_dc])
                fview = f_d.rearrange("(t p) d -> p t d", p=128)
                for part in range(4):
                    P.dma("sp", lambda q: q.dma_start(out=fview[:, part * 16:(part + 1) * 16, :],
                                                      in_=zt_[:].unsqueeze(1).to_broadcast([128, 16, D])), r=[B_dc], w=[B_f])
                sel = [sb(st, "d_sel%d" % i, [128, 1024], BF16) for i in range(4)]
                B_sel = [Buf() for _ in range(4)]
                idxf = sb(st, "d_idxf", [2, 1024], F32)
                idx2 = sb(st, "d_idx2", [128, 8], F32)
                idxi = [sb(st, "d_idxi%d" % i, [128, 8], I32) for i in range(2)]
                B_idxf, B_idx2 = Buf(), Buf()
                B_idxi = [Buf(), Buf()]
                X = [sb(st, "d_X%d" % i, [128, D], BF16) for i in range(16)]
                B_X = [Buf() for _ in range(16)]
                gat = [sb(st, "d_gat%d" % i, [128, 8, NE], F32) for i in range(2)]
                B_gat = [Buf(), Buf()]
                XT = sb(st, "d_XT", [128, 8, 1024], BF16)
                B_XT = Buf()
                AT = sb(st, "d_AT", [128, 8, 1024], BF16)
                B_AT = Buf()
                W = [[sb(st, "d_w%d_%d" % (m, i), [128, 8, D], BF16) for m in range(3)] for i in range(2)]
                B_W = [[[Buf() for _ in range(4)] for _ in range(3)] for _ in range(2)]
                sg = [sb(st, "d_sg%d" % i, [128, 512], F32) for i in range(2)]
                B_sg = [Buf(), Buf()]
                Ysb = [sb(st, "d_Y%d" % i, [128, D], F32) for i in range(2)]
                B_Y = [Buf(), Buf()]
                ips = [ps(st, "d_ips%d" % i, [128, 512]) for i in range(2)]
                B_ips = [Buf(), Buf()]
                tpx = ps(st, "d_tpx", [128, 8, 128], BF16)
                B_tpx = Buf()
                itp = ps(st, "d_itp", [128, 512])
                B_itp = Buf()
                gps = [ps(st, "d_gps%d" % i, [128, 512]) for i in range(2)]
                B_gps = [Buf(), Buf()]
                ups = [ps(st, "d_ups%d" % i, [128, 512]) for i in range(2)]
                B_ups = [Buf(), Buf()]
                wsrc = (wg_d, wu_d, wd_d)

                def load_w(e, slot):
                    for m in range(3):
                        for pi in range(4):
                            P.dma("pool", lambda q: q.dma_start(out=W[slot][m][:, 2 * pi:2 * pi + 2, :],
                                                                in_=wsrc[m][e][:, 2 * pi:2 * pi + 2, :]), w=[B_W[slot][m][pi]])

                elist = list(experts)
                ctr = dict(sel=0, g=0, y=0)

                def compaction(e, slot):
                    for i in range(NTILE):
                        si = ctr["sel"] % 4
                        ctr["sel"] += 1
                        P.op("dve", lambda v: v.tensor_scalar(out=sel[si][:], in0=iota[:], scalar1=posm[:, e, i:i + 1], scalar2=None,
                                                            op0=ALU.is_equal), r=[B_dc, B_posm], w=[B_sel[si]])
                        for half in range(2):
                            mm1(ips[half][0:2, :], tokb[:, i, :], sel[si][:, half * 512:(half + 1) * 512], i == 0, i == NTILE - 1,
                                [B_dc, B_sel[si]], [B_ips[half]])
                        if i % 4 == 3 and i != NTILE - 1:
                            yield
                    for half in range(2):
                        P.op("act", lambda a: a.copy(out=idxf[:, half * 512:(half + 1) * 512], in_=ips[half][0:2, :]),
                             r=[B_ips[half]], w=[B_idxf])
                    pe_group([(lambda pe, jt=jt: pe.transpose(out=itp[:, 2 * jt:2 * jt + 2], in_=idxf[0:2, jt * 128:(jt + 1) * 128],
                                                               identity=ident_f[0:2, 0:2])) for jt in range(8)],
                             r=[B_idxf, B_const], w=[B_itp])
                    P.op("dve", lambda v: v.tensor_reduce(out=idx2[:], in_=itp[:, 0:16].rearrange("p (j t) -> p j t", t=2),
                                                          axis=AX.X, op=ALU.add), r=[B_itp], w=[B_idx2])
                    P.op("dve", lambda v: v.tensor_copy(out=idxi[slot][:], in_=idx2[:]), r=[B_idx2], w=[B_idxi[slot]])
                    yield

                def gather(e, slot):
                    ii = idxi[slot]
                    for jt in range(8):
                        xj = X[slot * 8 + jt]
                        idma(lambda q: q.indirect_dma_start(out=xj[:], out_offset=None, in_=h2_d[:, :],
                                                            in_offset=IndirectOffsetOnAxis(ap=ii[:, jt:jt + 1], axis=0)),
                             r=[B_idxi[slot]] + B_h2t, w=[B_X[slot * 8 + jt]])
                        idma(lambda q: q.indirect_dma_start(out=gat[slot][:, jt, :], out_offset=None, in_=aff_d[:, :],
                                                            in_offset=IndirectOffsetOnAxis(ap=ii[:, jt:jt + 1], axis=0)),
                             r=[B_idxi[slot]] + B_afft, w=[B_gat[slot]])

                load_w(elist[0], 0)
                for _ in compaction(elist[0], 0):
                    pass
                gather(elist[0], 0)
                for ei, e in enumerate(elist):
                    slot = ei % 2
                    ii = idxi[slot]
                    g_ = gat[slot]
                    nxt = None
                    if ei + 1 < len(elist):
                        load_w(elist[ei + 1], 1 - slot)
                        nxt = compaction(elist[ei + 1], 1 - slot)
                    for jt in range(8):
                        xj = X[slot * 8 + jt]
                        pe_group([(lambda pe, kc=kc: pe.transpose(out=tpx[:, kc, :], in_=xj[:, kc * 128:(kc + 1) * 128],
                                                                   identity=ident_bf[:])) for kc in range(8)],
                                 r=[B_X[slot * 8 + jt], B_const], w=[B_tpx])
                        if jt % 2 == 0:
                            P.op("act", lambda a: a.copy(out=XT[:, :, jt * 128:(jt + 1) * 128], in_=tpx[:]), r=[B_tpx], w=[B_XT])
                        else:
                            P.op("dve", lambda v: v.tensor_copy(out=XT[:, :, jt * 128:(jt + 1) * 128], in_=tpx[:]), r=[B_tpx], w=[B_XT])
                    wg_, wu_, wd_ = W[slot]
                    bwg, bwu, bwd = B_W[slot]
                    for fc in range(8):
                        for sh in range(2):
                            gi = ctr["g"] % 2
                            ctr["g"] += 1
                            cs_ = slice(sh * 512, (sh + 1) * 512)
                            P.mm(gps[gi][:], [(wg_[:, kc, fc * 128:(fc + 1) * 128], XT[:, kc, cs_]) for kc in range(8)],
                                 r=[B_XT] + bwg, w=[B_gps[gi]])
                            P.mm(ups[gi][:], [(wu_[:, kc, fc * 128:(fc + 1) * 128], XT[:, kc, cs_]) for kc in range(8)],
                                 r=[B_XT] + bwu, w=[B_ups[gi]])
                            P.op("act", lambda a: a.activation(out=sg[gi][:], in_=gps[gi][:], func=AF.Silu),
                                 r=[B_gps[gi]], w=[B_sg[gi]])
                            P.op("dve", lambda v: v.tensor_tensor(out=AT[:, fc, cs_], in0=ups[gi][:], in1=sg[gi][:], op=ALU.mult),
                                 r=[B_ups[gi], B_sg[gi]], w=[B_AT])
                            if nxt is not None:
                                next(nxt, None)
                    if nxt is not None:
                        for _ in nxt:
                            pass
                        gather(elist[ei + 1], 1 - slot)
                    for jt in range(8):
                        yi = ctr["y"] % 2
                        ctr["y"] += 1
                        for dh in range(2):
                            gi = ctr["g"] % 2
                            ctr["g"] += 1
                            P.mm(gps[gi][:], [(AT[:, fc, jt * 128:(jt + 1) * 128], wd_[:, fc, dh * 512:(dh + 1) * 512]) for fc in range(8)],
                                 r=[B_AT] + bwd, w=[B_gps[gi]])
                            P.op("dve", lambda v: v.tensor_scalar(out=Ysb[yi][:, dh * 512:(dh + 1) * 512], in0=gps[gi][:],
                                                                  scalar1=g_[:, jt, e:e + 1], scalar2=None, op0=ALU.mult),
                                 r=[B_gps[gi], B_gat[slot]], w=[B_Y[yi]])
                        idma(lambda q: q.indirect_dma_start(out=f_d[:, :], out_offset=IndirectOffsetOnAxis(ap=ii[:, jt:jt + 1], axis=0),
                                                            in_=Ysb[yi][:], in_offset=None, compute_op=ALU.add),
                             r=[B_Y[yi], B_idxi[slot]], w=[B_f])
                P.barrier()

        def phase_e():
            with ExitStack() as st:
                gpf, B_gpf = load_bc(st, "gpf", 7)
                fb = [sb(st, "e_f%d" % i, [128, D], F32) for i in range(2)]
                xb = [sb(st, "e_x%d" % i, [128, D], F32) for i in range(2)]
                tb = [sb(st, "e_t%d" % i, [128, D], F32) for i in range(2)]
                ob_ = [sb(st, "e_o%d" % i, [128, D], F32) for i in range(2)]
                junk = sb(st, "e_junk", [128, D], BF16)
                sm = sb(st, "e_sm", [128, 2, 2], F32)
                B_fb, B_xb, B_tb, B_ob, B_sm = [Buf(), Buf()], [Buf(), Buf()], [Buf(), Buf()], [Buf(), Buf()], [Buf(), Buf()]
                B_junk = Buf()
                B_out = [Buf() for _ in range(NTILE)]
                for tl in range(NTILE):
                    i2 = tl % 2
                    rows = slice(tl * 128, (tl + 1) * 128)
                    P.dma("sp", lambda q: q.dma_start(out=fb[i2][:], in_=f_d[rows, :]), r=[B_f], w=[B_fb[i2]])
                    P.dma("sp", lambda q: q.dma_start(out=xb[i2][:], in_=x1_d[rows, :]), r=[B_x1[tl]], w=[B_xb[i2]])
                    P.op("dve", lambda v: v.scalar_tensor_tensor(out=junk[:], in0=fb[i2][:], scalar=1.0, in1=fb[i2][:],
                                                                 op0=ALU.mult, op1=ALU.mult, accum_out=sm[:, i2, 0:1]),
                         r=[B_fb[i2]], w=[B_junk, B_sm[i2]])
                    rstd_from_ss(sm[:, i2, 0:1], D, sm[:, i2, 1:2], sm[:, i2, 0:1], [B_sm[i2]], [B_sm[i2]], B_sm[i2])
                    P.op("dve", lambda v: v.scalar_tensor_tensor(out=tb[i2][:], in0=fb[i2][:], scalar=sm[:, i2, 1:2], in1=gpf[:],
                                                                 op0=ALU.mult, op1=ALU.mult),
                         r=[B_fb[i2], B_sm[i2], B_gpf], w=[B_tb[i2]])
                    P.op("pool", lambda g: g.tensor_tensor(out=ob_[i2][:], in0=tb[i2][:], in1=xb[i2][:], op=ALU.add),
                         r=[B_tb[i2], B_xb[i2]], w=[B_ob[i2]])
                    P.dma("pool", lambda q: q.dma_start(out=out_d[rows, :], in_=ob_[i2][:]), r=[B_ob[i2]], w=[B_out[tl]])
                P.barrier()

        import os
        if stop_after == "0":
            return nc
        phase_a0()
        if stop_after == "A0":
            return nc
        if stop_after == "A1":
            phase_a1(heads=[int(x) for x in os.environ.get("A1_HEADS", "0").split(",")],
                     qchunks=[int(x) for x in os.environ.get("A1_QC", "0,9").split(",")])
            return nc
        if stop_after == "A2":
            phase_a2(heads=[int(x) for x in os.environ.get("A2_HEADS", "0").split(",")])
            return nc
        if not os.environ.get("SKIP_A1"):
            phase_a1()
        phase_a2()
        phase_b()
        if stop_after == "B":
            return nc
        phase_c()
        if stop_after == "C":
            return nc
        phase_d()
        phase_e()
        return nc


def _rope_tables():
    half = 32
    inv_freq = (1.0 / (10000.0 ** (np.arange(0, half, 2, dtype=np.float32) / np.float32(half)))).astype(np.float32)
    t = np.arange(T)
    r = (t // 64).astype(np.float32)
    c = (t % 64).astype(np.float32)
    ang_r = r[:, None] * inv_freq[None, :]
    ang_c = c[:, None] * inv_freq[None, :]
    ang = np.concatenate([ang_r, ang_r, ang_c, ang_c], axis=-1).astype(np.float32)
    cos = np.cos(ang).astype(np.float32)
    sin = np.sin(ang).astype(np.float32)
    sign = np.concatenate([-np.ones(16), np.ones(16), -np.ones(16), np.ones(16)]).astype(np.float32)
    sin = sin * sign[None, :]
    cosT = np.ones((128, S), np.float32)
    sinT = np.zeros((128, S), np.float32)
    cosT[:, TC:] = np.concatenate([cos.T, cos.T], axis=0)
    sinT[:, TC:] = np.concatenate([sin.T, sin.T], axis=0)
    return cosT, sinT


def _win_cols():
    rot = np.concatenate([np.arange(16, 32), np.arange(0, 16), np.arange(48, 64), np.arange(32, 48)])
    fm, tm, tg = [], [], []
    for h in range(NH):
        for off in (0, 512):
            base = off + h * 128
            fm.append(base + np.arange(128))
            fm.append(np.concatenate([base + rot, base + 64 + rot]))
        fm.append(1536 + h * 128 + np.arange(128))
        fm.append(2048 + h * 128 + np.arange(128))
        fm.append(3072 + h * 128 + np.arange(128))
        tm.append(1024 + h * 128 + np.arange(128))
        tm.append(2560 + h * 128 + np.arange(128))
        tg.append(3584 + h * 128 + np.arange(128))
    tm = tm + tg
    return np.concatenate(fm + tm)


def _kc(a):
    n = a.shape[-1]
    return np.ascontiguousarray(a.reshape(8, 128, n).transpose(1, 0, 2))


def prep_inputs(inp, n_cores):
    f = lambda k: np.asarray(inp[k], dtype=np.float32)
    x, c, ctx, c_ctx = f("x"), f("c"), f("ctx"), f("c_ctx")
    cosT, sinT = _rope_tables()
    p = np.arange(128)
    blk = p // 64
    same = blk[:, None] == blk[None, :]
    cm = np.zeros((128, 4, 128), np.float32)
    cm[:, 0, :] = np.eye(128)
    cm[:, 1, :] = same & (p[:, None] <= p[None, :])
    cm[:, 2, :] = same & (p[:, None] >= p[None, :])
    cm[:, 3, :] = p[:, None] < p[None, :]
    j = np.arange(1024)
    rm = np.ones((128, 2, 1024), np.float32)
    rm[:, 0, j % 64 == 0] = 0.0
    rm[:, 1, j % 64 == 63] = 0.0
    iota = np.broadcast_to(j.astype(np.float32), (128, 1024)).copy()
    tt = np.arange(NTILE)[None, :] * 128 + p[:, None]
    tokhl = np.stack([64 * (tt // 64), tt % 64], axis=-1).astype(np.float32)
    hlb = f("hg_lower_bound").reshape(2, 2, 4, 128).transpose(3, 0, 1, 2).reshape(128, 16)
    shared = {
        "w_ada": _kc(f("w_ada")[0]),
        "b_ada": f("b_ada")[0][None, :],
        "norms": np.concatenate([f("norm_pre_mix")[0], f("norm_post_mix")[0], f("norm_pre_ffn")[0],
                                 f("norm_post_ffn")[0]])[None, :],
        "w_in": _kc(f("w_in")[0][:, _win_cols()]),
        "lamv": np.concatenate([f("da_lambda_q1")[0], f("da_lambda_k1")[0], f("da_lambda_q2")[0],
                                f("da_lambda_k2")[0]])[None, :],
        "subln": f("da_subln")[0][:, None],
        "hgn": f("hg_norm")[0][None, :],
        "hlb": np.ascontiguousarray(hlb),
        "w_out": _kc(f("w_out")[0]),
        "w_r": _kc(f("w_router")[0]),
        "w_gate": np.ascontiguousarray(f("w_gate")[0].reshape(NE, 8, 128, D).transpose(0, 2, 1, 3)),
        "w_up": np.ascontiguousarray(f("w_up")[0].reshape(NE, 8, 128, D).transpose(0, 2, 1, 3)),
        "w_down": np.ascontiguousarray(f("w_down")[0].reshape(NE, 8, 128, D).transpose(0, 2, 1, 3)),
        "cosT": cosT, "sinT": sinT, "cmasks": cm, "rmask": rm, "iota": iota, "tokhl": tokhl,
    }
    maps = []
    for i in range(n_cores):
        b = i % 2
        m = dict(shared)
        m["x"] = np.ascontiguousarray(x[b])
        m["ctx"] = np.ascontiguousarray(ctx[b])
        m["cc"] = _kc(np.stack([c[b], c_ctx], axis=1))
        maps.append(m)
    return maps


N_CORES = 2
_NC_CACHE = {}


def kernel(**inputs):
    if "nc" not in _NC_CACHE:
        _NC_CACHE["nc"] = build()
    nc = _NC_CACHE["nc"]
    maps = prep_inputs(inputs, N_CORES)
    res = run_bass_kernel_spmd(nc, maps, core_ids=list(range(N_CORES)))
    out = np.stack([np.asarray(res.results[b]["out"], dtype=np.float32) for b in range(2)], axis=0)
    return out
```

```python
import numpy as np
from contextlib import ExitStack
import concourse.bass as bass
import concourse.mybir as mybir
from concourse.bass import IndirectOffsetOnAxis
from concourse.bass_utils import run_bass_kernel_spmd

F32 = mybir.dt.float32
BF16 = mybir.dt.bfloat16
I32 = mybir.dt.int32
U32 = mybir.dt.uint32
AF = mybir.ActivationFunctionType
ALU = mybir.AluOpType
AX = mybir.AxisListType

D = 1024
T = 8192
TC = 256
S = T + TC
NH = 4
NE = 16
CAP = 1024
EPS = 1e-6
NTILE = T // 128
FM_BLOCKS = 7
TM_COLS = 384
HEAD_COLS = FM_BLOCKS * 128 + TM_COLS
FM_TOTAL = NH * FM_BLOCKS * 128
WCOLS = NH * HEAD_COLS


class Buf:
    __slots__ = ("w", "r")

    def __init__(self):
        self.w = None
        self.r = {}


class Prog:
    def __init__(self, nc, es, ndma=12):
        self.nc = nc
        self.eng = dict(pe=nc.tensor, act=nc.scalar, dve=nc.vector, pool=nc.gpsimd, sp=nc.sync)
        self.sem = {k: es.enter_context(nc.semaphore("s_" + k)) for k in self.eng}
        self.cnt = {k: 0 for k in self.eng}
        self.waited = {k: {} for k in self.eng}
        self.dsem = {q: [[es.enter_context(nc.semaphore("d_%s%d" % (q, i))), 0] for i in range(ndma)]
                     for q in ("sp", "pool")}
        self.dnext = {"sp": 0, "pool": 0}
        self.nwait = 0
        self.nops = 0
        import os
        self.limit = int(os.environ.get('OPLIMIT', '1000000000'))

    def _wait(self, e, ev):
        s, v = ev
        w = self.waited[e]
        if w.get(s.num, 0) < v:
            self.eng[e].wait_ge(s, v)
            w[s.num] = v
            self.nwait += 1

    def _deps(self, e, reads, writes):
        own = self.sem[e].num
        for b in reads:
            if b.w is not None:
                if not (e == "pe" and b.w[0].num == own):
                    self._wait(e, b.w)
        for b in writes:
            if b.w is not None:
                if not (e == "pe" and b.w[0].num == own):
                    self._wait(e, b.w)
            for ev in b.r.values():
                if ev[0].num == own:
                    continue
                self._wait(e, ev)

    def _mark(self, ev, reads, writes):
        k = ev[0].num
        for b in reads:
            old = b.r.get(k)
            if old is None or old[1] < ev[1]:
                b.r[k] = ev
        for b in writes:
            b.w = ev
            b.r = {}

    def op(self, e, fn, r=(), w=()):
        self.nops += 1
        if self.nops > self.limit:
            return None
        if self.nops == self.limit:
            print('LAST OP', e, fn.__code__.co_firstlineno)
        self._deps(e, r, w)
        ins = fn(self.eng[e])
        self.cnt[e] += 1
        ins.then_inc(self.sem[e], 1)
        ev = (self.sem[e], self.cnt[e])
        self._mark(ev, r, w)
        return ev

    def mm(self, out_ap, pairs, r=(), w=()):
        self.nops += 1
        if self.nops > self.limit:
            return None
        self._deps("pe", r, w)
        n = len(pairs)
        ins = None
        for i, (l, rh) in enumerate(pairs):
            ins = self.nc.tensor.matmul(out_ap, lhsT=l, rhs=rh, start=(i == 0), stop=(i == n - 1))
        self.cnt["pe"] += 1
        ins.then_inc(self.sem["pe"], 1)
        ev = (self.sem["pe"], self.cnt["pe"])
        self._mark(ev, r, w)
        return ev

    def dma(self, q, fn, r=(), w=()):
        self.nops += 1
        if self.nops > self.limit:
            return None
        slots = self.dsem[q]
        i = self.dnext[q]
        self.dnext[q] = (i + 1) % len(slots)
        s, v = slots[i]
        if v > 0:
            self._wait(q, (s, v))
        self._deps(q, r, w)
        ins = fn(self.eng[q])
        slots[i][1] = v + 16
        ins.then_inc(s, 16)
        ev = (s, v + 16)
        self._mark(ev, r, w)
        return ev

    def all_events(self):
        evs = [(self.sem[k], self.cnt[k]) for k in self.eng if self.cnt[k] > 0]
        for q in self.dsem:
            for s, v in self.dsem[q]:
                if v > 0:
                    evs.append((s, v))
        return evs

    def barrier(self, engines=None):
        evs = self.all_events()
        for e in (engines or self.eng):
            for ev in evs:
                if ev[0].num != self.sem[e].num:
                    self._wait(e, ev)


def build(stop_after=None, dbg=()):
    nc = bass.Bass("TRN2", target_bir_lowering=False)
    dbg = set(dbg)

    def din(name, shape, dt=F32):
        return nc.dram_tensor(name, list(shape), dt, kind="ExternalInput").ap()

    def dscr(name, shape, dt):
        kind = "ExternalOutput" if name in dbg else "Internal"
        return nc.dram_tensor(name, list(shape), dt, kind=kind).ap()

    x_d = din("x", [T, D])
    ctx_d = din("ctx", [TC, D])
    cc_d = din("cc", [128, 8, 2])
    wada_d = din("w_ada", [128, 8, 6 * D])
    bada_d = din("b_ada", [1, 6 * D])
    norms_d = din("norms", [1, 4 * D])
    win_d = din("w_in", [128, 8, WCOLS])
    lamv_d = din("lamv", [1, 256])
    subln_d = din("subln", [128, 1])
    hgn_d = din("hgn", [1, 128])
    hlb_d = din("hlb", [128, 16])
    wout_d = din("w_out", [128, 8, D])
    wr_d = din("w_r", [128, 8, NE])
    wg_d = din("w_gate", [NE, 128, 8, D])
    wu_d = din("w_up", [NE, 128, 8, D])
    wd_d = din("w_down", [NE, 128, 8, D])
    cos_d = din("cosT", [128, S])
    sin_d = din("sinT", [128, S])
    cm_d = din("cmasks", [128, 4, 128])
    rm_d = din("rmask", [128, 2, 1024])
    iota_d = din("iota", [128, 1024])
    tokhl_d = din("tokhl", [128, NTILE, 2])
    out_d = nc.dram_tensor("out", [T, D], F32, kind="ExternalOutput").ap()

    modrows_d = dscr("modrows", [8, D], F32)
    qkt_d = dscr("qkt", [NH, 2, 128, S], BF16)
    zt_d = dscr("zt", [NH, 3, 128, S], F32)
    vi_d = dscr("vi", [S, NH, 2, 128], BF16)
    g_d = dscr("gsil", [S, NH, 128], F32)
    mixt_d = dscr("mixt", [D, T], BF16)
    x1_d = dscr("x1", [T, D], F32)
    h2_d = dscr("h2", [T, D], BF16)
    aff_d = dscr("aff", [T, NE], F32)
    f_d = dscr("facc", [T, D], F32)

    B_modrows = Buf()
    B_qkt = [[Buf() for _ in range(17)] for _ in range(NH)]
    B_zt = [[Buf() for _ in range(17)] for _ in range(NH)]
    B_vi = [Buf() for _ in range(17)]
    B_g = [Buf() for _ in range(17)]
    B_mixt = [Buf() for _ in range(16)]
    B_x1 = [Buf() for _ in range(NTILE)]
    B_h2 = Buf()
    B_aff = Buf()
    B_f = Buf()

    es = ExitStack()
    with es:
        P = Prog(nc, es)

        def sb(stack, name, shape, dt):
            return stack.enter_context(nc.sbuf_tensor("sb_" + name, list(shape), dt))

        def ps(stack, name, shape, dt=F32):
            return stack.enter_context(nc.psum_tensor("ps_" + name, list(shape), dt))

        cm_f = sb(es, "cm_f", [128, 4, 128], F32)
        ident_bf = sb(es, "ident_bf", [128, 128], BF16)
        ones_bf = sb(es, "ones_bf", [128, 128], BF16)
        ones_f = sb(es, "ones_f", [128, 128], F32)
        neglam = sb(es, "neglam", [128, 1], F32)
        subln8 = sb(es, "subln8", [128, 1], F32)
        lbt = sb(es, "lbt", [128, 8], F32)
        omlt = sb(es, "omlt", [128, 8], F32)
        mhalf = sb(es, "mhalf", [128, 512], F32)
        epsb = sb(es, "epsb", [128, 1], F32)
        B_const = Buf()
        ident_f = cm_f[:, 0, :]

        P.dma("sp", lambda q: q.dma_start(out=cm_f[:], in_=cm_d[:]), w=[B_const])
        P.op("dve", lambda v: v.tensor_copy(out=ident_bf[:], in_=cm_f[:, 0, :]), r=[B_const], w=[B_const])
        P.op("dve", lambda v: v.memset(ones_bf[:], 1.0), w=[B_const])
        P.op("dve", lambda v: v.memset(ones_f[:], 1.0), w=[B_const])
        P.op("dve", lambda v: v.memset(mhalf[:], -0.5), w=[B_const])
        P.op("dve", lambda v: v.memset(epsb[:], EPS), w=[B_const])

        def rstd_from_ss(ss_ap, n, out_ap, tmp_ap, bufs_r, bufs_w, tmpbuf):
            P.op("dve", lambda v: v.tensor_scalar(out=tmp_ap, in0=ss_ap, scalar1=1.0 / n, scalar2=EPS,
                                                  op0=ALU.mult, op1=ALU.add), r=bufs_r, w=[tmpbuf])
            shp = list(tmp_ap.shape)
            P.op("pool", lambda g: g.tensor_tensor(out=out_ap, in0=tmp_ap, in1=mhalf[0:shp[0], 0:shp[1]],
                                                   op=ALU.pow), r=[tmpbuf, B_const], w=bufs_w)

        with ExitStack() as p0:
            scf = sb(p0, "scf", [128, 8, 2], F32)
            sct = sb(p0, "sct", [128, 8, 2], F32)
            wa = [sb(p0, "wa%d" % i, [128, 8, 512], F32) for i in range(2)]
            modl = sb(p0, "modl", [1, 6 * D], F32)
            modc = sb(p0, "modc", [1, 6 * D], F32)
            bada = sb(p0, "bada", [1, 6 * D], F32)
            nrm = sb(p0, "nrm", [1, 4 * D], F32)
            rows = sb(p0, "rows", [1, 8, D], F32)
            lamv = sb(p0, "lamv", [1, 256], F32)
            lamt = sb(p0, "lamt", [1, 8], F32)
            hlb = sb(p0, "hlb", [128, 16], F32)
            sl = sb(p0, "sl", [128, 1], F32)
            pm = [ps(p0, "pm%d" % i, [1, 512]) for i in range(4)]
            pl = ps(p0, "pl", [128, 1])
            B_sc, B_wa, B_modl, B_modc, B_bada, B_nrm, B_rows, B_lam, B_hlb = (
                Buf(), [Buf(), Buf()], Buf(), Buf(), Buf(), Buf(), Buf(), Buf(), Buf())
            B_pm = [Buf() for _ in range(4)]
            B_pl = Buf()

            P.dma("sp", lambda q: q.dma_start(out=scf[:], in_=cc_d[:]), w=[B_sc])
            P.dma("sp", lambda q: q.dma_start(out=bada[:], in_=bada_d[:]), w=[B_bada])
            P.dma("sp", lambda q: q.dma_start(out=nrm[:], in_=norms_d[:]), w=[B_nrm])
            P.dma("sp", lambda q: q.dma_start(out=lamv[:], in_=lamv_d[:]), w=[B_lam])
            P.dma("sp", lambda q: q.dma_start(out=hlb[:], in_=hlb_d[:]), w=[B_hlb])
            P.dma("sp", lambda q: q.dma_start(out=sl[:], in_=subln_d[:]), w=[B_hlb])
            P.op("act", lambda a: a.activation(out=sct[:], in_=scf[:], func=AF.Exp, scale=-1.0), r=[B_sc], w=[B_rows])
            P.op("dve", lambda v: v.tensor_scalar(out=sct[:], in0=sct[:], scalar1=1.0, scalar2=None, op0=ALU.add),
                 r=[B_rows], w=[B_rows])
            P.op("dve", lambda v: v.reciprocal(out=sct[:], in_=sct[:]), r=[B_rows], w=[B_rows])
            P.op("dve", lambda v: v.tensor_tensor(out=scf[:], in0=scf[:], in1=sct[:], op=ALU.mult),
                 r=[B_rows, B_sc], w=[B_sc])
            for ch in range(12):
                wb_ = wa[ch % 2]
                P.dma("sp", lambda q: q.dma_start(out=wb_[:], in_=wada_d[:, :, ch * 512:(ch + 1) * 512]),
                      w=[B_wa[ch % 2]])
                for j, (mod, bm) in enumerate(((modl, B_modl), (modc, B_modc))):
                    pmt = pm[(2 * ch + j) % 4]
                    bp = B_pm[(2 * ch + j) % 4]
                    P.mm(pmt[:], [(scf[:, kc, j:j + 1], wb_[:, kc, :]) for kc in range(8)],
                         r=[B_sc, B_wa[ch % 2]], w=[bp])
                    P.op("dve", lambda v: v.tensor_tensor(out=mod[:, ch * 512:(ch + 1) * 512], in0=pmt[:],
                                                          in1=bada[:, ch * 512:(ch + 1) * 512], op=ALU.add),
                         r=[bp, B_bada], w=[bm])
            def stt(dst, a, b, op0):
                P.op("dve", lambda v: v.scalar_tensor_tensor(out=rows[:, dst, :], in0=a, scalar=(1.0 if op0 == ALU.add else 1.0),
                                                             in1=b, op0=op0, op1=ALU.mult),
                     r=[B_modl, B_modc, B_nrm], w=[B_rows])
            stt(0, modl[:, D:2 * D], nrm[:, 0:D], ALU.add)
            P.op("dve", lambda v: v.tensor_copy(out=rows[:, 1, :], in_=modl[:, 0:D]), r=[B_modl], w=[B_rows])
            stt(2, modc[:, D:2 * D], nrm[:, 0:D], ALU.add)
            P.op("dve", lambda v: v.tensor_copy(out=rows[:, 3, :], in_=modc[:, 0:D]), r=[B_modc], w=[B_rows])
            stt(4, modl[:, 2 * D:3 * D], nrm[:, D:2 * D], ALU.mult)
            stt(5, modl[:, 4 * D:5 * D], nrm[:, 2 * D:3 * D], ALU.add)
            P.op("dve", lambda v: v.tensor_copy(out=rows[:, 6, :], in_=modl[:, 3 * D:4 * D]), r=[B_modl], w=[B_rows])
            stt(7, modl[:, 5 * D:6 * D], nrm[:, 3 * D:4 * D], ALU.mult)
            P.dma("pool", lambda q: q.dma_start(out=modrows_d[:, :].rearrange("(o r) d -> o r d", o=1), in_=rows[:]),
                  r=[B_rows], w=[B_modrows])
            P.op("dve", lambda v: v.tensor_tensor(out=lamv[:, 0:64], in0=lamv[:, 0:64], in1=lamv[:, 64:128], op=ALU.mult),
                 r=[B_lam], w=[B_lam])
            P.op("dve", lambda v: v.tensor_tensor(out=lamv[:, 128:192], in0=lamv[:, 128:192], in1=lamv[:, 192:256], op=ALU.mult),
                 r=[B_lam], w=[B_lam])
            P.op("dve", lambda v: v.tensor_reduce(out=lamt[:, 0:1], in_=lamv[:, 0:64], axis=AX.X, op=ALU.add),
                 r=[B_lam], w=[B_lam])
            P.op("dve", lambda v: v.tensor_reduce(out=lamt[:, 1:2], in_=lamv[:, 128:192], axis=AX.X, op=ALU.add),
                 r=[B_lam], w=[B_lam])
            P.op("act", lambda a: a.activation(out=lamt[:, 2:4], in_=lamt[:, 0:2], func=AF.Exp), r=[B_lam], w=[B_lam])
            P.op("dve", lambda v: v.scalar_tensor_tensor(out=lamt[:, 4:5], in0=lamt[:, 3:4], scalar=-0.2, in1=lamt[:, 2:3],
                                                         op0=ALU.add, op1=ALU.subtract), r=[B_lam], w=[B_lam])
            P.mm(pl[:], [(ones_f[0:1, :], lamt[0:1, 4:5])], r=[B_lam, B_const], w=[B_pl])
            P.op("dve", lambda v: v.tensor_copy(out=neglam[:], in_=pl[:]), r=[B_pl], w=[B_const])
            P.op("dve", lambda v: v.tensor_scalar(out=subln8[:], in0=sl[:], scalar1=0.8, scalar2=None, op0=ALU.mult),
                 r=[B_hlb], w=[B_const])
            P.op("dve", lambda v: v.tensor_tensor(out=hlb[:, 0:8], in0=hlb[:, 8:16], in1=hlb[:, 0:8], op=ALU.subtract),
                 r=[B_hlb], w=[B_hlb])
            P.op("act", lambda a: a.activation(out=hlb[:, 0:8], in_=hlb[:, 0:8], func=AF.Exp), r=[B_hlb], w=[B_hlb])
            P.op("dve", lambda v: v.tensor_scalar(out=hlb[:, 0:8], in0=hlb[:, 0:8], scalar1=1.0, scalar2=None, op0=ALU.add),
                 r=[B_hlb], w=[B_hlb])
            P.op("dve", lambda v: v.reciprocal(out=lbt[:], in_=hlb[:, 0:8]), r=[B_hlb], w=[B_const])
            P.op("dve", lambda v: v.tensor_scalar(out=omlt[:], in0=lbt[:], scalar1=-1.0, scalar2=1.0, op0=ALU.mult, op1=ALU.add),
                 r=[B_const], w=[B_const])
            P.barrier()

        def load_bc(stack, name, row):
            t = sb(stack, name, [128, D], F32)
            b = Buf()
            P.dma("sp", lambda q: q.dma_start(out=t[:], in_=modrows_d[row:row + 1, :].partition_broadcast(128)),
                  r=[B_modrows], w=[b])
            return t, b

        def pe_group(fns, r, w):
            P._deps("pe", r, w)
            ins = None
            for fn in fns:
                ins = fn(nc.tensor)
            P.cnt["pe"] += 1
            ins.then_inc(P.sem["pe"], 1)
            ev = (P.sem["pe"], P.cnt["pe"])
            P._mark(ev, r, w)
            return ev

        SCS = [(0, 2)] + [(2 + 4 * k, 4) for k in range(16)]

        def phase_a0():
            with ExitStack() as st:
                wbf = sb(st, "wbf", [128, 8, WCOLS], BF16)
                B_w = [[Buf() for _ in range(4)] for _ in range(8)]
                for kc in range(8):
                    for pi in range(4):
                        c0 = pi * 1280
                        P.dma("pool", lambda q: q.dma_start(out=wbf[:, kc, c0:c0 + 1280], in_=win_d[:, kc, c0:c0 + 1280]),
                              w=[B_w[kc][pi]])
                B_wall = [b for l in B_w for b in l]
                gm_l, B_gml = load_bc(st, "gm_l", 0)
                sh_l, B_shl = load_bc(st, "sh_l", 1)
                gm_c, B_gmc = load_bc(st, "gm_c", 2)
                sh_c, B_shc = load_bc(st, "sh_c", 3)
                xb = [sb(st, "xb%d" % i, [128, D], F32) for i in range(2)]
                B_xb = [Buf(), Buf()]
                junk = sb(st, "junk", [128, D], BF16)
                B_junk = Buf()
                hn = [sb(st, "hn%d" % i, [128, D], F32) for i in range(2)]
                B_hn = [Buf(), Buf()]
                hb = [sb(st, "hb%d" % i, [128, D], BF16) for i in range(4)]
                B_hb = [Buf() for _ in range(4)]
                ssm = sb(st, "ssm", [128, 8], F32)
                B_ss = [Buf() for _ in range(4)]
                hT = [sb(st, "hT%d" % i, [128, 8, 512], BF16) for i in range(2)]
                B_hT = [Buf(), Buf()]
                cs = [sb(st, "cs%d" % i, [128, 2, 512], F32) for i in range(2)]
                B_cs = [Buf(), Buf()]
                qks = [sb(st, "qks%d" % i, [128, 2, 512], BF16) for i in range(2)]
                B_qks = [Buf(), Buf()]
                zs = [sb(st, "zs%d" % i, [128, 3, 512], F32) for i in range(2)]
                B_zs = [Buf(), Buf()]
                t1 = sb(st, "t1", [128, 512], F32)
                t2 = sb(st, "t2", [128, 512], F32)
                B_t1, B_t2 = Buf(), Buf()
                vis = [sb(st, "vis%d" % i, [128, NH, 2, 128], BF16) for i in range(2)]
                B_vis = [Buf(), Buf()]
                gs = [sb(st, "gs%d" % i, [128, NH, 128], F32) for i in range(2)]
                B_gs = [Buf(), Buf()]
                tp = [ps(st, "tp%d" % i, [128, 8, 128], BF16) for i in range(2)]
                B_tp = [Buf(), Buf()]
                fm = [ps(st, "fm%d" % i, [128, 512]) for i in range(3)]
                B_fm = [Buf() for _ in range(3)]
                tm = ps(st, "tm", [128, 1536])
                B_tm = Buf()
                fmi = [0]
                tile_ctr = [0]

                def stage1(k):
                    t0, nt = SCS[k]
                    for j in range(nt):
                        i = tile_ctr[0]
                        tile_ctr[0] += 1
                        x_ = xb[i % 2]
                        st_ = t0 + j
                        src = ctx_d[st_ * 128:(st_ + 1) * 128, :] if k == 0 else x_d[(st_ - 2) * 128:(st_ - 1) * 128, :]
                        gm, bgm, sh, bsh = (gm_c, B_gmc, sh_c, B_shc) if k == 0 else (gm_l, B_gml, sh_l, B_shl)
                        P.dma("sp", lambda q: q.dma_start(out=x_[:], in_=src), w=[B_xb[i % 2]])
                        ss = ssm[:, 2 * j:2 * j + 1]
                        rs = ssm[:, 2 * j + 1:2 * j + 2]
                        P.op("dve", lambda v: v.scalar_tensor_tensor(out=junk[:], in0=x_[:], scalar=1.0, in1=x_[:],
                                                                     op0=ALU.mult, op1=ALU.mult, accum_out=ss),
                             r=[B_xb[i % 2]], w=[B_junk, B_ss[j]])
                        rstd_from_ss(ss, D, rs, ss, [B_ss[j]], [B_ss[j]], B_ss[j])
                        h_ = hn[i % 2]
                        P.op("dve", lambda v: v.scalar_tensor_tensor(out=h_[:], in0=x_[:], scalar=rs, in1=gm[:],
                                                                     op0=ALU.mult, op1=ALU.mult),
                             r=[B_xb[i % 2], B_ss[j], bgm], w=[B_hn[i % 2]])
                        P.op("pool", lambda g: g.tensor_tensor(out=hb[j][:], in0=h_[:], in1=sh[:], op=ALU.add),
                             r=[B_hn[i % 2], bsh], w=[B_hb[j]])

                def stage2(k):
                    t0, nt = SCS[k]
                    for j in range(nt):
                        tpp = tp[j % 2]
                        pe_group([(lambda pe, kc=kc: pe.transpose(out=tpp[:, kc, :], in_=hb[j][:, kc * 128:(kc + 1) * 128],
                                                                   identity=ident_bf[:])) for kc in range(8)],
                                 r=[B_hb[j], B_const], w=[B_tp[j % 2]])
                        P.op("act", lambda a: a.copy(out=hT[k % 2][:, :, j * 128:(j + 1) * 128], in_=tpp[:]),
                             r=[B_tp[j % 2]], w=[B_hT[k % 2]])

                def fm_mm(k, col0, n):
                    bi = fmi[0] % 3
                    fmi[0] += 1
                    P.mm(fm[bi][:, 0:n], [(wbf[:, kc, col0:col0 + 128], hT[k % 2][:, kc, 0:n]) for kc in range(8)],
                         r=[B_hT[k % 2]] + B_wall, w=[B_fm[bi]])
                    return bi

                def stage3(k):
                    t0, nt = SCS[k]
                    n = nt * 128
                    s0 = t0 * 128
                    c_ = cs[k % 2]
                    P.dma("sp", lambda q: q.dma_start(out=c_[:, 0, 0:n], in_=cos_d[:, s0:s0 + n]), w=[B_cs[k % 2]])
                    P.dma("sp", lambda q: q.dma_start(out=c_[:, 1, 0:n], in_=sin_d[:, s0:s0 + n]), w=[B_cs[k % 2]])
                    for h in range(NH):
                        hi = k * NH + h
                        qk_ = qks[hi % 2]
                        z_ = zs[hi % 2]
                        base = h * FM_BLOCKS * 128
                        for t in range(2):
                            b0 = fm_mm(k, base + (2 * t) * 128, n)
                            b1 = fm_mm(k, base + (2 * t + 1) * 128, n)
                            P.op("dve", lambda v: v.tensor_tensor(out=t1[:, 0:n], in0=fm[b0][:, 0:n], in1=c_[:, 0, 0:n], op=ALU.mult),
                                 r=[B_fm[b0], B_cs[k % 2]], w=[B_t1])
                            P.op("dve", lambda v: v.tensor_tensor(out=t2[:, 0:n], in0=fm[b1][:, 0:n], in1=c_[:, 1, 0:n], op=ALU.mult),
                                 r=[B_fm[b1], B_cs[k % 2]], w=[B_t2])
                            P.op("pool", lambda g: g.tensor_tensor(out=qk_[:, t, 0:n], in0=t1[:, 0:n], in1=t2[:, 0:n], op=ALU.add),
                                 r=[B_t1, B_t2], w=[B_qks[hi % 2]])
                        for t in range(3):
                            b0 = fm_mm(k, base + (4 + t) * 128, n)
                            P.op("act", lambda a: a.copy(out=z_[:, t, 0:n], in_=fm[b0][:, 0:n]), r=[B_fm[b0]], w=[B_zs[hi % 2]])
                        P.dma("pool", lambda q: q.dma_start(out=qkt_d[h].rearrange("t p s -> p t s")[:, :, s0:s0 + n],
                                                            in_=qk_[:, :, 0:n]), r=[B_qks[hi % 2]], w=[B_qkt[h][k]])
                        P.dma("pool", lambda q: q.dma_start(out=zt_d[h].rearrange("t p s -> p t s")[:, :, s0:s0 + n],
                                                            in_=z_[:, :, 0:n]), r=[B_zs[hi % 2]], w=[B_zt[h][k]])
                    for j in range(nt):
                        ti = t0 + j
                        for nb in range(3):
                            P.mm(tm[:, nb * 512:(nb + 1) * 512],
                                 [(hT[k % 2][:, kc, j * 128:(j + 1) * 128], wbf[:, kc, FM_TOTAL + nb * 512:FM_TOTAL + (nb + 1) * 512])
                                  for kc in range(8)], r=[B_hT[k % 2]] + B_wall, w=[B_tm])
                        v_ = vis[ti % 2]
                        g_ = gs[ti % 2]
                        vflat = v_[:].rearrange("p h t c -> p (h t c)")
                        P.op("dve", lambda v: v.tensor_copy(out=vflat[:, 0:512], in_=tm[:, 0:512]),
                             r=[B_tm], w=[B_vis[ti % 2]])
                        P.op("dve", lambda v: v.tensor_copy(out=vflat[:, 512:1024], in_=tm[:, 512:1024]),
                             r=[B_tm], w=[B_vis[ti % 2]])
                        P.op("act", lambda a: a.activation(out=g_[:].rearrange("p h c -> p (h c)"), in_=tm[:, 1024:1536], func=AF.Silu),
                             r=[B_tm], w=[B_gs[ti % 2]])
                        P.dma("pool", lambda q: q.dma_start(out=vi_d[ti * 128:(ti + 1) * 128], in_=v_[:]),
                              r=[B_vis[ti % 2]], w=[B_vi[k]])
                        P.dma("pool", lambda q: q.dma_start(out=g_d[ti * 128:(ti + 1) * 128], in_=g_[:]),
                              r=[B_gs[ti % 2]], w=[B_g[k]])

                stage1(0)
                stage2(0)
                for k in range(17):
                    if k + 1 < 17:
                        stage1(k + 1)
                    stage3(k)
                    if k + 1 < 17:
                        stage2(k + 1)
                P.barrier()

        def mm1(out_ap, lhsT, rhs, start, stop, r, w):
            P.nops += 1
            if P.nops > P.limit:
                return None
            P._deps("pe", r, w)
            ins = nc.tensor.matmul(out_ap, lhsT=lhsT, rhs=rhs, start=start, stop=stop)
            P.cnt["pe"] += 1
            ins.then_inc(P.sem["pe"], 1)
            ev = (P.sem["pe"], P.cnt["pe"])
            P._mark(ev, r, w)
            return ev

        NKT = S // 128

        def phase_a1(heads=range(NH), qchunks=range(16)):
            with ExitStack() as st:
                kt = [sb(st, "kt%d" % i, [128, S], BF16) for i in range(2)]
                vv = [sb(st, "vv%d" % i, [128, NKT, 128], BF16) for i in range(2)]
                B_kt = [Buf(), Buf()]
                B_vv = [Buf(), Buf()]
                qt = [sb(st, "qt%d" % i, [128, 512], BF16) for i in range(2)]
                B_qt = [Buf(), Buf()]
                p1 = [sb(st, "p1_%d" % i, [128, 512], BF16) for i in range(3)]
                p2 = [sb(st, "p2_%d" % i, [128, 512], BF16) for i in range(3)]
                B_p1 = [Buf() for _ in range(3)]
                B_p2 = [Buf() for _ in range(3)]
                acc1s = [sb(st, "acc1_%d" % i, [128, 512], F32) for i in range(2)]
                acc2s = [sb(st, "acc2_%d" % i, [128, 512], F32) for i in range(2)]
                B_acc1s, B_acc2s = [Buf(), Buf()], [Buf(), Buf()]
                o1c = sb(st, "o1c", [128, 512], F32)
                o2c = sb(st, "o2c", [128, 512], F32)
                B_o1c, B_o2c = Buf(), Buf()
                r1 = sb(st, "r1", [128, 512], F32)
                o1 = sb(st, "o1", [128, 512], F32)
                r2 = sb(st, "r2", [128, 512], F32)
                o2 = sb(st, "o2", [128, 512], F32)
                oo = sb(st, "oo", [128, 512], F32)
                sq = sb(st, "sq", [128, 512], F32)
                rs = sb(st, "rs", [128, 512], F32)
                ob = [sb(st, "ob%d" % i, [128, 512], BF16) for i in range(2)]
                B_r1, B_o1, B_r2, B_o2, B_oo, B_sq, B_rs = Buf(), Buf(), Buf(), Buf(), Buf(), Buf(), Buf()
                B_ob = [Buf(), Buf()]
                s1 = [ps(st, "s1_%d" % i, [128, 512]) for i in range(2)]
                s2 = [ps(st, "s2_%d" % i, [128, 512]) for i in range(2)]
                B_s1 = [Buf(), Buf()]
                B_s2 = [Buf(), Buf()]
                o1p = ps(st, "o1p", [128, 512])
                o2p = ps(st, "o2p", [128, 512])
                l1p = ps(st, "l1p", [128, 512])
                l2p = ps(st, "l2p", [128, 512])
                B_o1p, B_o2p, B_l1p, B_l2p = Buf(), Buf(), Buf(), Buf()
                def finalize(h, qc, acc1, acc2, B_acc1, B_acc2, o_, bo):
                    P.mm(l1p[:], [(ones_f[:], acc1[:])], r=[B_acc1, B_const], w=[B_l1p])
                    P.mm(l2p[:], [(ones_f[:], acc2[:])], r=[B_acc2, B_const], w=[B_l2p])
                    P.op("act", lambda a: a.activation(out=r1[:], in_=l1p[:], func=AF.Ln), r=[B_l1p], w=[B_r1])
                    P.op("act", lambda a: a.activation(out=r1[:], in_=r1[:], func=AF.Exp, scale=-1.0), r=[B_r1], w=[B_r1])
                    P.op("dve", lambda v: v.tensor_tensor(out=o1[:], in0=o1c[:], in1=r1[:], op=ALU.mult),
                         r=[B_o1c, B_r1], w=[B_o1])
                    P.op("act", lambda a: a.activation(out=r2[:], in_=l2p[:], func=AF.Ln), r=[B_l2p], w=[B_r2])
                    P.op("act", lambda a: a.activation(out=r2[:], in_=r2[:], func=AF.Exp, scale=-1.0), r=[B_r2], w=[B_r2])
                    P.op("dve", lambda v: v.tensor_tensor(out=o2[:], in0=o2c[:], in1=r2[:], op=ALU.mult),
                         r=[B_o2c, B_r2], w=[B_o2])
                    P.op("dve", lambda v: v.scalar_tensor_tensor(out=oo[:], in0=o2[:], scalar=neglam[:, 0:1], in1=o1[:],
                                                                 op0=ALU.mult, op1=ALU.add),
                         r=[B_o2, B_o1, B_const], w=[B_oo])
                    P.op("dve", lambda v: v.tensor_tensor(out=sq[:], in0=oo[:], in1=oo[:], op=ALU.mult),
                         r=[B_oo], w=[B_sq])
                    P.mm(l1p[:], [(ones_f[:], sq[:])], r=[B_sq, B_const], w=[B_l1p])
                    P.op("act", lambda a: a.activation(out=rs[:], in_=l1p[:], func=AF.Ln, scale=1.0 / 128, bias=epsb[:, 0:1]),
                         r=[B_l1p, B_const], w=[B_rs])
                    P.op("act", lambda a: a.activation(out=rs[:], in_=rs[:], func=AF.Exp, scale=-0.5), r=[B_rs], w=[B_rs])
                    P.op("dve", lambda v: v.scalar_tensor_tensor(out=o_[:], in0=oo[:], scalar=subln8[:, 0:1], in1=rs[:],
                                                                 op0=ALU.mult, op1=ALU.mult),
                         r=[B_oo, B_rs, B_const], w=[bo])
                    P.dma("pool", lambda q: q.dma_start(out=mixt_d[h * 128:(h + 1) * 128, qc * 512:(qc + 1) * 512], in_=o_[:]),
                          r=[bo], w=[B_mixt[qc]])

                pending = None
                cnt = 0
                hl = list(heads)

                def load_kv(hi):
                    hh = hl[hi]
                    P.dma("sp", lambda q: q.dma_start(out=kt[hi % 2][:], in_=qkt_d[hh, 1]), r=B_qkt[hh], w=[B_kt[hi % 2]])
                    vsrc = vi_d[:, hh, 0, :].rearrange("(t p) c -> p t c", p=128)
                    for part in range(3):
                        P.dma("sp", lambda q: q.dma_start(out=vv[hi % 2][:, part * 22:(part + 1) * 22, :],
                                                          in_=vsrc[:, part * 22:(part + 1) * 22, :]),
                              r=B_vi, w=[B_vv[hi % 2]])

                load_kv(0)
                for hi, h in enumerate(hl):
                    k_ = kt[hi % 2]
                    v_ = vv[hi % 2]
                    if hi + 1 < len(hl):
                        load_kv(hi + 1)
                    for qc in qchunks:
                        q_ = qt[cnt % 2]
                        bq = B_qt[cnt % 2]
                        o_ = ob[cnt % 2]
                        bo = B_ob[cnt % 2]
                        acc1, acc2 = acc1s[cnt % 2], acc2s[cnt % 2]
                        B_acc1, B_acc2 = B_acc1s[cnt % 2], B_acc2s[cnt % 2]
                        cnt += 1
                        P.dma("sp", lambda q: q.dma_start(out=q_[:], in_=qkt_d[h, 0][:, TC + qc * 512:TC + (qc + 1) * 512]),
                              r=B_qkt[h], w=[bq])

                        def scores(i):
                            mm1(s1[i % 2][:], k_[0:64, i * 128:(i + 1) * 128], q_[0:64, :], True, True,
                                [B_kt[hi % 2], bq], [B_s1[i % 2]])
                            mm1(s2[i % 2][:], k_[64:128, i * 128:(i + 1) * 128], q_[64:128, :], True, True,
                                [B_kt[hi % 2], bq], [B_s2[i % 2]])

                        scores(0)
                        scores(1)
                        for i in range(NKT):
                            if i == 8 and pending is not None:
                                finalize(*pending)
                                pending = None
                            pa, pb = p1[i % 3], p2[i % 3]
                            P.op("act", lambda a: a.activation(out=pa[:], in_=s1[i % 2][:], func=AF.Exp, scale=0.125),
                                 r=[B_s1[i % 2]], w=[B_p1[i % 3]])
                            P.op("act", lambda a: a.activation(out=pb[:], in_=s2[i % 2][:], func=AF.Exp, scale=0.125),
                                 r=[B_s2[i % 2]], w=[B_p2[i % 3]])
                            st_, sp_ = (i == 0), (i == NKT - 1)
                            P._deps("pe", [B_p1[i % 3], B_p2[i % 3]], [])
                            mm1(o1p[:], v_[:, i, :], pa[:], st_, sp_, [B_vv[hi % 2], B_p1[i % 3]], [B_o1p])
                            mm1(o2p[:], v_[:, i, :], pb[:], st_, sp_, [B_vv[hi % 2], B_p2[i % 3]], [B_o2p])
                            if i == 0:
                                P.op("dve", lambda v: v.tensor_copy(out=acc1[:], in_=pa[:]), r=[B_p1[i % 3]], w=[B_acc1])
                                P.op("dve", lambda v: v.tensor_copy(out=acc2[:], in_=pb[:]), r=[B_p2[i % 3]], w=[B_acc2])
                            else:
                                P.op("dve", lambda v: v.tensor_tensor(out=acc1[:], in0=acc1[:], in1=pa[:], op=ALU.add),
                                     r=[B_p1[i % 3], B_acc1], w=[B_acc1])
                                P.op("dve", lambda v: v.tensor_tensor(out=acc2[:], in0=acc2[:], in1=pb[:], op=ALU.add),
                                     r=[B_p2[i % 3], B_acc2], w=[B_acc2])
                            if i + 2 < NKT:
                                scores(i + 2)
                        P.op("act", lambda a: a.copy(out=o1c[:], in_=o1p[:]), r=[B_o1p], w=[B_o1c])
                        P.op("dve", lambda v: v.tensor_copy(out=o2c[:], in_=o2p[:]), r=[B_o2p], w=[B_o2c])
                        pending = (h, qc, acc1, acc2, B_acc1, B_acc2, o_, bo)
                if pending is not None:
                    finalize(*pending)
                P.barrier()

        def phase_a2(heads=range(NH)):
            with ExitStack() as st:
                o_accs = [sb(st, "o_acc%d" % i, [128, NTILE, 128], F32) for i in range(2)]
                B_oaccs = [[Buf() for _ in range(NTILE)] for _ in range(2)]
                rm = sb(st, "rm", [128, 2, 512], F32)
                cmA = sb(st, "cmA", [128, 512], BF16)
                cmB = sb(st, "cmB", [128, 512], BF16)
                hgn = sb(st, "hgn_b", [128, 128], F32)
                B_c2 = Buf()
                P.dma("sp", lambda q: q.dma_start(out=rm[:], in_=rm_d[:, :, 0:512]), w=[B_c2])
                P.dma("sp", lambda q: q.dma_start(out=hgn[:], in_=hgn_d[0:1, :].partition_broadcast(128)), w=[B_c2])
                P.op("dve", lambda v: v.memset(cmA[:], 0.0), w=[B_c2])
                P.op("dve", lambda v: v.memset(cmB[:], 0.0), w=[B_c2])
                P.op("dve", lambda v: v.memset(cmA[:].rearrange("p (t c) -> p t c", c=128)[:, :, 0:64], 1.0), w=[B_c2])
                P.op("dve", lambda v: v.memset(cmB[:].rearrange("p (t c) -> p t c", c=128)[:, :, 64:128], 1.0), w=[B_c2])

                def dbl(name, shape, dt):
                    return [sb(st, "%s%d" % (name, i), shape, dt) for i in range(2)], [Buf(), Buf()]
                zin, B_zin = dbl("zin", [128, 2, 512], F32)
                vt, B_vt = dbl("vt", [128, 4, 128], BF16)
                gt, B_gt = dbl("gt", [128, 4, 128], F32)
                e_, B_e = dbl("e_", [128, 512], F32)
                f_, B_f_ = dbl("f_", [128, 512], F32)
                lf, B_lf = dbl("lf", [128, 512], F32)
                kk, B_kk = dbl("kk", [128, 512], F32)
                bc, B_bc = dbl("bc", [128, 512], F32)
                ep, B_ep = dbl("ep", [128, 512], F32)
                en, B_en = dbl("en", [128, 512], F32)
                kdf, B_kdf = dbl("kdf", [128, 512], F32)
                Qd, B_Qd = dbl("Qd", [128, 512], BF16)
                QdA, B_QdA = dbl("QdA", [128, 512], BF16)
                QdB, B_QdB = dbl("QdB", [128, 512], BF16)
                Kd, B_Kd = dbl("Kd", [128, 512], BF16)
                K2T, B_K2T = dbl("K2T", [128, 512], BF16)
                dec, B_dec = dbl("dec", [128, 8], F32)
                k2all, B_k2all = dbl("k2all", [128, 4, 128], BF16)
                scma, B_scma = dbl("scma", [128, 4, 128], BF16)
                Sball, B_Sball = dbl("Sball", [128, 8, 128], BF16)
                ScarA, B_ScarA = dbl("ScarA", [128, 128], BF16)
                ScarB, B_ScarB = dbl("ScarB", [128, 128], BF16)
                Sst2 = [sb(st, "Sst%d" % i, [128, 128], F32) for i in range(2)]
                B_S2 = [Buf(), Buf()]
                otot, B_otot = dbl("otot", [128, 4, 128], F32)
                osq, B_osq = dbl("osq", [128, 4, 128], F32)
                osm4, B_osm4 = dbl("osm4", [128, 8], F32)
                yb4, B_yb4 = dbl("yb4", [128, 4, 128], BF16)
                mixs, B_mixs = dbl("mixs", [128, 512], BF16)
                tpb = ps(st, "tpb", [128, 1024], BF16)
                B_tpb = Buf()
                scp = ps(st, "scp", [128, 512])
                B_scp = Buf()
                ups2 = [ps(st, "ups%d" % i, [128, 1024]) for i in range(2)]
                B_ups2 = [[Buf() for _ in range(8)] for _ in range(2)]
                ops2 = [ps(st, "ops%d" % i, [128, 512]) for i in range(2)]
                B_ops2 = [Buf(), Buf()]
                ctr = dict(sc=0, tile=0, ch=0, tp=0)
                for i_ in range(2):
                    P.op("dve", lambda v: v.memset(QdA[i_][:], 0.0), w=[B_QdA[i_]])
                    P.op("dve", lambda v: v.memset(QdB[i_][:], 0.0), w=[B_QdB[i_]])

                def chain(h, d):
                    if True:
                        Sst = Sst2[d]
                        B_S = B_S2[d]
                        Scar, B_Scar = (ScarA, B_ScarA) if d == 0 else (ScarB, B_ScarB)
                        o_acc = o_accs[d]
                        B_oacc = B_oaccs[d]
                        cp = 0
                        col = d * 4 + h
                        lb_ap = lbt[:, col:col + 1]
                        oml_ap = omlt[:, col:col + 1]
                        P.op("dve", lambda v: v.memset(Sst[:], 0.0), w=[B_S])
                        P.op("dve", lambda v: v.memset(Scar[0][:], 0.0), w=[B_Scar[0]])
                        P.op("dve", lambda v: v.memset(Scar[1][:], 0.0), w=[B_Scar[1]])
                        order = list(range(17)) if d == 0 else [0] + list(range(16, 0, -1))
                        for k in order:
                            t0, nt = SCS[k]
                            n = nt * 128
                            s0 = t0 * 128
                            nch = n // 64
                            lat = k >= 1
                            i2 = d
                            z_ = zin[i2]
                            P.dma("sp", lambda q: q.dma_start(out=z_[:, 0, 0:n], in_=zt_d[h, d][:, s0:s0 + n]),
                                  r=B_zt[h], w=[B_zin[i2]])
                            P.dma("sp", lambda q: q.dma_start(out=z_[:, 1, 0:n], in_=zt_d[h, 2][:, s0:s0 + n]),
                                  r=B_zt[h], w=[B_zin[i2]])
                            P.dma("sp", lambda q: q.dma_start(
                                out=vt[i2][:, 0:nt, :], in_=vi_d[s0:s0 + n, h, 1, :].rearrange("(t p) c -> p t c", p=128)),
                                r=B_vi, w=[B_vt[i2]])
                            zz = z_[:, 0, 0:n]
                            hq = z_[:, 1, 0:n]
                            P.op("act", lambda a: a.activation(out=e_[i2][:, 0:n], in_=zz, func=AF.Exp, scale=-1.0),
                                 r=[B_zin[i2]], w=[B_e[i2]])
                            yield
                            P.op("dve", lambda v: v.tensor_scalar(out=e_[i2][:, 0:n], in0=e_[i2][:, 0:n], scalar1=1.0, scalar2=None,
                                                                  op0=ALU.add), r=[B_e[i2]], w=[B_e[i2]])
                            yield
                            P.op("act", lambda a: a.activation(out=e_[i2][:, 0:n], in_=e_[i2][:, 0:n], func=AF.Ln), r=[B_e[i2]], w=[B_e[i2]])
                            P.op("act", lambda a: a.activation(out=e_[i2][:, 0:n], in_=e_[i2][:, 0:n], func=AF.Exp, scale=-1.0),
                                 r=[B_e[i2]], w=[B_e[i2]])
                            yield
                            P.op("dve", lambda v: v.tensor_scalar(out=f_[i2][:, 0:n], in0=e_[i2][:, 0:n], scalar1=oml_ap, scalar2=lb_ap,
                                                                  op0=ALU.mult, op1=ALU.add), r=[B_e[i2], B_const], w=[B_f_[i2]])
                            yield
                            P.op("act", lambda a: a.activation(out=lf[i2][:, 0:n], in_=f_[i2][:, 0:n], func=AF.Ln),
                                 r=[B_f_[i2]], w=[B_lf[i2]])
                            P.op("act", lambda a: a.activation(out=kk[i2][:, 0:n], in_=f_[i2][:, 0:n], func=AF.Copy, scale=-1.0, bias=1.0),
                                 r=[B_f_[i2]], w=[B_kk[i2]])
                            yield
                            if d == 0:
                                P.op("dve", lambda v: v.tensor_tensor_scan(out=bc[i2][:, 0:n], data0=rm[:, 0, 0:n], data1=lf[i2][:, 0:n],
                                                                           initial=0.0, op0=ALU.mult, op1=ALU.add),
                                     r=[B_lf[i2], B_c2], w=[B_bc[i2]])
                            else:
                                P.op("dve", lambda v: v.tensor_tensor_scan(out=bc[i2][:, 0:n][:, ::-1], data0=rm[:, 1, 0:n][:, ::-1],
                                                                           data1=lf[i2][:, 0:n][:, ::-1],
                                                                           initial=0.0, op0=ALU.mult, op1=ALU.add),
                                     r=[B_lf[i2], B_c2], w=[B_bc[i2]])
                            yield
                            P.op("act", lambda a: a.activation(out=ep[i2][:, 0:n], in_=bc[i2][:, 0:n], func=AF.Exp),
                                 r=[B_bc[i2]], w=[B_ep[i2]])
                            P.op("act", lambda a: a.activation(out=en[i2][:, 0:n], in_=bc[i2][:, 0:n], func=AF.Exp, scale=-1.0),
                                 r=[B_bc[i2]], w=[B_en[i2]])
                            yield
                            if lat:
                                P.op("dve", lambda v: v.tensor_tensor(out=Qd[i2][:, 0:n], in0=hq, in1=ep[i2][:, 0:n], op=ALU.mult),
                                     r=[B_zin[i2], B_ep[i2]], w=[B_Qd[i2]])
                                P.op("act", lambda a: a.copy(out=QdA[i2][:, 0:n].rearrange("p (t c) -> p t c", c=128)[:, :, 0:64],
                                                             in_=Qd[i2][:, 0:n].rearrange("p (t c) -> p t c", c=128)[:, :, 0:64]),
                                     r=[B_Qd[i2]], w=[B_QdA[i2]])
                                P.op("act", lambda a: a.copy(out=QdB[i2][:, 0:n].rearrange("p (t c) -> p t c", c=128)[:, :, 64:128],
                                                             in_=Qd[i2][:, 0:n].rearrange("p (t c) -> p t c", c=128)[:, :, 64:128]),
                                     r=[B_Qd[i2]], w=[B_QdB[i2]])
                            P.op("pool", lambda g: g.tensor_tensor(out=kdf[i2][:, 0:n], in0=kk[i2][:, 0:n], in1=en[i2][:, 0:n], op=ALU.mult),
                                 r=[B_kk[i2], B_en[i2]], w=[B_kdf[i2]])
                            if lat:
                                P.op("act", lambda a: a.copy(out=Kd[i2][:, 0:n], in_=kdf[i2][:, 0:n]),
                                     r=[B_kdf[i2]], w=[B_Kd[i2]])
                            endcol = 63 if d == 0 else 0
                            P.op("dve", lambda v: v.tensor_copy(out=dec[i2][:, 0:nch],
                                                                in_=ep[i2][:, 0:n].rearrange("p (c j) -> p c j", j=64)[:, :, endcol]),
                                 r=[B_ep[i2]], w=[B_dec[i2]])
                            P.op("dve", lambda v: v.tensor_tensor(
                                out=K2T[i2][:, 0:n].rearrange("p (c j) -> p c j", j=64),
                                in0=kdf[i2][:, 0:n].rearrange("p (c j) -> p c j", j=64),
                                in1=dec[i2][:, 0:nch].unsqueeze(2).to_broadcast([128, nch, 64]), op=ALU.mult),
                                r=[B_kdf[i2], B_dec[i2]], w=[B_K2T[i2]])
                            yield
                            pe_group([(lambda pe, j=j: pe.transpose(out=tpb[:, j * 128:(j + 1) * 128], in_=K2T[i2][:, j * 128:(j + 1) * 128],
                                                                     identity=ident_bf[:])) for j in range(nt)],
                                     r=[B_K2T[i2], B_const], w=[B_tpb])
                            P.op("act", lambda a: a.copy(out=k2all[d][:, 0:nt, :].rearrange("p t c -> p (t c)"), in_=tpb[:, 0:n]),
                                 r=[B_tpb], w=[B_k2all[d]])
                            for gc in range(nch):
                                j, c = gc // 2, gc % 2
                                rows = slice(c * 64, (c + 1) * 64)
                                uo = c * 512 + j * 128
                                mm1(ups2[d][:, uo:uo + 128], k2all[d][rows, j, :], vt[i2][rows, j, :], True, True,
                                    [B_k2all[d], B_vt[i2]], [B_ups2[d][c]])
                            if lat:
                                for j in range(nt):
                                    cs_ = slice(j * 128, (j + 1) * 128)
                                    mm1(scp[:, cs_], Kd[i2][:, cs_], Qd[i2][:, cs_], True, True, [B_Kd[i2], B_Qd[i2]], [B_scp])
                                P.op("dve", lambda v: v.tensor_tensor(
                                    out=scma[d][:], in0=scp[:, :].rearrange("p (t c) -> p t c", c=128),
                                    in1=cm_f[:, 1 + d, :].unsqueeze(1).to_broadcast([128, 4, 128]), op=ALU.mult),
                                    r=[B_scp, B_const], w=[B_scma[d]])
                            yield
                            seq = list(range(nch)) if d == 0 else list(range(nch - 1, -1, -1))
                            for m, gc in enumerate(seq):
                                last = m == nch - 1
                                uo = (gc % 2) * 512 + (gc // 2) * 128
                                if last:
                                    dst, bdst = Scar[1 - cp][:], B_Scar[1 - cp]
                                else:
                                    dst, bdst = Sball[d][:, seq[m + 1], :], B_Sball[d]
                                if lat or last:
                                    P.op("dve", lambda v: v.scalar_tensor_tensor(out=dst, in0=Sst[:], scalar=dec[i2][:, gc:gc + 1],
                                                                                 in1=ups2[d][:, uo:uo + 128],
                                                                                 op0=ALU.mult, op1=ALU.add),
                                         r=[B_S, B_dec[i2], B_ups2[d][gc % 2]], w=[bdst])
                                P.op("dve", lambda v: v.scalar_tensor_tensor(out=Sst[:], in0=Sst[:], scalar=dec[i2][:, gc:gc + 1],
                                                                             in1=ups2[d][:, uo:uo + 128],
                                                                             op0=ALU.mult, op1=ALU.add),
                                     r=[B_S, B_dec[i2], B_ups2[d][gc % 2]], w=[B_S])
                            yield
                            if lat:
                                for j in range(nt):
                                    cs_ = slice(j * 128, (j + 1) * 128)
                                    mm1(ops2[d][:, cs_], scma[d][:, j, :], vt[i2][:, j, :], True, False,
                                        [B_scma[d], B_vt[i2]], [B_ops2[d]])
                                    for c in range(2):
                                        gc = 2 * j + c
                                        qsel = QdA if c == 0 else QdB
                                        bq = B_QdA if c == 0 else B_QdB
                                        if gc == seq[0]:
                                            sap, bs = Scar[cp][:], B_Scar[cp]
                                        else:
                                            sap, bs = Sball[d][:, gc, :], B_Sball[d]
                                        mm1(ops2[d][:, cs_], qsel[i2][:, cs_], sap, False, c == 1, [bq[i2], bs], [B_ops2[d]])
                                gt0 = (k - 1) * 4
                                P.op("act", lambda a: a.copy(out=o_acc[:, gt0:gt0 + 4, :].rearrange("p t c -> p (t c)"), in_=ops2[d][:, :]),
                                     r=[B_ops2[d]], w=[B_oacc[gt0 + jj] for jj in range(4)])
                            cp = 1 - cp
                            yield

                def combine(h):
                    for k in range(1, 17):
                        t0, nt = SCS[k]
                        s0 = t0 * 128
                        i2 = k % 2
                        gt0 = (k - 1) * 4
                        P.dma("sp", lambda q: q.dma_start(
                            out=gt[i2][:, 0:4, :], in_=g_d[s0:s0 + 512, h, :].rearrange("(t p) c -> p t c", p=128)),
                            r=B_g, w=[B_gt[i2]])
                        ro = [B_oaccs[dd][gt0 + jj] for dd in range(2) for jj in range(4)]
                        P.op("dve", lambda v: v.tensor_tensor(out=otot[i2][:], in0=o_accs[0][:, gt0:gt0 + 4, :],
                                                              in1=o_accs[1][:, gt0:gt0 + 4, :], op=ALU.add), r=ro, w=[B_otot[i2]])
                        P.op("dve", lambda v: v.tensor_tensor(out=osq[i2][:], in0=otot[i2][:], in1=otot[i2][:], op=ALU.mult),
                             r=[B_otot[i2]], w=[B_osq[i2]])
                        P.op("dve", lambda v: v.tensor_reduce(out=osm4[i2][:, 0:4], in_=osq[i2][:], axis=AX.X, op=ALU.add),
                             r=[B_osq[i2]], w=[B_osm4[i2]])
                        rstd_from_ss(osm4[i2][:, 0:4], 128, osm4[i2][:, 4:8], osm4[i2][:, 0:4], [B_osm4[i2]], [B_osm4[i2]], B_osm4[i2])
                        P.op("dve", lambda v: v.tensor_tensor(out=otot[i2][:], in0=otot[i2][:],
                                                              in1=osm4[i2][:, 4:8].unsqueeze(2).to_broadcast([128, 4, 128]), op=ALU.mult),
                             r=[B_otot[i2], B_osm4[i2]], w=[B_otot[i2]])
                        P.op("dve", lambda v: v.tensor_tensor(out=otot[i2][:], in0=otot[i2][:],
                                                              in1=hgn[:].unsqueeze(1).to_broadcast([128, 4, 128]), op=ALU.mult),
                             r=[B_otot[i2], B_c2], w=[B_otot[i2]])
                        P.op("pool", lambda g: g.tensor_tensor(out=yb4[i2][:], in0=otot[i2][:], in1=gt[i2][:, 0:4, :], op=ALU.mult),
                             r=[B_otot[i2], B_gt[i2]], w=[B_yb4[i2]])
                        pe_group([(lambda pe, j=j: pe.transpose(out=tpb[:, j * 128:(j + 1) * 128], in_=yb4[i2][:, j, :],
                                                                 identity=ident_bf[:])) for j in range(4)],
                                 r=[B_yb4[i2], B_const], w=[B_tpb])
                        P.op("act", lambda a: a.copy(out=mixs[i2][:], in_=tpb[:, 0:512]), r=[B_tpb], w=[B_mixs[i2]])
                        P.dma("pool", lambda q: q.dma_start(
                            out=mixt_d[512 + h * 128:512 + (h + 1) * 128, (k - 1) * 512:k * 512], in_=mixs[i2][:]),
                            r=[B_mixs[i2]], w=[B_mixt[k - 1]])

                for h in heads:
                    alive = [chain(h, 0), chain(h, 1)]
                    while alive:
                        for g_ in list(alive):
                            try:
                                next(g_)
                            except StopIteration:
                                alive.remove(g_)
                    combine(h)
                P.barrier()

        AFF = sb(es, "AFF", [128, NTILE, NE], F32)
        B_AFF = [Buf() for _ in range(NTILE)]
        B_h2t = [Buf() for _ in range(NTILE)]
        B_afft = [Buf() for _ in range(NTILE)]

        def phase_b():
            with ExitStack() as st:
                wo = sb(st, "wo", [128, 8, D], BF16)
                B_wo = [Buf() for _ in range(4)]
                for pi in range(4):
                    P.dma("pool", lambda q: q.dma_start(out=wo[:, 2 * pi:2 * pi + 2, :], in_=wout_d[:, 2 * pi:2 * pi + 2, :]),
                          w=[B_wo[pi]])
                wr = sb(st, "wr", [128, 8, NE], F32)
                B_wr = Buf()
                P.dma("sp", lambda q: q.dma_start(out=wr[:], in_=wr_d[:]), w=[B_wr])
                gpm, B_gpm = load_bc(st, "gpm", 4)
                g2m, B_g2m = load_bc(st, "g2m", 5)
                sh2, B_sh2 = load_bc(st, "sh2", 6)
                mix = [sb(st, "mix%d" % i, [128, 8, 512], BF16) for i in range(2)]
                B_mix = [Buf(), Buf()]
                xb = [sb(st, "bxb%d" % i, [128, D], F32) for i in range(2)]
                B_xb = [Buf(), Buf()]
                tt = [sb(st, "btt%d" % i, [128, D], F32) for i in range(2)]
                B_tt = [Buf(), Buf()]
                x1 = [sb(st, "bx1%d" % i, [128, D], F32) for i in range(2)]
                B_x1s = [Buf(), Buf()]
                h2f = [sb(st, "h2f%d" % i, [128, D], F32) for i in range(2)]
                B_h2f = [Buf(), Buf()]
                h2b = [sb(st, "h2b%d" % i, [128, D], BF16) for i in range(2)]
                B_h2b = [Buf(), Buf()]
                junk = sb(st, "bjunk", [128, D], BF16)
                B_junk = Buf()
                h2T = [sb(st, "h2T%d" % i, [128, 8, 128], F32) for i in range(2)]
                B_h2T = [Buf(), Buf()]
                sm = sb(st, "bsm", [128, 2, 8], F32)
                B_sm = [Buf(), Buf()]
                ee = sb(st, "bee", [128, 2, NE], F32)
                yps = [ps(st, "yps%d" % i, [128, D]) for i in range(2)]
                B_yps = [Buf(), Buf()]
                trp = ps(st, "trp", [128, D])
                B_trp = Buf()
                lgp = ps(st, "lgp", [128, 512])
                B_lgp = Buf()
                for sc in range(16):
                    m_ = mix[sc % 2]
                    P.dma("sp", lambda q: q.dma_start(out=m_[:], in_=mixt_d[:, sc * 512:(sc + 1) * 512].rearrange("(kc p) t -> p kc t", p=128)),
                          r=[B_mixt[sc]], w=[B_mix[sc % 2]])
                    for j in range(4):
                        tl = sc * 4 + j
                        i2 = tl % 2
                        y_ = yps[i2]
                        for half in range(2):
                            P.mm(y_[:, half * 512:(half + 1) * 512],
                                 [(m_[:, kc, j * 128:(j + 1) * 128], wo[:, kc, half * 512:(half + 1) * 512]) for kc in range(8)],
                                 r=[B_mix[sc % 2]] + B_wo, w=[B_yps[i2]])
                        s_ = sm[:, i2, :]
                        for half in range(2):
                            P.op("act", lambda a: a.activation(out=junk[:, half * 512:(half + 1) * 512], in_=y_[:, half * 512:(half + 1) * 512],
                                                               func=AF.Square, accum_out=s_[:, half:half + 1]),
                                 r=[B_yps[i2]], w=[B_junk, B_sm[i2]])
                        P.op("dve", lambda v: v.tensor_tensor(out=s_[:, 2:3], in0=s_[:, 0:1], in1=s_[:, 1:2], op=ALU.add),
                             r=[B_sm[i2]], w=[B_sm[i2]])
                        rstd_from_ss(s_[:, 2:3], D, s_[:, 3:4], s_[:, 2:3], [B_sm[i2]], [B_sm[i2]], B_sm[i2])
                        P.dma("sp", lambda q: q.dma_start(out=xb[i2][:], in_=x_d[tl * 128:(tl + 1) * 128, :]), w=[B_xb[i2]])
                        for half in range(2):
                            hs = slice(half * 512, (half + 1) * 512)
                            P.op("dve", lambda v: v.scalar_tensor_tensor(out=tt[i2][:, hs], in0=y_[:, hs], scalar=s_[:, 3:4], in1=gpm[:, hs],
                                                                         op0=ALU.mult, op1=ALU.mult),
                                 r=[B_yps[i2], B_sm[i2], B_gpm], w=[B_tt[i2]])
                        P.op("dve", lambda g: g.tensor_tensor(out=x1[i2][:], in0=tt[i2][:], in1=xb[i2][:], op=ALU.add),
                             r=[B_tt[i2], B_xb[i2]], w=[B_x1s[i2]])
                        P.dma("pool", lambda q: q.dma_start(out=x1_d[tl * 128:(tl + 1) * 128, :], in_=x1[i2][:]),
                              r=[B_x1s[i2]], w=[B_x1[tl]])
                        P.op("dve", lambda v: v.scalar_tensor_tensor(out=junk[:], in0=x1[i2][:], scalar=1.0, in1=x1[i2][:],
                                                                     op0=ALU.mult, op1=ALU.mult, accum_out=s_[:, 4:5]),
                             r=[B_x1s[i2]], w=[B_junk, B_sm[i2]])
                        rstd_from_ss(s_[:, 4:5], D, s_[:, 5:6], s_[:, 4:5], [B_sm[i2]], [B_sm[i2]], B_sm[i2])
                        P.op("dve", lambda v: v.scalar_tensor_tensor(out=tt[i2][:], in0=x1[i2][:], scalar=s_[:, 5:6], in1=g2m[:],
                                                                     op0=ALU.mult, op1=ALU.mult),
                             r=[B_x1s[i2], B_sm[i2], B_g2m], w=[B_tt[i2]])
                        P.op("dve", lambda g: g.tensor_tensor(out=h2f[i2][:], in0=tt[i2][:], in1=sh2[:], op=ALU.add),
                             r=[B_tt[i2], B_sh2], w=[B_h2f[i2]])
                        P.op("act", lambda a: a.copy(out=h2b[i2][:], in_=h2f[i2][:]), r=[B_h2f[i2]], w=[B_h2b[i2]])
                        P.dma("pool", lambda q: q.dma_start(out=h2_d[tl * 128:(tl + 1) * 128, :], in_=h2b[i2][:]),
                              r=[B_h2b[i2]], w=[B_h2t[tl]])
                        pe_group([(lambda pe, kc=kc: pe.transpose(out=trp[:, kc * 128:(kc + 1) * 128],
                                                                   in_=h2f[i2][:, kc * 128:(kc + 1) * 128], identity=ident_f))
                                  for kc in range(8)], r=[B_h2f[i2], B_const], w=[B_trp])
                        P.op("act", lambda a: a.copy(out=h2T[i2][:, 0:4, :].rearrange("p k t -> p (k t)"), in_=trp[:, 0:512]),
                             r=[B_trp], w=[B_h2T[i2]])
                        P.op("dve", lambda v: v.tensor_copy(out=h2T[i2][:, 4:8, :].rearrange("p k t -> p (k t)"), in_=trp[:, 512:1024]),
                             r=[B_trp], w=[B_h2T[i2]])
                        P.mm(lgp[:, 0:NE], [(h2T[i2][:, kc, :], wr[:, kc, :]) for kc in range(8)],
                             r=[B_h2T[i2], B_wr], w=[B_lgp])
                        P.op("dve", lambda v: v.tensor_reduce(out=s_[:, 6:7], in_=lgp[:, 0:NE], axis=AX.X, op=ALU.max, negate=True),
                             r=[B_lgp], w=[B_sm[i2]])
                        P.op("act", lambda a: a.activation(out=ee[:, i2, :], in_=lgp[:, 0:NE], func=AF.Exp, bias=s_[:, 6:7],
                                                           accum_out=s_[:, 7:8]), r=[B_lgp, B_sm[i2]], w=[B_sm[i2]])
                        P.op("dve", lambda v: v.reciprocal(out=s_[:, 7:8], in_=s_[:, 7:8]), r=[B_sm[i2]], w=[B_sm[i2]])
                        P.op("dve", lambda v: v.tensor_scalar(out=AFF[:, tl, :], in0=ee[:, i2, :], scalar1=s_[:, 7:8], scalar2=None,
                                                              op0=ALU.mult), r=[B_sm[i2]], w=[B_AFF[tl]])
                        P.dma("pool", lambda q: q.dma_start(out=aff_d[tl * 128:(tl + 1) * 128, :], in_=AFF[:, tl, :]),
                              r=[B_AFF[tl]], w=[B_afft[tl]])
                P.barrier()

        posm = sb(es, "posm", [128, NE, NTILE], F32)
        B_posm = Buf()

        def phase_c():
            with ExitStack() as st:
                lo = sb(st, "c_lo", [128, NE], F32)
                hi = sb(st, "c_hi", [128, NE], F32)
                mid = sb(st, "c_mid", [128, NE], F32)
                ge = sb(st, "c_ge", [128, NTILE, NE], F32)
                cntp = sb(st, "c_cntp", [128, NE], F32)
                mge = sb(st, "c_mge", [128, NE], U32)
                mlt = sb(st, "c_mlt", [128, NE], U32)
                Mt = sb(st, "c_Mt", [128, NE, NTILE], F32)
                Psc = sb(st, "c_Psc", [128, NE, NTILE], F32)
                rmc = sb(st, "c_rmc", [128, 1024], F32)
                Tt = sb(st, "c_Tt", [128, NE], BF16)
                Lbf = sb(st, "c_Lbf", [128, 128], BF16)
                off = sb(st, "c_off", [128, NE], F32)
                cps = ps(st, "c_cps", [128, 512])
                B_lo, B_hi, B_mid, B_ge, B_cntp, B_m, B_cps, B_x = Buf(), Buf(), Buf(), Buf(), Buf(), Buf(), Buf(), Buf()
                P.dma("sp", lambda q: q.dma_start(out=rmc[:], in_=rm_d[:, 0, :]), w=[B_x])
                P.op("dve", lambda v: v.tensor_copy(out=Lbf[:], in_=cm_f[:, 3, :]), r=[B_const], w=[B_x])
                P.op("dve", lambda v: v.memset(lo[:], 0.0), w=[B_lo])
                P.op("dve", lambda v: v.memset(hi[:], 2.0), w=[B_hi])
                for it in range(34):
                    P.op("dve", lambda v: v.tensor_tensor(out=mid[:], in0=lo[:], in1=hi[:], op=ALU.add), r=[B_lo, B_hi], w=[B_mid])
                    P.op("dve", lambda v: v.tensor_scalar(out=mid[:], in0=mid[:], scalar1=0.5, scalar2=None, op0=ALU.mult),
                         r=[B_mid], w=[B_mid])
                    P.op("dve", lambda v: v.tensor_tensor(out=ge[:], in0=AFF[:], in1=mid[:].unsqueeze(1).to_broadcast([128, NTILE, NE]),
                                                          op=ALU.is_ge), r=B_AFF + [B_mid], w=[B_ge])
                    P.op("dve", lambda v: v.tensor_reduce(out=cntp[:], in_=ge[:].rearrange("p i e -> p e i"), axis=AX.X, op=ALU.add),
                         r=[B_ge], w=[B_cntp])
                    P.mm(cps[:, 0:NE], [(ones_f[:], cntp[:])], r=[B_cntp, B_const], w=[B_cps])
                    P.op("dve", lambda v: v.tensor_scalar(out=mge[:], in0=cps[:, 0:NE], scalar1=float(CAP), scalar2=None, op0=ALU.is_ge),
                         r=[B_cps], w=[B_m])
                    P.op("dve", lambda v: v.tensor_scalar(out=mlt[:], in0=cps[:, 0:NE], scalar1=float(CAP), scalar2=None, op0=ALU.is_lt),
                         r=[B_cps], w=[B_m])
                    P.op("dve", lambda v: v.copy_predicated(out=lo[:], mask=mge[:], data=mid[:]), r=[B_m, B_mid], w=[B_lo])
                    P.op("dve", lambda v: v.copy_predicated(out=hi[:], mask=mlt[:], data=mid[:]), r=[B_m, B_mid], w=[B_hi])
                P.op("dve", lambda v: v.tensor_tensor(out=ge[:], in0=AFF[:], in1=lo[:].unsqueeze(1).to_broadcast([128, NTILE, NE]),
                                                      op=ALU.is_ge), r=B_AFF + [B_lo], w=[B_ge])
                P.op("dve", lambda v: v.tensor_copy(out=Mt[:], in_=ge[:].rearrange("p i e -> p e i")), r=[B_ge], w=[B_x])
                P.op("dve", lambda v: v.tensor_tensor_scan(out=Psc[:].rearrange("p e i -> p (e i)"), data0=rmc[:],
                                                           data1=Mt[:].rearrange("p e i -> p (e i)"), initial=0.0,
                                                           op0=ALU.mult, op1=ALU.add), r=[B_x], w=[B_x])
                P.op("dve", lambda v: v.tensor_copy(out=Tt[:], in_=Psc[:, :, NTILE - 1]), r=[B_x], w=[B_x])
                P.mm(cps[:, 0:NE], [(Lbf[:], Tt[:])], r=[B_x], w=[B_cps])
                P.op("dve", lambda v: v.tensor_copy(out=off[:], in_=cps[:, 0:NE]), r=[B_cps], w=[B_x])
                P.op("dve", lambda v: v.tensor_tensor(out=Psc[:], in0=Psc[:], in1=off[:].unsqueeze(2).to_broadcast([128, NE, NTILE]),
                                                      op=ALU.add), r=[B_x], w=[B_x])
                P.op("dve", lambda v: v.tensor_tensor(out=Psc[:], in0=Psc[:], in1=Mt[:], op=ALU.mult), r=[B_x], w=[B_x])
                P.op("dve", lambda v: v.tensor_scalar(out=posm[:], in0=Psc[:], scalar1=-1.0, scalar2=None, op0=ALU.add),
                     r=[B_x], w=[B_posm])
                P.barrier()

        def idma(fn, r, w):
            return P.dma("pool", fn, r=r, w=w)

        def phase_d(experts=range(NE)):
            with ExitStack() as st:
                iota = sb(st, "d_iota", [128, 1024], F32)
                tokf = sb(st, "d_tokf", [128, NTILE, 2], F32)
                tokb = sb(st, "d_tokb", [128, NTILE, 2], BF16)
                zt_ = sb(st, "d_zero", [128, D], F32)
                B_dc = Buf()
                P.dma("sp", lambda q: q.dma_start(out=iota[:], in_=iota_d[:]), w=[B_dc])
                P.dma("sp", lambda q: q.dma_start(out=tokf[:], in_=tokhl_d[:]), w=[B_dc])
                P.op("dve", lambda v: v.tensor_copy(out=tokb[:], in_=tokf[:]), r=[B_dc], w=[B_dc])
                P.op("dve", lambda v: v.memset(zt_[:], 0.0), w=[B_dc])
                fview = f_d.rearrange("(t p) d -> p t d", p=128)
                for part in range(4):
                    P.dma("sp", lambda q: q.dma_start(out=fview[:, part * 16:(part + 1) * 16, :],
                                                      in_=zt_[:].unsqueeze(1).to_broadcast([128, 16, D])), r=[B_dc], w=[B_f])
                sel = [sb(st, "d_sel%d" % i, [128, 1024], BF16) for i in range(4)]
                B_sel = [Buf() for _ in range(4)]
                idxf = sb(st, "d_idxf", [2, 1024], F32)
                idx2 = sb(st, "d_idx2", [128, 8], F32)
                idxi = [sb(st, "d_idxi%d" % i, [128, 8], I32) for i in range(2)]
                B_idxf, B_idx2 = Buf(), Buf()
                B_idxi = [Buf(), Buf()]
                X = [sb(st, "d_X%d" % i, [128, D], BF16) for i in range(16)]
                B_X = [Buf() for _ in range(16)]
                gat = [sb(st, "d_gat%d" % i, [128, 8, NE], F32) for i in range(2)]
                B_gat = [Buf(), Buf()]
                XT = sb(st, "d_XT", [128, 8, 1024], BF16)
                B_XT = Buf()
                AT = sb(st, "d_AT", [128, 8, 1024], BF16)
                B_AT = Buf()
                W = [[sb(st, "d_w%d_%d" % (m, i), [128, 8, D], BF16) for m in range(3)] for i in range(2)]
                B_W = [[[Buf() for _ in range(4)] for _ in range(3)] for _ in range(2)]
                sg = [sb(st, "d_sg%d" % i, [128, 512], F32) for i in range(2)]
                B_sg = [Buf(), Buf()]
                Ysb = [sb(st, "d_Y%d" % i, [128, D], F32) for i in range(2)]
                B_Y = [Buf(), Buf()]
                ips = [ps(st, "d_ips%d" % i, [128, 512]) for i in range(2)]
                B_ips = [Buf(), Buf()]
                tpx = ps(st, "d_tpx", [128, 8, 128], BF16)
                B_tpx = Buf()
                itp = ps(st, "d_itp", [128, 512])
                B_itp = Buf()
                gps = [ps(st, "d_gps%d" % i, [128, 512]) for i in range(2)]
                B_gps = [Buf(), Buf()]
                ups = [ps(st, "d_ups%d" % i, [128, 512]) for i in range(2)]
                B_ups = [Buf(), Buf()]
                wsrc = (wg_d, wu_d, wd_d)

                def load_w(e, slot):
                    for m in range(3):
                        for pi in range(4):
                            P.dma("pool", lambda q: q.dma_start(out=W[slot][m][:, 2 * pi:2 * pi + 2, :],
                                                                in_=wsrc[m][e][:, 2 * pi:2 * pi + 2, :]), w=[B_W[slot][m][pi]])

                elist = list(experts)
                ctr = dict(sel=0, g=0, y=0)

                def compaction(e, slot):
                    for i in range(NTILE):
                        si = ctr["sel"] % 4
                        ctr["sel"] += 1
                        P.op("dve", lambda v: v.tensor_scalar(out=sel[si][:], in0=iota[:], scalar1=posm[:, e, i:i + 1], scalar2=None,
                                                            op0=ALU.is_equal), r=[B_dc, B_posm], w=[B_sel[si]])
                        for half in range(2):
                            mm1(ips[half][0:2, :], tokb[:, i, :], sel[si][:, half * 512:(half + 1) * 512], i == 0, i == NTILE - 1,
                                [B_dc, B_sel[si]], [B_ips[half]])
                        if i % 4 == 3 and i != NTILE - 1:
                            yield
                    for half in range(2):
                        P.op("act", lambda a: a.copy(out=idxf[:, half * 512:(half + 1) * 512], in_=ips[half][0:2, :]),
                             r=[B_ips[half]], w=[B_idxf])
                    pe_group([(lambda pe, jt=jt: pe.transpose(out=itp[:, 2 * jt:2 * jt + 2], in_=idxf[0:2, jt * 128:(jt + 1) * 128],
                                                               identity=ident_f[0:2, 0:2])) for jt in range(8)],
                             r=[B_idxf, B_const], w=[B_itp])
                    P.op("dve", lambda v: v.tensor_reduce(out=idx2[:], in_=itp[:, 0:16].rearrange("p (j t) -> p j t", t=2),
                                                          axis=AX.X, op=ALU.add), r=[B_itp], w=[B_idx2])
                    P.op("dve", lambda v: v.tensor_copy(out=idxi[slot][:], in_=idx2[:]), r=[B_idx2], w=[B_idxi[slot]])
                    yield

                def gather(e, slot):
                    ii = idxi[slot]
                    for jt in range(8):
                        xj = X[slot * 8 + jt]
                        idma(lambda q: q.indirect_dma_start(out=xj[:], out_offset=None, in_=h2_d[:, :],
                                                            in_offset=IndirectOffsetOnAxis(ap=ii[:, jt:jt + 1], axis=0)),
                             r=[B_idxi[slot]] + B_h2t, w=[B_X[slot * 8 + jt]])
                        idma(lambda q: q.indirect_dma_start(out=gat[slot][:, jt, :], out_offset=None, in_=aff_d[:, :],
                                                            in_offset=IndirectOffsetOnAxis(ap=ii[:, jt:jt + 1], axis=0)),
                             r=[B_idxi[slot]] + B_afft, w=[B_gat[slot]])

                load_w(elist[0], 0)
                for _ in compaction(elist[0], 0):
                    pass
                gather(elist[0], 0)
                for ei, e in enumerate(elist):
                    slot = ei % 2
                    ii = idxi[slot]
                    g_ = gat[slot]
                    nxt = None
                    if ei + 1 < len(elist):
                        load_w(elist[ei + 1], 1 - slot)
                        nxt = compaction(elist[ei + 1], 1 - slot)
                    for jt in range(8):
                        xj = X[slot * 8 + jt]
                        pe_group([(lambda pe, kc=kc: pe.transpose(out=tpx[:, kc, :], in_=xj[:, kc * 128:(kc + 1) * 128],
                                                                   identity=ident_bf[:])) for kc in range(8)],
                                 r=[B_X[slot * 8 + jt], B_const], w=[B_tpx])
                        if jt % 2 == 0:
                            P.op("act", lambda a: a.copy(out=XT[:, :, jt * 128:(jt + 1) * 128], in_=tpx[:]), r=[B_tpx], w=[B_XT])
                        else:
                            P.op("dve", lambda v: v.tensor_copy(out=XT[:, :, jt * 128:(jt + 1) * 128], in_=tpx[:]), r=[B_tpx], w=[B_XT])
                    wg_, wu_, wd_ = W[slot]
                    bwg, bwu, bwd = B_W[slot]
                    for fc in range(8):
                        for sh in range(2):
                            gi = ctr["g"] % 2
                            ctr["g"] += 1
                            cs_ = slice(sh * 512, (sh + 1) * 512)
                            P.mm(gps[gi][:], [(wg_[:, kc, fc * 128:(fc + 1) * 128], XT[:, kc, cs_]) for kc in range(8)],
                                 r=[B_XT] + bwg, w=[B_gps[gi]])
                            P.mm(ups[gi][:], [(wu_[:, kc, fc * 128:(fc + 1) * 128], XT[:, kc, cs_]) for kc in range(8)],
                                 r=[B_XT] + bwu, w=[B_ups[gi]])
                            P.op("act", lambda a: a.activation(out=sg[gi][:], in_=gps[gi][:], func=AF.Silu),
                                 r=[B_gps[gi]], w=[B_sg[gi]])
                            P.op("dve", lambda v: v.tensor_tensor(out=AT[:, fc, cs_], in0=ups[gi][:], in1=sg[gi][:], op=ALU.mult),
                                 r=[B_ups[gi], B_sg[gi]], w=[B_AT])
                            if nxt is not None:
                                next(nxt, None)
                    if nxt is not None:
                        for _ in nxt:
                            pass
                        gather(elist[ei + 1], 1 - slot)
                    for jt in range(8):
                        yi = ctr["y"] % 2
                        ctr["y"] += 1
                        for dh in range(2):
                            gi = ctr["g"] % 2
                            ctr["g"] += 1
                            P.mm(gps[gi][:], [(AT[:, fc, jt * 128:(jt + 1) * 128], wd_[:, fc, dh * 512:(dh + 1) * 512]) for fc in range(8)],
                                 r=[B_AT] + bwd, w=[B_gps[gi]])
                            P.op("dve", lambda v: v.tensor_scalar(out=Ysb[yi][:, dh * 512:(dh + 1) * 512], in0=gps[gi][:],
                                                                  scalar1=g_[:, jt, e:e + 1], scalar2=None, op0=ALU.mult),
                                 r=[B_gps[gi], B_gat[slot]], w=[B_Y[yi]])
                        idma(lambda q: q.indirect_dma_start(out=f_d[:, :], out_offset=IndirectOffsetOnAxis(ap=ii[:, jt:jt + 1], axis=0),
                                                            in_=Ysb[yi][:], in_offset=None, compute_op=ALU.add),
                             r=[B_Y[yi], B_idxi[slot]], w=[B_f])
                P.barrier()

        def phase_e():
            with ExitStack() as st:
                gpf, B_gpf = load_bc(st, "gpf", 7)
                fb = [sb(st, "e_f%d" % i, [128, D], F32) for i in range(2)]
                xb = [sb(st, "e_x%d" % i, [128, D], F32) for i in range(2)]
                tb = [sb(st, "e_t%d" % i, [128, D], F32) for i in range(2)]
                ob_ = [sb(st, "e_o%d" % i, [128, D], F32) for i in range(2)]
                junk = sb(st, "e_junk", [128, D], BF16)
                sm = sb(st, "e_sm", [128, 2, 2], F32)
                B_fb, B_xb, B_tb, B_ob, B_sm = [Buf(), Buf()], [Buf(), Buf()], [Buf(), Buf()], [Buf(), Buf()], [Buf(), Buf()]
                B_junk = Buf()
                B_out = [Buf() for _ in range(NTILE)]
                for tl in range(NTILE):
                    i2 = tl % 2
                    rows = slice(tl * 128, (tl + 1) * 128)
                    P.dma("sp", lambda q: q.dma_start(out=fb[i2][:], in_=f_d[rows, :]), r=[B_f], w=[B_fb[i2]])
                    P.dma("sp", lambda q: q.dma_start(out=xb[i2][:], in_=x1_d[rows, :]), r=[B_x1[tl]], w=[B_xb[i2]])
                    P.op("dve", lambda v: v.scalar_tensor_tensor(out=junk[:], in0=fb[i2][:], scalar=1.0, in1=fb[i2][:],
                                                                 op0=ALU.mult, op1=ALU.mult, accum_out=sm[:, i2, 0:1]),
                         r=[B_fb[i2]], w=[B_junk, B_sm[i2]])
                    rstd_from_ss(sm[:, i2, 0:1], D, sm[:, i2, 1:2], sm[:, i2, 0:1], [B_sm[i2]], [B_sm[i2]], B_sm[i2])
                    P.op("dve", lambda v: v.scalar_tensor_tensor(out=tb[i2][:], in0=fb[i2][:], scalar=sm[:, i2, 1:2], in1=gpf[:],
                                                                 op0=ALU.mult, op1=ALU.mult),
                         r=[B_fb[i2], B_sm[i2], B_gpf], w=[B_tb[i2]])
                    P.op("dve", lambda g: g.tensor_tensor(out=ob_[i2][:], in0=tb[i2][:], in1=xb[i2][:], op=ALU.add),
                         r=[B_tb[i2], B_xb[i2]], w=[B_ob[i2]])
                    P.dma("pool", lambda q: q.dma_start(out=out_d[rows, :], in_=ob_[i2][:]), r=[B_ob[i2]], w=[B_out[tl]])
                P.barrier()

        import os
        if stop_after == "0":
            return nc
        phase_a0()
        if stop_after == "A0":
            return nc
        if stop_after == "A1":
            phase_a1(heads=[int(x) for x in os.environ.get("A1_HEADS", "0").split(",")],
                     qchunks=[int(x) for x in os.environ.get("A1_QC", "0,9").split(",")])
            return nc
        if stop_after == "A2":
            phase_a2(heads=[int(x) for x in os.environ.get("A2_HEADS", "0").split(",")])
            return nc
        if not os.environ.get("SKIP_A1"):
            phase_a1()
        phase_a2()
        phase_b()
        if stop_after == "B":
            return nc
        phase_c()
        if stop_after == "C":
            return nc
        phase_d()
        phase_e()
        return nc


def _rope_tables():
    half = 32
    inv_freq = (1.0 / (10000.0 ** (np.arange(0, half, 2, dtype=np.float32) / np.float32(half)))).astype(np.float32)
    t = np.arange(T)
    r = (t // 64).astype(np.float32)
    c = (t % 64).astype(np.float32)
    ang_r = r[:, None] * inv_freq[None, :]
    ang_c = c[:, None] * inv_freq[None, :]
    ang = np.concatenate([ang_r, ang_r, ang_c, ang_c], axis=-1).astype(np.float32)
    cos = np.cos(ang).astype(np.float32)
    sin = np.sin(ang).astype(np.float32)
    sign = np.concatenate([-np.ones(16), np.ones(16), -np.ones(16), np.ones(16)]).astype(np.float32)
    sin = sin * sign[None, :]
    cosT = np.ones((128, S), np.float32)
    sinT = np.zeros((128, S), np.float32)
    cosT[:, TC:] = np.concatenate([cos.T, cos.T], axis=0)
    sinT[:, TC:] = np.concatenate([sin.T, sin.T], axis=0)
    return cosT, sinT


def _win_cols():
    rot = np.concatenate([np.arange(16, 32), np.arange(0, 16), np.arange(48, 64), np.arange(32, 48)])
    fm, tm, tg = [], [], []
    for h in range(NH):
        for off in (0, 512):
            base = off + h * 128
            fm.append(base + np.arange(128))
            fm.append(np.concatenate([base + rot, base + 64 + rot]))
        fm.append(1536 + h * 128 + np.arange(128))
        fm.append(2048 + h * 128 + np.arange(128))
        fm.append(3072 + h * 128 + np.arange(128))
        tm.append(1024 + h * 128 + np.arange(128))
        tm.append(2560 + h * 128 + np.arange(128))
        tg.append(3584 + h * 128 + np.arange(128))
    tm = tm + tg
    return np.concatenate(fm + tm)


def _kc(a):
    n = a.shape[-1]
    return np.ascontiguousarray(a.reshape(8, 128, n).transpose(1, 0, 2))


def prep_inputs(inp, n_cores):
    f = lambda k: np.asarray(inp[k], dtype=np.float32)
    x, c, ctx, c_ctx = f("x"), f("c"), f("ctx"), f("c_ctx")
    cosT, sinT = _rope_tables()
    p = np.arange(128)
    blk = p // 64
    same = blk[:, None] == blk[None, :]
    cm = np.zeros((128, 4, 128), np.float32)
    cm[:, 0, :] = np.eye(128)
    cm[:, 1, :] = same & (p[:, None] <= p[None, :])
    cm[:, 2, :] = same & (p[:, None] >= p[None, :])
    cm[:, 3, :] = p[:, None] < p[None, :]
    j = np.arange(1024)
    rm = np.ones((128, 2, 1024), np.float32)
    rm[:, 0, j % 64 == 0] = 0.0
    rm[:, 1, j % 64 == 63] = 0.0
    iota = np.broadcast_to(j.astype(np.float32), (128, 1024)).copy()
    tt = np.arange(NTILE)[None, :] * 128 + p[:, None]
    tokhl = np.stack([64 * (tt // 64), tt % 64], axis=-1).astype(np.float32)
    hlb = f("hg_lower_bound").reshape(2, 2, 4, 128).transpose(3, 0, 1, 2).reshape(128, 16)
    shared = {
        "w_ada": _kc(f("w_ada")[0]),
        "b_ada": f("b_ada")[0][None, :],
        "norms": np.concatenate([f("norm_pre_mix")[0], f("norm_post_mix")[0], f("norm_pre_ffn")[0],
                                 f("norm_post_ffn")[0]])[None, :],
        "w_in": _kc(f("w_in")[0][:, _win_cols()]),
        "lamv": np.concatenate([f("da_lambda_q1")[0], f("da_lambda_k1")[0], f("da_lambda_q2")[0],
                                f("da_lambda_k2")[0]])[None, :],
        "subln": f("da_subln")[0][:, None],
        "hgn": f("hg_norm")[0][None, :],
        "hlb": np.ascontiguousarray(hlb),
        "w_out": _kc(f("w_out")[0]),
        "w_r": _kc(f("w_router")[0]),
        "w_gate": np.ascontiguousarray(f("w_gate")[0].reshape(NE, 8, 128, D).transpose(0, 2, 1, 3)),
        "w_up": np.ascontiguousarray(f("w_up")[0].reshape(NE, 8, 128, D).transpose(0, 2, 1, 3)),
        "w_down": np.ascontiguousarray(f("w_down")[0].reshape(NE, 8, 128, D).transpose(0, 2, 1, 3)),
        "cosT": cosT, "sinT": sinT, "cmasks": cm, "rmask": rm, "iota": iota, "tokhl": tokhl,
    }
    maps = []
    for i in range(n_cores):
        b = i % 2
        m = dict(shared)
        m["x"] = np.ascontiguousarray(x[b])
        m["ctx"] = np.ascontiguousarray(ctx[b])
        m["cc"] = _kc(np.stack([c[b], c_ctx], axis=1))
        maps.append(m)
    return maps


N_CORES = 2
_NC_CACHE = {}


def kernel(**inputs):
    if "nc" not in _NC_CACHE:
        _NC_CACHE["nc"] = build()
    nc = _NC_CACHE["nc"]
    maps = prep_inputs(inputs, N_CORES)
    res = run_bass_kernel_spmd(nc, maps, core_ids=list(range(N_CORES)))
    out = np.stack([np.asarray(res.results[b]["out"], dtype=np.float32) for b in range(2)], axis=0)
    return out
```

```python
import numpy as np
from contextlib import ExitStack
import concourse.bass as bass
import concourse.mybir as mybir
from concourse.bass import IndirectOffsetOnAxis
from concourse.bass_utils import run_bass_kernel_spmd

F32 = mybir.dt.float32
BF16 = mybir.dt.bfloat16
I32 = mybir.dt.int32
U32 = mybir.dt.uint32
AF = mybir.ActivationFunctionType
ALU = mybir.AluOpType
AX = mybir.AxisListType

D = 1024
T = 8192
TC = 256
S = T + TC
NH = 4
NE = 16
CAP = 1024
EPS = 1e-6
NTILE = T // 128
FM_BLOCKS = 7
TM_COLS = 384
HEAD_COLS = FM_BLOCKS * 128 + TM_COLS
FM_TOTAL = NH * FM_BLOCKS * 128
WCOLS = NH * HEAD_COLS


class Buf:
    __slots__ = ("w", "r")

    def __init__(self):
        self.w = None
        self.r = {}


class Prog:
    def __init__(self, nc, es, ndma=12):
        self.nc = nc
        self.eng = dict(pe=nc.tensor, act=nc.scalar, dve=nc.vector, pool=nc.gpsimd, sp=nc.sync)
        self.sem = {k: es.enter_context(nc.semaphore("s_" + k)) for k in self.eng}
        self.cnt = {k: 0 for k in self.eng}
        self.waited = {k: {} for k in self.eng}
        self.dsem = {q: [[es.enter_context(nc.semaphore("d_%s%d" % (q, i))), 0] for i in range(ndma)]
                     for q in ("sp", "pool")}
        self.dnext = {"sp": 0, "pool": 0}
        self.nwait = 0
        self.nops = 0
        import os
        self.limit = int(os.environ.get('OPLIMIT', '1000000000'))

    def _wait(self, e, ev):
        s, v = ev
        w = self.waited[e]
        if w.get(s.num, 0) < v:
            self.eng[e].wait_ge(s, v)
            w[s.num] = v
            self.nwait += 1

    def _deps(self, e, reads, writes):
        own = self.sem[e].num
        for b in reads:
            if b.w is not None:
                if not (e == "pe" and b.w[0].num == own):
                    self._wait(e, b.w)
        for b in writes:
            if b.w is not None:
                if not (e == "pe" and b.w[0].num == own):
                    self._wait(e, b.w)
            for ev in b.r.values():
                if ev[0].num == own:
                    continue
                self._wait(e, ev)

    def _mark(self, ev, reads, writes):
        k = ev[0].num
        for b in reads:
            old = b.r.get(k)
            if old is None or old[1] < ev[1]:
                b.r[k] = ev
        for b in writes:
            b.w = ev
            b.r = {}

    def op(self, e, fn, r=(), w=()):
        self.nops += 1
        if self.nops > self.limit:
            return None
        if self.nops == self.limit:
            print('LAST OP', e, fn.__code__.co_firstlineno)
        self._deps(e, r, w)
        ins = fn(self.eng[e])
        self.cnt[e] += 1
        ins.then_inc(self.sem[e], 1)
        ev = (self.sem[e], self.cnt[e])
        self._mark(ev, r, w)
        return ev

    def mm(self, out_ap, pairs, r=(), w=()):
        self.nops += 1
        if self.nops > self.limit:
            return None
        self._deps("pe", r, w)
        n = len(pairs)
        ins = None
        for i, (l, rh) in enumerate(pairs):
            ins = self.nc.tensor.matmul(out_ap, lhsT=l, rhs=rh, start=(i == 0), stop=(i == n - 1))
        self.cnt["pe"] += 1
        ins.then_inc(self.sem["pe"], 1)
        ev = (self.sem["pe"], self.cnt["pe"])
        self._mark(ev, r, w)
        return ev

    def dma(self, q, fn, r=(), w=()):
        self.nops += 1
        if self.nops > self.limit:
            return None
        slots = self.dsem[q]
        i = self.dnext[q]
        self.dnext[q] = (i + 1) % len(slots)
        s, v = slots[i]
        if v > 0:
            self._wait(q, (s, v))
        self._deps(q, r, w)
        ins = fn(self.eng[q])
        slots[i][1] = v + 16
        ins.then_inc(s, 16)
        ev = (s, v + 16)
        self._mark(ev, r, w)
        return ev

    def all_events(self):
        evs = [(self.sem[k], self.cnt[k]) for k in self.eng if self.cnt[k] > 0]
        for q in self.dsem:
            for s, v in self.dsem[q]:
                if v > 0:
                    evs.append((s, v))
        return evs

    def barrier(self, engines=None):
        evs = self.all_events()
        for e in (engines or self.eng):
            for ev in evs:
                if ev[0].num != self.sem[e].num:
                    self._wait(e, ev)


def build(stop_after=None, dbg=()):
    nc = bass.Bass("TRN2", target_bir_lowering=False)
    dbg = set(dbg)

    def din(name, shape, dt=F32):
        return nc.dram_tensor(name, list(shape), dt, kind="ExternalInput").ap()

    def dscr(name, shape, dt):
        kind = "ExternalOutput" if name in dbg else "Internal"
        return nc.dram_tensor(name, list(shape), dt, kind=kind).ap()

    x_d = din("x", [T, D])
    ctx_d = din("ctx", [TC, D])
    cc_d = din("cc", [128, 8, 2])
    wada_d = din("w_ada", [128, 8, 6 * D])
    bada_d = din("b_ada", [1, 6 * D])
    norms_d = din("norms", [1, 4 * D])
    win_d = din("w_in", [128, 8, WCOLS])
    lamv_d = din("lamv", [1, 256])
    subln_d = din("subln", [128, 1])
    hgn_d = din("hgn", [1, 128])
    hlb_d = din("hlb", [128, 16])
    wout_d = din("w_out", [128, 8, D])
    wr_d = din("w_r", [128, 8, NE])
    wg_d = din("w_gate", [NE, 128, 8, D])
    wu_d = din("w_up", [NE, 128, 8, D])
    wd_d = din("w_down", [NE, 128, 8, D])
    cos_d = din("cosT", [128, S])
    sin_d = din("sinT", [128, S])
    cm_d = din("cmasks", [128, 4, 128])
    rm_d = din("rmask", [128, 2, 1024])
    iota_d = din("iota", [128, 1024])
    tokhl_d = din("tokhl", [128, NTILE, 2])
    out_d = nc.dram_tensor("out", [T, D], F32, kind="ExternalOutput").ap()

    modrows_d = dscr("modrows", [8, D], F32)
    qkt_d = dscr("qkt", [NH, 2, 128, S], BF16)
    zt_d = dscr("zt", [NH, 3, 128, S], F32)
    vi_d = dscr("vi", [S, NH, 2, 128], BF16)
    g_d = dscr("gsil", [S, NH, 128], F32)
    mixt_d = dscr("mixt", [D, T], BF16)
    x1_d = dscr("x1", [T, D], F32)
    h2_d = dscr("h2", [T, D], BF16)
    aff_d = dscr("aff", [T, NE], F32)
    f_d = dscr("facc", [T, D], F32)

    B_modrows = Buf()
    B_qkt = [[Buf() for _ in range(17)] for _ in range(NH)]
    B_zt = [[Buf() for _ in range(17)] for _ in range(NH)]
    B_vi = [Buf() for _ in range(17)]
    B_g = [Buf() for _ in range(17)]
    B_mixt = [Buf() for _ in range(16)]
    B_x1 = [Buf() for _ in range(NTILE)]
    B_h2 = Buf()
    B_aff = Buf()
    B_f = Buf()

    es = ExitStack()
    with es:
        P = Prog(nc, es)

        def sb(stack, name, shape, dt):
            return stack.enter_context(nc.sbuf_tensor("sb_" + name, list(shape), dt))

        def ps(stack, name, shape, dt=F32):
            return stack.enter_context(nc.psum_tensor("ps_" + name, list(shape), dt))

        cm_f = sb(es, "cm_f", [128, 4, 128], F32)
        ident_bf = sb(es, "ident_bf", [128, 128], BF16)
        ones_bf = sb(es, "ones_bf", [128, 128], BF16)
        ones_f = sb(es, "ones_f", [128, 128], F32)
        neglam = sb(es, "neglam", [128, 1], F32)
        subln8 = sb(es, "subln8", [128, 1], F32)
        lbt = sb(es, "lbt", [128, 8], F32)
        omlt = sb(es, "omlt", [128, 8], F32)
        mhalf = sb(es, "mhalf", [128, 512], F32)
        epsb = sb(es, "epsb", [128, 1], F32)
        B_const = Buf()
        ident_f = cm_f[:, 0, :]

        P.dma("sp", lambda q: q.dma_start(out=cm_f[:], in_=cm_d[:]), w=[B_const])
        P.op("dve", lambda v: v.tensor_copy(out=ident_bf[:], in_=cm_f[:, 0, :]), r=[B_const], w=[B_const])
        P.op("dve", lambda v: v.memset(ones_bf[:], 1.0), w=[B_const])
        P.op("dve", lambda v: v.memset(ones_f[:], 1.0), w=[B_const])
        P.op("dve", lambda v: v.memset(mhalf[:], -0.5), w=[B_const])
        P.op("dve", lambda v: v.memset(epsb[:], EPS), w=[B_const])

        def rstd_from_ss(ss_ap, n, out_ap, tmp_ap, bufs_r, bufs_w, tmpbuf):
            P.op("dve", lambda v: v.tensor_scalar(out=tmp_ap, in0=ss_ap, scalar1=1.0 / n, scalar2=EPS,
                                                  op0=ALU.mult, op1=ALU.add), r=bufs_r, w=[tmpbuf])
            shp = list(tmp_ap.shape)
            P.op("pool", lambda g: g.tensor_tensor(out=out_ap, in0=tmp_ap, in1=mhalf[0:shp[0], 0:shp[1]],
                                                   op=ALU.pow), r=[tmpbuf, B_const], w=bufs_w)

        with ExitStack() as p0:
            scf = sb(p0, "scf", [128, 8, 2], F32)
            sct = sb(p0, "sct", [128, 8, 2], F32)
            wa = [sb(p0, "wa%d" % i, [128, 8, 512], F32) for i in range(2)]
            modl = sb(p0, "modl", [1, 6 * D], F32)
            modc = sb(p0, "modc", [1, 6 * D], F32)
            bada = sb(p0, "bada", [1, 6 * D], F32)
            nrm = sb(p0, "nrm", [1, 4 * D], F32)
            rows = sb(p0, "rows", [1, 8, D], F32)
            lamv = sb(p0, "lamv", [1, 256], F32)
            lamt = sb(p0, "lamt", [1, 8], F32)
            hlb = sb(p0, "hlb", [128, 16], F32)
            sl = sb(p0, "sl", [128, 1], F32)
            pm = [ps(p0, "pm%d" % i, [1, 512]) for i in range(4)]
            pl = ps(p0, "pl", [128, 1])
            B_sc, B_wa, B_modl, B_modc, B_bada, B_nrm, B_rows, B_lam, B_hlb = (
                Buf(), [Buf(), Buf()], Buf(), Buf(), Buf(), Buf(), Buf(), Buf(), Buf())
            B_pm = [Buf() for _ in range(4)]
            B_pl = Buf()

            P.dma("sp", lambda q: q.dma_start(out=scf[:], in_=cc_d[:]), w=[B_sc])
            P.dma("sp", lambda q: q.dma_start(out=bada[:], in_=bada_d[:]), w=[B_bada])
            P.dma("sp", lambda q: q.dma_start(out=nrm[:], in_=norms_d[:]), w=[B_nrm])
            P.dma("sp", lambda q: q.dma_start(out=lamv[:], in_=lamv_d[:]), w=[B_lam])
            P.dma("sp", lambda q: q.dma_start(out=hlb[:], in_=hlb_d[:]), w=[B_hlb])
            P.dma("sp", lambda q: q.dma_start(out=sl[:], in_=subln_d[:]), w=[B_hlb])
            P.op("act", lambda a: a.activation(out=sct[:], in_=scf[:], func=AF.Exp, scale=-1.0), r=[B_sc], w=[B_rows])
            P.op("dve", lambda v: v.tensor_scalar(out=sct[:], in0=sct[:], scalar1=1.0, scalar2=None, op0=ALU.add),
                 r=[B_rows], w=[B_rows])
            P.op("dve", lambda v: v.reciprocal(out=sct[:], in_=sct[:]), r=[B_rows], w=[B_rows])
            P.op("dve", lambda v: v.tensor_tensor(out=scf[:], in0=scf[:], in1=sct[:], op=ALU.mult),
                 r=[B_rows, B_sc], w=[B_sc])
            for ch in range(12):
                wb_ = wa[ch % 2]
                P.dma("sp", lambda q: q.dma_start(out=wb_[:], in_=wada_d[:, :, ch * 512:(ch + 1) * 512]),
                      w=[B_wa[ch % 2]])
                for j, (mod, bm) in enumerate(((modl, B_modl), (modc, B_modc))):
                    pmt = pm[(2 * ch + j) % 4]
                    bp = B_pm[(2 * ch + j) % 4]
                    P.mm(pmt[:], [(scf[:, kc, j:j + 1], wb_[:, kc, :]) for kc in range(8)],
                         r=[B_sc, B_wa[ch % 2]], w=[bp])
                    P.op("dve", lambda v: v.tensor_tensor(out=mod[:, ch * 512:(ch + 1) * 512], in0=pmt[:],
                                                          in1=bada[:, ch * 512:(ch + 1) * 512], op=ALU.add),
                         r=[bp, B_bada], w=[bm])
            def stt(dst, a, b, op0):
                P.op("dve", lambda v: v.scalar_tensor_tensor(out=rows[:, dst, :], in0=a, scalar=(1.0 if op0 == ALU.add else 1.0),
                                                             in1=b, op0=op0, op1=ALU.mult),
                     r=[B_modl, B_modc, B_nrm], w=[B_rows])
            stt(0, modl[:, D:2 * D], nrm[:, 0:D], ALU.add)
            P.op("dve", lambda v: v.tensor_copy(out=rows[:, 1, :], in_=modl[:, 0:D]), r=[B_modl], w=[B_rows])
            stt(2, modc[:, D:2 * D], nrm[:, 0:D], ALU.add)
            P.op("dve", lambda v: v.tensor_copy(out=rows[:, 3, :], in_=modc[:, 0:D]), r=[B_modc], w=[B_rows])
            stt(4, modl[:, 2 * D:3 * D], nrm[:, D:2 * D], ALU.mult)
            stt(5, modl[:, 4 * D:5 * D], nrm[:, 2 * D:3 * D], ALU.add)
            P.op("dve", lambda v: v.tensor_copy(out=rows[:, 6, :], in_=modl[:, 3 * D:4 * D]), r=[B_modl], w=[B_rows])
            stt(7, modl[:, 5 * D:6 * D], nrm[:, 3 * D:4 * D], ALU.mult)
            P.dma("pool", lambda q: q.dma_start(out=modrows_d[:, :].rearrange("(o r) d -> o r d", o=1), in_=rows[:]),
                  r=[B_rows], w=[B_modrows])
            P.op("dve", lambda v: v.tensor_tensor(out=lamv[:, 0:64], in0=lamv[:, 0:64], in1=lamv[:, 64:128], op=ALU.mult),
                 r=[B_lam], w=[B_lam])
            P.op("dve", lambda v: v.tensor_tensor(out=lamv[:, 128:192], in0=lamv[:, 128:192], in1=lamv[:, 192:256], op=ALU.mult),
                 r=[B_lam], w=[B_lam])
            P.op("dve", lambda v: v.tensor_reduce(out=lamt[:, 0:1], in_=lamv[:, 0:64], axis=AX.X, op=ALU.add),
                 r=[B_lam], w=[B_lam])
            P.op("dve", lambda v: v.tensor_reduce(out=lamt[:, 1:2], in_=lamv[:, 128:192], axis=AX.X, op=ALU.add),
                 r=[B_lam], w=[B_lam])
            P.op("act", lambda a: a.activation(out=lamt[:, 2:4], in_=lamt[:, 0:2], func=AF.Exp), r=[B_lam], w=[B_lam])
            P.op("dve", lambda v: v.scalar_tensor_tensor(out=lamt[:, 4:5], in0=lamt[:, 3:4], scalar=-0.2, in1=lamt[:, 2:3],
                                                         op0=ALU.add, op1=ALU.subtract), r=[B_lam], w=[B_lam])
            P.mm(pl[:], [(ones_f[0:1, :], lamt[0:1, 4:5])], r=[B_lam, B_const], w=[B_pl])
            P.op("dve", lambda v: v.tensor_copy(out=neglam[:], in_=pl[:]), r=[B_pl], w=[B_const])
            P.op("dve", lambda v: v.tensor_scalar(out=subln8[:], in0=sl[:], scalar1=0.8, scalar2=None, op0=ALU.mult),
                 r=[B_hlb], w=[B_const])
            P.op("dve", lambda v: v.tensor_tensor(out=hlb[:, 0:8], in0=hlb[:, 8:16], in1=hlb[:, 0:8], op=ALU.subtract),
                 r=[B_hlb], w=[B_hlb])
            P.op("act", lambda a: a.activation(out=hlb[:, 0:8], in_=hlb[:, 0:8], func=AF.Exp), r=[B_hlb], w=[B_hlb])
            P.op("dve", lambda v: v.tensor_scalar(out=hlb[:, 0:8], in0=hlb[:, 0:8], scalar1=1.0, scalar2=None, op0=ALU.add),
                 r=[B_hlb], w=[B_hlb])
            P.op("dve", lambda v: v.reciprocal(out=lbt[:], in_=hlb[:, 0:8]), r=[B_hlb], w=[B_const])
            P.op("dve", lambda v: v.tensor_scalar(out=omlt[:], in0=lbt[:], scalar1=-1.0, scalar2=1.0, op0=ALU.mult, op1=ALU.add),
                 r=[B_const], w=[B_const])
            P.barrier()

        def load_bc(stack, name, row):
            t = sb(stack, name, [128, D], F32)
            b = Buf()
            P.dma("sp", lambda q: q.dma_start(out=t[:], in_=modrows_d[row:row + 1, :].partition_broadcast(128)),
                  r=[B_modrows], w=[b])
            return t, b

        def pe_group(fns, r, w):
            P._deps("pe", r, w)
            ins = None
            for fn in fns:
                ins = fn(nc.tensor)
            P.cnt["pe"] += 1
            ins.then_inc(P.sem["pe"], 1)
            ev = (P.sem["pe"], P.cnt["pe"])
            P._mark(ev, r, w)
            return ev

        SCS = [(0, 2)] + [(2 + 4 * k, 4) for k in range(16)]

        def phase_a0():
            with ExitStack() as st:
                wbf = sb(st, "wbf", [128, 8, WCOLS], BF16)
                B_w = [[Buf() for _ in range(4)] for _ in range(8)]
                for kc in range(8):
                    for pi in range(4):
                        c0 = pi * 1280
                        P.dma("pool", lambda q: q.dma_start(out=wbf[:, kc, c0:c0 + 1280], in_=win_d[:, kc, c0:c0 + 1280]),
                              w=[B_w[kc][pi]])
                B_wall = [b for l in B_w for b in l]
                gm_l, B_gml = load_bc(st, "gm_l", 0)
                sh_l, B_shl = load_bc(st, "sh_l", 1)
                gm_c, B_gmc = load_bc(st, "gm_c", 2)
                sh_c, B_shc = load_bc(st, "sh_c", 3)
                xb = [sb(st, "xb%d" % i, [128, D], F32) for i in range(2)]
                B_xb = [Buf(), Buf()]
                junk = sb(st, "junk", [128, D], BF16)
                B_junk = Buf()
                hn = [sb(st, "hn%d" % i, [128, D], F32) for i in range(2)]
                B_hn = [Buf(), Buf()]
                hb = [sb(st, "hb%d" % i, [128, D], BF16) for i in range(4)]
                B_hb = [Buf() for _ in range(4)]
                ssm = sb(st, "ssm", [128, 8], F32)
                B_ss = [Buf() for _ in range(4)]
                hT = [sb(st, "hT%d" % i, [128, 8, 512], BF16) for i in range(2)]
                B_hT = [Buf(), Buf()]
                cs = [sb(st, "cs%d" % i, [128, 2, 512], F32) for i in range(2)]
                B_cs = [Buf(), Buf()]
                qks = [sb(st, "qks%d" % i, [128, 2, 512], BF16) for i in range(2)]
                B_qks = [Buf(), Buf()]
                zs = [sb(st, "zs%d" % i, [128, 3, 512], F32) for i in range(2)]
                B_zs = [Buf(), Buf()]
                t1 = sb(st, "t1", [128, 512], F32)
                t2 = sb(st, "t2", [128, 512], F32)
                B_t1, B_t2 = Buf(), Buf()
                vis = [sb(st, "vis%d" % i, [128, NH, 2, 128], BF16) for i in range(2)]
                B_vis = [Buf(), Buf()]
                gs = [sb(st, "gs%d" % i, [128, NH, 128], F32) for i in range(2)]
                B_gs = [Buf(), Buf()]
                tp = [ps(st, "tp%d" % i, [128, 8, 128], BF16) for i in range(2)]
                B_tp = [Buf(), Buf()]
                fm = [ps(st, "fm%d" % i, [128, 512]) for i in range(3)]
                B_fm = [Buf() for _ in range(3)]
                tm = ps(st, "tm", [128, 1536])
                B_tm = Buf()
                fmi = [0]
                tile_ctr = [0]

                def stage1(k):
                    t0, nt = SCS[k]
                    for j in range(nt):
                        i = tile_ctr[0]
                        tile_ctr[0] += 1
                        x_ = xb[i % 2]
                        st_ = t0 + j
                        src = ctx_d[st_ * 128:(st_ + 1) * 128, :] if k == 0 else x_d[(st_ - 2) * 128:(st_ - 1) * 128, :]
                        gm, bgm, sh, bsh = (gm_c, B_gmc, sh_c, B_shc) if k == 0 else (gm_l, B_gml, sh_l, B_shl)
                        P.dma("sp", lambda q: q.dma_start(out=x_[:], in_=src), w=[B_xb[i % 2]])
                        ss = ssm[:, 2 * j:2 * j + 1]
                        rs = ssm[:, 2 * j + 1:2 * j + 2]
                        P.op("dve", lambda v: v.scalar_tensor_tensor(out=junk[:], in0=x_[:], scalar=1.0, in1=x_[:],
                                                                     op0=ALU.mult, op1=ALU.mult, accum_out=ss),
                             r=[B_xb[i % 2]], w=[B_junk, B_ss[j]])
                        rstd_from_ss(ss, D, rs, ss, [B_ss[j]], [B_ss[j]], B_ss[j])
                        h_ = hn[i % 2]
                        P.op("dve", lambda v: v.scalar_tensor_tensor(out=h_[:], in0=x_[:], scalar=rs, in1=gm[:],
                                                                     op0=ALU.mult, op1=ALU.mult),
                             r=[B_xb[i % 2], B_ss[j], bgm], w=[B_hn[i % 2]])
                        P.op("dve", lambda g: g.tensor_tensor(out=hb[j][:], in0=h_[:], in1=sh[:], op=ALU.add),
                             r=[B_hn[i % 2], bsh], w=[B_hb[j]])

                def stage2(k):
                    t0, nt = SCS[k]
                    for j in range(nt):
                        tpp = tp[j % 2]
                        pe_group([(lambda pe, kc=kc: pe.transpose(out=tpp[:, kc, :], in_=hb[j][:, kc * 128:(kc + 1) * 128],
                                                                   identity=ident_bf[:])) for kc in range(8)],
                                 r=[B_hb[j], B_const], w=[B_tp[j % 2]])
                        P.op("act", lambda a: a.copy(out=hT[k % 2][:, :, j * 128:(j + 1) * 128], in_=tpp[:]),
                             r=[B_tp[j % 2]], w=[B_hT[k % 2]])

                def fm_mm(k, col0, n):
                    bi = fmi[0] % 3
                    fmi[0] += 1
                    P.mm(fm[bi][:, 0:n], [(wbf[:, kc, col0:col0 + 128], hT[k % 2][:, kc, 0:n]) for kc in range(8)],
                         r=[B_hT[k % 2]] + B_wall, w=[B_fm[bi]])
                    return bi

                def stage3(k):
                    t0, nt = SCS[k]
                    n = nt * 128
                    s0 = t0 * 128
                    c_ = cs[k % 2]
                    P.dma("sp", lambda q: q.dma_start(out=c_[:, 0, 0:n], in_=cos_d[:, s0:s0 + n]), w=[B_cs[k % 2]])
                    P.dma("sp", lambda q: q.dma_start(out=c_[:, 1, 0:n], in_=sin_d[:, s0:s0 + n]), w=[B_cs[k % 2]])
                    for h in range(NH):
                        hi = k * NH + h
                        qk_ = qks[hi % 2]
                        z_ = zs[hi % 2]
                        base = h * FM_BLOCKS * 128
                        for t in range(2):
                            b0 = fm_mm(k, base + (2 * t) * 128, n)
                            b1 = fm_mm(k, base + (2 * t + 1) * 128, n)
                            P.op("dve", lambda v: v.tensor_tensor(out=t1[:, 0:n], in0=fm[b0][:, 0:n], in1=c_[:, 0, 0:n], op=ALU.mult),
                                 r=[B_fm[b0], B_cs[k % 2]], w=[B_t1])
                            P.op("dve", lambda v: v.tensor_tensor(out=t2[:, 0:n], in0=fm[b1][:, 0:n], in1=c_[:, 1, 0:n], op=ALU.mult),
                                 r=[B_fm[b1], B_cs[k % 2]], w=[B_t2])
                            P.op("dve", lambda g: g.tensor_tensor(out=qk_[:, t, 0:n], in0=t1[:, 0:n], in1=t2[:, 0:n], op=ALU.add),
                                 r=[B_t1, B_t2], w=[B_qks[hi % 2]])
                        for t in range(3):
                            b0 = fm_mm(k, base + (4 + t) * 128, n)
                            P.op("act", lambda a: a.copy(out=z_[:, t, 0:n], in_=fm[b0][:, 0:n]), r=[B_fm[b0]], w=[B_zs[hi % 2]])
                        P.dma("pool", lambda q: q.dma_start(out=qkt_d[h].rearrange("t p s -> p t s")[:, :, s0:s0 + n],
                                                            in_=qk_[:, :, 0:n]), r=[B_qks[hi % 2]], w=[B_qkt[h][k]])
                        P.dma("pool", lambda q: q.dma_start(out=zt_d[h].rearrange("t p s -> p t s")[:, :, s0:s0 + n],
                                                            in_=z_[:, :, 0:n]), r=[B_zs[hi % 2]], w=[B_zt[h][k]])
                    for j in range(nt):
                        ti = t0 + j
                        for nb in range(3):
                            P.mm(tm[:, nb * 512:(nb + 1) * 512],
                                 [(hT[k % 2][:, kc, j * 128:(j + 1) * 128], wbf[:, kc, FM_TOTAL + nb * 512:FM_TOTAL + (nb + 1) * 512])
                                  for kc in range(8)], r=[B_hT[k % 2]] + B_wall, w=[B_tm])
                        v_ = vis[ti % 2]
                        g_ = gs[ti % 2]
                        vflat = v_[:].rearrange("p h t c -> p (h t c)")
                        P.op("dve", lambda v: v.tensor_copy(out=vflat[:, 0:512], in_=tm[:, 0:512]),
                             r=[B_tm], w=[B_vis[ti % 2]])
                        P.op("dve", lambda v: v.tensor_copy(out=vflat[:, 512:1024], in_=tm[:, 512:1024]),
                             r=[B_tm], w=[B_vis[ti % 2]])
                        P.op("act", lambda a: a.activation(out=g_[:].rearrange("p h c -> p (h c)"), in_=tm[:, 1024:1536], func=AF.Silu),
                             r=[B_tm], w=[B_gs[ti % 2]])
                        P.dma("pool", lambda q: q.dma_start(out=vi_d[ti * 128:(ti + 1) * 128], in_=v_[:]),
                              r=[B_vis[ti % 2]], w=[B_vi[k]])
                        P.dma("pool", lambda q: q.dma_start(out=g_d[ti * 128:(ti + 1) * 128], in_=g_[:]),
                              r=[B_gs[ti % 2]], w=[B_g[k]])

                stage1(0)
                stage2(0)
                for k in range(17):
                    if k + 1 < 17:
                        stage1(k + 1)
                    stage3(k)
                    if k + 1 < 17:
                        stage2(k + 1)
                P.barrier()

        def mm1(out_ap, lhsT, rhs, start, stop, r, w):
            P.nops += 1
            if P.nops > P.limit:
                return None
            P._deps("pe", r, w)
            ins = nc.tensor.matmul(out_ap, lhsT=lhsT, rhs=rhs, start=start, stop=stop)
            P.cnt["pe"] += 1
            ins.then_inc(P.sem["pe"], 1)
            ev = (P.sem["pe"], P.cnt["pe"])
            P._mark(ev, r, w)
            return ev

        NKT = S // 128

        def phase_a1(heads=range(NH), qchunks=range(16)):
            with ExitStack() as st:
                kt = [sb(st, "kt%d" % i, [128, S], BF16) for i in range(2)]
                vv = [sb(st, "vv%d" % i, [128, NKT, 128], BF16) for i in range(2)]
                B_kt = [Buf(), Buf()]
                B_vv = [Buf(), Buf()]
                qt = [sb(st, "qt%d" % i, [128, 512], BF16) for i in range(2)]
                B_qt = [Buf(), Buf()]
                p1 = [sb(st, "p1_%d" % i, [128, 512], BF16) for i in range(3)]
                p2 = [sb(st, "p2_%d" % i, [128, 512], BF16) for i in range(3)]
                B_p1 = [Buf() for _ in range(3)]
                B_p2 = [Buf() for _ in range(3)]
                acc1s = [sb(st, "acc1_%d" % i, [128, 512], F32) for i in range(2)]
                acc2s = [sb(st, "acc2_%d" % i, [128, 512], F32) for i in range(2)]
                B_acc1s, B_acc2s = [Buf(), Buf()], [Buf(), Buf()]
                o1c = sb(st, "o1c", [128, 512], F32)
                o2c = sb(st, "o2c", [128, 512], F32)
                B_o1c, B_o2c = Buf(), Buf()
                r1 = sb(st, "r1", [128, 512], F32)
                o1 = sb(st, "o1", [128, 512], F32)
                r2 = sb(st, "r2", [128, 512], F32)
                o2 = sb(st, "o2", [128, 512], F32)
                oo = sb(st, "oo", [128, 512], F32)
                sq = sb(st, "sq", [128, 512], F32)
                rs = sb(st, "rs", [128, 512], F32)
                ob = [sb(st, "ob%d" % i, [128, 512], BF16) for i in range(2)]
                B_r1, B_o1, B_r2, B_o2, B_oo, B_sq, B_rs = Buf(), Buf(), Buf(), Buf(), Buf(), Buf(), Buf()
                B_ob = [Buf(), Buf()]
                s1 = [ps(st, "s1_%d" % i, [128, 512]) for i in range(2)]
                s2 = [ps(st, "s2_%d" % i, [128, 512]) for i in range(2)]
                B_s1 = [Buf(), Buf()]
                B_s2 = [Buf(), Buf()]
                o1p = ps(st, "o1p", [128, 512])
                o2p = ps(st, "o2p", [128, 512])
                l1p = ps(st, "l1p", [128, 512])
                l2p = ps(st, "l2p", [128, 512])
                B_o1p, B_o2p, B_l1p, B_l2p = Buf(), Buf(), Buf(), Buf()
                def finalize(h, qc, acc1, acc2, B_acc1, B_acc2, o_, bo):
                    P.mm(l1p[:], [(ones_f[:], acc1[:])], r=[B_acc1, B_const], w=[B_l1p])
                    P.mm(l2p[:], [(ones_f[:], acc2[:])], r=[B_acc2, B_const], w=[B_l2p])
                    P.op("act", lambda a: a.activation(out=r1[:], in_=l1p[:], func=AF.Ln), r=[B_l1p], w=[B_r1])
                    P.op("act", lambda a: a.activation(out=r1[:], in_=r1[:], func=AF.Exp, scale=-1.0), r=[B_r1], w=[B_r1])
                    P.op("dve", lambda v: v.tensor_tensor(out=o1[:], in0=o1c[:], in1=r1[:], op=ALU.mult),
                         r=[B_o1c, B_r1], w=[B_o1])
                    P.op("act", lambda a: a.activation(out=r2[:], in_=l2p[:], func=AF.Ln), r=[B_l2p], w=[B_r2])
                    P.op("act", lambda a: a.activation(out=r2[:], in_=r2[:], func=AF.Exp, scale=-1.0), r=[B_r2], w=[B_r2])
                    P.op("dve", lambda v: v.tensor_tensor(out=o2[:], in0=o2c[:], in1=r2[:], op=ALU.mult),
                         r=[B_o2c, B_r2], w=[B_o2])
                    P.op("dve", lambda v: v.scalar_tensor_tensor(out=oo[:], in0=o2[:], scalar=neglam[:, 0:1], in1=o1[:],
                                                                 op0=ALU.mult, op1=ALU.add),
                         r=[B_o2, B_o1, B_const], w=[B_oo])
                    P.op("dve", lambda v: v.tensor_tensor(out=sq[:], in0=oo[:], in1=oo[:], op=ALU.mult),
                         r=[B_oo], w=[B_sq])
                    P.mm(l1p[:], [(ones_f[:], sq[:])], r=[B_sq, B_const], w=[B_l1p])
                    P.op("act", lambda a: a.activation(out=rs[:], in_=l1p[:], func=AF.Ln, scale=1.0 / 128, bias=epsb[:, 0:1]),
                         r=[B_l1p, B_const], w=[B_rs])
                    P.op("act", lambda a: a.activation(out=rs[:], in_=rs[:], func=AF.Exp, scale=-0.5), r=[B_rs], w=[B_rs])
                    P.op("dve", lambda v: v.scalar_tensor_tensor(out=o_[:], in0=oo[:], scalar=subln8[:, 0:1], in1=rs[:],
                                                                 op0=ALU.mult, op1=ALU.mult),
                         r=[B_oo, B_rs, B_const], w=[bo])
                    P.dma("pool", lambda q: q.dma_start(out=mixt_d[h * 128:(h + 1) * 128, qc * 512:(qc + 1) * 512], in_=o_[:]),
                          r=[bo], w=[B_mixt[qc]])

                pending = None
                cnt = 0
                hl = list(heads)

                def load_kv(hi):
                    hh = hl[hi]
                    P.dma("sp", lambda q: q.dma_start(out=kt[hi % 2][:], in_=qkt_d[hh, 1]), r=B_qkt[hh], w=[B_kt[hi % 2]])
                    vsrc = vi_d[:, hh, 0, :].rearrange("(t p) c -> p t c", p=128)
                    for part in range(3):
                        P.dma("sp", lambda q: q.dma_start(out=vv[hi % 2][:, part * 22:(part + 1) * 22, :],
                                                          in_=vsrc[:, part * 22:(part + 1) * 22, :]),
                              r=B_vi, w=[B_vv[hi % 2]])

                load_kv(0)
                for hi, h in enumerate(hl):
                    k_ = kt[hi % 2]
                    v_ = vv[hi % 2]
                    if hi + 1 < len(hl):
                        load_kv(hi + 1)
                    for qc in qchunks:
                        q_ = qt[cnt % 2]
                        bq = B_qt[cnt % 2]
                        o_ = ob[cnt % 2]
                        bo = B_ob[cnt % 2]
                        acc1, acc2 = acc1s[cnt % 2], acc2s[cnt % 2]
                        B_acc1, B_acc2 = B_acc1s[cnt % 2], B_acc2s[cnt % 2]
                        cnt += 1
                        P.dma("sp", lambda q: q.dma_start(out=q_[:], in_=qkt_d[h, 0][:, TC + qc * 512:TC + (qc + 1) * 512]),
                              r=B_qkt[h], w=[bq])

                        def scores(i):
                            mm1(s1[i % 2][:], k_[0:64, i * 128:(i + 1) * 128], q_[0:64, :], True, True,
                                [B_kt[hi % 2], bq], [B_s1[i % 2]])
                            mm1(s2[i % 2][:], k_[64:128, i * 128:(i + 1) * 128], q_[64:128, :], True, True,
                                [B_kt[hi % 2], bq], [B_s2[i % 2]])

                        scores(0)
                        scores(1)
                        for i in range(NKT):
                            if i == 8 and pending is not None:
                                finalize(*pending)
                                pending = None
                            pa, pb = p1[i % 3], p2[i % 3]
                            P.op("act", lambda a: a.activation(out=pa[:], in_=s1[i % 2][:], func=AF.Exp, scale=0.125),
                                 r=[B_s1[i % 2]], w=[B_p1[i % 3]])
                            P.op("act", lambda a: a.activation(out=pb[:], in_=s2[i % 2][:], func=AF.Exp, scale=0.125),
                                 r=[B_s2[i % 2]], w=[B_p2[i % 3]])
                            st_, sp_ = (i == 0), (i == NKT - 1)
                            P._deps("pe", [B_p1[i % 3], B_p2[i % 3]], [])
                            mm1(o1p[:], v_[:, i, :], pa[:], st_, sp_, [B_vv[hi % 2], B_p1[i % 3]], [B_o1p])
                            mm1(o2p[:], v_[:, i, :], pb[:], st_, sp_, [B_vv[hi % 2], B_p2[i % 3]], [B_o2p])
                            if i == 0:
                                P.op("dve", lambda v: v.tensor_copy(out=acc1[:], in_=pa[:]), r=[B_p1[i % 3]], w=[B_acc1])
                                P.op("dve", lambda v: v.tensor_copy(out=acc2[:], in_=pb[:]), r=[B_p2[i % 3]], w=[B_acc2])
                            else:
                                P.op("dve", lambda v: v.tensor_tensor(out=acc1[:], in0=acc1[:], in1=pa[:], op=ALU.add),
                                     r=[B_p1[i % 3], B_acc1], w=[B_acc1])
                                P.op("dve", lambda v: v.tensor_tensor(out=acc2[:], in0=acc2[:], in1=pb[:], op=ALU.add),
                                     r=[B_p2[i % 3], B_acc2], w=[B_acc2])
                            if i + 2 < NKT:
                                scores(i + 2)
                        P.op("act", lambda a: a.copy(out=o1c[:], in_=o1p[:]), r=[B_o1p], w=[B_o1c])
                        P.op("dve", lambda v: v.tensor_copy(out=o2c[:], in_=o2p[:]), r=[B_o2p], w=[B_o2c])
                        pending = (h, qc, acc1, acc2, B_acc1, B_acc2, o_, bo)
                if pending is not None:
                    finalize(*pending)
                P.barrier()

        def phase_a2(heads=range(NH)):
            with ExitStack() as st:
                o_accs = [sb(st, "o_acc%d" % i, [128, NTILE, 128], F32) for i in range(2)]
                B_oaccs = [[Buf() for _ in range(NTILE)] for _ in range(2)]
                rm = sb(st, "rm", [128, 2, 512], F32)
                cmA = sb(st, "cmA", [128, 512], BF16)
                cmB = sb(st, "cmB", [128, 512], BF16)
                hgn = sb(st, "hgn_b", [128, 128], F32)
                B_c2 = Buf()
                P.dma("sp", lambda q: q.dma_start(out=rm[:], in_=rm_d[:, :, 0:512]), w=[B_c2])
                P.dma("sp", lambda q: q.dma_start(out=hgn[:], in_=hgn_d[0:1, :].partition_broadcast(128)), w=[B_c2])
                P.op("dve", lambda v: v.memset(cmA[:], 0.0), w=[B_c2])
                P.op("dve", lambda v: v.memset(cmB[:], 0.0), w=[B_c2])
                P.op("dve", lambda v: v.memset(cmA[:].rearrange("p (t c) -> p t c", c=128)[:, :, 0:64], 1.0), w=[B_c2])
                P.op("dve", lambda v: v.memset(cmB[:].rearrange("p (t c) -> p t c", c=128)[:, :, 64:128], 1.0), w=[B_c2])

                def dbl(name, shape, dt):
                    return [sb(st, "%s%d" % (name, i), shape, dt) for i in range(2)], [Buf(), Buf()]
                zin, B_zin = dbl("zin", [128, 2, 512], F32)
                vt, B_vt = dbl("vt", [128, 4, 128], BF16)
                gt, B_gt = dbl("gt", [128, 4, 128], F32)
                e_, B_e = dbl("e_", [128, 512], F32)
                f_, B_f_ = dbl("f_", [128, 512], F32)
                lf, B_lf = dbl("lf", [128, 512], F32)
                kk, B_kk = dbl("kk", [128, 512], F32)
                bc, B_bc = dbl("bc", [128, 512], F32)
                ep, B_ep = dbl("ep", [128, 512], F32)
                en, B_en = dbl("en", [128, 512], F32)
                kdf, B_kdf = dbl("kdf", [128, 512], F32)
                Qd, B_Qd = dbl("Qd", [128, 512], BF16)
                QdA, B_QdA = dbl("QdA", [128, 512], BF16)
                QdB, B_QdB = dbl("QdB", [128, 512], BF16)
                Kd, B_Kd = dbl("Kd", [128, 512], BF16)
                K2T, B_K2T = dbl("K2T", [128, 512], BF16)
                dec, B_dec = dbl("dec", [128, 8], F32)
                k2all, B_k2all = dbl("k2all", [128, 4, 128], BF16)
                scma, B_scma = dbl("scma", [128, 4, 128], BF16)
                Sball, B_Sball = dbl("Sball", [128, 8, 128], BF16)
                ScarA, B_ScarA = dbl("ScarA", [128, 128], BF16)
                ScarB, B_ScarB = dbl("ScarB", [128, 128], BF16)
                Sst2 = [sb(st, "Sst%d" % i, [128, 128], F32) for i in range(2)]
                B_S2 = [Buf(), Buf()]
                otot, B_otot = dbl("otot", [128, 4, 128], F32)
                osq, B_osq = dbl("osq", [128, 4, 128], F32)
                osm4, B_osm4 = dbl("osm4", [128, 8], F32)
                yb4, B_yb4 = dbl("yb4", [128, 4, 128], BF16)
                mixs, B_mixs = dbl("mixs", [128, 512], BF16)
                tpb = ps(st, "tpb", [128, 1024], BF16)
                B_tpb = Buf()
                scp = ps(st, "scp", [128, 512])
                B_scp = Buf()
                ups2 = [ps(st, "ups%d" % i, [128, 1024]) for i in range(2)]
                B_ups2 = [[Buf() for _ in range(8)] for _ in range(2)]
                ops2 = [ps(st, "ops%d" % i, [128, 512]) for i in range(2)]
                B_ops2 = [Buf(), Buf()]
                ctr = dict(sc=0, tile=0, ch=0, tp=0)
                for i_ in range(2):
                    P.op("dve", lambda v: v.memset(QdA[i_][:], 0.0), w=[B_QdA[i_]])
                    P.op("dve", lambda v: v.memset(QdB[i_][:], 0.0), w=[B_QdB[i_]])

                def chain(h, d):
                    if True:
                        Sst = Sst2[d]
                        B_S = B_S2[d]
                        Scar, B_Scar = (ScarA, B_ScarA) if d == 0 else (ScarB, B_ScarB)
                        o_acc = o_accs[d]
                        B_oacc = B_oaccs[d]
                        cp = 0
                        col = d * 4 + h
                        lb_ap = lbt[:, col:col + 1]
                        oml_ap = omlt[:, col:col + 1]
                        P.op("dve", lambda v: v.memset(Sst[:], 0.0), w=[B_S])
                        P.op("dve", lambda v: v.memset(Scar[0][:], 0.0), w=[B_Scar[0]])
                        P.op("dve", lambda v: v.memset(Scar[1][:], 0.0), w=[B_Scar[1]])
                        order = list(range(17)) if d == 0 else [0] + list(range(16, 0, -1))
                        for k in order:
                            t0, nt = SCS[k]
                            n = nt * 128
                            s0 = t0 * 128
                            nch = n // 64
                            lat = k >= 1
                            i2 = d
                            z_ = zin[i2]
                            P.dma("sp", lambda q: q.dma_start(out=z_[:, 0, 0:n], in_=zt_d[h, d][:, s0:s0 + n]),
                                  r=B_zt[h], w=[B_zin[i2]])
                            P.dma("sp", lambda q: q.dma_start(out=z_[:, 1, 0:n], in_=zt_d[h, 2][:, s0:s0 + n]),
                                  r=B_zt[h], w=[B_zin[i2]])
                            P.dma("sp", lambda q: q.dma_start(
                                out=vt[i2][:, 0:nt, :], in_=vi_d[s0:s0 + n, h, 1, :].rearrange("(t p) c -> p t c", p=128)),
                                r=B_vi, w=[B_vt[i2]])
                            zz = z_[:, 0, 0:n]
                            hq = z_[:, 1, 0:n]
                            P.op("act", lambda a: a.activation(out=e_[i2][:, 0:n], in_=zz, func=AF.Exp, scale=-1.0),
                                 r=[B_zin[i2]], w=[B_e[i2]])
                            yield
                            P.op("dve", lambda v: v.tensor_scalar(out=e_[i2][:, 0:n], in0=e_[i2][:, 0:n], scalar1=1.0, scalar2=None,
                                                                  op0=ALU.add), r=[B_e[i2]], w=[B_e[i2]])
                            yield
                            P.op("act", lambda a: a.activation(out=e_[i2][:, 0:n], in_=e_[i2][:, 0:n], func=AF.Ln), r=[B_e[i2]], w=[B_e[i2]])
                            P.op("act", lambda a: a.activation(out=e_[i2][:, 0:n], in_=e_[i2][:, 0:n], func=AF.Exp, scale=-1.0),
                                 r=[B_e[i2]], w=[B_e[i2]])
                            yield
                            P.op("dve", lambda v: v.tensor_scalar(out=f_[i2][:, 0:n], in0=e_[i2][:, 0:n], scalar1=oml_ap, scalar2=lb_ap,
                                                                  op0=ALU.mult, op1=ALU.add), r=[B_e[i2], B_const], w=[B_f_[i2]])
                            yield
                            P.op("act", lambda a: a.activation(out=lf[i2][:, 0:n], in_=f_[i2][:, 0:n], func=AF.Ln),
                                 r=[B_f_[i2]], w=[B_lf[i2]])
                            P.op("act", lambda a: a.activation(out=kk[i2][:, 0:n], in_=f_[i2][:, 0:n], func=AF.Copy, scale=-1.0, bias=1.0),
                                 r=[B_f_[i2]], w=[B_kk[i2]])
                            yield
                            if d == 0:
                                P.op("dve", lambda v: v.tensor_tensor_scan(out=bc[i2][:, 0:n], data0=rm[:, 0, 0:n], data1=lf[i2][:, 0:n],
                                                                           initial=0.0, op0=ALU.mult, op1=ALU.add),
                                     r=[B_lf[i2], B_c2], w=[B_bc[i2]])
                            else:
                                P.op("dve", lambda v: v.tensor_tensor_scan(out=bc[i2][:, 0:n][:, ::-1], data0=rm[:, 1, 0:n][:, ::-1],
                                                                           data1=lf[i2][:, 0:n][:, ::-1],
                                                                           initial=0.0, op0=ALU.mult, op1=ALU.add),
                                     r=[B_lf[i2], B_c2], w=[B_bc[i2]])
                            yield
                            P.op("act", lambda a: a.activation(out=ep[i2][:, 0:n], in_=bc[i2][:, 0:n], func=AF.Exp),
                                 r=[B_bc[i2]], w=[B_ep[i2]])
                            P.op("act", lambda a: a.activation(out=en[i2][:, 0:n], in_=bc[i2][:, 0:n], func=AF.Exp, scale=-1.0),
                                 r=[B_bc[i2]], w=[B_en[i2]])
                            yield
                            if lat:
                                P.op("dve", lambda v: v.tensor_tensor(out=Qd[i2][:, 0:n], in0=hq, in1=ep[i2][:, 0:n], op=ALU.mult),
                                     r=[B_zin[i2], B_ep[i2]], w=[B_Qd[i2]])
                                P.op("act", lambda a: a.copy(out=QdA[i2][:, 0:n].rearrange("p (t c) -> p t c", c=128)[:, :, 0:64],
                                                             in_=Qd[i2][:, 0:n].rearrange("p (t c) -> p t c", c=128)[:, :, 0:64]),
                                     r=[B_Qd[i2]], w=[B_QdA[i2]])
                                P.op("act", lambda a: a.copy(out=QdB[i2][:, 0:n].rearrange("p (t c) -> p t c", c=128)[:, :, 64:128],
                                                             in_=Qd[i2][:, 0:n].rearrange("p (t c) -> p t c", c=128)[:, :, 64:128]),
                                     r=[B_Qd[i2]], w=[B_QdB[i2]])
                            P.op("dve", lambda g: g.tensor_tensor(out=kdf[i2][:, 0:n], in0=kk[i2][:, 0:n], in1=en[i2][:, 0:n], op=ALU.mult),
                                 r=[B_kk[i2], B_en[i2]], w=[B_kdf[i2]])
                            if lat:
                                P.op("act", lambda a: a.copy(out=Kd[i2][:, 0:n], in_=kdf[i2][:, 0:n]),
                                     r=[B_kdf[i2]], w=[B_Kd[i2]])
                            endcol = 63 if d == 0 else 0
                            P.op("dve", lambda v: v.tensor_copy(out=dec[i2][:, 0:nch],
                                                                in_=ep[i2][:, 0:n].rearrange("p (c j) -> p c j", j=64)[:, :, endcol]),
                                 r=[B_ep[i2]], w=[B_dec[i2]])
                            P.op("dve", lambda v: v.tensor_tensor(
                                out=K2T[i2][:, 0:n].rearrange("p (c j) -> p c j", j=64),
                                in0=kdf[i2][:, 0:n].rearrange("p (c j) -> p c j", j=64),
                                in1=dec[i2][:, 0:nch].unsqueeze(2).to_broadcast([128, nch, 64]), op=ALU.mult),
                                r=[B_kdf[i2], B_dec[i2]], w=[B_K2T[i2]])
                            yield
                            pe_group([(lambda pe, j=j: pe.transpose(out=tpb[:, j * 128:(j + 1) * 128], in_=K2T[i2][:, j * 128:(j + 1) * 128],
                                                                     identity=ident_bf[:])) for j in range(nt)],
                                     r=[B_K2T[i2], B_const], w=[B_tpb])
                            P.op("act", lambda a: a.copy(out=k2all[d][:, 0:nt, :].rearrange("p t c -> p (t c)"), in_=tpb[:, 0:n]),
                                 r=[B_tpb], w=[B_k2all[d]])
                            for gc in range(nch):
                                j, c = gc // 2, gc % 2
                                rows = slice(c * 64, (c + 1) * 64)
                                uo = c * 512 + j * 128
                                mm1(ups2[d][:, uo:uo + 128], k2all[d][rows, j, :], vt[i2][rows, j, :], True, True,
                                    [B_k2all[d], B_vt[i2]], [B_ups2[d][c]])
                            if lat:
                                for j in range(nt):
                                    cs_ = slice(j * 128, (j + 1) * 128)
                                    mm1(scp[:, cs_], Kd[i2][:, cs_], Qd[i2][:, cs_], True, True, [B_Kd[i2], B_Qd[i2]], [B_scp])
                                P.op("dve", lambda v: v.tensor_tensor(
                                    out=scma[d][:], in0=scp[:, :].rearrange("p (t c) -> p t c", c=128),
                                    in1=cm_f[:, 1 + d, :].unsqueeze(1).to_broadcast([128, 4, 128]), op=ALU.mult),
                                    r=[B_scp, B_const], w=[B_scma[d]])
                            yield
                            seq = list(range(nch)) if d == 0 else list(range(nch - 1, -1, -1))
                            for m, gc in enumerate(seq):
                                last = m == nch - 1
                                uo = (gc % 2) * 512 + (gc // 2) * 128
                                if last:
                                    dst, bdst = Scar[1 - cp][:], B_Scar[1 - cp]
                                else:
                                    dst, bdst = Sball[d][:, seq[m + 1], :], B_Sball[d]
                                if lat or last:
                                    P.op("dve", lambda v: v.scalar_tensor_tensor(out=dst, in0=Sst[:], scalar=dec[i2][:, gc:gc + 1],
                                                                                 in1=ups2[d][:, uo:uo + 128],
                                                                                 op0=ALU.mult, op1=ALU.add),
                                         r=[B_S, B_dec[i2], B_ups2[d][gc % 2]], w=[bdst])
                                P.op("dve", lambda v: v.scalar_tensor_tensor(out=Sst[:], in0=Sst[:], scalar=dec[i2][:, gc:gc + 1],
                                                                             in1=ups2[d][:, uo:uo + 128],
                                                                             op0=ALU.mult, op1=ALU.add),
                                     r=[B_S, B_dec[i2], B_ups2[d][gc % 2]], w=[B_S])
                            yield
                            if lat:
                                for j in range(nt):
                                    cs_ = slice(j * 128, (j + 1) * 128)
                                    mm1(ops2[d][:, cs_], scma[d][:, j, :], vt[i2][:, j, :], True, False,
                                        [B_scma[d], B_vt[i2]], [B_ops2[d]])
                                    for c in range(2):
                                        gc = 2 * j + c
                                        qsel = QdA if c == 0 else QdB
                                        bq = B_QdA if c == 0 else B_QdB
                                        if gc == seq[0]:
                                            sap, bs = Scar[cp][:], B_Scar[cp]
                                        else:
                                            sap, bs = Sball[d][:, gc, :], B_Sball[d]
                                        mm1(ops2[d][:, cs_], qsel[i2][:, cs_], sap, False, c == 1, [bq[i2], bs], [B_ops2[d]])
                                gt0 = (k - 1) * 4
                                P.op("act", lambda a: a.copy(out=o_acc[:, gt0:gt0 + 4, :].rearrange("p t c -> p (t c)"), in_=ops2[d][:, :]),
                                     r=[B_ops2[d]], w=[B_oacc[gt0 + jj] for jj in range(4)])
                            cp = 1 - cp
                            yield

                def combine(h):
                    for k in range(1, 17):
                        t0, nt = SCS[k]
                        s0 = t0 * 128
                        i2 = k % 2
                        gt0 = (k - 1) * 4
                        P.dma("sp", lambda q: q.dma_start(
                            out=gt[i2][:, 0:4, :], in_=g_d[s0:s0 + 512, h, :].rearrange("(t p) c -> p t c", p=128)),
                            r=B_g, w=[B_gt[i2]])
                        ro = [B_oaccs[dd][gt0 + jj] for dd in range(2) for jj in range(4)]
                        P.op("dve", lambda v: v.tensor_tensor(out=otot[i2][:], in0=o_accs[0][:, gt0:gt0 + 4, :],
                                                              in1=o_accs[1][:, gt0:gt0 + 4, :], op=ALU.add), r=ro, w=[B_otot[i2]])
                        P.op("dve", lambda v: v.tensor_tensor(out=osq[i2][:], in0=otot[i2][:], in1=otot[i2][:], op=ALU.mult),
                             r=[B_otot[i2]], w=[B_osq[i2]])
                        P.op("dve", lambda v: v.tensor_reduce(out=osm4[i2][:, 0:4], in_=osq[i2][:], axis=AX.X, op=ALU.add),
                             r=[B_osq[i2]], w=[B_osm4[i2]])
                        rstd_from_ss(osm4[i2][:, 0:4], 128, osm4[i2][:, 4:8], osm4[i2][:, 0:4], [B_osm4[i2]], [B_osm4[i2]], B_osm4[i2])
                        P.op("dve", lambda v: v.tensor_tensor(out=otot[i2][:], in0=otot[i2][:],
                                                              in1=osm4[i2][:, 4:8].unsqueeze(2).to_broadcast([128, 4, 128]), op=ALU.mult),
                             r=[B_otot[i2], B_osm4[i2]], w=[B_otot[i2]])
                        P.op("dve", lambda v: v.tensor_tensor(out=otot[i2][:], in0=otot[i2][:],
                                                              in1=hgn[:].unsqueeze(1).to_broadcast([128, 4, 128]), op=ALU.mult),
                             r=[B_otot[i2], B_c2], w=[B_otot[i2]])
                        P.op("pool", lambda g: g.tensor_tensor(out=yb4[i2][:], in0=otot[i2][:], in1=gt[i2][:, 0:4, :], op=ALU.mult),
                             r=[B_otot[i2], B_gt[i2]], w=[B_yb4[i2]])
                        pe_group([(lambda pe, j=j: pe.transpose(out=tpb[:, j * 128:(j + 1) * 128], in_=yb4[i2][:, j, :],
                                                                 identity=ident_bf[:])) for j in range(4)],
                                 r=[B_yb4[i2], B_const], w=[B_tpb])
                        P.op("act", lambda a: a.copy(out=mixs[i2][:], in_=tpb[:, 0:512]), r=[B_tpb], w=[B_mixs[i2]])
                        P.dma("pool", lambda q: q.dma_start(
                            out=mixt_d[512 + h * 128:512 + (h + 1) * 128, (k - 1) * 512:k * 512], in_=mixs[i2][:]),
                            r=[B_mixs[i2]], w=[B_mixt[k - 1]])

                for h in heads:
                    alive = [chain(h, 0), chain(h, 1)]
                    while alive:
                        for g_ in list(alive):
                            try:
                                next(g_)
                            except StopIteration:
                                alive.remove(g_)
                    combine(h)
                P.barrier()

        AFF = sb(es, "AFF", [128, NTILE, NE], F32)
        B_AFF = [Buf() for _ in range(NTILE)]
        B_h2t = [Buf() for _ in range(NTILE)]
        B_afft = [Buf() for _ in range(NTILE)]

        def phase_b():
            with ExitStack() as st:
                wo = sb(st, "wo", [128, 8, D], BF16)
                B_wo = [Buf() for _ in range(4)]
                for pi in range(4):
                    P.dma("pool", lambda q: q.dma_start(out=wo[:, 2 * pi:2 * pi + 2, :], in_=wout_d[:, 2 * pi:2 * pi + 2, :]),
                          w=[B_wo[pi]])
                wr = sb(st, "wr", [128, 8, NE], F32)
                B_wr = Buf()
                P.dma("sp", lambda q: q.dma_start(out=wr[:], in_=wr_d[:]), w=[B_wr])
                gpm, B_gpm = load_bc(st, "gpm", 4)
                g2m, B_g2m = load_bc(st, "g2m", 5)
                sh2, B_sh2 = load_bc(st, "sh2", 6)
                mix = [sb(st, "mix%d" % i, [128, 8, 512], BF16) for i in range(2)]
                B_mix = [Buf(), Buf()]
                xb = [sb(st, "bxb%d" % i, [128, D], F32) for i in range(2)]
                B_xb = [Buf(), Buf()]
                tt = [sb(st, "btt%d" % i, [128, D], F32) for i in range(2)]
                B_tt = [Buf(), Buf()]
                x1 = [sb(st, "bx1%d" % i, [128, D], F32) for i in range(2)]
                B_x1s = [Buf(), Buf()]
                h2f = [sb(st, "h2f%d" % i, [128, D], F32) for i in range(2)]
                B_h2f = [Buf(), Buf()]
                h2b = [sb(st, "h2b%d" % i, [128, D], BF16) for i in range(2)]
                B_h2b = [Buf(), Buf()]
                junk = sb(st, "bjunk", [128, D], BF16)
                B_junk = Buf()
                h2T = [sb(st, "h2T%d" % i, [128, 8, 128], F32) for i in range(2)]
                B_h2T = [Buf(), Buf()]
                sm = sb(st, "bsm", [128, 2, 8], F32)
                B_sm = [Buf(), Buf()]
                ee = sb(st, "bee", [128, 2, NE], F32)
                yps = [ps(st, "yps%d" % i, [128, D]) for i in range(2)]
                B_yps = [Buf(), Buf()]
                trp = ps(st, "trp", [128, D])
                B_trp = Buf()
                lgp = ps(st, "lgp", [128, 512])
                B_lgp = Buf()
                for sc in range(16):
                    m_ = mix[sc % 2]
                    P.dma("sp", lambda q: q.dma_start(out=m_[:], in_=mixt_d[:, sc * 512:(sc + 1) * 512].rearrange("(kc p) t -> p kc t", p=128)),
                          r=[B_mixt[sc]], w=[B_mix[sc % 2]])
                    for j in range(4):
                        tl = sc * 4 + j
                        i2 = tl % 2
                        y_ = yps[i2]
                        for half in range(2):
                            P.mm(y_[:, half * 512:(half + 1) * 512],
                                 [(m_[:, kc, j * 128:(j + 1) * 128], wo[:, kc, half * 512:(half + 1) * 512]) for kc in range(8)],
                                 r=[B_mix[sc % 2]] + B_wo, w=[B_yps[i2]])
                        s_ = sm[:, i2, :]
                        for half in range(2):
                            P.op("act", lambda a: a.activation(out=junk[:, half * 512:(half + 1) * 512], in_=y_[:, half * 512:(half + 1) * 512],
                                                               func=AF.Square, accum_out=s_[:, half:half + 1]),
                                 r=[B_yps[i2]], w=[B_junk, B_sm[i2]])
                        P.op("dve", lambda v: v.tensor_tensor(out=s_[:, 2:3], in0=s_[:, 0:1], in1=s_[:, 1:2], op=ALU.add),
                             r=[B_sm[i2]], w=[B_sm[i2]])
                        rstd_from_ss(s_[:, 2:3], D, s_[:, 3:4], s_[:, 2:3], [B_sm[i2]], [B_sm[i2]], B_sm[i2])
                        P.dma("sp", lambda q: q.dma_start(out=xb[i2][:], in_=x_d[tl * 128:(tl + 1) * 128, :]), w=[B_xb[i2]])
                        for half in range(2):
                            hs = slice(half * 512, (half + 1) * 512)
                            P.op("dve", lambda v: v.scalar_tensor_tensor(out=tt[i2][:, hs], in0=y_[:, hs], scalar=s_[:, 3:4], in1=gpm[:, hs],
                                                                         op0=ALU.mult, op1=ALU.mult),
                                 r=[B_yps[i2], B_sm[i2], B_gpm], w=[B_tt[i2]])
                        P.op("dve", lambda g: g.tensor_tensor(out=x1[i2][:], in0=tt[i2][:], in1=xb[i2][:], op=ALU.add),
                             r=[B_tt[i2], B_xb[i2]], w=[B_x1s[i2]])
                        P.dma("pool", lambda q: q.dma_start(out=x1_d[tl * 128:(tl + 1) * 128, :], in_=x1[i2][:]),
                              r=[B_x1s[i2]], w=[B_x1[tl]])
                        P.op("dve", lambda v: v.scalar_tensor_tensor(out=junk[:], in0=x1[i2][:], scalar=1.0, in1=x1[i2][:],
                                                                     op0=ALU.mult, op1=ALU.mult, accum_out=s_[:, 4:5]),
                             r=[B_x1s[i2]], w=[B_junk, B_sm[i2]])
                        rstd_from_ss(s_[:, 4:5], D, s_[:, 5:6], s_[:, 4:5], [B_sm[i2]], [B_sm[i2]], B_sm[i2])
                        P.op("dve", lambda v: v.scalar_tensor_tensor(out=tt[i2][:], in0=x1[i2][:], scalar=s_[:, 5:6], in1=g2m[:],
                                                                     op0=ALU.mult, op1=ALU.mult),
                             r=[B_x1s[i2], B_sm[i2], B_g2m], w=[B_tt[i2]])
                        P.op("dve", lambda g: g.tensor_tensor(out=h2f[i2][:], in0=tt[i2][:], in1=sh2[:], op=ALU.add),
                             r=[B_tt[i2], B_sh2], w=[B_h2f[i2]])
                        P.op("act", lambda a: a.copy(out=h2b[i2][:], in_=h2f[i2][:]), r=[B_h2f[i2]], w=[B_h2b[i2]])
                        P.dma("pool", lambda q: q.dma_start(out=h2_d[tl * 128:(tl + 1) * 128, :], in_=h2b[i2][:]),
                              r=[B_h2b[i2]], w=[B_h2t[tl]])
                        pe_group([(lambda pe, kc=kc: pe.transpose(out=trp[:, kc * 128:(kc + 1) * 128],
                                                                   in_=h2f[i2][:, kc * 128:(kc + 1) * 128], identity=ident_f))
                                  for kc in range(8)], r=[B_h2f[i2], B_const], w=[B_trp])
                        P.op("act", lambda a: a.copy(out=h2T[i2][:, 0:4, :].rearrange("p k t -> p (k t)"), in_=trp[:, 0:512]),
                             r=[B_trp], w=[B_h2T[i2]])
                        P.op("dve", lambda v: v.tensor_copy(out=h2T[i2][:, 4:8, :].rearrange("p k t -> p (k t)"), in_=trp[:, 512:1024]),
                             r=[B_trp], w=[B_h2T[i2]])
                        P.mm(lgp[:, 0:NE], [(h2T[i2][:, kc, :], wr[:, kc, :]) for kc in range(8)],
                             r=[B_h2T[i2], B_wr], w=[B_lgp])
                        P.op("dve", lambda v: v.tensor_reduce(out=s_[:, 6:7], in_=lgp[:, 0:NE], axis=AX.X, op=ALU.max, negate=True),
                             r=[B_lgp], w=[B_sm[i2]])
                        P.op("act", lambda a: a.activation(out=ee[:, i2, :], in_=lgp[:, 0:NE], func=AF.Exp, bias=s_[:, 6:7],
                                                           accum_out=s_[:, 7:8]), r=[B_lgp, B_sm[i2]], w=[B_sm[i2]])
                        P.op("dve", lambda v: v.reciprocal(out=s_[:, 7:8], in_=s_[:, 7:8]), r=[B_sm[i2]], w=[B_sm[i2]])
                        P.op("dve", lambda v: v.tensor_scalar(out=AFF[:, tl, :], in0=ee[:, i2, :], scalar1=s_[:, 7:8], scalar2=None,
                                                              op0=ALU.mult), r=[B_sm[i2]], w=[B_AFF[tl]])
                        P.dma("pool", lambda q: q.dma_start(out=aff_d[tl * 128:(tl + 1) * 128, :], in_=AFF[:, tl, :]),
                              r=[B_AFF[tl]], w=[B_afft[tl]])
                P.barrier()

        posm = sb(es, "posm", [128, NE, NTILE], F32)
        B_posm = Buf()

        def phase_c():
            with ExitStack() as st:
                lo = sb(st, "c_lo", [128, NE], F32)
                hi = sb(st, "c_hi", [128, NE], F32)
                mid = sb(st, "c_mid", [128, NE], F32)
                ge = sb(st, "c_ge", [128, NTILE, NE], F32)
                cntp = sb(st, "c_cntp", [128, NE], F32)
                mge = sb(st, "c_mge", [128, NE], U32)
                mlt = sb(st, "c_mlt", [128, NE], U32)
                Mt = sb(st, "c_Mt", [128, NE, NTILE], F32)
                Psc = sb(st, "c_Psc", [128, NE, NTILE], F32)
                rmc = sb(st, "c_rmc", [128, 1024], F32)
                Tt = sb(st, "c_Tt", [128, NE], BF16)
                Lbf = sb(st, "c_Lbf", [128, 128], BF16)
                off = sb(st, "c_off", [128, NE], F32)
                cps = ps(st, "c_cps", [128, 512])
                B_lo, B_hi, B_mid, B_ge, B_cntp, B_m, B_cps, B_x = Buf(), Buf(), Buf(), Buf(), Buf(), Buf(), Buf(), Buf()
                P.dma("sp", lambda q: q.dma_start(out=rmc[:], in_=rm_d[:, 0, :]), w=[B_x])
                P.op("dve", lambda v: v.tensor_copy(out=Lbf[:], in_=cm_f[:, 3, :]), r=[B_const], w=[B_x])
                P.op("dve", lambda v: v.memset(lo[:], 0.0), w=[B_lo])
                P.op("dve", lambda v: v.memset(hi[:], 2.0), w=[B_hi])
                for it in range(34):
                    P.op("dve", lambda v: v.tensor_tensor(out=mid[:], in0=lo[:], in1=hi[:], op=ALU.add), r=[B_lo, B_hi], w=[B_mid])
                    P.op("dve", lambda v: v.tensor_scalar(out=mid[:], in0=mid[:], scalar1=0.5, scalar2=None, op0=ALU.mult),
                         r=[B_mid], w=[B_mid])
                    P.op("dve", lambda v: v.tensor_tensor(out=ge[:], in0=AFF[:], in1=mid[:].unsqueeze(1).to_broadcast([128, NTILE, NE]),
                                                          op=ALU.is_ge), r=B_AFF + [B_mid], w=[B_ge])
                    P.op("dve", lambda v: v.tensor_reduce(out=cntp[:], in_=ge[:].rearrange("p i e -> p e i"), axis=AX.X, op=ALU.add),
                         r=[B_ge], w=[B_cntp])
                    P.mm(cps[:, 0:NE], [(ones_f[:], cntp[:])], r=[B_cntp, B_const], w=[B_cps])
                    P.op("dve", lambda v: v.tensor_scalar(out=mge[:], in0=cps[:, 0:NE], scalar1=float(CAP), scalar2=None, op0=ALU.is_ge),
                         r=[B_cps], w=[B_m])
                    P.op("dve", lambda v: v.tensor_scalar(out=mlt[:], in0=cps[:, 0:NE], scalar1=float(CAP), scalar2=None, op0=ALU.is_lt),
                         r=[B_cps], w=[B_m])
                    P.op("dve", lambda v: v.copy_predicated(out=lo[:], mask=mge[:], data=mid[:]), r=[B_m, B_mid], w=[B_lo])
                    P.op("dve", lambda v: v.copy_predicated(out=hi[:], mask=mlt[:], data=mid[:]), r=[B_m, B_mid], w=[B_hi])
                P.op("dve", lambda v: v.tensor_tensor(out=ge[:], in0=AFF[:], in1=lo[:].unsqueeze(1).to_broadcast([128, NTILE, NE]),
                                                      op=ALU.is_ge), r=B_AFF + [B_lo], w=[B_ge])
                P.op("dve", lambda v: v.tensor_copy(out=Mt[:], in_=ge[:].rearrange("p i e -> p e i")), r=[B_ge], w=[B_x])
                P.op("dve", lambda v: v.tensor_tensor_scan(out=Psc[:].rearrange("p e i -> p (e i)"), data0=rmc[:],
                                                           data1=Mt[:].rearrange("p e i -> p (e i)"), initial=0.0,
                                                           op0=ALU.mult, op1=ALU.add), r=[B_x], w=[B_x])
                P.op("dve", lambda v: v.tensor_copy(out=Tt[:], in_=Psc[:, :, NTILE - 1]), r=[B_x], w=[B_x])
                P.mm(cps[:, 0:NE], [(Lbf[:], Tt[:])], r=[B_x], w=[B_cps])
                P.op("dve", lambda v: v.tensor_copy(out=off[:], in_=cps[:, 0:NE]), r=[B_cps], w=[B_x])
                P.op("dve", lambda v: v.tensor_tensor(out=Psc[:], in0=Psc[:], in1=off[:].unsqueeze(2).to_broadcast([128, NE, NTILE]),
                                                      op=ALU.add), r=[B_x], w=[B_x])
                P.op("dve", lambda v: v.tensor_tensor(out=Psc[:], in0=Psc[:], in1=Mt[:], op=ALU.mult), r=[B_x], w=[B_x])
                P.op("dve", lambda v: v.tensor_scalar(out=posm[:], in0=Psc[:], scalar1=-1.0, scalar2=None, op0=ALU.add),
                     r=[B_x], w=[B_posm])
                P.barrier()

        def idma(fn, r, w):
            return P.dma("pool", fn, r=r, w=w)

        def phase_d(experts=range(NE)):
            with ExitStack() as st:
                iota = sb(st, "d_iota", [128, 1024], F32)
                tokf = sb(st, "d_tokf", [128, NTILE, 2], F32)
                tokb = sb(st, "d_tokb", [128, NTILE, 2], BF16)
                zt_ = sb(st, "d_zero", [128, D], F32)
                B_dc = Buf()
                P.dma("sp", lambda q: q.dma_start(out=iota[:], in_=iota_d[:]), w=[B_dc])
                P.dma("sp", lambda q: q.dma_start(out=tokf[:], in_=tokhl_d[:]), w=[B_dc])
                P.op("dve", lambda v: v.tensor_copy(out=tokb[:], in_=tokf[:]), r=[B_dc], w=[B_dc])
                P.op("dve", lambda v: v.memset(zt_[:], 0.0), w=[B_dc])
                fview = f_d.rearrange("(t p) d -> p t d", p=128)
                for part in range(4):
                    P.dma("sp", lambda q: q.dma_start(out=fview[:, part * 16:(part + 1) * 16, :],
                                                      in_=zt_[:].unsqueeze(1).to_broadcast([128, 16, D])), r=[B_dc], w=[B_f])
                sel = [sb(st, "d_sel%d" % i, [128, 1024], BF16) for i in range(4)]
                B_sel = [Buf() for _ in range(4)]
                idxf = sb(st, "d_idxf", [2, 1024], F32)
                idx2 = sb(st, "d_idx2", [128, 8], F32)
                idxi = [sb(st, "d_idxi%d" % i, [128, 8], I32) for i in range(2)]
                B_idxf, B_idx2 = Buf(), Buf()
                B_idxi = [Buf(), Buf()]
                X = [sb(st, "d_X%d" % i, [128, D], BF16) for i in range(16)]
                B_X = [Buf() for _ in range(16)]
                gat = [sb(st, "d_gat%d" % i, [128, 8, NE], F32) for i in range(2)]
                B_gat = [Buf(), Buf()]
                XT = sb(st, "d_XT", [128, 8, 1024], BF16)
                B_XT = Buf()
                AT = sb(st, "d_AT", [128, 8, 1024], BF16)
                B_AT = Buf()
                W = [[sb(st, "d_w%d_%d" % (m, i), [128, 8, D], BF16) for m in range(3)] for i in range(2)]
                B_W = [[[Buf() for _ in range(4)] for _ in range(3)] for _ in range(2)]
                sg = [sb(st, "d_sg%d" % i, [128, 512], F32) for i in range(2)]
                B_sg = [Buf(), Buf()]
                Ysb = [sb(st, "d_Y%d" % i, [128, D], F32) for i in range(2)]
                B_Y = [Buf(), Buf()]
                ips = [ps(st, "d_ips%d" % i, [128, 512]) for i in range(2)]
                B_ips = [Buf(), Buf()]
                tpx = ps(st, "d_tpx", [128, 8, 128], BF16)
                B_tpx = Buf()
                itp = ps(st, "d_itp", [128, 512])
                B_itp = Buf()
                gps = [ps(st, "d_gps%d" % i, [128, 512]) for i in range(2)]
                B_gps = [Buf(), Buf()]
                ups = [ps(st, "d_ups%d" % i, [128, 512]) for i in range(2)]
                B_ups = [Buf(), Buf()]
                wsrc = (wg_d, wu_d, wd_d)

                def load_w(e, slot):
                    for m in range(3):
                        for pi in range(4):
                            P.dma("pool", lambda q: q.dma_start(out=W[slot][m][:, 2 * pi:2 * pi + 2, :],
                                                                in_=wsrc[m][e][:, 2 * pi:2 * pi + 2, :]), w=[B_W[slot][m][pi]])

                elist = list(experts)
                ctr = dict(sel=0, g=0, y=0)

                def compaction(e, slot):
                    for i in range(NTILE):
                        si = ctr["sel"] % 4
                        ctr["sel"] += 1
                        P.op("dve", lambda v: v.tensor_scalar(out=sel[si][:], in0=iota[:], scalar1=posm[:, e, i:i + 1], scalar2=None,
                                                            op0=ALU.is_equal), r=[B_dc, B_posm], w=[B_sel[si]])
                        for half in range(2):
                            mm1(ips[half][0:2, :], tokb[:, i, :], sel[si][:, half * 512:(half + 1) * 512], i == 0, i == NTILE - 1,
                                [B_dc, B_sel[si]], [B_ips[half]])
                        if i % 4 == 3 and i != NTILE - 1:
                            yield
                    for half in range(2):
                        P.op("act", lambda a: a.copy(out=idxf[:, half * 512:(half + 1) * 512], in_=ips[half][0:2, :]),
                             r=[B_ips[half]], w=[B_idxf])
                    pe_group([(lambda pe, jt=jt: pe.transpose(out=itp[:, 2 * jt:2 * jt + 2], in_=idxf[0:2, jt * 128:(jt + 1) * 128],
                                                               identity=ident_f[0:2, 0:2])) for jt in range(8)],
                             r=[B_idxf, B_const], w=[B_itp])
                    P.op("dve", lambda v: v.tensor_reduce(out=idx2[:], in_=itp[:, 0:16].rearrange("p (j t) -> p j t", t=2),
                                                          axis=AX.X, op=ALU.add), r=[B_itp], w=[B_idx2])
                    P.op("dve", lambda v: v.tensor_copy(out=idxi[slot][:], in_=idx2[:]), r=[B_idx2], w=[B_idxi[slot]])
                    yield

                def gather(e, slot):
                    ii = idxi[slot]
                    for jt in range(8):
                        xj = X[slot * 8 + jt]
                        idma(lambda q: q.indirect_dma_start(out=xj[:], out_offset=None, in_=h2_d[:, :],
                                                            in_offset=IndirectOffsetOnAxis(ap=ii[:, jt:jt + 1], axis=0)),
                             r=[B_idxi[slot]] + B_h2t, w=[B_X[slot * 8 + jt]])
                        idma(lambda q: q.indirect_dma_start(out=gat[slot][:, jt, :], out_offset=None, in_=aff_d[:, :],
                                                            in_offset=IndirectOffsetOnAxis(ap=ii[:, jt:jt + 1], axis=0)),
                             r=[B_idxi[slot]] + B_afft, w=[B_gat[slot]])

                load_w(elist[0], 0)
                for _ in compaction(elist[0], 0):
                    pass
                gather(elist[0], 0)
                for ei, e in enumerate(elist):
                    slot = ei % 2
                    ii = idxi[slot]
                    g_ = gat[slot]
                    nxt = None
                    if ei + 1 < len(elist):
                        load_w(elist[ei + 1], 1 - slot)
                        nxt = compaction(elist[ei + 1], 1 - slot)
                    for jt in range(8):
                        xj = X[slot * 8 + jt]
                        pe_group([(lambda pe, kc=kc: pe.transpose(out=tpx[:, kc, :], in_=xj[:, kc * 128:(kc + 1) * 128],
                                                                   identity=ident_bf[:])) for kc in range(8)],
                                 r=[B_X[slot * 8 + jt], B_const], w=[B_tpx])
                        if jt % 2 == 0:
                            P.op("act", lambda a: a.copy(out=XT[:, :, jt * 128:(jt + 1) * 128], in_=tpx[:]), r=[B_tpx], w=[B_XT])
                        else:
                            P.op("dve", lambda v: v.tensor_copy(out=XT[:, :, jt * 128:(jt + 1) * 128], in_=tpx[:]), r=[B_tpx], w=[B_XT])
                    wg_, wu_, wd_ = W[slot]
                    bwg, bwu, bwd = B_W[slot]
                    for fc in range(8):
                        for sh in range(2):
                            gi = ctr["g"] % 2
                            ctr["g"] += 1
                            cs_ = slice(sh * 512, (sh + 1) * 512)
                            P.mm(gps[gi][:], [(wg_[:, kc, fc * 128:(fc + 1) * 128], XT[:, kc, cs_]) for kc in range(8)],
                                 r=[B_XT] + bwg, w=[B_gps[gi]])
                            P.mm(ups[gi][:], [(wu_[:, kc, fc * 128:(fc + 1) * 128], XT[:, kc, cs_]) for kc in range(8)],
                                 r=[B_XT] + bwu, w=[B_ups[gi]])
                            P.op("act", lambda a: a.activation(out=sg[gi][:], in_=gps[gi][:], func=AF.Silu),
                                 r=[B_gps[gi]], w=[B_sg[gi]])
                            P.op("dve", lambda v: v.tensor_tensor(out=AT[:, fc, cs_], in0=ups[gi][:], in1=sg[gi][:], op=ALU.mult),
                                 r=[B_ups[gi], B_sg[gi]], w=[B_AT])
                            if nxt is not None:
                                next(nxt, None)
                    if nxt is not None:
                        for _ in nxt:
                            pass
                        gather(elist[ei + 1], 1 - slot)
                    for jt in range(8):
                        yi = ctr["y"] % 2
                        ctr["y"] += 1
                        for dh in range(2):
                            gi = ctr["g"] % 2
                            ctr["g"] += 1
                            P.mm(gps[gi][:], [(AT[:, fc, jt * 128:(jt + 1) * 128], wd_[:, fc, dh * 512:(dh + 1) * 512]) for fc in range(8)],
                                 r=[B_AT] + bwd, w=[B_gps[gi]])
                            P.op("dve", lambda v: v.tensor_scalar(out=Ysb[yi][:, dh * 512:(dh + 1) * 512], in0=gps[gi][:],
                                                                  scalar1=g_[:, jt, e:e + 1], scalar2=None, op0=ALU.mult),
                                 r=[B_gps[gi], B_gat[slot]], w=[B_Y[yi]])
                        idma(lambda q: q.indirect_dma_start(out=f_d[:, :], out_offset=IndirectOffsetOnAxis(ap=ii[:, jt:jt + 1], axis=0),
                                                            in_=Ysb[yi][:], in_offset=None, compute_op=ALU.add),
                             r=[B_Y[yi], B_idxi[slot]], w=[B_f])
                P.barrier()

        def phase_e():
            with ExitStack() as st:
                gpf, B_gpf = load_bc(st, "gpf", 7)
                fb = [sb(st, "e_f%d" % i, [128, D], F32) for i in range(2)]
                xb = [sb(st, "e_x%d" % i, [128, D], F32) for i in range(2)]
                tb = [sb(st, "e_t%d" % i, [128, D], F32) for i in range(2)]
                ob_ = [sb(st, "e_o%d" % i, [128, D], F32) for i in range(2)]
                junk = sb(st, "e_junk", [128, D], BF16)
                sm = sb(st, "e_sm", [128, 2, 2], F32)
                B_fb, B_xb, B_tb, B_ob, B_sm = [Buf(), Buf()], [Buf(), Buf()], [Buf(), Buf()], [Buf(), Buf()], [Buf(), Buf()]
                B_junk = Buf()
                B_out = [Buf() for _ in range(NTILE)]
                for tl in range(NTILE):
                    i2 = tl % 2
                    rows = slice(tl * 128, (tl + 1) * 128)
                    P.dma("sp", lambda q: q.dma_start(out=fb[i2][:], in_=f_d[rows, :]), r=[B_f], w=[B_fb[i2]])
                    P.dma("sp", lambda q: q.dma_start(out=xb[i2][:], in_=x1_d[rows, :]), r=[B_x1[tl]], w=[B_xb[i2]])
                    P.op("dve", lambda v: v.scalar_tensor_tensor(out=junk[:], in0=fb[i2][:], scalar=1.0, in1=fb[i2][:],
                                                                 op0=ALU.mult, op1=ALU.mult, accum_out=sm[:, i2, 0:1]),
                         r=[B_fb[i2]], w=[B_junk, B_sm[i2]])
                    rstd_from_ss(sm[:, i2, 0:1], D, sm[:, i2, 1:2], sm[:, i2, 0:1], [B_sm[i2]], [B_sm[i2]], B_sm[i2])
                    P.op("dve", lambda v: v.scalar_tensor_tensor(out=tb[i2][:], in0=fb[i2][:], scalar=sm[:, i2, 1:2], in1=gpf[:],
                                                                 op0=ALU.mult, op1=ALU.mult),
                         r=[B_fb[i2], B_sm[i2], B_gpf], w=[B_tb[i2]])
                    P.op("dve", lambda g: g.tensor_tensor(out=ob_[i2][:], in0=tb[i2][:], in1=xb[i2][:], op=ALU.add),
                         r=[B_tb[i2], B_xb[i2]], w=[B_ob[i2]])
                    P.dma("pool", lambda q: q.dma_start(out=out_d[rows, :], in_=ob_[i2][:]), r=[B_ob[i2]], w=[B_out[tl]])
                P.barrier()

        import os
        if stop_after == "0":
            return nc
        phase_a0()
        if stop_after == "A0":
            return nc
        if stop_after == "A1":
            phase_a1(heads=[int(x) for x in os.environ.get("A1_HEADS", "0").split(",")],
                     qchunks=[int(x) for x in os.environ.get("A1_QC", "0,9").split(",")])
            return nc
        if stop_after == "A2":
            phase_a2(heads=[int(x) for x in os.environ.get("A2_HEADS", "0").split(",")])
            return nc
        if not os.environ.get("SKIP_A1"):
            phase_a1()
        phase_a2()
        phase_b()
        if stop_after == "B":
            return nc
        phase_c()
        if stop_after == "C":
            return nc
        phase_d()
        phase_e()
        return nc


def _rope_tables():
    half = 32
    inv_freq = (1.0 / (10000.0 ** (np.arange(0, half, 2, dtype=np.float32) / np.float32(half)))).astype(np.float32)
    t = np.arange(T)
    r = (t // 64).astype(np.float32)
    c = (t % 64).astype(np.float32)
    ang_r = r[:, None] * inv_freq[None, :]
    ang_c = c[:, None] * inv_freq[None, :]
    ang = np.concatenate([ang_r, ang_r, ang_c, ang_c], axis=-1).astype(np.float32)
    cos = np.cos(ang).astype(np.float32)
    sin = np.sin(ang).astype(np.float32)
    sign = np.concatenate([-np.ones(16), np.ones(16), -np.ones(16), np.ones(16)]).astype(np.float32)
    sin = sin * sign[None, :]
    cosT = np.ones((128, S), np.float32)
    sinT = np.zeros((128, S), np.float32)
    cosT[:, TC:] = np.concatenate([cos.T, cos.T], axis=0)
    sinT[:, TC:] = np.concatenate([sin.T, sin.T], axis=0)
    return cosT, sinT


def _win_cols():
    rot = np.concatenate([np.arange(16, 32), np.arange(0, 16), np.arange(48, 64), np.arange(32, 48)])
    fm, tm, tg = [], [], []
    for h in range(NH):
        for off in (0, 512):
            base = off + h * 128
            fm.append(base + np.arange(128))
            fm.append(np.concatenate([base + rot, base + 64 + rot]))
        fm.append(1536 + h * 128 + np.arange(128))
        fm.append(2048 + h * 128 + np.arange(128))
        fm.append(3072 + h * 128 + np.arange(128))
        tm.append(1024 + h * 128 + np.arange(128))
        tm.append(2560 + h * 128 + np.arange(128))
        tg.append(3584 + h * 128 + np.arange(128))
    tm = tm + tg
    return np.concatenate(fm + tm)


def _kc(a):
    n = a.shape[-1]
    return np.ascontiguousarray(a.reshape(8, 128, n).transpose(1, 0, 2))


def prep_inputs(inp, n_cores):
    f = lambda k: np.asarray(inp[k], dtype=np.float32)
    x, c, ctx, c_ctx = f("x"), f("c"), f("ctx"), f("c_ctx")
    cosT, sinT = _rope_tables()
    p = np.arange(128)
    blk = p // 64
    same = blk[:, None] == blk[None, :]
    cm = np.zeros((128, 4, 128), np.float32)
    cm[:, 0, :] = np.eye(128)
    cm[:, 1, :] = same & (p[:, None] <= p[None, :])
    cm[:, 2, :] = same & (p[:, None] >= p[None, :])
    cm[:, 3, :] = p[:, None] < p[None, :]
    j = np.arange(1024)
    rm = np.ones((128, 2, 1024), np.float32)
    rm[:, 0, j % 64 == 0] = 0.0
    rm[:, 1, j % 64 == 63] = 0.0
    iota = np.broadcast_to(j.astype(np.float32), (128, 1024)).copy()
    tt = np.arange(NTILE)[None, :] * 128 + p[:, None]
    tokhl = np.stack([64 * (tt // 64), tt % 64], axis=-1).astype(np.float32)
    hlb = f("hg_lower_bound").reshape(2, 2, 4, 128).transpose(3, 0, 1, 2).reshape(128, 16)
    shared = {
        "w_ada": _kc(f("w_ada")[0]),
        "b_ada": f("b_ada")[0][None, :],
        "norms": np.concatenate([f("norm_pre_mix")[0], f("norm_post_mix")[0], f("norm_pre_ffn")[0],
                                 f("norm_post_ffn")[0]])[None, :],
        "w_in": _kc(f("w_in")[0][:, _win_cols()]),
        "lamv": np.concatenate([f("da_lambda_q1")[0], f("da_lambda_k1")[0], f("da_lambda_q2")[0],
                                f("da_lambda_k2")[0]])[None, :],
        "subln": f("da_subln")[0][:, None],
        "hgn": f("hg_norm")[0][None, :],
        "hlb": np.ascontiguousarray(hlb),
        "w_out": _kc(f("w_out")[0]),
        "w_r": _kc(f("w_router")[0]),
        "w_gate": np.ascontiguousarray(f("w_gate")[0].reshape(NE, 8, 128, D).transpose(0, 2, 1, 3)),
        "w_up": np.ascontiguousarray(f("w_up")[0].reshape(NE, 8, 128, D).transpose(0, 2, 1, 3)),
        "w_down": np.ascontiguousarray(f("w_down")[0].reshape(NE, 8, 128, D).transpose(0, 2, 1, 3)),
        "cosT": cosT, "sinT": sinT, "cmasks": cm, "rmask": rm, "iota": iota, "tokhl": tokhl,
    }
    maps = []
    for i in range(n_cores):
        b = i % 2
        m = dict(shared)
        m["x"] = np.ascontiguousarray(x[b])
        m["ctx"] = np.ascontiguousarray(ctx[b])
        m["cc"] = _kc(np.stack([c[b], c_ctx], axis=1))
        maps.append(m)
    return maps


N_CORES = 2
_NC_CACHE = {}


def kernel(**inputs):
    if "nc" not in _NC_CACHE:
        _NC_CACHE["nc"] = build()
    nc = _NC_CACHE["nc"]
    maps = prep_inputs(inputs, N_CORES)
    res = run_bass_kernel_spmd(nc, maps, core_ids=list(range(N_CORES)))
    out = np.stack([np.asarray(res.results[b]["out"], dtype=np.float32) for b in range(2)], axis=0)
    return out
```
